# Optimizing a Trainium2 kernel written in Bass

```python
import jax
import jax.numpy as jnp
from jax import lax
import numpy as np

D_MODEL = 1024
BATCH = 4
SEQ = 4096
DEPTH = 4

GRID_W = 64
CTX_LEN = 256
N_MOD = 6
NORM_EPS = 1e-6
CHUNK = 64
RET_HEADS = 8
RET_DK = 64
RET_DV = 64
RET_W = RET_HEADS * RET_DV
ROPE_BASE = 10000.0
HG_HEADS = 4
HG_DK = 128
HG_DV = 128
HG_W = HG_HEADS * HG_DK
LRU_W = 512
LRU_BLOCKS = 8
LRU_BD = LRU_W // LRU_BLOCKS
LRU_C = 8.0
CONV_W = 4
CONV_PAD = (2, 1)
N_EXPERTS = 32
TOP_K = 4
D_EXPERT = D_MODEL
SWIGLU_ALPHA = 1.702
SWIGLU_LIMIT = 7.0
MOE_BLOCK = 256
SPLIT_SIZES = (RET_W, RET_W, RET_W, RET_W, HG_W, HG_W, HG_W, HG_W, HG_W, LRU_W, LRU_W, D_MODEL, D_MODEL, D_MODEL)
IN_COLS = sum(SPLIT_SIZES)

kernel_name = 'hybrid_retention_hgrn2_rglru_moe_dit'


def rmsnorm(x, g):
    xf = x.astype(jnp.float32)
    y = xf * lax.rsqrt(jnp.mean(xf * xf, axis=-1, keepdims=True) + NORM_EPS)
    return (y * g.astype(jnp.float32)).astype(x.dtype)


def modulate(h, shift, scale):
    return h * (1 + scale) + shift


def heads(t, n):
    B, L, W = t.shape
    return t.reshape(B, L, n, W // n).transpose(0, 2, 1, 3)


def head_rmsnorm(o, g):
    B, H, L, d = o.shape
    of = o.astype(jnp.float32)
    of = of * lax.rsqrt(jnp.mean(of * of, axis=-1, keepdims=True) + NORM_EPS)
    return of.transpose(0, 2, 1, 3).reshape(B, L, H * d) * g.astype(jnp.float32)


def axial_rotary_tables(n_tokens):
    rows = n_tokens // GRID_W
    row = jnp.repeat(jnp.arange(rows, dtype=jnp.float32), GRID_W)
    col = jnp.tile(jnp.arange(GRID_W, dtype=jnp.float32), rows)
    n_freq = RET_DK // 4
    inv = ROPE_BASE ** (-jnp.arange(n_freq, dtype=jnp.float32) / n_freq)
    ang = jnp.concatenate([row[:, None] * inv, col[:, None] * inv], axis=-1)
    return jnp.cos(ang), jnp.sin(ang)


def apply_rotary(t, cos, sin):
    half = t.shape[-1] // 2
    t1, t2 = t[..., :half], t[..., half:]
    return jnp.concatenate([t1 * cos - t2 * sin, t1 * sin + t2 * cos], axis=-1).astype(t.dtype)


def chunk_scan(q, k, v, log_f, s0, include_diag):
    f32 = jnp.float32
    B, H, L, _ = q.shape
    n = L // CHUNK

    def blocks(t):
        return t.astype(f32).reshape(B, H, n, CHUNK, t.shape[-1]).transpose(2, 0, 1, 3, 4)

    mask = jnp.tril(jnp.ones((CHUNK, CHUNK), dtype=bool), 0 if include_diag else -1)
    scalar_decay = log_f.shape[-1] == 1

    def step(S, xs):
        qb, kb, vb, lfb = xs
        g = jnp.cumsum(lfb, axis=2)
        g_last = g[:, :, -1:]
        rel = jnp.exp(jnp.where(mask[:, :, None], g[:, :, :, None] - g[:, :, None], -jnp.inf))
        if scalar_decay:
            scores = jnp.einsum('bhid,bhjd->bhij', qb, kb) * rel[..., 0]
        else:
            scores = jnp.einsum('bhid,bhjd,bhijd->bhij', qb, kb, rel)
        o = jnp.einsum('bhij,bhjv->bhiv', scores, vb) + jnp.einsum('bhid,bhdv->bhiv', qb * jnp.exp(g), S)
        S_new = jnp.exp(g_last)[:, :, 0, :, None] * S + jnp.einsum('bhjd,bhjv->bhdv', kb * jnp.exp(g_last - g), vb)
        return S_new, o

    s_fin, o = lax.scan(step, s0.astype(f32), (blocks(q), blocks(k), blocks(v), blocks(log_f)))
    return o.transpose(1, 2, 0, 3, 4).reshape(B, H, L, v.shape[-1]), s_fin


def bidir_linear_recurrence(q, v, k_f, lf_f, k_b, lf_b, n_ctx, bwd_diag):
    c_ = lambda t: t[:, :, :n_ctx]
    l_ = lambda t: t[:, :, n_ctx:]
    r_ = lambda t: jnp.flip(t, axis=2)
    B, H, _, dk = q.shape
    s0 = jnp.zeros((B, H, dk, v.shape[-1]), jnp.float32)
    oc_f, sc_f = chunk_scan(c_(q), c_(k_f), c_(v), c_(lf_f), s0, True)
    ol_f, _ = chunk_scan(l_(q), l_(k_f), l_(v), l_(lf_f), sc_f, True)
    oc_b, sc_b = chunk_scan(r_(c_(q)), r_(c_(k_b)), r_(c_(v)), r_(c_(lf_b)), s0, bwd_diag)
    ol_b, _ = chunk_scan(r_(l_(q)), r_(l_(k_b)), r_(l_(v)), r_(l_(lf_b)), sc_b, bwd_diag)
    return oc_f + r_(oc_b), ol_f + r_(ol_b)


def hgrn_forget(z, lb):
    z = z.astype(jnp.float32)
    log_f = jnp.logaddexp(jnp.log(lb), jnp.log1p(-lb) + jax.nn.log_sigmoid(z))
    key_in = (1.0 - lb) * jax.nn.sigmoid(-z)
    return heads(key_in, HG_HEADS), heads(log_f, HG_HEADS)


def depthwise_conv(x, w, b):
    y = lax.conv_general_dilated(x, w[:, None, :].astype(x.dtype), window_strides=(1,), padding=[CONV_PAD],
                                 dimension_numbers=('NWC', 'WIO', 'NWC'), feature_group_count=x.shape[-1])
    return y + b.astype(x.dtype)


def rglru_coeffs(xs, w_a, b_a, w_x, b_x, lam):
    f32 = jnp.float32
    B, L, C = xs.shape
    xf = xs.astype(f32)
    xb = xf.reshape(B, L, LRU_BLOCKS, LRU_BD)
    r = jax.nn.sigmoid(jnp.einsum('blhi,hij->blhj', xb, w_a.astype(f32)).reshape(B, L, C) + b_a.astype(f32))
    i = jax.nn.sigmoid(jnp.einsum('blhi,hij->blhj', xb, w_x.astype(f32)).reshape(B, L, C) + b_x.astype(f32))
    log_a = -LRU_C * r * jax.nn.softplus(-lam.astype(f32))
    a = jnp.exp(log_a)
    b = jnp.sqrt(-jnp.expm1(2.0 * log_a)) * (i * xf)
    return a, b


def linear_scan(a, b, h0):
    b = b.at[:, 0].add(a[:, 0] * h0)
    _, h = lax.associative_scan(lambda l, r: (l[0] * r[0], r[0] * l[1] + r[1]), (a, b), axis=1)
    return h


def bidir_rglru(a_f, b_f, a_b, b_b, n_ctx):
    c_ = lambda t: t[:, :n_ctx]
    l_ = lambda t: t[:, n_ctx:]
    r_ = lambda t: jnp.flip(t, axis=1)
    h0 = jnp.zeros((a_f.shape[0], a_f.shape[2]), a_f.dtype)
    hc_f = linear_scan(c_(a_f), c_(b_f), h0)
    hl_f = linear_scan(l_(a_f), l_(b_f), hc_f[:, -1])
    hc_b = linear_scan(r_(c_(a_b)), r_(c_(b_b)), h0)
    hl_b = linear_scan(r_(l_(a_b)), r_(l_(b_b)), hc_b[:, -1])
    return hc_f + r_(hc_b), hl_f + r_(hl_b)


def merge_branches(o_ret, o_hg, y_lru, cols, p):
    rg, hgt, lgt, gr, gh, gl = cols
    ret = (head_rmsnorm(o_ret, p['ret_gn_g']) * jax.nn.silu(rg)) @ p['w_ret_o']
    hgr = (head_rmsnorm(o_hg, p['hgrn_gn_g']) * jax.nn.sigmoid(hgt)) @ p['w_hgrn_o']
    lru = (y_lru * jax.nn.gelu(lgt)) @ p['w_lru_o']
    merged = jax.nn.sigmoid(gr) * ret + jax.nn.sigmoid(gh) * hgr + jax.nn.sigmoid(gl) * lru
    return merged @ p['w_out']


def moe_ffn(h, router_w, router_b, w_gu, b_gu, w_down, b_down):
    T, D = h.shape
    logits = h.astype(jnp.float32) @ router_w.astype(jnp.float32) + router_b.astype(jnp.float32)
    top_val, top_idx = lax.top_k(logits, TOP_K)
    top_w = jax.nn.softmax(top_val, axis=-1).astype(h.dtype)
    n_assign = T * TOP_K
    flat_e = top_idx.reshape(-1)
    flat_tok = jnp.repeat(jnp.arange(T, dtype=jnp.int32), TOP_K)
    order = jnp.argsort(flat_e)
    se, stok, sw = flat_e[order], flat_tok[order], top_w.reshape(-1)[order]
    counts = jnp.bincount(flat_e, length=N_EXPERTS)
    padded = (counts + MOE_BLOCK - 1) // MOE_BLOCK * MOE_BLOCK
    pad_end = jnp.cumsum(padded)
    pad_start = pad_end - padded
    grp_start = jnp.cumsum(counts) - counts
    dest = pad_start[se] + jnp.arange(n_assign, dtype=jnp.int32) - grp_start[se]
    n_blocks = -(-n_assign // MOE_BLOCK) + N_EXPERTS
    slot_tok = jnp.zeros((n_blocks * MOE_BLOCK,), jnp.int32).at[dest].set(stok)
    block_exp = jnp.minimum(jnp.searchsorted(pad_end, jnp.arange(n_blocks, dtype=jnp.int32) * MOE_BLOCK, side='right'),
                            N_EXPERTS - 1)

    def run_block(args):
        tok, e = args
        xb = h[tok]
        gu = xb @ w_gu[e] + b_gu[e]
        gate = jnp.minimum(gu[:, :D_EXPERT], SWIGLU_LIMIT)
        up = jnp.clip(gu[:, D_EXPERT:], -SWIGLU_LIMIT, SWIGLU_LIMIT)
        glu = gate * jax.nn.sigmoid(SWIGLU_ALPHA * gate)
        return ((up + 1) * glu) @ w_down[e] + b_down[e]

    y_slots = lax.map(run_block, (slot_tok.reshape(n_blocks, MOE_BLOCK), block_exp)).reshape(-1, D)
    y = y_slots[dest] * sw[:, None]
    return jnp.zeros((T, D), y.dtype).at[stok].add(y)


def trunk_layer(xc, xl, mod_c, mod_l, p, cos, sin, last):
    f32 = jnp.float32
    B, Lc, D = xc.shape
    L = xl.shape[1]
    Lt = Lc + L
    sh1c, sc1c, g1c, sh2c, sc2c, g2c = [mod_c[i] for i in range(N_MOD)]
    sh1l, sc1l, g1l, sh2l, sc2l, g2l = [mod_l[:, None, i] for i in range(N_MOD)]

    hc = modulate(rmsnorm(xc, p['norm1_g']), sh1c, sc1c)
    hl = modulate(rmsnorm(xl, p['norm1_g']), sh1l, sc1l)
    u = jnp.concatenate([hc, hl], axis=1) @ p['w_in']
    points = np.cumsum(SPLIT_SIZES)[:-1].tolist()
    rq, rk, rv, rg, hq, hff, hfb, hi, hgt, lx, lgt, gr, gh, gl = jnp.split(u, points, axis=-1)

    q = heads(rq, RET_HEADS)
    k = heads(rk, RET_HEADS) * RET_DK ** -0.5
    v = heads(rv, RET_HEADS)
    q = jnp.concatenate([q[:, :, :Lc], apply_rotary(q[:, :, Lc:], cos, sin)], axis=2)
    k = jnp.concatenate([k[:, :, :Lc], apply_rotary(k[:, :, Lc:], cos, sin)], axis=2)
    log_gamma = jax.nn.log_sigmoid(p['ret_decay'].astype(f32))
    lg_f = jnp.broadcast_to(log_gamma[0][None, :, None, None], (B, RET_HEADS, Lt, 1))
    lg_b = jnp.broadcast_to(log_gamma[1][None, :, None, None], (B, RET_HEADS, Lt, 1))
    ret_c, ret_l = bidir_linear_recurrence(q, v, k, lg_f, k, lg_b, Lc, bwd_diag=False)

    hq_h = heads(jax.nn.silu(hq), HG_HEADS)
    hv_h = heads(hi, HG_HEADS)
    kf, lff = hgrn_forget(hff, p['hgrn_lb'][0])
    kb, lfb = hgrn_forget(hfb, p['hgrn_lb'][1])
    hg_c, hg_l = bidir_linear_recurrence(hq_h, hv_h, kf, lff, kb, lfb, Lc, bwd_diag=True)

    xconv = jnp.concatenate([depthwise_conv(lx[:, :Lc], p['lru_conv_w'], p['lru_conv_b']),
                             depthwise_conv(lx[:, Lc:], p['lru_conv_w'], p['lru_conv_b'])], axis=1)
    a_f, b_f = rglru_coeffs(xconv, p['lru_wa'][0], p['lru_ba'][0], p['lru_wx'][0], p['lru_bx'][0], p['lru_lambda'][0])
    a_b, b_b = rglru_coeffs(xconv, p['lru_wa'][1], p['lru_ba'][1], p['lru_wx'][1], p['lru_bx'][1], p['lru_lambda'][1])
    lru_c, lru_l = bidir_rglru(a_f, b_f, a_b, b_b, Lc)

    side_l = lambda t: t[:, Lc:]
    side_c = lambda t: t[:, :Lc]
    mix_l = merge_branches(ret_l, hg_l, lru_l, [side_l(t) for t in (rg, hgt, lgt, gr, gh, gl)], p)
    xl = xl + (g1l * mix_l).astype(xl.dtype)
    h2l = modulate(rmsnorm(xl, p['norm2_g']), sh2l, sc2l)

    moe_args = (p['router_w'], p['router_b'], p['exp_w_gu'], p['exp_b_gu'], p['exp_w_down'], p['exp_b_down'])
    if last:
        ff_l = moe_ffn(h2l.reshape(-1, D), *moe_args).reshape(B, L, D)
        return None, xl + (g2l * ff_l).astype(xl.dtype)

    mix_c = merge_branches(ret_c, hg_c, lru_c, [side_c(t) for t in (rg, hgt, lgt, gr, gh, gl)], p)
    xc = xc + (g1c * mix_c).astype(xc.dtype)
    h2c = modulate(rmsnorm(xc, p['norm2_g']), sh2c, sc2c)

    ff = moe_ffn(jnp.concatenate([h2c, h2l], axis=1).reshape(-1, D), *moe_args).reshape(B, Lt, D)
    xc = xc + (g2c * ff[:, :Lc]).astype(xc.dtype)
    xl = xl + (g2l * ff[:, Lc:]).astype(xl.dtype)
    return xc, xl


def setup_inputs(seed: int = 0) -> dict:
    key = jax.random.key(seed)
    ks = list(jax.random.split(key, 40))
    f32 = jnp.float32

    def nrm(shape, scale):
        return jax.random.normal(ks.pop(), shape, f32) * scale

    D, E, F = D_MODEL, N_EXPERTS, D_EXPERT
    gamma0 = 1.0 - 2.0 ** (-5.0 - jnp.arange(RET_HEADS, dtype=f32))
    ret_logit0 = jnp.log(gamma0) - jnp.log1p(-gamma0)
    a0 = jax.random.uniform(ks.pop(), (DEPTH, 2, LRU_W), f32, minval=0.9, maxval=0.999)
    s_lam = a0 ** (1.0 / LRU_C)
    return {
        'x': nrm((BATCH, SEQ, D), 1.0),
        'c': nrm((BATCH, D), 1.0),
        'ctx': nrm((BATCH, CTX_LEN, D), 1.0),
        'c_ctx': nrm((D,), 1.0),
        'mod_w': nrm((DEPTH, D, N_MOD * D), 0.5 * D ** -0.5),
        'mod_b': nrm((DEPTH, N_MOD * D), 0.02),
        'norm1_g': 1.0 + nrm((DEPTH, D), 0.02),
        'norm2_g': 1.0 + nrm((DEPTH, D), 0.02),
        'final_g': 1.0 + nrm((D,), 0.02),
        'w_in': nrm((DEPTH, D, IN_COLS), D ** -0.5),
        'ret_decay': ret_logit0[None, None, :] + nrm((DEPTH, 2, RET_HEADS), 0.05),
        'ret_gn_g': 1.0 + nrm((DEPTH, RET_W), 0.02),
        'w_ret_o': nrm((DEPTH, RET_W, D), RET_W ** -0.5),
        'hgrn_lb_logits': nrm((DEPTH, 2, HG_W), 0.5),
        'hgrn_gn_g': 1.0 + nrm((DEPTH, HG_W), 0.02),
        'w_hgrn_o': nrm((DEPTH, HG_W, D), HG_W ** -0.5),
        'lru_conv_w': nrm((DEPTH, CONV_W, LRU_W), CONV_W ** -0.5),
        'lru_conv_b': nrm((DEPTH, LRU_W), 0.02),
        'lru_wa': nrm((DEPTH, 2, LRU_BLOCKS, LRU_BD, LRU_BD), LRU_BD ** -0.5),
        'lru_ba': nrm((DEPTH, 2, LRU_W), 0.02),
        'lru_wx': nrm((DEPTH, 2, LRU_BLOCKS, LRU_BD, LRU_BD), LRU_BD ** -0.5),
        'lru_bx': nrm((DEPTH, 2, LRU_W), 0.02),
        'lru_lambda': jnp.log(s_lam) - jnp.log1p(-s_lam),
        'w_lru_o': nrm((DEPTH, LRU_W, D), LRU_W ** -0.5),
        'w_out': nrm((DEPTH, D, D), D ** -0.5),
        'router_w': nrm((DEPTH, D, E), D ** -0.5),
        'router_b': nrm((DEPTH, E), 0.01),
        'exp_w_gu': nrm((DEPTH, E, D, 2 * F), D ** -0.5),
        'exp_b_gu': nrm((DEPTH, E, 2 * F), 0.02),
        'exp_w_down': nrm((DEPTH, E, F, D), F ** -0.5),
        'exp_b_down': nrm((DEPTH, E, D), 0.02),
    }


def reference(x, c, ctx, c_ctx, mod_w, mod_b, norm1_g, norm2_g, final_g, w_in, ret_decay, ret_gn_g, w_ret_o,
              hgrn_lb_logits, hgrn_gn_g, w_hgrn_o, lru_conv_w, lru_conv_b, lru_wa, lru_ba, lru_wx, lru_bx,
              lru_lambda, w_lru_o, w_out, router_w, router_b, exp_w_gu, exp_b_gu, exp_w_down, exp_b_down):
    B, L, D = x.shape
    cos, sin = axial_rotary_tables(L)
    lb_cum = jnp.cumsum(jax.nn.softmax(hgrn_lb_logits.astype(jnp.float32), axis=0), axis=0)
    lower_bounds = lb_cum - lb_cum[0]
    s_c = jax.nn.silu(c)
    s_ctx = jax.nn.silu(c_ctx)
    xc, xl = ctx, x
    for l in range(DEPTH):
        mod_l = (s_c @ mod_w[l] + mod_b[l]).reshape(B, N_MOD, D)
        mod_c = (s_ctx @ mod_w[l] + mod_b[l]).reshape(N_MOD, D)
        p = dict(norm1_g=norm1_g[l], norm2_g=norm2_g[l], w_in=w_in[l], ret_decay=ret_decay[l],
                 ret_gn_g=ret_gn_g[l], w_ret_o=w_ret_o[l], hgrn_lb=lower_bounds[l], hgrn_gn_g=hgrn_gn_g[l],
                 w_hgrn_o=w_hgrn_o[l], lru_conv_w=lru_conv_w[l], lru_conv_b=lru_conv_b[l], lru_wa=lru_wa[l],
                 lru_ba=lru_ba[l], lru_wx=lru_wx[l], lru_bx=lru_bx[l], lru_lambda=lru_lambda[l],
                 w_lru_o=w_lru_o[l], w_out=w_out[l], router_w=router_w[l], router_b=router_b[l],
                 exp_w_gu=exp_w_gu[l], exp_b_gu=exp_b_gu[l], exp_w_down=exp_w_down[l], exp_b_down=exp_b_down[l])
        xc, xl = trunk_layer(xc, xl, mod_c, mod_l, p, cos, sin, last=(l == DEPTH - 1))
    return rmsnorm(xl, final_g)
```

```python
import contextlib
import numpy as np
import concourse.bass as bass
import concourse.mybir as mybir
from concourse.bass_utils import run_bass_kernel_spmd

F32 = mybir.dt.float32
BF16 = mybir.dt.bfloat16
ALU = mybir.AluOpType
AF = mybir.ActivationFunctionType
AX = mybir.AxisListType

D = 1024
KC = 8
CTX = 256
NEXP = 32
EPS = 1e-6
IN_COLS = 8704


class T:
    __slots__ = ("h", "w", "r", "name")

    def __init__(self, h, name):
        self.h = h
        self.name = name
        self.w = {}
        self.r = {}

    def __getitem__(self, k):
        return self.h[k]


class Prog:
    NDMA = 8

    def __init__(self, nc, same_engine_sync=True):
        self.nc = nc
        self.stack = [contextlib.ExitStack()]
        self.eng = {"pe": nc.tensor, "act": nc.scalar, "dve": nc.vector, "pool": nc.gpsimd, "sp": nc.sync}
        self.semh = {}
        self.cnt = {}
        self.known = {k: {} for k in self.eng}
        for k in self.eng:
            self.semh[k] = self.stack[0].enter_context(nc.semaphore("s_" + k))
            self.cnt[k] = 0
        self.dq = {}
        for q in ("sp", "pool", "act"):
            sems = []
            for i in range(self.NDMA):
                key = "d_%s%d" % (q, i)
                self.semh[key] = self.stack[0].enter_context(nc.semaphore(key))
                self.cnt[key] = 0
                sems.append(key)
            self.dq[q] = [sems, 0]
        self.same = same_engine_sync
        self.uid = 0
        self.ninst = 0

    @contextlib.contextmanager
    def scope(self):
        es = contextlib.ExitStack()
        self.stack.append(es)
        try:
            yield
        finally:
            self.barrier()
            self.stack.pop()
            es.close()

    def sb(self, shape, dt=F32, name=None):
        self.uid += 1
        name = (name or "t") + "_%d" % self.uid
        h = self.stack[-1].enter_context(self.nc.sbuf_tensor(name, list(shape), dt))
        return T(h, name)

    def ps(self, shape, dt=F32, name=None):
        self.uid += 1
        name = (name or "p") + "_%d" % self.uid
        h = self.stack[-1].enter_context(self.nc.psum_tensor(name, list(shape), dt))
        return T(h, name)

    def dram(self, name, shape, dt, kind="Internal"):
        h = self.nc.dram_tensor(name, list(shape), dt, kind=kind)
        return T(h.ap(), name)

    def _wait(self, ek, need):
        e = self.eng[ek]
        kn = self.known[ek]
        for s, v in need.items():
            if s == ek and (not self.same or ek == "pe"):
                continue
            if kn.get(s, 0) < v:
                e.wait_ge(self.semh[s], v)
                kn[s] = v

    @staticmethod
    def _merge(d, s):
        for k, v in s.items():
            if d.get(k, 0) < v:
                d[k] = v

    def _need(self, r, w, skip_waw=False):
        need = {}
        for t in r:
            self._merge(need, t.w)
        for t in w:
            if not skip_waw:
                self._merge(need, t.w)
            self._merge(need, t.r)
        return need

    def _record(self, tok, r, w, merge=False):
        for t in w:
            if merge:
                self._merge(t.w, tok)
            else:
                t.w = dict(tok)
                t.r = {}
        for t in r:
            if t not in w:
                self._merge(t.r, tok)

    def op(self, ek, fn, r=(), w=()):
        self._wait(ek, self._need(r, w))
        inst = fn(self.eng[ek])
        self.cnt[ek] += 1
        inst.then_inc(self.semh[ek], 1)
        self.ninst += 1
        self._record({ek: self.cnt[ek]}, r, w)
        return inst

    def dma(self, q, out_ap, in_ap, r=(), w=(), merge=False):
        sems, i = self.dq[q]
        key = sems[i % len(sems)]
        self.dq[q][1] = i + 1
        need = self._need(r, w, skip_waw=merge)
        if self.cnt[key] > 0:
            need[key] = max(need.get(key, 0), self.cnt[key])
        self._wait(q, need)
        inst = self.eng[q].dma_start(out=out_ap, in_=in_ap)
        self.cnt[key] += 16
        inst.then_inc(self.semh[key], 16)
        self.ninst += 1
        self._record({key: self.cnt[key]}, r, w, merge=merge)
        return inst

    def barrier(self):
        allv = {k: v for k, v in self.cnt.items() if v > 0}
        for ek in self.eng:
            self._wait(ek, dict(allv))

    def finish(self):
        self.barrier()
        while self.stack:
            self.stack.pop().close()

    def tt(self, ek, out, in0, in1, op, r, w):
        return self.op(ek, lambda e: e.tensor_tensor(out=out, in0=in0, in1=in1, op=op), r=r, w=w)

    def ts(self, ek, out, in0, s1, s2, op0, op1, r, w):
        if s2 is None:
            return self.op(ek, lambda e: e.tensor_scalar(out=out, in0=in0, scalar1=s1, scalar2=None, op0=op0), r=r, w=w)
        return self.op(ek, lambda e: e.tensor_scalar(out=out, in0=in0, scalar1=s1, scalar2=s2, op0=op0, op1=op1), r=r, w=w)

    def stt(self, ek, out, in0, scalar, in1, op0, op1, r, w):
        return self.op(ek, lambda e: e.scalar_tensor_tensor(out=out, in0=in0, scalar=scalar, in1=in1, op0=op0, op1=op1), r=r, w=w)

    def act(self, out, in_, func, r, w, bias=None, scale=None):
        kw = {}
        if bias is not None:
            kw["bias"] = bias
        if scale is not None:
            kw["scale"] = scale
        return self.op("act", lambda e: e.activation(out=out, in_=in_, func=func, **kw), r=r, w=w)

    def copy(self, ek, out, in_, r, w):
        if ek == "act":
            return self.op("act", lambda e: e.copy(out=out, in_=in_), r=r, w=w)
        return self.op(ek, lambda e: e.tensor_copy(out=out, in_=in_), r=r, w=w)

    def mm(self, out, lhsT, rhs, start, stop, r, w):
        return self.op("pe", lambda e: e.matmul(out, lhsT=lhsT, rhs=rhs, start=start, stop=stop), r=r, w=w)

    def tr(self, out, in_, ident, r, w):
        return self.op("pe", lambda e: e.transpose(out=out, in_=in_, identity=ident), r=r, w=w)


C_ID = 0
C_MF = 128
C_MB = 256
C_TF = 384
C_TB = 448
C_RF = 512
C_RB = 576
C_IDX = 640
C_ONE = 646
NCONST = 774

FV_N1 = 0
FV_N2 = 8
FV_MODB = 16
FV_CW = 64
FV_CB = 80
FV_BA = 84
FV_BX = 92
FV_LAM = 100
FV_FG = 108
FV_BGU = 116
NFV = 628

G_RQ, G_RK, G_RV, G_RG, G_HQ, G_HFF, G_HFB, G_HI, G_HGT, G_LX, G_LGT = [512 * i for i in range(11)]
G_GR = 5632
G_GH = 6656
G_GL = 7680


def make_consts():
    c = np.zeros((128, NCONST), np.float32)
    p = np.arange(128)
    c[:, C_ID:C_ID + 128] = np.eye(128)
    c[:, C_MF:C_MF + 128] = (p[None, :] >= p[:, None])
    c[:, C_MB:C_MB + 128] = (p[:, None] > p[None, :])
    q = np.arange(64)
    c[:64, C_TF:C_TF + 64] = (q[:, None] <= q[None, :])
    c[:64, C_TB:C_TB + 64] = (q[:, None] >= q[None, :])
    c[:64, C_RF:C_RF + 64] = (q[:, None] > q[None, :])
    c[:64, C_RB:C_RB + 64] = (q[:, None] < q[None, :])
    c[:, C_IDX + 0] = p + 1
    c[:, C_IDX + 1] = 127 - p
    c[:, C_IDX + 2] = 128 - p
    c[:, C_IDX + 3] = p
    c[:, C_IDX + 4] = -(p + 1)
    c[:, C_IDX + 5] = p - 128
    c[:, C_ONE:C_ONE + 128] = 1.0
    return c


def make_rot(lat):
    n = np.arange(lat)
    row = (n // 64).astype(np.float32)
    col = (n % 64).astype(np.float32)
    inv = (10000.0 ** (-np.arange(16, dtype=np.float32) / 16)).astype(np.float32)
    ang = np.concatenate([row[:, None] * inv, col[:, None] * inv], axis=-1).astype(np.float32)
    cs, sn = np.cos(ang), np.sin(ang)
    return np.concatenate([cs, sn, 0.125 * cs, 0.125 * sn], axis=-1).astype(np.float32)


def build(lat, depth, dbg=()):
    S = CTX + lat
    nc = bass.Bass("TRN2", target_bir_lowering=False)
    p = Prog(nc)
    tiles = [(0, 256, True)] + [(CTX + 512 * i, 512, False) for i in range(lat // 512)]
    NT128 = S // 128

    def din(name, shape):
        return p.dram(name, shape, F32, kind="ExternalInput")

    xin = din("xin", [S, D])
    scin = din("scin", [128, KC * 2])
    consts_d = din("consts", [128, NCONST])
    rot_d = din("rot", [lat, 128])
    fv_d = din("fv", [depth, 128, NFV])
    lblT_d = din("lblT", [128, 4 * 8])
    lbl_d = din("hgrn_lb_logits", [1, 4 * 2 * 512])
    mod_w = din("mod_w", [depth, D, 6 * D])
    w_in = din("w_in", [depth, D, IN_COLS])
    ret_decay = din("ret_decay", [depth, 16])
    ret_gn = din("ret_gn_g", [depth, 512])
    hg_gn = din("hgrn_gn_g", [depth, 512])
    w_ret_o = din("w_ret_o", [depth, 512, D])
    w_hg_o = din("w_hgrn_o", [depth, 512, D])
    w_lru_o = din("w_lru_o", [depth, 512, D])
    w_out = din("w_out", [depth, D, D])
    lru_bd = din("lru_bd", [depth, 2, 2, 4, 128, 128])
    router_w = din("router_w", [depth, D, NEXP])
    router_b = din("router_b", [depth, NEXP])
    w_gu = din("exp_w_gu", [depth, NEXP, D, 2 * D])
    w_dn = din("exp_w_down", [depth, NEXP, D, D])
    b_dn = din("exp_b_down", [depth, NEXP, D])
    out_d = p.dram("out", [lat, D], F32, kind="ExternalOutput")

    def scr(name, shape, dt=F32):
        kind = "ExternalOutput" if name in dbg else "Internal"
        return p.dram(name, shape, dt, kind=kind)

    xT = scr("xT", [KC, 128, S])
    r_q = scr("r_q", [S, 512], BF16)
    r_k = scr("r_k", [S, 512], BF16)
    r_v = scr("r_v", [S, 512], BF16)
    r_g = scr("r_g", [S, 512])
    h_qT = scr("h_qT", [4, 128, S])
    h_kT = [scr("h_kfT", [4, 128, S]), scr("h_kbT", [4, 128, S])]
    h_k = [scr("h_kf", [S, 512]), scr("h_kb", [S, 512])]
    h_lf = [scr("h_lff", [S, 512]), scr("h_lfb", [S, 512])]
    h_v = scr("h_v", [S, 512], BF16)
    h_g = scr("h_g", [S, 512])
    l_xT = scr("l_xT", [4, 128, S])
    l_gT = scr("l_gT", [4, 128, S])
    g_T = scr("g_T", [3, KC, 128, S])
    o_ret = scr("o_ret", [S, 512])
    o_hg = scr("o_hg", [S, 512])
    retnT = scr("retnT", [4, 128, S], BF16)
    hgnT = scr("hgnT", [4, 128, S], BF16)
    lruT = scr("lruT", [4, 128, S], BF16)
    h2T_d = scr("h2T", [KC, 128, S], BF16)
    lbrow_d = scr("lbrow", [4, 128, 2048])
    wmoe_d = scr("wmoe", [S, NEXP])

    cst = p.sb([128, NCONST], F32, "cst")
    identb = p.sb([128, 128], BF16, "identb")
    sT = p.sb([128, KC * 2], F32, "sT")
    lbT = p.sb([128, 32], F32, "lbT")
    omlT = p.sb([128, 32], F32, "omlT")
    fv = p.sb([128, NFV], F32, "fv")
    modv = p.sb([128, 6, KC, 2], F32, "modv")
    psb = [p.ps([128, 512], F32, "psf%d" % i) for i in range(6)]
    psh = [p.ps([128, 1024], BF16, "psh%d" % i) for i in range(2)]
    rr = {"f": 0, "h": 0}

    def nps():
        rr["f"] += 1
        return psb[rr["f"] % 6]

    def npsh():
        rr["h"] += 1
        return psh[rr["h"] % 2]

    ident = cst[:, C_ID:C_ID + 128]
    ones = cst[:, C_ONE:C_ONE + 128]

    p.dma("sp", cst[:], consts_d[:], w=[cst])
    p.dma("sp", sT[:], scin[:], w=[sT])
    p.copy("dve", identb[:], ident, r=[cst], w=[identb])
    with p.scope():
        t0 = p.sb([128, KC * 2])
        p.act(t0[:], sT[:], AF.Sigmoid, r=[sT], w=[t0])
        p.tt("dve", sT[:], sT[:], t0[:], ALU.mult, r=[sT, t0], w=[sT])
        e0 = p.sb([128, 32])
        p.dma("sp", e0[:], lblT_d[:], w=[e0])
        p.act(e0[:], e0[:], AF.Exp, r=[e0], w=[e0])
        sm = p.sb([128, 8])
        p.tt("dve", sm[:], e0[:, 0:8], e0[:, 8:16], ALU.add, r=[e0], w=[sm])
        p.tt("dve", sm[:], sm[:], e0[:, 16:24], ALU.add, r=[e0, sm], w=[sm])
        p.tt("dve", sm[:], sm[:], e0[:, 24:32], ALU.add, r=[e0, sm], w=[sm])
        p.op("dve", lambda e: e.reciprocal(out=sm[:], in_=sm[:]), r=[sm], w=[sm])
        p.op("dve", lambda e: e.memset(lbT[:, 0:8], 0.0), w=[lbT])
        for l in range(1, 4):
            p.tt("dve", e0[:, 8 * l:8 * l + 8], e0[:, 8 * l:8 * l + 8], sm[:], ALU.mult, r=[e0, sm], w=[e0])
            p.tt("dve", lbT[:, 8 * l:8 * l + 8], lbT[:, 8 * l - 8:8 * l], e0[:, 8 * l:8 * l + 8], ALU.add, r=[e0, lbT], w=[lbT])
        p.ts("dve", omlT[:], lbT[:], -1.0, 1.0, ALU.mult, ALU.add, r=[lbT], w=[omlT])
        er = p.sb([128, 4, 1024])
        p.dma("sp", er[:].rearrange("p l n -> p (l n)"), lbl_d[:].partition_broadcast(128), w=[er])
        p.act(er[:], er[:], AF.Exp, r=[er], w=[er])
        sr = p.sb([128, 1024])
        p.tt("dve", sr[:], er[:, 0, :], er[:, 1, :], ALU.add, r=[er], w=[sr])
        p.tt("dve", sr[:], sr[:], er[:, 2, :], ALU.add, r=[er, sr], w=[sr])
        p.tt("dve", sr[:], sr[:], er[:, 3, :], ALU.add, r=[er, sr], w=[sr])
        p.op("dve", lambda e: e.reciprocal(out=sr[:], in_=sr[:]), r=[sr], w=[sr])
        lbr = p.sb([128, 2048])
        p.op("dve", lambda e: e.memset(lbr[:, 0:1024], 0.0), w=[lbr])
        for l in range(4):
            if l > 0:
                p.tt("dve", er[:, l, :], er[:, l, :], sr[:], ALU.mult, r=[er, sr], w=[er])
                p.tt("dve", lbr[:, 0:1024], lbr[:, 0:1024], er[:, l, :], ALU.add, r=[er, lbr], w=[lbr])
            p.ts("dve", lbr[:, 1024:2048], lbr[:, 0:1024], -1.0, 1.0, ALU.mult, ALU.add, r=[lbr], w=[lbr])
            p.dma("sp", lbrow_d[l], lbr[:], r=[lbr], w=[lbrow_d], merge=True)

    with p.scope():
        xts = [p.sb([128, D], F32, "xt") for _ in range(2)]
        xo = [p.sb([128, KC, 128], F32, "xo") for _ in range(2)]
        for t in range(NT128):
            xt = xts[t % 2]
            p.dma("sp", xt[:], xin[t * 128:(t + 1) * 128, :], w=[xt])
            o = xo[t % 2]
            for half in range(2):
                ps = nps()
                for j in range(4):
                    kc = half * 4 + j
                    p.tr(ps[:, j * 128:(j + 1) * 128], xt[:, kc * 128:(kc + 1) * 128], ident, r=[xt, cst], w=[ps])
                p.copy("dve" if half == 0 else "act", o[:, half * 4:half * 4 + 4, :].rearrange("p k n -> p (k n)"), ps[:], r=[ps], w=[o])
            p.dma("sp", xT[:, :, t * 128:(t + 1) * 128].rearrange("k p n -> p k n"), o[:], r=[o], w=[xT], merge=True)

    for l in range(depth):
        last = (l == depth - 1)
        p.dma("sp", fv[:], fv_d[l], w=[fv])
        with p.scope():
            mraw = p.sb([128, 48, 2], F32, "mraw")
            wts = [p.sb([128, KC, 512], F32, "modw") for _ in range(2)]
            for g in range(12):
                wt = wts[g % 2]
                p.dma("sp", wt[:], mod_w[l, :, g * 512:(g + 1) * 512].rearrange("(k p) n -> p k n", p=128), w=[wt])
                ps = nps()
                for m in range(4):
                    for kc in range(KC):
                        p.mm(ps[:, m * 2:m * 2 + 2], wt[:, kc, m * 128:(m + 1) * 128], sT[:, kc * 2:kc * 2 + 2],
                             kc == 0, kc == KC - 1, r=[wt, sT], w=[ps])
                p.copy("dve", mraw[:, g * 4:g * 4 + 4, :].rearrange("p a b -> p (a b)"), ps[:, 0:8], r=[ps], w=[mraw])
            p.tt("dve", mraw[:], mraw[:], fv[:, FV_MODB:FV_MODB + 48].unsqueeze(2).to_broadcast([128, 48, 2]), ALU.add, r=[mraw, fv], w=[mraw])
            for (dst, src, ng) in ((0, 1, FV_N1), (3, 4, FV_N2)):
                p.ts("dve", modv[:, dst], mraw[:, src * 8:src * 8 + 8, :], 1.0, None, ALU.add, None, r=[mraw], w=[modv])
                p.tt("dve", modv[:, dst], modv[:, dst], fv[:, ng:ng + 8].unsqueeze(2).to_broadcast([128, 8, 2]), ALU.mult, r=[modv, fv], w=[modv])
            for (dst, src) in ((1, 0), (2, 2), (4, 3), (5, 5)):
                p.copy("dve", modv[:, dst], mraw[:, src * 8:src * 8 + 8, :], r=[mraw], w=[modv])

        with p.scope():
            hT = p.sb([128, KC, S], BF16, "hT")
            with p.scope():
                xt = p.sb([128, KC, 512], F32, "xa")
                sq = p.sb([128, KC, 512], F32, "sq")
                rstd = p.sb([128, 512], F32, "rstd")
                for (t0, N, isctx) in tiles:
                    col = 0 if isctx else 1
                    p.dma("sp", xt[:, :, :N], xT[:, :, t0:t0 + N].rearrange("k p n -> p k n"), r=[xT], w=[xt])
                    p.act(sq[:, :, :N], xt[:, :, :N], AF.Square, r=[xt], w=[sq])
                    ps = nps()
                    for kc in range(KC):
                        p.mm(ps[:, :N], ones, sq[:, kc, :N], kc == 0, kc == KC - 1, r=[cst, sq], w=[ps])
                    p.act(rstd[:, :N], ps[:, :N], AF.Ln, r=[ps], w=[rstd], bias=EPS, scale=1.0 / D)
                    p.act(rstd[:, :N], rstd[:, :N], AF.Exp, r=[rstd], w=[rstd], scale=-0.5)
                    for kc in range(KC):
                        p.stt("dve", sq[:, kc, :N], xt[:, kc, :N], modv[:, 0, kc, col:col + 1], rstd[:, :N], ALU.mult, ALU.mult,
                              r=[xt, modv, rstd], w=[sq])
                        p.act(hT[:, kc, t0:t0 + N], sq[:, kc, :N], AF.Identity, r=[sq, modv], w=[hT], bias=modv[:, 1, kc, col:col + 1])

            lbr = p.sb([128, 2048], F32, "lbr")
            p.dma("sp", lbr[:], lbrow_d[l], r=[lbrow_d], w=[lbr])
            wgs = [p.sb([128, KC, 512], BF16, "wg") for _ in range(2)]
            gi = [0]

            def load_wg(c0):
                wg = wgs[gi[0] % 2]
                gi[0] += 1
                p.dma("pool", wg[:], w_in[l, :, c0:c0 + 512].rearrange("(k p) n -> p k n", p=128), w=[wg])
                return wg

            rot = [p.sb([128, 128], F32, "rot") for _ in range(2)]
            tmpA = [p.sb([128, 512], F32, "tmpA") for _ in range(2)]
            tmpB = [p.sb([128, 512], F32, "tmpB") for _ in range(2)]
            obf = [p.sb([128, 512], BF16, "obf") for _ in range(2)]
            of32 = [p.sb([128, 512], F32, "of32") for _ in range(2)]
            of32b = [p.sb([128, 512], F32, "of32b") for _ in range(2)]
            cnt = [0]

            def tm_loop(wg, handler):
                for t in range(NT128):
                    ps = nps()
                    for kc in range(KC):
                        p.mm(ps[:], hT[:, kc, t * 128:(t + 1) * 128], wg[:, kc, :], kc == 0, kc == KC - 1, r=[hT, wg], w=[ps])
                    cnt[0] += 1
                    handler(t, ps, cnt[0] % 2)

            def fm_loop(wg, handler):
                for (t0, N, isctx) in tiles:
                    for m in range(4):
                        ps = nps()
                        for kc in range(KC):
                            p.mm(ps[:, :N], wg[:, kc, m * 128:(m + 1) * 128], hT[:, kc, t0:t0 + N], kc == 0, kc == KC - 1, r=[hT, wg], w=[ps])
                        cnt[0] += 1
                        handler(t0, N, m, ps, cnt[0] % 2)

            def store_tm(dst, t, src):
                p.dma("sp", dst[t * 128:(t + 1) * 128, :], src[:], r=[src], w=[dst], merge=True)

            def store_fm(dst3, m, t0, N, src):
                p.dma("sp", dst3[m, :, t0:t0 + N], src[:, :N], r=[src], w=[dst3], merge=True)

            def h_rot(dst, cofs):
                def h(t, ps, b):
                    o = obf[b]
                    if t < 2:
                        if cofs == 0:
                            p.copy("dve", o[:], ps[:], r=[ps], w=[o])
                        else:
                            p.ts("dve", o[:], ps[:], 0.125, None, ALU.mult, None, r=[ps], w=[o])
                    else:
                        rt = rot[b]
                        p.dma("sp", rt[:], rot_d[(t - 2) * 128:(t - 1) * 128, :], w=[rt])
                        pv = ps[:].rearrange("p (h two d) -> p h two d", h=8, two=2)
                        ov = o[:].rearrange("p (h two d) -> p h two d", h=8, two=2)
                        cs = rt[:, cofs:cofs + 32].unsqueeze(1).to_broadcast([128, 8, 32])
                        sn = rt[:, cofs + 32:cofs + 64].unsqueeze(1).to_broadcast([128, 8, 32])
                        a = tmpA[b][:, 0:256].rearrange("p (h d) -> p h d", h=8)
                        bb = tmpA[b][:, 256:512].rearrange("p (h d) -> p h d", h=8)
                        p.tt("dve", a, pv[:, :, 0, :], cs, ALU.mult, r=[ps, rt], w=[tmpA[b]])
                        p.tt("dve", bb, pv[:, :, 1, :], sn, ALU.mult, r=[ps, rt], w=[tmpA[b]])
                        p.tt("dve", ov[:, :, 0, :], a, bb, ALU.subtract, r=[tmpA[b]], w=[o])
                        p.tt("dve", a, pv[:, :, 0, :], sn, ALU.mult, r=[ps, rt], w=[tmpA[b]])
                        p.tt("dve", bb, pv[:, :, 1, :], cs, ALU.mult, r=[ps, rt], w=[tmpA[b]])
                        p.tt("dve", ov[:, :, 1, :], a, bb, ALU.add, r=[tmpA[b]], w=[o])
                    store_tm(dst, t, o)
                return h

            def h_copy_bf(dst):
                def h(t, ps, b):
                    p.copy("act", obf[b][:], ps[:], r=[ps], w=[obf[b]])
                    store_tm(dst, t, obf[b])
                return h

            def h_silu_tm(dst):
                def h(t, ps, b):
                    p.act(tmpA[b][:], ps[:], AF.Sigmoid, r=[ps], w=[tmpA[b]])
                    p.tt("dve", of32[b][:], tmpA[b][:], ps[:], ALU.mult, r=[ps, tmpA[b]], w=[of32[b]])
                    store_tm(dst, t, of32[b])
                return h

            def h_sig_tm(dst):
                def h(t, ps, b):
                    p.act(of32[b][:], ps[:], AF.Sigmoid, r=[ps], w=[of32[b]])
                    store_tm(dst, t, of32[b])
                return h

            def h_forget_tm(d):
                lb = lbr[:, d * 512:(d + 1) * 512]
                oml = lbr[:, 1024 + d * 512:1024 + (d + 1) * 512]

                def h(t, ps, b):
                    p.act(tmpA[b][:], ps[:], AF.Sigmoid, r=[ps], w=[tmpA[b]])
                    p.tt("dve", tmpA[b][:], tmpA[b][:], oml, ALU.mult, r=[tmpA[b], lbr], w=[tmpA[b]])
                    p.tt("dve", tmpB[b][:], tmpA[b][:], lb, ALU.add, r=[tmpA[b], lbr], w=[tmpB[b]])
                    p.tt("pool", of32[b][:], oml, tmpA[b][:], ALU.subtract, r=[tmpA[b], lbr], w=[of32[b]])
                    p.act(of32b[b][:], tmpB[b][:], AF.Ln, r=[tmpB[b]], w=[of32b[b]])
                    store_tm(h_k[d], t, of32[b])
                    store_tm(h_lf[d], t, of32b[b])
                return h

            def h_forget_fm(d):
                def h(t0, N, m, ps, b):
                    p.act(tmpA[b][:, :N], ps[:, :N], AF.Sigmoid, r=[ps], w=[tmpA[b]], scale=-1.0)
                    p.ts("dve", of32[b][:, :N], tmpA[b][:, :N], omlT[:, l * 8 + d * 4 + m:l * 8 + d * 4 + m + 1], None, ALU.mult, None,
                         r=[tmpA[b], omlT], w=[of32[b]])
                    store_fm(h_kT[d], m, t0, N, of32[b])
                return h

            def h_silu_fm(dst):
                def h(t0, N, m, ps, b):
                    p.act(tmpA[b][:, :N], ps[:, :N], AF.Sigmoid, r=[ps], w=[tmpA[b]])
                    p.tt("dve", of32[b][:, :N], tmpA[b][:, :N], ps[:, :N], ALU.mult, r=[ps, tmpA[b]], w=[of32[b]])
                    store_fm(dst, m, t0, N, of32[b])
                return h

            def h_copy_fm(dst):
                def h(t0, N, m, ps, b):
                    p.copy("act", of32[b][:, :N], ps[:, :N], r=[ps], w=[of32[b]])
                    store_fm(dst, m, t0, N, of32[b])
                return h

            def h_gelu_fm(dst):
                def h(t0, N, m, ps, b):
                    p.act(tmpA[b][:, :N], ps[:, :N], AF.Square, r=[ps], w=[tmpA[b]])
                    p.ts("dve", tmpA[b][:, :N], tmpA[b][:, :N], 0.044715, 1.0, ALU.mult, ALU.add, r=[tmpA[b]], w=[tmpA[b]])
                    p.tt("dve", tmpA[b][:, :N], tmpA[b][:, :N], ps[:, :N], ALU.mult, r=[tmpA[b], ps], w=[tmpA[b]])
                    p.act(tmpA[b][:, :N], tmpA[b][:, :N], AF.Sigmoid, r=[tmpA[b]], w=[tmpA[b]], scale=1.5957691216057308)
                    p.tt("dve", of32[b][:, :N], tmpA[b][:, :N], ps[:, :N], ALU.mult, r=[tmpA[b], ps], w=[of32[b]])
                    store_fm(dst, m, t0, N, of32[b])
                return h

            def h_sig_fm(dst, gidx, half):
                def h(t0, N, m, ps, b):
                    p.act(of32[b][:, :N], ps[:, :N], AF.Sigmoid, r=[ps], w=[of32[b]])
                    p.dma("sp", dst[gidx, half * 4 + m, :, t0:t0 + N], of32[b][:, :N], r=[of32[b]], w=[dst], merge=True)
                return h

            plan = [
                (G_RQ, "tm", h_rot(r_q, 0)), (G_RK, "tm", h_rot(r_k, 64)), (G_RV, "tm", h_copy_bf(r_v)), (G_RG, "tm", h_silu_tm(r_g)),
                (G_HQ, "fm", h_silu_fm(h_qT)),
                (G_HFF, "tm", h_forget_tm(0)), (G_HFF, "fm", h_forget_fm(0)),
                (G_HFB, "tm", h_forget_tm(1)), (G_HFB, "fm", h_forget_fm(1)),
                (G_HI, "tm", h_copy_bf(h_v)), (G_HGT, "tm", h_sig_tm(h_g)),
                (G_LX, "fm", h_copy_fm(l_xT)), (G_LGT, "fm", h_gelu_fm(l_gT)),
                (G_GR, "fm", h_sig_fm(g_T, 0, 0)), (G_GR + 512, "fm", h_sig_fm(g_T, 0, 1)),
                (G_GH, "fm", h_sig_fm(g_T, 1, 0)), (G_GH + 512, "fm", h_sig_fm(g_T, 1, 1)),
                (G_GL, "fm", h_sig_fm(g_T, 2, 0)), (G_GL + 512, "fm", h_sig_fm(g_T, 2, 1)),
            ]
            prev = (None, None)
            for (c0, kind, hd) in plan:
                if c0 != prev[0]:
                    wg = load_wg(c0)
                    prev = (c0, wg)
                (tm_loop if kind == "tm" else fm_loop)(prev[1], hd)

        with p.scope():
            lgb = p.sb([128, 16], F32, "lgb")
            p.dma("sp", lgb[:], ret_decay[l:l + 1, :].partition_broadcast(128), w=[lgb])
            p.act(lgb[:], lgb[:], AF.Exp, r=[lgb], w=[lgb], scale=-1.0)
            p.act(lgb[:], lgb[:], AF.Ln, r=[lgb], w=[lgb], bias=1.0)
            p.ts("dve", lgb[:], lgb[:], -1.0, None, ALU.mult, None, r=[lgb], w=[lgb])
            gnrow = p.sb([128, 512], F32, "gnrow")
            p.dma("sp", gnrow[:], ret_gn[l:l + 1, :].partition_broadcast(128), w=[gnrow])
            av = p.sb([128, 2, 8], F32, "av")
            bv = p.sb([128, 2, 8], F32, "bv")
            iv = p.sb([128, 2, 8], F32, "iv")
            Dt = p.sb([128, 2, 8, 128], F32, "Dt")
            Gt = p.sb([64, 2, 512], F32, "Gt")
            idx = lambda i: cst[:, C_IDX + i:C_IDX + i + 1]
            for d in range(2):
                lg = lgb[:, d * 8:d * 8 + 8]
                p.act(av[:, d, :], lg, AF.Exp, r=[lgb, cst], w=[av], scale=idx(0) if d == 0 else idx(2))
                p.act(bv[:, d, :], lg, AF.Exp, r=[lgb, cst], w=[bv], scale=idx(1) if d == 0 else idx(3))
                p.act(iv[:, d, :], lg, AF.Exp, r=[lgb, cst], w=[iv], scale=idx(4) if d == 0 else idx(5))
                msk = cst[:, C_MF:C_MF + 128] if d == 0 else cst[:, C_MB:C_MB + 128]
                for h in range(8):
                    p.ts("dve", Dt[:, d, h, :], msk, iv[:, d, h:h + 1], None, ALU.mult, None, r=[cst, iv], w=[Dt])
                p.act(Gt[:, d, :].rearrange("p (h v) -> p h v", h=8), lgb[0:64, d * 8:d * 8 + 8].unsqueeze(2).to_broadcast([64, 8, 64]),
                      AF.Exp, r=[lgb], w=[Gt], scale=128.0)

            Sst = p.sb([64, 512], F32, "Sst")
            Sbf = p.sb([64, 512], BF16, "Sbf")
            NB = 2
            qt = [p.sb([128, 512], BF16, "rq") for _ in range(NB)]
            kt = [p.sb([128, 512], BF16, "rk") for _ in range(NB)]
            vt = [p.sb([128, 512], BF16, "rv") for _ in range(NB)]
            qd = [p.sb([128, 512], BF16, "qd") for _ in range(NB)]
            kd = [p.sb([128, 512], BF16, "kd") for _ in range(NB)]
            qdT = [p.sb([64, 8, 128], BF16, "qdT") for _ in range(NB)]
            kTt = [p.sb([64, 8, 128], BF16, "kTt") for _ in range(NB)]
            Pm = [p.sb([128, 8, 128], BF16, "Pm") for _ in range(NB)]
            osb = [p.sb([128, 512], F32, "osb") for _ in range(NB)]
            oprev = [p.sb([128, 512], F32, "oprev") for _ in range(NB)]
            gate = [p.sb([128, 512], F32, "gate") for _ in range(NB)]
            ssq = [p.sb([128, 8], F32, "ssq") for _ in range(NB)]
            ybf = [p.sb([128, 512], BF16, "ybf") for _ in range(NB)]
            yT = [p.sb([128, 4, 128], BF16, "yT") for _ in range(NB)]
            for d in range(2):
                p.op("dve", lambda e: e.memset(Sst[:], 0.0), w=[Sst])
                p.op("dve", lambda e: e.memset(Sbf[:], 0.0), w=[Sbf])
                order = list(range(NT128)) if d == 0 else [1, 0] + list(range(NT128 - 1, 1, -1))
                for it, t in enumerate(order):
                    b = it % NB
                    if last and d == 1 and t < 2:
                        pass
                    rows = slice(t * 128, (t + 1) * 128)
                    p.dma("sp", qt[b][:], r_q[rows, :], r=[r_q], w=[qt[b]])
                    p.dma("sp", kt[b][:], r_k[rows, :], r=[r_k], w=[kt[b]])
                    p.dma("sp", vt[b][:], r_v[rows, :], r=[r_v], w=[vt[b]])
                    v3 = lambda tl: tl[:].rearrange("p (h e) -> p h e", h=8)
                    p.tt("dve", v3(qd[b]), v3(qt[b]), av[:, d, :].unsqueeze(2).to_broadcast([128, 8, 64]), ALU.mult, r=[qt[b], av], w=[qd[b]])
                    p.tt("pool", v3(kd[b]), v3(kt[b]), bv[:, d, :].unsqueeze(2).to_broadcast([128, 8, 64]), ALU.mult, r=[kt[b], bv], w=[kd[b]])
                    ph = npsh()
                    for h in range(8):
                        p.tr(ph[0:64, h * 128:(h + 1) * 128], qd[b][:, h * 64:(h + 1) * 64], identb[:], r=[qd[b], identb], w=[ph])
                    p.copy("act", qdT[b][:].rearrange("p h n -> p (h n)"), ph[0:64, :], r=[ph], w=[qdT[b]])
                    ph = npsh()
                    for h in range(8):
                        p.tr(ph[0:64, h * 128:(h + 1) * 128], kt[b][:, h * 64:(h + 1) * 64], identb[:], r=[kt[b], identb], w=[ph])
                    p.copy("dve", kTt[b][:].rearrange("p h n -> p (h n)"), ph[0:64, :], r=[ph], w=[kTt[b]])
                    for hh in range(2):
                        ps = nps()
                        for j in range(4):
                            h = hh * 4 + j
                            p.mm(ps[:, j * 128:(j + 1) * 128], kTt[b][:, h, :], qdT[b][:, h, :], True, True, r=[kTt[b], qdT[b]], w=[ps])
                        p.tt("dve", Pm[b][:, hh * 4:hh * 4 + 4, :].rearrange("p h n -> p (h n)"), ps[:],
                             Dt[:, d, hh * 4:hh * 4 + 4, :].rearrange("p h n -> p (h n)"), ALU.mult, r=[ps, Dt], w=[Pm[b]])
                    po = nps()
                    for h in range(8):
                        cs = slice(h * 64, (h + 1) * 64)
                        p.mm(po[:, cs], Pm[b][:, h, :], vt[b][:, cs], True, False, r=[Pm[b], vt[b]], w=[po])
                        p.mm(po[:, cs], qdT[b][:, h, :], Sbf[:, cs], False, True, r=[qdT[b], Sbf], w=[po])
                    psS = nps()
                    for h in range(8):
                        cs = slice(h * 64, (h + 1) * 64)
                        p.mm(psS[0:64, cs], kd[b][:, cs], vt[b][:, cs], True, True, r=[kd[b], vt[b]], w=[psS])
                    p.tt("dve", Sst[:], Sst[:], Gt[:, d, :], ALU.mult, r=[Sst, Gt], w=[Sst])
                    p.tt("dve", Sst[:], Sst[:], psS[0:64, :], ALU.add, r=[Sst, psS], w=[Sst])
                    p.copy("act", Sbf[:], Sst[:], r=[Sst], w=[Sbf])
                    if d == 0:
                        p.copy("act", osb[b][:], po[:], r=[po], w=[osb[b]])
                        p.dma("sp", o_ret[rows, :], osb[b][:], r=[osb[b]], w=[o_ret], merge=True)
                    else:
                        if last and t < 2:
                            continue
                        p.dma("sp", oprev[b][:], o_ret[rows, :], r=[o_ret], w=[oprev[b]])
                        p.dma("sp", gate[b][:], r_g[rows, :], r=[r_g], w=[gate[b]])
                        p.tt("dve", osb[b][:], po[:], oprev[b][:], ALU.add, r=[po, oprev[b]], w=[osb[b]])
                        p.tt("pool", oprev[b][:], osb[b][:], osb[b][:], ALU.mult, r=[osb[b]], w=[oprev[b]])
                        p.op("dve", lambda e: e.reduce_sum(out=ssq[b][:], in_=oprev[b][:].rearrange("p (h e) -> p h e", h=8), axis=AX.X),
                             r=[oprev[b]], w=[ssq[b]])
                        p.act(ssq[b][:], ssq[b][:], AF.Ln, r=[ssq[b]], w=[ssq[b]], bias=EPS, scale=1.0 / 64)
                        p.act(ssq[b][:], ssq[b][:], AF.Exp, r=[ssq[b]], w=[ssq[b]], scale=-0.5)
                        p.tt("dve", v3(osb[b]), v3(osb[b]), ssq[b][:].unsqueeze(2).to_broadcast([128, 8, 64]), ALU.mult, r=[osb[b], ssq[b]], w=[osb[b]])
                        p.tt("pool", gate[b][:], gate[b][:], gnrow[:], ALU.mult, r=[gate[b], gnrow], w=[gate[b]])
                        p.tt("dve", ybf[b][:], osb[b][:], gate[b][:], ALU.mult, r=[osb[b], gate[b]], w=[ybf[b]])
                        ph = npsh()
                        for c in range(4):
                            p.tr(ph[:, c * 128:(c + 1) * 128], ybf[b][:, c * 128:(c + 1) * 128], identb[:], r=[ybf[b], identb], w=[ph])
                        p.copy("act", yT[b][:].rearrange("p c n -> p (c n)"), ph[:, 0:512], r=[ph], w=[yT[b]])
                        p.dma("sp", retnT[:, :, t * 128:(t + 1) * 128].rearrange("c p n -> p c n"), yT[b][:], r=[yT[b]], w=[retnT], merge=True)

        with p.scope():
            gnrow = p.sb([128, 512], F32, "hgnrow")
            p.dma("sp", gnrow[:], hg_gn[l:l + 1, :].partition_broadcast(128), w=[gnrow])
            Sst = p.sb([128, 4, 128], F32, "hS")
            Sbf = p.sb([128, 4, 128], BF16, "hSbf")
            NB = 2
            mk = lambda shape, dt, nm: [p.sb(shape, dt, nm) for _ in range(NB)]
            lft = mk([64, 512], F32, "lft")
            ktm = mk([64, 512], F32, "ktm")
            vt = mk([64, 512], BF16, "hv")
            qTt = mk([128, 4, 64], F32, "qTt")
            kTt = mk([128, 4, 64], F32, "hkT")
            gsb = mk([128, 4, 64], F32, "gsb")
            gref = mk([128, 8], F32, "gref")
            E1 = mk([128, 4, 64], F32, "E1")
            E3 = mk([128, 4, 64], F32, "E3")
            qtl = mk([128, 4, 64], BF16, "qtl")
            ktl = mk([128, 4, 64], BF16, "ktl")
            qg = mk([128, 4, 64], BF16, "qg")
            ER = mk([64, 512], F32, "ER")
            kdec = mk([64, 512], BF16, "kdec")
            Pm = mk([64, 4, 64], BF16, "hP")
            osb = mk([64, 512], F32, "hosb")
            oprev = mk([64, 512], F32, "hoprev")
            gate = mk([64, 512], F32, "hgate")
            ssq = mk([64, 4], F32, "hssq")
            ybf = mk([64, 512], BF16, "hybf")
            yT = mk([128, 4, 64], BF16, "hyT")
            NCH = S // 64
            M = 32
            mint = [p.sb([64, 4, 64], mybir.dt.int32, "mint%d" % i) for i in range(2)]
            zer = p.sb([64, 4, 64], F32, "zer")
            p.op("dve", lambda e: e.memset(zer[:], 0.0), w=[zer])
            for i, cc in enumerate((C_TF, C_TB)):
                p.copy("dve", mint[i][:], cst[0:64, cc:cc + 64].unsqueeze(1).to_broadcast([64, 4, 64]), r=[cst], w=[mint[i]])
            for d in range(2):
                p.op("dve", lambda e: e.memset(Sst[:], 0.0), w=[Sst])
                p.op("dve", lambda e: e.memset(Sbf[:], 0.0), w=[Sbf])
                tri = cst[0:64, C_TF:C_TF + 64] if d == 0 else cst[0:64, C_TB:C_TB + 64]
                rem = cst[0:64, C_RF:C_RF + 64] if d == 0 else cst[0:64, C_RB:C_RB + 64]
                lastcol = 63 if d == 0 else 0
                order = list(range(NCH)) if d == 0 else [3, 2, 1, 0] + list(range(NCH - 1, 3, -1))
                for it, c in enumerate(order):
                    b = it % NB
                    rows = slice(c * 64, (c + 1) * 64)
                    p.dma("sp", lft[b][:], h_lf[d][rows, :], r=[h_lf[d]], w=[lft[b]])
                    p.dma("sp", ktm[b][:], h_k[d][rows, :], r=[h_k[d]], w=[ktm[b]])
                    p.dma("sp", vt[b][:], h_v[rows, :], r=[h_v], w=[vt[b]])
                    p.dma("sp", qTt[b][:], h_qT[:, :, c * 64:(c + 1) * 64].rearrange("h p n -> p h n"), r=[h_qT], w=[qTt[b]])
                    p.dma("sp", kTt[b][:], h_kT[d][:, :, c * 64:(c + 1) * 64].rearrange("h p n -> p h n"), r=[h_kT[d]], w=[kTt[b]])
                    pg = nps()
                    for h in range(4):
                        p.mm(pg[:, h * 64:(h + 1) * 64], lft[b][:, h * 128:(h + 1) * 128], tri, True, True, r=[lft[b], cst], w=[pg])
                    pr = nps()
                    for h in range(4):
                        p.mm(pr[0:64, h * 128:(h + 1) * 128], rem, lft[b][:, h * 128:(h + 1) * 128], True, True, r=[lft[b], cst], w=[pr])
                    p.copy("dve", gsb[b][:].rearrange("p h n -> p (h n)"), pg[:, 0:256], r=[pg], w=[gsb[b]])
                    p.copy("dve", gref[b][:, 0:4], gsb[b][:, :, M], r=[gsb[b]], w=[gref[b]])
                    p.ts("dve", gref[b][:, 4:8], gref[b][:, 0:4], -1.0, None, ALU.mult, None, r=[gref[b]], w=[gref[b]])
                    for h in range(4):
                        p.act(E1[b][:, h, :], gsb[b][:, h, :], AF.Exp, r=[gsb[b], gref[b]], w=[E1[b]], bias=gref[b][:, 4 + h:5 + h])
                    p.tt("dve", qtl[b][:], qTt[b][:], E1[b][:], ALU.mult, r=[qTt[b], E1[b]], w=[qtl[b]])
                    for h in range(4):
                        p.act(E1[b][:, h, :], gsb[b][:, h, :], AF.Exp, r=[gsb[b], gref[b], qtl[b]], w=[E1[b]], bias=gref[b][:, h:h + 1], scale=-1.0)
                    p.tt("dve", ktl[b][:], kTt[b][:], E1[b][:], ALU.mult, r=[kTt[b], E1[b]], w=[ktl[b]])
                    p.act(E3[b][:], gsb[b][:], AF.Exp, r=[gsb[b]], w=[E3[b]])
                    p.tt("pool", qg[b][:], qTt[b][:], E3[b][:], ALU.mult, r=[qTt[b], E3[b]], w=[qg[b]])
                    p.act(ER[b][:], pr[0:64, :], AF.Exp, r=[pr], w=[ER[b]])
                    p.tt("pool", kdec[b][:], ktm[b][:], ER[b][:], ALU.mult, r=[ktm[b], ER[b]], w=[kdec[b]])
                    psc = nps()
                    for h in range(4):
                        p.mm(psc[0:64, h * 64:(h + 1) * 64], ktl[b][:, h, :], qtl[b][:, h, :], True, True, r=[ktl[b], qtl[b]], w=[psc])
                    p.op("dve", lambda e: e.select(out=Pm[b][:], mask=mint[d][:], on_true=psc[0:64, 0:256].rearrange("p (h n) -> p h n", h=4),
                                                   on_false=zer[:]), r=[psc, mint[d], zer], w=[Pm[b]])
                    po = nps()
                    for h in range(4):
                        cs = slice(h * 128, (h + 1) * 128)
                        p.mm(po[0:64, cs], Pm[b][:, h, :], vt[b][:, cs], True, False, r=[Pm[b], vt[b]], w=[po])
                        p.mm(po[0:64, cs], qg[b][:, h, :], Sbf[:, h, :], False, True, r=[qg[b], Sbf], w=[po])
                    psS = nps()
                    for h in range(4):
                        cs = slice(h * 128, (h + 1) * 128)
                        p.mm(psS[:, cs], kdec[b][:, cs], vt[b][:, cs], True, True, r=[kdec[b], vt[b]], w=[psS])
                    for h in range(4):
                        p.stt("dve", Sst[:, h, :], Sst[:, h, :], E3[b][:, h, lastcol:lastcol + 1], psS[:, h * 128:(h + 1) * 128], ALU.mult, ALU.add,
                              r=[Sst, E3[b], psS], w=[Sst])
                    p.copy("act", Sbf[:], Sst[:], r=[Sst], w=[Sbf])
                    if d == 0:
                        p.copy("act", osb[b][:], po[0:64, :], r=[po], w=[osb[b]])
                        p.dma("sp", o_hg[rows, :], osb[b][:], r=[osb[b]], w=[o_hg], merge=True)
                    else:
                        if last and c < 4:
                            continue
                        p.dma("sp", oprev[b][:], o_hg[rows, :], r=[o_hg], w=[oprev[b]])
                        p.dma("sp", gate[b][:], h_g[rows, :], r=[h_g], w=[gate[b]])
                        p.tt("dve", osb[b][:], po[0:64, :], oprev[b][:], ALU.add, r=[po, oprev[b]], w=[osb[b]])
                        p.tt("pool", oprev[b][:], osb[b][:], osb[b][:], ALU.mult, r=[osb[b]], w=[oprev[b]])
                        p.op("dve", lambda e: e.reduce_sum(out=ssq[b][:], in_=oprev[b][:].rearrange("p (h e) -> p h e", h=4), axis=AX.X),
                             r=[oprev[b]], w=[ssq[b]])
                        p.act(ssq[b][:], ssq[b][:], AF.Ln, r=[ssq[b]], w=[ssq[b]], bias=EPS, scale=1.0 / 128)
                        p.act(ssq[b][:], ssq[b][:], AF.Exp, r=[ssq[b]], w=[ssq[b]], scale=-0.5)
                        o3 = osb[b][:].rearrange("p (h e) -> p h e", h=4)
                        p.tt("dve", o3, o3, ssq[b][:].unsqueeze(2).to_broadcast([64, 4, 128]), ALU.mult, r=[osb[b], ssq[b]], w=[osb[b]])
                        p.tt("pool", gate[b][:], gate[b][:], gnrow[0:64, :], ALU.mult, r=[gate[b], gnrow], w=[gate[b]])
                        p.tt("dve", ybf[b][:], osb[b][:], gate[b][:], ALU.mult, r=[osb[b], gate[b]], w=[ybf[b]])
                        ph = npsh()
                        for h in range(4):
                            p.tr(ph[:, h * 64:(h + 1) * 64], ybf[b][:, h * 128:(h + 1) * 128], identb[0:64, 0:64], r=[ybf[b], identb], w=[ph])
                        p.copy("act", yT[b][:].rearrange("p c n -> p (c n)"), ph[:, 0:256], r=[ph], w=[yT[b]])
                        p.dma("sp", hgnT[:, :, c * 64:(c + 1) * 64].rearrange("c p n -> p c n"), yT[b][:], r=[yT[b]], w=[hgnT], merge=True)

        with p.scope():
            xs = p.sb([128, S], F32, "lx")
            xc = p.sb([128, S], F32, "lxc")
            at = p.sb([128, S], F32, "la")
            bt = p.sb([128, S], F32, "lb")
            hf = p.sb([128, S], F32, "lhf")
            hb = p.sb([128, S], F32, "lhb")
            gl = p.sb([128, S], F32, "lgl")
            ybf = p.sb([128, S], BF16, "lybf")
            wbd = p.sb([128, 128], F32, "wbd")
            wbx = p.sb([128, 128], F32, "wbx")
            cneg = p.sb([128, 8], F32, "cneg")
            p.act(cneg[:], fv[:, FV_LAM:FV_LAM + 8], AF.Exp, r=[fv], w=[cneg], scale=-1.0)
            p.act(cneg[:], cneg[:], AF.Ln, r=[cneg], w=[cneg], bias=1.0)
            p.ts("dve", cneg[:], cneg[:], -8.0, None, ALU.mult, None, r=[cneg], w=[cneg])
            tA = [p.sb([128, 512], F32, "ltA") for _ in range(2)]
            tB = [p.sb([128, 512], F32, "ltB") for _ in range(2)]
            segs = [(0, CTX), (CTX, S)]
            for c in range(4):
                p.dma("sp", xs[:], l_xT[c], r=[l_xT], w=[xs])
                p.dma("sp", gl[:], l_gT[c], r=[l_gT], w=[gl])
                cw = lambda w: fv[:, FV_CW + w * 4 + c:FV_CW + w * 4 + c + 1]
                p.ts("dve", xc[:], xs[:], cw(2), fv[:, FV_CB + c:FV_CB + c + 1], ALU.mult, ALU.add, r=[xs, fv], w=[xc])
                for w in (0, 1, 3):
                    o = w - 2
                    for (a, bnd) in segs:
                        lo, hi = max(a, a - o), min(bnd, bnd - o)
                        p.stt("dve", xc[:, lo:hi], xs[:, lo + o:hi + o], cw(w), xc[:, lo:hi], ALU.mult, ALU.add, r=[xs, fv, xc], w=[xc])
                for d in range(2):
                    p.dma("sp", wbd[:], lru_bd[l, 0, d, c], w=[wbd])
                    p.dma("sp", wbx[:], lru_bd[l, 1, d, c], w=[wbx])
                    for i, (t0, N, isctx) in enumerate(tiles):
                        b = i % 2
                        pa = nps()
                        p.mm(pa[:, :N], wbd[:], xc[:, t0:t0 + N], True, True, r=[wbd, xc], w=[pa])
                        px = nps()
                        p.mm(px[:, :N], wbx[:], xc[:, t0:t0 + N], True, True, r=[wbx, xc], w=[px])
                        p.act(tA[b][:, :N], pa[:, :N], AF.Sigmoid, r=[pa, fv], w=[tA[b]], bias=fv[:, FV_BA + d * 4 + c:FV_BA + d * 4 + c + 1])
                        p.act(tB[b][:, :N], px[:, :N], AF.Sigmoid, r=[px, fv], w=[tB[b]], bias=fv[:, FV_BX + d * 4 + c:FV_BX + d * 4 + c + 1])
                        p.act(at[:, t0:t0 + N], tA[b][:, :N], AF.Exp, r=[tA[b], cneg], w=[at], scale=cneg[:, d * 4 + c:d * 4 + c + 1])
                        p.tt("dve", tB[b][:, :N], tB[b][:, :N], xc[:, t0:t0 + N], ALU.mult, r=[tB[b], xc], w=[tB[b]])
                        p.tt("dve", tA[b][:, :N], at[:, t0:t0 + N], at[:, t0:t0 + N], ALU.mult, r=[at], w=[tA[b]])
                        p.ts("dve", tA[b][:, :N], tA[b][:, :N], -1.0, 1.0, ALU.mult, ALU.add, r=[tA[b]], w=[tA[b]])
                        p.act(tA[b][:, :N], tA[b][:, :N], AF.Ln, r=[tA[b]], w=[tA[b]])
                        p.act(tA[b][:, :N], tA[b][:, :N], AF.Exp, r=[tA[b]], w=[tA[b]], scale=0.5)
                        p.tt("dve", bt[:, t0:t0 + N], tA[b][:, :N], tB[b][:, :N], ALU.mult, r=[tA[b], tB[b]], w=[bt])
                    PC = 256
                    if d == 0:
                        for s0 in range(0, S, PC):
                            init = 0.0 if s0 == 0 else hf[:, s0 - 1:s0]
                            p.op("dve", lambda e: e.tensor_tensor_scan(out=hf[:, s0:s0 + PC], data0=at[:, s0:s0 + PC], data1=bt[:, s0:s0 + PC],
                                                                       initial=init, op0=ALU.mult, op1=ALU.add), r=[at, bt, hf], w=[hf])
                    else:
                        pieces = [(0, CTX, None)] + [(s0, s0 + PC, None) for s0 in range(S - PC, CTX - 1, -PC)]
                        prev_first = None
                        for (a0, a1, _) in pieces:
                            init = 0.0 if prev_first is None else hb[:, prev_first:prev_first + 1]
                            rv = lambda tl: tl[:, a0:a1][:, ::-1]
                            p.op("dve", lambda e: e.tensor_tensor_scan(out=rv(hb), data0=rv(at), data1=rv(bt),
                                                                       initial=init, op0=ALU.mult, op1=ALU.add), r=[at, bt, hb], w=[hb])
                            prev_first = a0
                p.tt("dve", hf[:], hf[:], hb[:], ALU.add, r=[hf, hb], w=[hf])
                p.tt("dve", ybf[:], hf[:], gl[:], ALU.mult, r=[hf, gl], w=[ybf])
                p.dma("sp", lruT[c], ybf[:], r=[ybf], w=[lruT], merge=True)

        with p.scope():
            wro = p.sb([128, 4, D], BF16, "wro")
            who = p.sb([128, 4, D], BF16, "who")
            wlo = p.sb([128, 4, D], BF16, "wlo")
            wo = p.sb([128, KC, D], BF16, "wo")
            wr = p.sb([128, KC, NEXP], F32, "wr")
            rbrow = p.sb([128, NEXP], F32, "rbrow")
            p.dma("pool", wro[:], w_ret_o[l].rearrange("(k p) n -> p k n", p=128), w=[wro])
            p.dma("pool", who[:], w_hg_o[l].rearrange("(k p) n -> p k n", p=128), w=[who])
            p.dma("pool", wlo[:], w_lru_o[l].rearrange("(k p) n -> p k n", p=128), w=[wlo])
            p.dma("pool", wo[:], w_out[l].rearrange("(k p) n -> p k n", p=128), w=[wo])
            p.dma("sp", wr[:], router_w[l].rearrange("(k p) n -> p k n", p=128), w=[wr])
            p.dma("sp", rbrow[:], router_b[l:l + 1, :].partition_broadcast(128), w=[rbrow])
            bt3 = [p.sb([128, 4, 512], BF16, "br%d" % i) for i in range(3)]
            gts = [[p.sb([128, 512], F32, "gt%d" % i) for i in range(3)] for _ in range(2)]
            mg = p.sb([128, KC, 512], BF16, "mg")
            t1 = [p.sb([128, 512], F32, "et1") for _ in range(2)]
            t2 = [p.sb([128, 512], F32, "et2") for _ in range(2)]
            xo = p.sb([128, KC, 512], F32, "exo")
            xn = p.sb([128, KC, 512], F32, "exn")
            sq = p.sb([128, KC, 512], F32, "esq")
            rstd = p.sb([128, 512], F32, "erstd")
            h2b = p.sb([128, KC, 512], BF16, "h2b")
            lgt = p.sb([128, NEXP], F32, "lgt")
            top8 = p.sb([128, 8], F32, "top8")
            wrow = p.sb([128, NEXP], F32, "wrow")
            ssum = p.sb([128, 2], F32, "ssum")
            for (t0, N, isctx) in tiles:
                if last and isctx:
                    continue
                col = 0 if isctx else 1
                for i, src in enumerate((retnT, hgnT, lruT)):
                    p.dma("sp", bt3[i][:, :, :N], src[:, :, t0:t0 + N].rearrange("c p n -> p c n"), r=[src], w=[bt3[i]])
                p.dma("sp", xo[:, :, :N], xT[:, :, t0:t0 + N].rearrange("k p n -> p k n"), r=[xT], w=[xo])
                for m in range(KC):
                    b = m % 2
                    for i in range(3):
                        p.dma("sp", gts[b][i][:, :N], g_T[i, m, :, t0:t0 + N], r=[g_T], w=[gts[b][i]])
                    pss = []
                    for i, wgt in enumerate((wro, who, wlo)):
                        ps = nps()
                        for kc in range(4):
                            p.mm(ps[:, :N], wgt[:, kc, m * 128:(m + 1) * 128], bt3[i][:, kc, :N], kc == 0, kc == 3, r=[wgt, bt3[i]], w=[ps])
                        pss.append(ps)
                    p.tt("dve", t1[b][:, :N], pss[0][:, :N], gts[b][0][:, :N], ALU.mult, r=[pss[0], gts[b][0]], w=[t1[b]])
                    p.tt("dve", t2[b][:, :N], pss[1][:, :N], gts[b][1][:, :N], ALU.mult, r=[pss[1], gts[b][1]], w=[t2[b]])
                    p.tt("pool", t1[b][:, :N], t1[b][:, :N], t2[b][:, :N], ALU.add, r=[t1[b], t2[b]], w=[t1[b]])
                    p.tt("dve", t2[b][:, :N], pss[2][:, :N], gts[b][2][:, :N], ALU.mult, r=[pss[2], gts[b][2]], w=[t2[b]])
                    p.tt("pool", mg[:, m, :N], t1[b][:, :N], t2[b][:, :N], ALU.add, r=[t1[b], t2[b]], w=[mg])
                for m in range(KC):
                    ps = nps()
                    for kc in range(KC):
                        p.mm(ps[:, :N], wo[:, kc, m * 128:(m + 1) * 128], mg[:, kc, :N], kc == 0, kc == KC - 1, r=[wo, mg], w=[ps])
                    p.stt("dve", xn[:, m, :N], ps[:, :N], modv[:, 2, m, col:col + 1], xo[:, m, :N], ALU.mult, ALU.add, r=[ps, modv, xo], w=[xn])
                p.dma("sp", xT[:, :, t0:t0 + N].rearrange("k p n -> p k n"), xn[:, :, :N], r=[xn], w=[xT], merge=True)
                p.act(sq[:, :, :N], xn[:, :, :N], AF.Square, r=[xn], w=[sq])
                ps = nps()
                for kc in range(KC):
                    p.mm(ps[:, :N], ones, sq[:, kc, :N], kc == 0, kc == KC - 1, r=[cst, sq], w=[ps])
                p.act(rstd[:, :N], ps[:, :N], AF.Ln, r=[ps], w=[rstd], bias=EPS, scale=1.0 / D)
                p.act(rstd[:, :N], rstd[:, :N], AF.Exp, r=[rstd], w=[rstd], scale=-0.5)
                for kc in range(KC):
                    p.stt("dve", sq[:, kc, :N], xn[:, kc, :N], modv[:, 3, kc, col:col + 1], rstd[:, :N], ALU.mult, ALU.mult, r=[xn, modv, rstd], w=[sq])
                    p.act(sq[:, kc, :N], sq[:, kc, :N], AF.Identity, r=[sq, modv], w=[sq], bias=modv[:, 4, kc, col:col + 1])
                p.copy("pool", h2b[:, :, :N], sq[:, :, :N], r=[sq], w=[h2b])
                p.dma("sp", h2T_d[:, :, t0:t0 + N].rearrange("k p n -> p k n"), h2b[:, :, :N], r=[h2b], w=[h2T_d], merge=True)
                for s in range(N // 128):
                    ps = nps()
                    for kc in range(KC):
                        p.mm(ps[:, 0:NEXP], sq[:, kc, s * 128:(s + 1) * 128], wr[:, kc, :], kc == 0, kc == KC - 1, r=[sq, wr], w=[ps])
                    p.tt("dve", lgt[:], ps[:, 0:NEXP], rbrow[:], ALU.add, r=[ps, rbrow], w=[lgt])
                    p.op("dve", lambda e: e.max(out=top8[:], in_=lgt[:]), r=[lgt], w=[top8])
                    p.ts("dve", wrow[:], lgt[:], top8[:, 3:4], None, ALU.is_ge, None, r=[lgt, top8], w=[wrow])
                    p.ts("dve", ssum[:, 0:1], top8[:, 0:1], -1.0, None, ALU.mult, None, r=[top8], w=[ssum])
                    p.act(lgt[:], lgt[:], AF.Exp, r=[lgt, ssum], w=[lgt], bias=ssum[:, 0:1])
                    p.tt("dve", wrow[:], wrow[:], lgt[:], ALU.mult, r=[wrow, lgt], w=[wrow])
                    p.op("dve", lambda e: e.reduce_sum(out=ssum[:, 1:2], in_=wrow[:], axis=AX.X), r=[wrow], w=[ssum])
                    p.op("dve", lambda e: e.reciprocal(out=ssum[:, 1:2], in_=ssum[:, 1:2]), r=[ssum], w=[ssum])
                    p.ts("dve", wrow[:], wrow[:], ssum[:, 1:2], None, ALU.mult, None, r=[wrow, ssum], w=[wrow])
                    p.dma("sp", wmoe_d[t0 + s * 128:t0 + (s + 1) * 128, :], wrow[:], r=[wrow], w=[wmoe_d], merge=True)

        with p.scope():
            mtiles = [tl for tl in tiles if not (last and tl[2])]
            groups = [mtiles[i:i + 2] for i in range(0, len(mtiles), 2)]
            wgu = [p.sb([128, KC, 2 * D], BF16, "wgu%d" % i) for i in range(2)]
            wdn = [p.sb([128, KC, D], BF16, "wdn%d" % i) for i in range(2)]
            h2 = p.sb([128, KC, 1024], BF16, "mh2")
            accs = [p.sb([128, D], F32, "acc%d" % i) for i in range(8)]
            wsb = [p.sb([128, NEXP], F32, "mw%d" % i) for i in range(8)]
            aT = [p.sb([128, KC, 512], BF16, "aT%d" % i) for i in range(2)]
            g1 = [p.sb([128, 512], F32, "mg%d" % i) for i in range(2)]
            s1 = [p.sb([128, 512], F32, "ms%d" % i) for i in range(2)]
            u1 = [p.sb([128, 512], F32, "mu%d" % i) for i in range(2)]
            bdn = p.sb([NEXP, D], F32, "bdn")
            wT = p.sb([NEXP, 128], F32, "wT")
            xo = p.sb([128, 512], F32, "fxo")
            xn2 = p.sb([128, 512], F32, "fxn")
            p.dma("sp", bdn[:], b_dn[l], w=[bdn])
            ei = 0
            for grp in groups:
                gt0 = grp[0][0]
                gN = sum(tl[1] for tl in grp)
                nsub = gN // 128
                p.dma("sp", h2[:, :, :gN], h2T_d[:, :, gt0:gt0 + gN].rearrange("k p n -> p k n"), r=[h2T_d], w=[h2])
                for s in range(nsub):
                    p.dma("sp", wsb[s][:], wmoe_d[gt0 + s * 128:gt0 + (s + 1) * 128, :], r=[wmoe_d], w=[wsb[s]])
                for e in range(NEXP):
                    wb = ei % 2
                    ei += 1
                    for hf_ in range(2):
                        p.dma("pool", wgu[wb][:, :, hf_ * D:(hf_ + 1) * D], w_gu[l, e, :, hf_ * D:(hf_ + 1) * D].rearrange("(k p) n -> p k n", p=128),
                              w=[wgu[wb]], merge=(hf_ == 1))
                    p.dma("pool", wdn[wb][:], w_dn[l, e].rearrange("(k p) n -> p k n", p=128), w=[wdn[wb]])
                    off = 0
                    for ti, (t0, N, isctx) in enumerate(grp):
                        a = aT[ti % 2]
                        for m in range(KC):
                            b = m % 2
                            pg = nps()
                            for kc in range(KC):
                                p.mm(pg[:, :N], wgu[wb][:, kc, m * 128:(m + 1) * 128], h2[:, kc, off:off + N], kc == 0, kc == KC - 1, r=[wgu[wb], h2], w=[pg])
                            pu = nps()
                            for kc in range(KC):
                                p.mm(pu[:, :N], wgu[wb][:, kc, D + m * 128:D + (m + 1) * 128], h2[:, kc, off:off + N], kc == 0, kc == KC - 1, r=[wgu[wb], h2], w=[pu])
                            bg = fv[:, FV_BGU + e * 16 + m:FV_BGU + e * 16 + m + 1]
                            bu = fv[:, FV_BGU + e * 16 + 8 + m:FV_BGU + e * 16 + 8 + m + 1]
                            p.ts("dve", g1[b][:, :N], pg[:, :N], bg, 7.0, ALU.add, ALU.min, r=[pg, fv], w=[g1[b]])
                            p.act(s1[b][:, :N], g1[b][:, :N], AF.Sigmoid, r=[g1[b]], w=[s1[b]], scale=1.702)
                            p.ts("dve", u1[b][:, :N], pu[:, :N], bu, 7.0, ALU.add, ALU.min, r=[pu, fv], w=[u1[b]])
                            p.ts("pool", u1[b][:, :N], u1[b][:, :N], -7.0, 1.0, ALU.max, ALU.add, r=[u1[b]], w=[u1[b]])
                            p.tt("pool", g1[b][:, :N], g1[b][:, :N], s1[b][:, :N], ALU.mult, r=[g1[b], s1[b]], w=[g1[b]])
                            p.tt("dve", a[:, m, :N], g1[b][:, :N], u1[b][:, :N], ALU.mult, r=[g1[b], u1[b]], w=[a])
                        for s in range(N // 128):
                            si = off // 128 + s
                            for hf_ in range(2):
                                pd = nps()
                                for kc in range(KC):
                                    p.mm(pd[:], a[:, kc, s * 128:(s + 1) * 128], wdn[wb][:, kc, hf_ * 512:(hf_ + 1) * 512], kc == 0, kc == KC - 1, r=[a, wdn[wb]], w=[pd])
                                cs = slice(hf_ * 512, (hf_ + 1) * 512)
                                if e == 0:
                                    p.ts("dve", accs[si][:, cs], pd[:], wsb[si][:, e:e + 1], None, ALU.mult, None, r=[pd, wsb[si]], w=[accs[si]])
                                else:
                                    p.stt("dve", accs[si][:, cs], pd[:], wsb[si][:, e:e + 1], accs[si][:, cs], ALU.mult, ALU.add, r=[pd, wsb[si], accs[si]], w=[accs[si]])
                        off += N
                off = 0
                for (t0, N, isctx) in grp:
                    col = 0 if isctx else 1
                    for s in range(N // 128):
                        si = off // 128 + s
                        pt = nps()
                        p.tr(pt[0:NEXP, 0:128], wsb[si][:], ident, r=[wsb[si], cst], w=[pt])
                        p.copy("dve", wT[:], pt[0:NEXP, 0:128], r=[pt], w=[wT])
                        for hf_ in range(2):
                            pb = nps()
                            p.mm(pb[:], wT[:], bdn[:, hf_ * 512:(hf_ + 1) * 512], True, True, r=[wT, bdn], w=[pb])
                            cs = slice(hf_ * 512, (hf_ + 1) * 512)
                            p.tt("dve", accs[si][:, cs], accs[si][:, cs], pb[:], ALU.add, r=[accs[si], pb], w=[accs[si]])
                    for m in range(KC):
                        p.dma("sp", xo[:, :N], xT[m, :, t0:t0 + N], r=[xT], w=[xo])
                        pt = nps()
                        for s in range(N // 128):
                            si = off // 128 + s
                            p.tr(pt[:, s * 128:(s + 1) * 128], accs[si][:, m * 128:(m + 1) * 128], ident, r=[accs[si], cst], w=[pt])
                        p.stt("dve", xn2[:, :N], pt[:, :N], modv[:, 5, m, col:col + 1], xo[:, :N], ALU.mult, ALU.add, r=[pt, modv, xo], w=[xn2])
                        p.dma("sp", xT[m, :, t0:t0 + N], xn2[:, :N], r=[xn2], w=[xT], merge=True)
                    off += N

    with p.scope():
        xt = p.sb([128, KC, 512], F32, "fx")
        sq = p.sb([128, KC, 512], F32, "fsq")
        rstd = p.sb([128, 512], F32, "frstd")
        ot = [p.sb([128, D], F32, "fot%d" % i) for i in range(2)]
        p.dma("sp", fv[:], fv_d[0], w=[fv])
        for (t0, N, isctx) in tiles:
            if isctx:
                continue
            p.dma("sp", xt[:, :, :N], xT[:, :, t0:t0 + N].rearrange("k p n -> p k n"), r=[xT], w=[xt])
            p.act(sq[:, :, :N], xt[:, :, :N], AF.Square, r=[xt], w=[sq])
            ps = nps()
            for kc in range(KC):
                p.mm(ps[:, :N], ones, sq[:, kc, :N], kc == 0, kc == KC - 1, r=[cst, sq], w=[ps])
            p.act(rstd[:, :N], ps[:, :N], AF.Ln, r=[ps], w=[rstd], bias=EPS, scale=1.0 / D)
            p.act(rstd[:, :N], rstd[:, :N], AF.Exp, r=[rstd], w=[rstd], scale=-0.5)
            for kc in range(KC):
                p.stt("dve", sq[:, kc, :N], xt[:, kc, :N], fv[:, FV_FG + kc:FV_FG + kc + 1], rstd[:, :N], ALU.mult, ALU.mult, r=[xt, fv, rstd], w=[sq])
            for s in range(N // 128):
                o = ot[s % 2]
                for half in range(2):
                    ps = nps()
                    for j in range(4):
                        kc = half * 4 + j
                        p.tr(ps[:, j * 128:(j + 1) * 128], sq[:, kc, s * 128:(s + 1) * 128], ident, r=[sq, cst], w=[ps])
                    p.copy("dve" if half == 0 else "act", o[:, half * 512:(half + 1) * 512], ps[:], r=[ps], w=[o])
                r0 = t0 - CTX + s * 128
                p.dma("sp", out_d[r0:r0 + 128, :], o[:], r=[o], w=[out_d], merge=True)
    p.finish()
    return nc, p


def prep_shared(inputs, depth, lat):
    f = lambda a: np.ascontiguousarray(np.asarray(a, dtype=np.float32))
    fm = lambda v, n: np.asarray(v, np.float32).reshape(n, 128).T
    fv = np.zeros((depth, 128, NFV), np.float32)
    for l in range(depth):
        fv[l, :, FV_N1:FV_N1 + 8] = fm(inputs["norm1_g"][l], 8)
        fv[l, :, FV_N2:FV_N2 + 8] = fm(inputs["norm2_g"][l], 8)
        fv[l, :, FV_MODB:FV_MODB + 48] = fm(inputs["mod_b"][l], 48)
        cw = np.asarray(inputs["lru_conv_w"][l], np.float32)
        for w in range(4):
            fv[l, :, FV_CW + w * 4:FV_CW + w * 4 + 4] = fm(cw[w], 4)
        fv[l, :, FV_CB:FV_CB + 4] = fm(inputs["lru_conv_b"][l], 4)
        for d in range(2):
            fv[l, :, FV_BA + d * 4:FV_BA + d * 4 + 4] = fm(inputs["lru_ba"][l][d], 4)
            fv[l, :, FV_BX + d * 4:FV_BX + d * 4 + 4] = fm(inputs["lru_bx"][l][d], 4)
            fv[l, :, FV_LAM + d * 4:FV_LAM + d * 4 + 4] = fm(inputs["lru_lambda"][l][d], 4)
        fv[l, :, FV_FG:FV_FG + 8] = fm(inputs["final_g"], 8)
        bgu = np.asarray(inputs["exp_b_gu"][l], np.float32)
        fv[l, :, FV_BGU:FV_BGU + 512] = bgu.reshape(NEXP, 16, 128).transpose(2, 0, 1).reshape(128, 512)
    lbl = np.asarray(inputs["hgrn_lb_logits"], np.float32)
    if lbl.shape[0] < 4:
        lbl = np.concatenate([lbl, np.full((4 - lbl.shape[0], 2, 512), -100.0, np.float32)], axis=0)
    lblT = lbl.reshape(lbl.shape[0], 2, 4, 128).transpose(3, 0, 1, 2).reshape(128, lbl.shape[0] * 8)
    lblT4 = np.zeros((128, 32), np.float32)
    lblT4[:, :lblT.shape[1]] = lblT
    bd = np.zeros((depth, 2, 2, 4, 128, 128), np.float32)
    for l in range(depth):
        for ai, nm in enumerate(("lru_wa", "lru_wx")):
            wsrc = np.asarray(inputs[nm][l], np.float32)
            for d in range(2):
                for c in range(4):
                    for j in range(2):
                        bd[l, ai, d, c, j * 64:(j + 1) * 64, j * 64:(j + 1) * 64] = wsrc[d, c * 2 + j]
    sh = {
        "consts": make_consts(), "rot": make_rot(lat), "fv": fv, "lblT": lblT4,
        "hgrn_lb_logits": f(lbl).reshape(1, -1),
        "mod_w": f(inputs["mod_w"][:depth]), "w_in": f(inputs["w_in"][:depth]),
        "ret_decay": f(inputs["ret_decay"][:depth]).reshape(depth, 16),
        "ret_gn_g": f(inputs["ret_gn_g"][:depth]), "hgrn_gn_g": f(inputs["hgrn_gn_g"][:depth]),
        "w_ret_o": f(inputs["w_ret_o"][:depth]), "w_hgrn_o": f(inputs["w_hgrn_o"][:depth]), "w_lru_o": f(inputs["w_lru_o"][:depth]),
        "w_out": f(inputs["w_out"][:depth]), "lru_bd": bd,
        "router_w": f(inputs["router_w"][:depth]), "router_b": f(inputs["router_b"][:depth]),
        "exp_w_gu": f(inputs["exp_w_gu"][:depth]), "exp_w_down": f(inputs["exp_w_down"][:depth]), "exp_b_down": f(inputs["exp_b_down"][:depth]),
    }
    return sh


def core_inputs(inputs, b, sh):
    x = np.asarray(inputs["x"][b], np.float32)
    ctx = np.asarray(inputs["ctx"][b], np.float32)
    xin = np.ascontiguousarray(np.concatenate([ctx, x], axis=0))
    sc = np.stack([np.asarray(inputs["c_ctx"], np.float32), np.asarray(inputs["c"][b], np.float32)], axis=-1)
    scin = np.ascontiguousarray(sc.reshape(KC, 128, 2).transpose(1, 0, 2).reshape(128, KC * 2))
    m = dict(sh)
    m["xin"] = xin
    m["scin"] = scin
    return m


_CACHE = {}


def kernel(**inputs):
    B, L, _ = inputs["x"].shape
    depth = inputs["w_in"].shape[0]
    key = (L, depth)
    if key not in _CACHE:
        _CACHE[key] = build(L, depth)[0]
    nc = _CACHE[key]
    sh = prep_shared(inputs, depth, L)
    n = 8
    in_maps = [core_inputs(inputs, c % B, sh) for c in range(n)]
    res = run_bass_kernel_spmd(nc, in_maps, core_ids=list(range(n)))
    out = np.stack([np.asarray(res.results[b]["out"], np.float32) for b in range(B)], axis=0)
    return out
```

```python
import contextlib
import numpy as np
import concourse.bass as bass
import concourse.mybir as mybir
from concourse.bass_utils import run_bass_kernel_spmd

F32 = mybir.dt.float32
BF16 = mybir.dt.bfloat16
ALU = mybir.AluOpType
AF = mybir.ActivationFunctionType
AX = mybir.AxisListType

D = 1024
KC = 8
CTX = 256
NEXP = 32
EPS = 1e-6
IN_COLS = 8704


class T:
    __slots__ = ("h", "w", "r", "name")

    def __init__(self, h, name):
        self.h = h
        self.name = name
        self.w = {}
        self.r = {}

    def __getitem__(self, k):
        return self.h[k]


class Prog:
    NDMA = 8

    def __init__(self, nc, same_engine_sync=True):
        self.nc = nc
        self.stack = [contextlib.ExitStack()]
        self.eng = {"pe": nc.tensor, "act": nc.scalar, "dve": nc.vector, "pool": nc.gpsimd, "sp": nc.sync}
        self.semh = {}
        self.cnt = {}
        self.known = {k: {} for k in self.eng}
        for k in self.eng:
            self.semh[k] = self.stack[0].enter_context(nc.semaphore("s_" + k))
            self.cnt[k] = 0
        self.dq = {}
        for q in ("sp", "pool", "act"):
            sems = []
            for i in range(self.NDMA):
                key = "d_%s%d" % (q, i)
                self.semh[key] = self.stack[0].enter_context(nc.semaphore(key))
                self.cnt[key] = 0
                sems.append(key)
            self.dq[q] = [sems, 0]
        self.same = same_engine_sync
        self.uid = 0
        self.ninst = 0

    @contextlib.contextmanager
    def scope(self, name=None):
        es = contextlib.ExitStack()
        self.stack.append(es)
        if name is not None:
            es.enter_context(self.nc.named_scope(name))
        try:
            yield
        finally:
            self.barrier()
            self.stack.pop()
            es.close()

    def sb(self, shape, dt=F32, name=None):
        self.uid += 1
        name = (name or "t") + "_%d" % self.uid
        h = self.stack[-1].enter_context(self.nc.sbuf_tensor(name, list(shape), dt))
        return T(h, name)

    def ps(self, shape, dt=F32, name=None):
        self.uid += 1
        name = (name or "p") + "_%d" % self.uid
        h = self.stack[-1].enter_context(self.nc.psum_tensor(name, list(shape), dt))
        return T(h, name)

    def dram(self, name, shape, dt, kind="Internal"):
        h = self.nc.dram_tensor(name, list(shape), dt, kind=kind)
        return T(h.ap(), name)

    def _wait(self, ek, need):
        e = self.eng[ek]
        kn = self.known[ek]
        for s, v in need.items():
            if s == ek and (not self.same or ek == "pe"):
                continue
            if kn.get(s, 0) < v:
                e.wait_ge(self.semh[s], v)
                kn[s] = v

    @staticmethod
    def _merge(d, s):
        for k, v in s.items():
            if d.get(k, 0) < v:
                d[k] = v

    def _need(self, r, w, skip_waw=False):
        need = {}
        for t in r:
            self._merge(need, t.w)
        for t in w:
            if not skip_waw:
                self._merge(need, t.w)
            self._merge(need, t.r)
        return need

    def _record(self, tok, r, w, merge=False):
        for t in w:
            if merge:
                self._merge(t.w, tok)
            else:
                t.w = dict(tok)
                t.r = {}
        for t in r:
            if t not in w:
                self._merge(t.r, tok)

    def op(self, ek, fn, r=(), w=()):
        self._wait(ek, self._need(r, w))
        inst = fn(self.eng[ek])
        self.cnt[ek] += 1
        inst.then_inc(self.semh[ek], 1)
        self.ninst += 1
        self._record({ek: self.cnt[ek]}, r, w)
        return inst

    def dma(self, q, out_ap, in_ap, r=(), w=(), merge=False):
        sems, i = self.dq[q]
        key = sems[i % len(sems)]
        self.dq[q][1] = i + 1
        need = self._need(r, w, skip_waw=merge)
        if self.cnt[key] > 0:
            need[key] = max(need.get(key, 0), self.cnt[key])
        self._wait(q, need)
        inst = self.eng[q].dma_start(out=out_ap, in_=in_ap)
        self.cnt[key] += 16
        inst.then_inc(self.semh[key], 16)
        self.ninst += 1
        self._record({key: self.cnt[key]}, r, w, merge=merge)
        return inst

    def barrier(self):
        allv = {k: v for k, v in self.cnt.items() if v > 0}
        for ek in self.eng:
            self._wait(ek, dict(allv))

    def finish(self):
        self.barrier()
        while self.stack:
            self.stack.pop().close()

    def tt(self, ek, out, in0, in1, op, r, w):
        return self.op(ek, lambda e: e.tensor_tensor(out=out, in0=in0, in1=in1, op=op), r=r, w=w)

    def ts(self, ek, out, in0, s1, s2, op0, op1, r, w):
        if s2 is None:
            return self.op(ek, lambda e: e.tensor_scalar(out=out, in0=in0, scalar1=s1, scalar2=None, op0=op0), r=r, w=w)
        return self.op(ek, lambda e: e.tensor_scalar(out=out, in0=in0, scalar1=s1, scalar2=s2, op0=op0, op1=op1), r=r, w=w)

    def stt(self, ek, out, in0, scalar, in1, op0, op1, r, w):
        return self.op(ek, lambda e: e.scalar_tensor_tensor(out=out, in0=in0, scalar=scalar, in1=in1, op0=op0, op1=op1), r=r, w=w)

    def act(self, out, in_, func, r, w, bias=None, scale=None):
        kw = {}
        if bias is not None:
            kw["bias"] = bias
        if scale is not None:
            kw["scale"] = scale
        return self.op("act", lambda e: e.activation(out=out, in_=in_, func=func, **kw), r=r, w=w)

    def copy(self, ek, out, in_, r, w):
        if ek == "act":
            return self.op("act", lambda e: e.copy(out=out, in_=in_), r=r, w=w)
        return self.op(ek, lambda e: e.tensor_copy(out=out, in_=in_), r=r, w=w)

    def mm(self, out, lhsT, rhs, start, stop, r, w):
        return self.op("pe", lambda e: e.matmul(out, lhsT=lhsT, rhs=rhs, start=start, stop=stop), r=r, w=w)

    def tr(self, out, in_, ident, r, w):
        return self.op("pe", lambda e: e.transpose(out=out, in_=in_, identity=ident), r=r, w=w)


C_ID = 0
C_MF = 128
C_MB = 256
C_TF = 384
C_TB = 448
C_RF = 512
C_RB = 576
C_IDX = 640
C_ONE = 646
NCONST = 774

FV_N1 = 0
FV_N2 = 8
FV_MODB = 16
FV_CW = 64
FV_CB = 80
FV_BA = 84
FV_BX = 92
FV_LAM = 100
FV_FG = 108
FV_BGU = 116
NFV = 628

G_RQ, G_RK, G_RV, G_RG, G_HQ, G_HFF, G_HFB, G_HI, G_HGT, G_LX, G_LGT = [512 * i for i in range(11)]
G_GR = 5632
G_GH = 6656
G_GL = 7680


def make_consts():
    c = np.zeros((128, NCONST), np.float32)
    p = np.arange(128)
    c[:, C_ID:C_ID + 128] = np.eye(128)
    c[:, C_MF:C_MF + 128] = (p[None, :] >= p[:, None])
    c[:, C_MB:C_MB + 128] = (p[:, None] > p[None, :])
    q = np.arange(64)
    c[:64, C_TF:C_TF + 64] = (q[:, None] <= q[None, :])
    c[:64, C_TB:C_TB + 64] = (q[:, None] >= q[None, :])
    c[:64, C_RF:C_RF + 64] = (q[:, None] > q[None, :])
    c[:64, C_RB:C_RB + 64] = (q[:, None] < q[None, :])
    c[:, C_IDX + 0] = p + 1
    c[:, C_IDX + 1] = 127 - p
    c[:, C_IDX + 2] = 128 - p
    c[:, C_IDX + 3] = p
    c[:, C_IDX + 4] = -(p + 1)
    c[:, C_IDX + 5] = p - 128
    c[:, C_ONE:C_ONE + 128] = 1.0
    return c


def make_rot(lat):
    n = np.arange(lat)
    row = (n // 64).astype(np.float32)
    col = (n % 64).astype(np.float32)
    inv = (10000.0 ** (-np.arange(16, dtype=np.float32) / 16)).astype(np.float32)
    ang = np.concatenate([row[:, None] * inv, col[:, None] * inv], axis=-1).astype(np.float32)
    cs, sn = np.cos(ang), np.sin(ang)
    return np.concatenate([cs, sn, 0.125 * cs, 0.125 * sn], axis=-1).astype(np.float32)


def build(lat, depth, dbg=()):
    S = CTX + lat
    nc = bass.Bass("TRN2", target_bir_lowering=False)
    p = Prog(nc)
    tiles = [(0, 256, True)] + [(CTX + 512 * i, 512, False) for i in range(lat // 512)]
    NT128 = S // 128

    def din(name, shape):
        return p.dram(name, shape, F32, kind="ExternalInput")

    xin = din("xin", [S, D])
    scin = din("scin", [128, KC * 2])
    consts_d = din("consts", [128, NCONST])
    rot_d = din("rot", [lat, 128])
    fv_d = din("fv", [depth, 128, NFV])
    lblT_d = din("lblT", [128, 4 * 8])
    lbl_d = din("hgrn_lb_logits", [1, 4 * 2 * 512])
    mod_w = din("mod_w", [depth, D, 6 * D])
    w_in = din("w_in", [depth, D, IN_COLS])
    ret_decay = din("ret_decay", [depth, 16])
    ret_gn = din("ret_gn_g", [depth, 512])
    hg_gn = din("hgrn_gn_g", [depth, 512])
    w_ret_o = din("w_ret_o", [depth, 512, D])
    w_hg_o = din("w_hgrn_o", [depth, 512, D])
    w_lru_o = din("w_lru_o", [depth, 512, D])
    w_out = din("w_out", [depth, D, D])
    lru_bd = din("lru_bd", [depth, 2, 2, 4, 128, 128])
    router_w = din("router_w", [depth, D, NEXP])
    router_b = din("router_b", [depth, NEXP])
    w_gu = din("exp_w_gu", [depth, NEXP, D, 2 * D])
    w_dn = din("exp_w_down", [depth, NEXP, D, D])
    b_dn = din("exp_b_down", [depth, NEXP, D])
    out_d = p.dram("out", [lat, D], F32, kind="ExternalOutput")

    def scr(name, shape, dt=F32):
        kind = "ExternalOutput" if name in dbg else "Internal"
        return p.dram(name, shape, dt, kind=kind)

    xT = scr("xT", [KC, 128, S])
    r_q = scr("r_q", [S, 512], BF16)
    r_k = scr("r_k", [S, 512], BF16)
    r_v = scr("r_v", [S, 512], BF16)
    r_g = scr("r_g", [S, 512])
    h_qT = scr("h_qT", [4, 128, S])
    h_kT = [scr("h_kfT", [4, 128, S]), scr("h_kbT", [4, 128, S])]
    h_k = [scr("h_kf", [S, 512]), scr("h_kb", [S, 512])]
    h_lf = [scr("h_lff", [S, 512]), scr("h_lfb", [S, 512])]
    h_v = scr("h_v", [S, 512], BF16)
    h_g = scr("h_g", [S, 512])
    l_xT = scr("l_xT", [4, 128, S])
    l_gT = scr("l_gT", [4, 128, S])
    g_T = scr("g_T", [3, KC, 128, S])
    o_ret = scr("o_ret", [S, 512])
    o_hg = scr("o_hg", [S, 512])
    retnT = scr("retnT", [4, 128, S], BF16)
    hgnT = scr("hgnT", [4, 128, S], BF16)
    lruT = scr("lruT", [4, 128, S], BF16)
    h2T_d = scr("h2T", [KC, 128, S], BF16)
    lbrow_d = scr("lbrow", [4, 128, 2048])
    wmoe_d = scr("wmoe", [S, NEXP])
    wgu_bf = [[p.dram("wgubf_%d_%d" % (i, e), [D, 2 * D], BF16) for e in range(NEXP)] for i in range(2)]
    wdn_bf = [[p.dram("wdnbf_%d_%d" % (i, e), [D, D], BF16) for e in range(NEXP)] for i in range(2)]

    def convert_expert(ll, e):
        par = ll % 2
        for q4 in range(4):
            rs = slice(q4 * 256, (q4 + 1) * 256)
            p.dma("pool", wgu_bf[par][e][rs, :], w_gu[ll, e, rs, :], w=[wgu_bf[par][e]], merge=True)
        for q2 in range(2):
            rs = slice(q2 * 512, (q2 + 1) * 512)
            p.dma("pool", wdn_bf[par][e][rs, :], w_dn[ll, e, rs, :], w=[wdn_bf[par][e]], merge=True)

    cst = p.sb([128, NCONST], F32, "cst")
    identb = p.sb([128, 128], BF16, "identb")
    sT = p.sb([128, KC * 2], F32, "sT")
    lbT = p.sb([128, 32], F32, "lbT")
    omlT = p.sb([128, 32], F32, "omlT")
    fv = p.sb([128, NFV], F32, "fv")
    modv = p.sb([128, 6, KC, 2], F32, "modv")
    psb = [p.ps([128, 512], F32, "psf%d" % i) for i in range(6)]
    psh = [p.ps([128, 1024], BF16, "psh%d" % i) for i in range(2)]
    rr = {"f": 0, "h": 0}

    def nps():
        rr["f"] += 1
        return psb[rr["f"] % 6]

    def npsh():
        rr["h"] += 1
        return psh[rr["h"] % 2]

    ident = cst[:, C_ID:C_ID + 128]
    ones = cst[:, C_ONE:C_ONE + 128]

    p.dma("sp", cst[:], consts_d[:], w=[cst])
    p.dma("sp", sT[:], scin[:], w=[sT])
    p.copy("dve", identb[:], ident, r=[cst], w=[identb])
    with p.scope():
        t0 = p.sb([128, KC * 2])
        p.act(t0[:], sT[:], AF.Sigmoid, r=[sT], w=[t0])
        p.tt("dve", sT[:], sT[:], t0[:], ALU.mult, r=[sT, t0], w=[sT])
        e0 = p.sb([128, 32])
        p.dma("sp", e0[:], lblT_d[:], w=[e0])
        p.act(e0[:], e0[:], AF.Exp, r=[e0], w=[e0])
        sm = p.sb([128, 8])
        p.tt("dve", sm[:], e0[:, 0:8], e0[:, 8:16], ALU.add, r=[e0], w=[sm])
        p.tt("dve", sm[:], sm[:], e0[:, 16:24], ALU.add, r=[e0, sm], w=[sm])
        p.tt("dve", sm[:], sm[:], e0[:, 24:32], ALU.add, r=[e0, sm], w=[sm])
        p.op("dve", lambda e: e.reciprocal(out=sm[:], in_=sm[:]), r=[sm], w=[sm])
        p.op("dve", lambda e: e.memset(lbT[:, 0:8], 0.0), w=[lbT])
        for l in range(1, 4):
            p.tt("dve", e0[:, 8 * l:8 * l + 8], e0[:, 8 * l:8 * l + 8], sm[:], ALU.mult, r=[e0, sm], w=[e0])
            p.tt("dve", lbT[:, 8 * l:8 * l + 8], lbT[:, 8 * l - 8:8 * l], e0[:, 8 * l:8 * l + 8], ALU.add, r=[e0, lbT], w=[lbT])
        p.ts("dve", omlT[:], lbT[:], -1.0, 1.0, ALU.mult, ALU.add, r=[lbT], w=[omlT])
        er = p.sb([128, 4, 1024])
        p.dma("sp", er[:].rearrange("p l n -> p (l n)"), lbl_d[:].partition_broadcast(128), w=[er])
        p.act(er[:], er[:], AF.Exp, r=[er], w=[er])
        sr = p.sb([128, 1024])
        p.tt("dve", sr[:], er[:, 0, :], er[:, 1, :], ALU.add, r=[er], w=[sr])
        p.tt("dve", sr[:], sr[:], er[:, 2, :], ALU.add, r=[er, sr], w=[sr])
        p.tt("dve", sr[:], sr[:], er[:, 3, :], ALU.add, r=[er, sr], w=[sr])
        p.op("dve", lambda e: e.reciprocal(out=sr[:], in_=sr[:]), r=[sr], w=[sr])
        lbr = p.sb([128, 2048])
        p.op("dve", lambda e: e.memset(lbr[:, 0:1024], 0.0), w=[lbr])
        for l in range(4):
            if l > 0:
                p.tt("dve", er[:, l, :], er[:, l, :], sr[:], ALU.mult, r=[er, sr], w=[er])
                p.tt("dve", lbr[:, 0:1024], lbr[:, 0:1024], er[:, l, :], ALU.add, r=[er, lbr], w=[lbr])
            p.ts("dve", lbr[:, 1024:2048], lbr[:, 0:1024], -1.0, 1.0, ALU.mult, ALU.add, r=[lbr], w=[lbr])
            p.dma("sp", lbrow_d[l], lbr[:], r=[lbr], w=[lbrow_d], merge=True)

    for e in range(NEXP):
        convert_expert(0, e)

    with p.scope():
        xts = [p.sb([128, D], F32, "xt") for _ in range(2)]
        xo = [p.sb([128, KC, 128], F32, "xo") for _ in range(2)]
        for t in range(NT128):
            xt = xts[t % 2]
            p.dma("sp", xt[:], xin[t * 128:(t + 1) * 128, :], w=[xt])
            o = xo[t % 2]
            for half in range(2):
                ps = nps()
                for j in range(4):
                    kc = half * 4 + j
                    p.tr(ps[:, j * 128:(j + 1) * 128], xt[:, kc * 128:(kc + 1) * 128], ident, r=[xt, cst], w=[ps])
                p.copy("dve" if half == 0 else "act", o[:, half * 4:half * 4 + 4, :].rearrange("p k n -> p (k n)"), ps[:], r=[ps], w=[o])
            p.dma("sp", xT[:, :, t * 128:(t + 1) * 128].rearrange("k p n -> p k n"), o[:], r=[o], w=[xT], merge=True)

    for l in range(depth):
        last = (l == depth - 1)
        p.dma("sp", fv[:], fv_d[l], w=[fv])
        with p.scope("mod%d" % l):
            mraw = p.sb([128, 48, 2], F32, "mraw")
            wts = [p.sb([128, KC, 512], F32, "modw") for _ in range(2)]
            for g in range(12):
                wt = wts[g % 2]
                p.dma("sp", wt[:], mod_w[l, :, g * 512:(g + 1) * 512].rearrange("(k p) n -> p k n", p=128), w=[wt])
                ps = nps()
                for m in range(4):
                    for kc in range(KC):
                        p.mm(ps[:, m * 2:m * 2 + 2], wt[:, kc, m * 128:(m + 1) * 128], sT[:, kc * 2:kc * 2 + 2],
                             kc == 0, kc == KC - 1, r=[wt, sT], w=[ps])
                p.copy("dve", mraw[:, g * 4:g * 4 + 4, :].rearrange("p a b -> p (a b)"), ps[:, 0:8], r=[ps], w=[mraw])
            p.tt("dve", mraw[:], mraw[:], fv[:, FV_MODB:FV_MODB + 48].unsqueeze(2).to_broadcast([128, 48, 2]), ALU.add, r=[mraw, fv], w=[mraw])
            for (dst, src, ng) in ((0, 1, FV_N1), (3, 4, FV_N2)):
                p.ts("dve", modv[:, dst], mraw[:, src * 8:src * 8 + 8, :], 1.0, None, ALU.add, None, r=[mraw], w=[modv])
                p.tt("dve", modv[:, dst], modv[:, dst], fv[:, ng:ng + 8].unsqueeze(2).to_broadcast([128, 8, 2]), ALU.mult, r=[modv, fv], w=[modv])
            for (dst, src) in ((1, 0), (2, 2), (4, 3), (5, 5)):
                p.copy("dve", modv[:, dst], mraw[:, src * 8:src * 8 + 8, :], r=[mraw], w=[modv])

        with p.scope("A%d" % l):
            hT = p.sb([128, KC, S], BF16, "hT")
            with p.scope():
                xt = p.sb([128, KC, 512], F32, "xa")
                sq = p.sb([128, KC, 512], F32, "sq")
                rstd = p.sb([128, 512], F32, "rstd")
                for (t0, N, isctx) in tiles:
                    col = 0 if isctx else 1
                    p.dma("sp", xt[:, :, :N], xT[:, :, t0:t0 + N].rearrange("k p n -> p k n"), r=[xT], w=[xt])
                    p.act(sq[:, :, :N], xt[:, :, :N], AF.Square, r=[xt], w=[sq])
                    ps = nps()
                    for kc in range(KC):
                        p.mm(ps[:, :N], ones, sq[:, kc, :N], kc == 0, kc == KC - 1, r=[cst, sq], w=[ps])
                    p.act(rstd[:, :N], ps[:, :N], AF.Ln, r=[ps], w=[rstd], bias=EPS, scale=1.0 / D)
                    p.act(rstd[:, :N], rstd[:, :N], AF.Exp, r=[rstd], w=[rstd], scale=-0.5)
                    for kc in range(KC):
                        p.stt("dve", sq[:, kc, :N], xt[:, kc, :N], modv[:, 0, kc, col:col + 1], rstd[:, :N], ALU.mult, ALU.mult,
                              r=[xt, modv, rstd], w=[sq])
                        p.act(hT[:, kc, t0:t0 + N], sq[:, kc, :N], AF.Identity, r=[sq, modv], w=[hT], bias=modv[:, 1, kc, col:col + 1])

            lbr = p.sb([128, 2048], F32, "lbr")
            p.dma("sp", lbr[:], lbrow_d[l], r=[lbrow_d], w=[lbr])
            wgs = [p.sb([128, KC, 512], BF16, "wg") for _ in range(2)]
            gi = [0]

            def load_wg(c0):
                wg = wgs[gi[0] % 2]
                gi[0] += 1
                p.dma("pool", wg[:], w_in[l, :, c0:c0 + 512].rearrange("(k p) n -> p k n", p=128), w=[wg])
                return wg

            rot = [p.sb([128, 128], F32, "rot") for _ in range(2)]
            tmpA = [p.sb([128, 512], F32, "tmpA") for _ in range(2)]
            tmpB = [p.sb([128, 512], F32, "tmpB") for _ in range(2)]
            obf = [p.sb([128, 512], BF16, "obf") for _ in range(2)]
            of32 = [p.sb([128, 512], F32, "of32") for _ in range(2)]
            of32b = [p.sb([128, 512], F32, "of32b") for _ in range(2)]
            cnt = [0]

            def tm_loop(wg, handler):
                for t in range(NT128):
                    ps = nps()
                    for kc in range(KC):
                        p.mm(ps[:], hT[:, kc, t * 128:(t + 1) * 128], wg[:, kc, :], kc == 0, kc == KC - 1, r=[hT, wg], w=[ps])
                    cnt[0] += 1
                    handler(t, ps, cnt[0] % 2)

            def fm_loop(wg, handler):
                for (t0, N, isctx) in tiles:
                    for m in range(4):
                        ps = nps()
                        for kc in range(KC):
                            p.mm(ps[:, :N], wg[:, kc, m * 128:(m + 1) * 128], hT[:, kc, t0:t0 + N], kc == 0, kc == KC - 1, r=[hT, wg], w=[ps])
                        cnt[0] += 1
                        handler(t0, N, m, ps, cnt[0] % 2)

            def store_tm(dst, t, src):
                p.dma("sp", dst[t * 128:(t + 1) * 128, :], src[:], r=[src], w=[dst], merge=True)

            def store_fm(dst3, m, t0, N, src):
                p.dma("sp", dst3[m, :, t0:t0 + N], src[:, :N], r=[src], w=[dst3], merge=True)

            def h_rot(dst, cofs):
                def h(t, ps, b):
                    o = obf[b]
                    if t < 2:
                        if cofs == 0:
                            p.copy("dve", o[:], ps[:], r=[ps], w=[o])
                        else:
                            p.ts("dve", o[:], ps[:], 0.125, None, ALU.mult, None, r=[ps], w=[o])
                    else:
                        rt = rot[b]
                        p.dma("sp", rt[:], rot_d[(t - 2) * 128:(t - 1) * 128, :], w=[rt])
                        pv = ps[:].rearrange("p (h two d) -> p h two d", h=8, two=2)
                        ov = o[:].rearrange("p (h two d) -> p h two d", h=8, two=2)
                        cs = rt[:, cofs:cofs + 32].unsqueeze(1).to_broadcast([128, 8, 32])
                        sn = rt[:, cofs + 32:cofs + 64].unsqueeze(1).to_broadcast([128, 8, 32])
                        a = tmpA[b][:, 0:256].rearrange("p (h d) -> p h d", h=8)
                        bb = tmpA[b][:, 256:512].rearrange("p (h d) -> p h d", h=8)
                        p.tt("dve", a, pv[:, :, 0, :], cs, ALU.mult, r=[ps, rt], w=[tmpA[b]])
                        p.tt("dve", bb, pv[:, :, 1, :], sn, ALU.mult, r=[ps, rt], w=[tmpA[b]])
                        p.tt("dve", ov[:, :, 0, :], a, bb, ALU.subtract, r=[tmpA[b]], w=[o])
                        p.tt("dve", a, pv[:, :, 0, :], sn, ALU.mult, r=[ps, rt], w=[tmpA[b]])
                        p.tt("dve", bb, pv[:, :, 1, :], cs, ALU.mult, r=[ps, rt], w=[tmpA[b]])
                        p.tt("dve", ov[:, :, 1, :], a, bb, ALU.add, r=[tmpA[b]], w=[o])
                    store_tm(dst, t, o)
                return h

            def h_copy_bf(dst):
                def h(t, ps, b):
                    p.copy("act", obf[b][:], ps[:], r=[ps], w=[obf[b]])
                    store_tm(dst, t, obf[b])
                return h

            def h_silu_tm(dst):
                def h(t, ps, b):
                    p.act(tmpA[b][:], ps[:], AF.Sigmoid, r=[ps], w=[tmpA[b]])
                    p.tt("dve", of32[b][:], tmpA[b][:], ps[:], ALU.mult, r=[ps, tmpA[b]], w=[of32[b]])
                    store_tm(dst, t, of32[b])
                return h

            def h_sig_tm(dst):
                def h(t, ps, b):
                    p.act(of32[b][:], ps[:], AF.Sigmoid, r=[ps], w=[of32[b]])
                    store_tm(dst, t, of32[b])
                return h

            def h_forget_tm(d):
                lb = lbr[:, d * 512:(d + 1) * 512]
                oml = lbr[:, 1024 + d * 512:1024 + (d + 1) * 512]

                def h(t, ps, b):
                    p.act(tmpA[b][:], ps[:], AF.Sigmoid, r=[ps], w=[tmpA[b]])
                    p.tt("dve", tmpA[b][:], tmpA[b][:], oml, ALU.mult, r=[tmpA[b], lbr], w=[tmpA[b]])
                    p.tt("dve", tmpB[b][:], tmpA[b][:], lb, ALU.add, r=[tmpA[b], lbr], w=[tmpB[b]])
                    p.tt("pool", of32[b][:], oml, tmpA[b][:], ALU.subtract, r=[tmpA[b], lbr], w=[of32[b]])
                    p.act(of32b[b][:], tmpB[b][:], AF.Ln, r=[tmpB[b]], w=[of32b[b]])
                    store_tm(h_k[d], t, of32[b])
                    store_tm(h_lf[d], t, of32b[b])
                return h

            def h_forget_fm(d):
                def h(t0, N, m, ps, b):
                    p.act(tmpA[b][:, :N], ps[:, :N], AF.Sigmoid, r=[ps], w=[tmpA[b]], scale=-1.0)
                    p.ts("dve", of32[b][:, :N], tmpA[b][:, :N], omlT[:, l * 8 + d * 4 + m:l * 8 + d * 4 + m + 1], None, ALU.mult, None,
                         r=[tmpA[b], omlT], w=[of32[b]])
                    store_fm(h_kT[d], m, t0, N, of32[b])
                return h

            def h_silu_fm(dst):
                def h(t0, N, m, ps, b):
                    p.act(tmpA[b][:, :N], ps[:, :N], AF.Sigmoid, r=[ps], w=[tmpA[b]])
                    p.tt("dve", of32[b][:, :N], tmpA[b][:, :N], ps[:, :N], ALU.mult, r=[ps, tmpA[b]], w=[of32[b]])
                    store_fm(dst, m, t0, N, of32[b])
                return h

            def h_copy_fm(dst):
                def h(t0, N, m, ps, b):
                    p.copy("act", of32[b][:, :N], ps[:, :N], r=[ps], w=[of32[b]])
                    store_fm(dst, m, t0, N, of32[b])
                return h

            def h_gelu_fm(dst):
                def h(t0, N, m, ps, b):
                    p.act(tmpA[b][:, :N], ps[:, :N], AF.Square, r=[ps], w=[tmpA[b]])
                    p.ts("dve", tmpA[b][:, :N], tmpA[b][:, :N], 0.044715, 1.0, ALU.mult, ALU.add, r=[tmpA[b]], w=[tmpA[b]])
                    p.tt("dve", tmpA[b][:, :N], tmpA[b][:, :N], ps[:, :N], ALU.mult, r=[tmpA[b], ps], w=[tmpA[b]])
                    p.act(tmpA[b][:, :N], tmpA[b][:, :N], AF.Sigmoid, r=[tmpA[b]], w=[tmpA[b]], scale=1.5957691216057308)
                    p.tt("dve", of32[b][:, :N], tmpA[b][:, :N], ps[:, :N], ALU.mult, r=[tmpA[b], ps], w=[of32[b]])
                    store_fm(dst, m, t0, N, of32[b])
                return h

            def h_sig_fm(dst, gidx, half):
                def h(t0, N, m, ps, b):
                    p.act(of32[b][:, :N], ps[:, :N], AF.Sigmoid, r=[ps], w=[of32[b]])
                    p.dma("sp", dst[gidx, half * 4 + m, :, t0:t0 + N], of32[b][:, :N], r=[of32[b]], w=[dst], merge=True)
                return h

            plan = [
                (G_RQ, "tm", h_rot(r_q, 0)), (G_RK, "tm", h_rot(r_k, 64)), (G_RV, "tm", h_copy_bf(r_v)), (G_RG, "tm", h_silu_tm(r_g)),
                (G_HQ, "fm", h_silu_fm(h_qT)),
                (G_HFF, "tm", h_forget_tm(0)), (G_HFF, "fm", h_forget_fm(0)),
                (G_HFB, "tm", h_forget_tm(1)), (G_HFB, "fm", h_forget_fm(1)),
                (G_HI, "tm", h_copy_bf(h_v)), (G_HGT, "tm", h_sig_tm(h_g)),
                (G_LX, "fm", h_copy_fm(l_xT)), (G_LGT, "fm", h_gelu_fm(l_gT)),
                (G_GR, "fm", h_sig_fm(g_T, 0, 0)), (G_GR + 512, "fm", h_sig_fm(g_T, 0, 1)),
                (G_GH, "fm", h_sig_fm(g_T, 1, 0)), (G_GH + 512, "fm", h_sig_fm(g_T, 1, 1)),
                (G_GL, "fm", h_sig_fm(g_T, 2, 0)), (G_GL + 512, "fm", h_sig_fm(g_T, 2, 1)),
            ]
            prev = (None, None)
            for (c0, kind, hd) in plan:
                if c0 != prev[0]:
                    wg = load_wg(c0)
                    prev = (c0, wg)
                (tm_loop if kind == "tm" else fm_loop)(prev[1], hd)

        with p.scope("B%d" % l):
            lgb = p.sb([128, 16], F32, "lgb")
            p.dma("sp", lgb[:], ret_decay[l:l + 1, :].partition_broadcast(128), w=[lgb])
            p.act(lgb[:], lgb[:], AF.Exp, r=[lgb], w=[lgb], scale=-1.0)
            p.act(lgb[:], lgb[:], AF.Ln, r=[lgb], w=[lgb], bias=1.0)
            p.ts("dve", lgb[:], lgb[:], -1.0, None, ALU.mult, None, r=[lgb], w=[lgb])
            gnrow = p.sb([128, 512], F32, "gnrow")
            p.dma("sp", gnrow[:], ret_gn[l:l + 1, :].partition_broadcast(128), w=[gnrow])
            av = p.sb([128, 2, 8], F32, "av")
            bv = p.sb([128, 2, 8], F32, "bv")
            iv = p.sb([128, 2, 8], F32, "iv")
            Dt = p.sb([128, 2, 8, 128], F32, "Dt")
            Gt = p.sb([64, 2, 512], F32, "Gt")
            idx = lambda i: cst[:, C_IDX + i:C_IDX + i + 1]
            for d in range(2):
                lg = lgb[:, d * 8:d * 8 + 8]
                p.act(av[:, d, :], lg, AF.Exp, r=[lgb, cst], w=[av], scale=idx(0) if d == 0 else idx(2))
                p.act(bv[:, d, :], lg, AF.Exp, r=[lgb, cst], w=[bv], scale=idx(1) if d == 0 else idx(3))
                p.act(iv[:, d, :], lg, AF.Exp, r=[lgb, cst], w=[iv], scale=idx(4) if d == 0 else idx(5))
                msk = cst[:, C_MF:C_MF + 128] if d == 0 else cst[:, C_MB:C_MB + 128]
                for h in range(8):
                    p.ts("dve", Dt[:, d, h, :], msk, iv[:, d, h:h + 1], None, ALU.mult, None, r=[cst, iv], w=[Dt])
                p.act(Gt[:, d, :].rearrange("p (h v) -> p h v", h=8), lgb[0:64, d * 8:d * 8 + 8].unsqueeze(2).to_broadcast([64, 8, 64]),
                      AF.Exp, r=[lgb], w=[Gt], scale=128.0)

            Sst = p.sb([64, 512], F32, "Sst")
            Sbf = p.sb([64, 512], BF16, "Sbf")
            NB = 2
            qt = [p.sb([128, 512], BF16, "rq") for _ in range(NB)]
            kt = [p.sb([128, 512], BF16, "rk") for _ in range(NB)]
            vt = [p.sb([128, 512], BF16, "rv") for _ in range(NB)]
            qd = [p.sb([128, 512], BF16, "qd") for _ in range(NB)]
            kd = [p.sb([128, 512], BF16, "kd") for _ in range(NB)]
            qdT = [p.sb([64, 8, 128], BF16, "qdT") for _ in range(NB)]
            kTt = [p.sb([64, 8, 128], BF16, "kTt") for _ in range(NB)]
            Pm = [p.sb([128, 8, 128], BF16, "Pm") for _ in range(NB)]
            osb = [p.sb([128, 512], F32, "osb") for _ in range(NB)]
            oprev = [p.sb([128, 512], F32, "oprev") for _ in range(NB)]
            gate = [p.sb([128, 512], F32, "gate") for _ in range(NB)]
            ssq = [p.sb([128, 8], F32, "ssq") for _ in range(NB)]
            ybf = [p.sb([128, 512], BF16, "ybf") for _ in range(NB)]
            yT = [p.sb([128, 4, 128], BF16, "yT") for _ in range(NB)]
            for d in range(2):
                p.op("dve", lambda e: e.memset(Sst[:], 0.0), w=[Sst])
                p.op("dve", lambda e: e.memset(Sbf[:], 0.0), w=[Sbf])
                order = list(range(NT128)) if d == 0 else [1, 0] + list(range(NT128 - 1, 1, -1))
                for it, t in enumerate(order):
                    b = it % NB
                    if last and d == 1 and t < 2:
                        pass
                    rows = slice(t * 128, (t + 1) * 128)
                    p.dma("sp", qt[b][:], r_q[rows, :], r=[r_q], w=[qt[b]])
                    p.dma("sp", kt[b][:], r_k[rows, :], r=[r_k], w=[kt[b]])
                    p.dma("sp", vt[b][:], r_v[rows, :], r=[r_v], w=[vt[b]])
                    v3 = lambda tl: tl[:].rearrange("p (h e) -> p h e", h=8)
                    p.tt("dve", v3(qd[b]), v3(qt[b]), av[:, d, :].unsqueeze(2).to_broadcast([128, 8, 64]), ALU.mult, r=[qt[b], av], w=[qd[b]])
                    p.tt("pool", v3(kd[b]), v3(kt[b]), bv[:, d, :].unsqueeze(2).to_broadcast([128, 8, 64]), ALU.mult, r=[kt[b], bv], w=[kd[b]])
                    ph = npsh()
                    for h in range(8):
                        p.tr(ph[0:64, h * 128:(h + 1) * 128], qd[b][:, h * 64:(h + 1) * 64], identb[:], r=[qd[b], identb], w=[ph])
                    p.copy("act", qdT[b][:].rearrange("p h n -> p (h n)"), ph[0:64, :], r=[ph], w=[qdT[b]])
                    ph = npsh()
                    for h in range(8):
                        p.tr(ph[0:64, h * 128:(h + 1) * 128], kt[b][:, h * 64:(h + 1) * 64], identb[:], r=[kt[b], identb], w=[ph])
                    p.copy("dve", kTt[b][:].rearrange("p h n -> p (h n)"), ph[0:64, :], r=[ph], w=[kTt[b]])
                    for hh in range(2):
                        ps = nps()
                        for j in range(4):
                            h = hh * 4 + j
                            p.mm(ps[:, j * 128:(j + 1) * 128], kTt[b][:, h, :], qdT[b][:, h, :], True, True, r=[kTt[b], qdT[b]], w=[ps])
                        p.tt("dve", Pm[b][:, hh * 4:hh * 4 + 4, :].rearrange("p h n -> p (h n)"), ps[:],
                             Dt[:, d, hh * 4:hh * 4 + 4, :].rearrange("p h n -> p (h n)"), ALU.mult, r=[ps, Dt], w=[Pm[b]])
                    po = nps()
                    for h in range(8):
                        cs = slice(h * 64, (h + 1) * 64)
                        p.mm(po[:, cs], Pm[b][:, h, :], vt[b][:, cs], True, False, r=[Pm[b], vt[b]], w=[po])
                        p.mm(po[:, cs], qdT[b][:, h, :], Sbf[:, cs], False, True, r=[qdT[b], Sbf], w=[po])
                    psS = nps()
                    for h in range(8):
                        cs = slice(h * 64, (h + 1) * 64)
                        p.mm(psS[0:64, cs], kd[b][:, cs], vt[b][:, cs], True, True, r=[kd[b], vt[b]], w=[psS])
                    p.tt("dve", Sst[:], Sst[:], Gt[:, d, :], ALU.mult, r=[Sst, Gt], w=[Sst])
                    p.tt("dve", Sst[:], Sst[:], psS[0:64, :], ALU.add, r=[Sst, psS], w=[Sst])
                    p.copy("act", Sbf[:], Sst[:], r=[Sst], w=[Sbf])
                    if d == 0:
                        p.copy("act", osb[b][:], po[:], r=[po], w=[osb[b]])
                        p.dma("sp", o_ret[rows, :], osb[b][:], r=[osb[b]], w=[o_ret], merge=True)
                    else:
                        if last and t < 2:
                            continue
                        p.dma("sp", oprev[b][:], o_ret[rows, :], r=[o_ret], w=[oprev[b]])
                        p.dma("sp", gate[b][:], r_g[rows, :], r=[r_g], w=[gate[b]])
                        p.tt("dve", osb[b][:], po[:], oprev[b][:], ALU.add, r=[po, oprev[b]], w=[osb[b]])
                        p.tt("pool", oprev[b][:], osb[b][:], osb[b][:], ALU.mult, r=[osb[b]], w=[oprev[b]])
                        p.op("dve", lambda e: e.reduce_sum(out=ssq[b][:], in_=oprev[b][:].rearrange("p (h e) -> p h e", h=8), axis=AX.X),
                             r=[oprev[b]], w=[ssq[b]])
                        p.act(ssq[b][:], ssq[b][:], AF.Ln, r=[ssq[b]], w=[ssq[b]], bias=EPS, scale=1.0 / 64)
                        p.act(ssq[b][:], ssq[b][:], AF.Exp, r=[ssq[b]], w=[ssq[b]], scale=-0.5)
                        p.tt("dve", v3(osb[b]), v3(osb[b]), ssq[b][:].unsqueeze(2).to_broadcast([128, 8, 64]), ALU.mult, r=[osb[b], ssq[b]], w=[osb[b]])
                        p.tt("pool", gate[b][:], gate[b][:], gnrow[:], ALU.mult, r=[gate[b], gnrow], w=[gate[b]])
                        p.tt("dve", ybf[b][:], osb[b][:], gate[b][:], ALU.mult, r=[osb[b], gate[b]], w=[ybf[b]])
                        ph = npsh()
                        for c in range(4):
                            p.tr(ph[:, c * 128:(c + 1) * 128], ybf[b][:, c * 128:(c + 1) * 128], identb[:], r=[ybf[b], identb], w=[ph])
                        p.copy("act", yT[b][:].rearrange("p c n -> p (c n)"), ph[:, 0:512], r=[ph], w=[yT[b]])
                        p.dma("sp", retnT[:, :, t * 128:(t + 1) * 128].rearrange("c p n -> p c n"), yT[b][:], r=[yT[b]], w=[retnT], merge=True)

        with p.scope("C%d" % l):
            gnrow = p.sb([128, 512], F32, "hgnrow")
            p.dma("sp", gnrow[:], hg_gn[l:l + 1, :].partition_broadcast(128), w=[gnrow])
            Sst = p.sb([128, 4, 128], F32, "hS")
            Sbf = p.sb([128, 4, 128], BF16, "hSbf")
            NB = 2
            mk = lambda shape, dt, nm: [p.sb(shape, dt, nm) for _ in range(NB)]
            lft = mk([64, 512], F32, "lft")
            ktm = mk([64, 512], F32, "ktm")
            vt = mk([64, 512], BF16, "hv")
            qTt = mk([128, 4, 64], F32, "qTt")
            kTt = mk([128, 4, 64], F32, "hkT")
            gsb = mk([128, 4, 64], F32, "gsb")
            gref = mk([128, 8], F32, "gref")
            E1 = mk([128, 4, 64], F32, "E1")
            E3 = mk([128, 4, 64], F32, "E3")
            qtl = mk([128, 4, 64], BF16, "qtl")
            ktl = mk([128, 4, 64], BF16, "ktl")
            qg = mk([128, 4, 64], BF16, "qg")
            ER = mk([64, 512], F32, "ER")
            kdec = mk([64, 512], BF16, "kdec")
            Pm = mk([64, 4, 64], BF16, "hP")
            osb = mk([64, 512], F32, "hosb")
            oprev = mk([64, 512], F32, "hoprev")
            gate = mk([64, 512], F32, "hgate")
            ssq = mk([64, 4], F32, "hssq")
            ybf = mk([64, 512], BF16, "hybf")
            yT = mk([128, 4, 64], BF16, "hyT")
            NCH = S // 64
            M = 32
            mint = [p.sb([64, 4, 64], mybir.dt.int32, "mint%d" % i) for i in range(2)]
            zer = p.sb([64, 4, 64], F32, "zer")
            p.op("dve", lambda e: e.memset(zer[:], 0.0), w=[zer])
            for i, cc in enumerate((C_TF, C_TB)):
                p.copy("dve", mint[i][:], cst[0:64, cc:cc + 64].unsqueeze(1).to_broadcast([64, 4, 64]), r=[cst], w=[mint[i]])
            for d in range(2):
                p.op("dve", lambda e: e.memset(Sst[:], 0.0), w=[Sst])
                p.op("dve", lambda e: e.memset(Sbf[:], 0.0), w=[Sbf])
                tri = cst[0:64, C_TF:C_TF + 64] if d == 0 else cst[0:64, C_TB:C_TB + 64]
                rem = cst[0:64, C_RF:C_RF + 64] if d == 0 else cst[0:64, C_RB:C_RB + 64]
                lastcol = 63 if d == 0 else 0
                order = list(range(NCH)) if d == 0 else [3, 2, 1, 0] + list(range(NCH - 1, 3, -1))
                for it, c in enumerate(order):
                    b = it % NB
                    rows = slice(c * 64, (c + 1) * 64)
                    p.dma("sp", lft[b][:], h_lf[d][rows, :], r=[h_lf[d]], w=[lft[b]])
                    p.dma("sp", ktm[b][:], h_k[d][rows, :], r=[h_k[d]], w=[ktm[b]])
                    p.dma("sp", vt[b][:], h_v[rows, :], r=[h_v], w=[vt[b]])
                    p.dma("sp", qTt[b][:], h_qT[:, :, c * 64:(c + 1) * 64].rearrange("h p n -> p h n"), r=[h_qT], w=[qTt[b]])
                    p.dma("sp", kTt[b][:], h_kT[d][:, :, c * 64:(c + 1) * 64].rearrange("h p n -> p h n"), r=[h_kT[d]], w=[kTt[b]])
                    pg = nps()
                    for h in range(4):
                        p.mm(pg[:, h * 64:(h + 1) * 64], lft[b][:, h * 128:(h + 1) * 128], tri, True, True, r=[lft[b], cst], w=[pg])
                    pr = nps()
                    for h in range(4):
                        p.mm(pr[0:64, h * 128:(h + 1) * 128], rem, lft[b][:, h * 128:(h + 1) * 128], True, True, r=[lft[b], cst], w=[pr])
                    p.copy("dve", gsb[b][:].rearrange("p h n -> p (h n)"), pg[:, 0:256], r=[pg], w=[gsb[b]])
                    p.copy("dve", gref[b][:, 0:4], gsb[b][:, :, M], r=[gsb[b]], w=[gref[b]])
                    p.ts("dve", gref[b][:, 4:8], gref[b][:, 0:4], -1.0, None, ALU.mult, None, r=[gref[b]], w=[gref[b]])
                    for h in range(4):
                        p.act(E1[b][:, h, :], gsb[b][:, h, :], AF.Exp, r=[gsb[b], gref[b]], w=[E1[b]], bias=gref[b][:, 4 + h:5 + h])
                    p.tt("dve", qtl[b][:], qTt[b][:], E1[b][:], ALU.mult, r=[qTt[b], E1[b]], w=[qtl[b]])
                    for h in range(4):
                        p.act(E1[b][:, h, :], gsb[b][:, h, :], AF.Exp, r=[gsb[b], gref[b], qtl[b]], w=[E1[b]], bias=gref[b][:, h:h + 1], scale=-1.0)
                    p.tt("dve", ktl[b][:], kTt[b][:], E1[b][:], ALU.mult, r=[kTt[b], E1[b]], w=[ktl[b]])
                    p.act(E3[b][:], gsb[b][:], AF.Exp, r=[gsb[b]], w=[E3[b]])
                    p.tt("pool", qg[b][:], qTt[b][:], E3[b][:], ALU.mult, r=[qTt[b], E3[b]], w=[qg[b]])
                    p.act(ER[b][:], pr[0:64, :], AF.Exp, r=[pr], w=[ER[b]])
                    p.tt("pool", kdec[b][:], ktm[b][:], ER[b][:], ALU.mult, r=[ktm[b], ER[b]], w=[kdec[b]])
                    psc = nps()
                    for h in range(4):
                        p.mm(psc[0:64, h * 64:(h + 1) * 64], ktl[b][:, h, :], qtl[b][:, h, :], True, True, r=[ktl[b], qtl[b]], w=[psc])
                    p.op("dve", lambda e: e.select(out=Pm[b][:], mask=mint[d][:], on_true=psc[0:64, 0:256].rearrange("p (h n) -> p h n", h=4),
                                                   on_false=zer[:]), r=[psc, mint[d], zer], w=[Pm[b]])
                    po = nps()
                    for h in range(4):
                        cs = slice(h * 128, (h + 1) * 128)
                        p.mm(po[0:64, cs], Pm[b][:, h, :], vt[b][:, cs], True, False, r=[Pm[b], vt[b]], w=[po])
                        p.mm(po[0:64, cs], qg[b][:, h, :], Sbf[:, h, :], False, True, r=[qg[b], Sbf], w=[po])
                    psS = nps()
                    for h in range(4):
                        cs = slice(h * 128, (h + 1) * 128)
                        p.mm(psS[:, cs], kdec[b][:, cs], vt[b][:, cs], True, True, r=[kdec[b], vt[b]], w=[psS])
                    for h in range(4):
                        p.stt("dve", Sst[:, h, :], Sst[:, h, :], E3[b][:, h, lastcol:lastcol + 1], psS[:, h * 128:(h + 1) * 128], ALU.mult, ALU.add,
                              r=[Sst, E3[b], psS], w=[Sst])
                    p.copy("act", Sbf[:], Sst[:], r=[Sst], w=[Sbf])
                    if d == 0:
                        p.copy("act", osb[b][:], po[0:64, :], r=[po], w=[osb[b]])
                        p.dma("sp", o_hg[rows, :], osb[b][:], r=[osb[b]], w=[o_hg], merge=True)
                    else:
                        if last and c < 4:
                            continue
                        p.dma("sp", oprev[b][:], o_hg[rows, :], r=[o_hg], w=[oprev[b]])
                        p.dma("sp", gate[b][:], h_g[rows, :], r=[h_g], w=[gate[b]])
                        p.tt("dve", osb[b][:], po[0:64, :], oprev[b][:], ALU.add, r=[po, oprev[b]], w=[osb[b]])
                        p.tt("pool", oprev[b][:], osb[b][:], osb[b][:], ALU.mult, r=[osb[b]], w=[oprev[b]])
                        p.op("dve", lambda e: e.reduce_sum(out=ssq[b][:], in_=oprev[b][:].rearrange("p (h e) -> p h e", h=4), axis=AX.X),
                             r=[oprev[b]], w=[ssq[b]])
                        p.act(ssq[b][:], ssq[b][:], AF.Ln, r=[ssq[b]], w=[ssq[b]], bias=EPS, scale=1.0 / 128)
                        p.act(ssq[b][:], ssq[b][:], AF.Exp, r=[ssq[b]], w=[ssq[b]], scale=-0.5)
                        o3 = osb[b][:].rearrange("p (h e) -> p h e", h=4)
                        p.tt("dve", o3, o3, ssq[b][:].unsqueeze(2).to_broadcast([64, 4, 128]), ALU.mult, r=[osb[b], ssq[b]], w=[osb[b]])
                        p.tt("pool", gate[b][:], gate[b][:], gnrow[0:64, :], ALU.mult, r=[gate[b], gnrow], w=[gate[b]])
                        p.tt("dve", ybf[b][:], osb[b][:], gate[b][:], ALU.mult, r=[osb[b], gate[b]], w=[ybf[b]])
                        ph = npsh()
                        for h in range(4):
                            p.tr(ph[:, h * 64:(h + 1) * 64], ybf[b][:, h * 128:(h + 1) * 128], identb[0:64, 0:64], r=[ybf[b], identb], w=[ph])
                        p.copy("act", yT[b][:].rearrange("p c n -> p (c n)"), ph[:, 0:256], r=[ph], w=[yT[b]])
                        p.dma("sp", hgnT[:, :, c * 64:(c + 1) * 64].rearrange("c p n -> p c n"), yT[b][:], r=[yT[b]], w=[hgnT], merge=True)

        with p.scope("D%d" % l):
            xs = p.sb([128, S], F32, "lx")
            xc = p.sb([128, S], F32, "lxc")
            at = p.sb([128, S], F32, "la")
            bt = p.sb([128, S], F32, "lb")
            hf = p.sb([128, S], F32, "lhf")
            hb = p.sb([128, S], F32, "lhb")
            gl = p.sb([128, S], F32, "lgl")
            ybf = p.sb([128, S], BF16, "lybf")
            wbd = p.sb([128, 128], F32, "wbd")
            wbx = p.sb([128, 128], F32, "wbx")
            cneg = p.sb([128, 8], F32, "cneg")
            p.act(cneg[:], fv[:, FV_LAM:FV_LAM + 8], AF.Exp, r=[fv], w=[cneg], scale=-1.0)
            p.act(cneg[:], cneg[:], AF.Ln, r=[cneg], w=[cneg], bias=1.0)
            p.ts("dve", cneg[:], cneg[:], -8.0, None, ALU.mult, None, r=[cneg], w=[cneg])
            tA = [p.sb([128, 512], F32, "ltA") for _ in range(2)]
            tB = [p.sb([128, 512], F32, "ltB") for _ in range(2)]
            segs = [(0, CTX), (CTX, S)]
            for c in range(4):
                p.dma("sp", xs[:], l_xT[c], r=[l_xT], w=[xs])
                p.dma("sp", gl[:], l_gT[c], r=[l_gT], w=[gl])
                cw = lambda w: fv[:, FV_CW + w * 4 + c:FV_CW + w * 4 + c + 1]
                p.ts("dve", xc[:], xs[:], cw(2), fv[:, FV_CB + c:FV_CB + c + 1], ALU.mult, ALU.add, r=[xs, fv], w=[xc])
                for w in (0, 1, 3):
                    o = w - 2
                    for (a, bnd) in segs:
                        lo, hi = max(a, a - o), min(bnd, bnd - o)
                        p.stt("dve", xc[:, lo:hi], xs[:, lo + o:hi + o], cw(w), xc[:, lo:hi], ALU.mult, ALU.add, r=[xs, fv, xc], w=[xc])
                for d in range(2):
                    p.dma("sp", wbd[:], lru_bd[l, 0, d, c], w=[wbd])
                    p.dma("sp", wbx[:], lru_bd[l, 1, d, c], w=[wbx])
                    for i, (t0, N, isctx) in enumerate(tiles):
                        b = i % 2
                        pa = nps()
                        p.mm(pa[:, :N], wbd[:], xc[:, t0:t0 + N], True, True, r=[wbd, xc], w=[pa])
                        px = nps()
                        p.mm(px[:, :N], wbx[:], xc[:, t0:t0 + N], True, True, r=[wbx, xc], w=[px])
                        p.act(tA[b][:, :N], pa[:, :N], AF.Sigmoid, r=[pa, fv], w=[tA[b]], bias=fv[:, FV_BA + d * 4 + c:FV_BA + d * 4 + c + 1])
                        p.act(tB[b][:, :N], px[:, :N], AF.Sigmoid, r=[px, fv], w=[tB[b]], bias=fv[:, FV_BX + d * 4 + c:FV_BX + d * 4 + c + 1])
                        p.act(at[:, t0:t0 + N], tA[b][:, :N], AF.Exp, r=[tA[b], cneg], w=[at], scale=cneg[:, d * 4 + c:d * 4 + c + 1])
                        p.tt("dve", tB[b][:, :N], tB[b][:, :N], xc[:, t0:t0 + N], ALU.mult, r=[tB[b], xc], w=[tB[b]])
                        p.tt("dve", tA[b][:, :N], at[:, t0:t0 + N], at[:, t0:t0 + N], ALU.mult, r=[at], w=[tA[b]])
                        p.ts("dve", tA[b][:, :N], tA[b][:, :N], -1.0, 1.0, ALU.mult, ALU.add, r=[tA[b]], w=[tA[b]])
                        p.act(tA[b][:, :N], tA[b][:, :N], AF.Ln, r=[tA[b]], w=[tA[b]])
                        p.act(tA[b][:, :N], tA[b][:, :N], AF.Exp, r=[tA[b]], w=[tA[b]], scale=0.5)
                        p.tt("dve", bt[:, t0:t0 + N], tA[b][:, :N], tB[b][:, :N], ALU.mult, r=[tA[b], tB[b]], w=[bt])
                    PC = 256
                    if d == 0:
                        for s0 in range(0, S, PC):
                            init = 0.0 if s0 == 0 else hf[:, s0 - 1:s0]
                            p.op("dve", lambda e: e.tensor_tensor_scan(out=hf[:, s0:s0 + PC], data0=at[:, s0:s0 + PC], data1=bt[:, s0:s0 + PC],
                                                                       initial=init, op0=ALU.mult, op1=ALU.add), r=[at, bt, hf], w=[hf])
                    else:
                        pieces = [(0, CTX, None)] + [(s0, s0 + PC, None) for s0 in range(S - PC, CTX - 1, -PC)]
                        prev_first = None
                        for (a0, a1, _) in pieces:
                            init = 0.0 if prev_first is None else hb[:, prev_first:prev_first + 1]
                            rv = lambda tl: tl[:, a0:a1][:, ::-1]
                            p.op("dve", lambda e: e.tensor_tensor_scan(out=rv(hb), data0=rv(at), data1=rv(bt),
                                                                       initial=init, op0=ALU.mult, op1=ALU.add), r=[at, bt, hb], w=[hb])
                            prev_first = a0
                p.tt("dve", hf[:], hf[:], hb[:], ALU.add, r=[hf, hb], w=[hf])
                p.tt("dve", ybf[:], hf[:], gl[:], ALU.mult, r=[hf, gl], w=[ybf])
                p.dma("sp", lruT[c], ybf[:], r=[ybf], w=[lruT], merge=True)

        with p.scope("E%d" % l):
            wro = p.sb([128, 4, D], BF16, "wro")
            who = p.sb([128, 4, D], BF16, "who")
            wlo = p.sb([128, 4, D], BF16, "wlo")
            wo = p.sb([128, KC, D], BF16, "wo")
            wr = p.sb([128, KC, NEXP], F32, "wr")
            rbrow = p.sb([128, NEXP], F32, "rbrow")
            p.dma("pool", wro[:], w_ret_o[l].rearrange("(k p) n -> p k n", p=128), w=[wro])
            p.dma("pool", who[:], w_hg_o[l].rearrange("(k p) n -> p k n", p=128), w=[who])
            p.dma("pool", wlo[:], w_lru_o[l].rearrange("(k p) n -> p k n", p=128), w=[wlo])
            p.dma("pool", wo[:], w_out[l].rearrange("(k p) n -> p k n", p=128), w=[wo])
            p.dma("sp", wr[:], router_w[l].rearrange("(k p) n -> p k n", p=128), w=[wr])
            p.dma("sp", rbrow[:], router_b[l:l + 1, :].partition_broadcast(128), w=[rbrow])
            bt3 = [p.sb([128, 4, 512], BF16, "br%d" % i) for i in range(3)]
            gts = [[p.sb([128, 512], F32, "gt%d" % i) for i in range(3)] for _ in range(2)]
            mg = p.sb([128, KC, 512], BF16, "mg")
            t1 = [p.sb([128, 512], F32, "et1") for _ in range(2)]
            t2 = [p.sb([128, 512], F32, "et2") for _ in range(2)]
            xo = p.sb([128, KC, 512], F32, "exo")
            xn = p.sb([128, KC, 512], F32, "exn")
            sq = p.sb([128, KC, 512], F32, "esq")
            rstd = p.sb([128, 512], F32, "erstd")
            h2b = p.sb([128, KC, 512], BF16, "h2b")
            lgt = p.sb([128, NEXP], F32, "lgt")
            top8 = p.sb([128, 8], F32, "top8")
            wrow = p.sb([128, NEXP], F32, "wrow")
            ssum = p.sb([128, 2], F32, "ssum")
            for (t0, N, isctx) in tiles:
                if last and isctx:
                    continue
                col = 0 if isctx else 1
                for i, src in enumerate((retnT, hgnT, lruT)):
                    p.dma("sp", bt3[i][:, :, :N], src[:, :, t0:t0 + N].rearrange("c p n -> p c n"), r=[src], w=[bt3[i]])
                p.dma("sp", xo[:, :, :N], xT[:, :, t0:t0 + N].rearrange("k p n -> p k n"), r=[xT], w=[xo])
                for m in range(KC):
                    b = m % 2
                    for i in range(3):
                        p.dma("sp", gts[b][i][:, :N], g_T[i, m, :, t0:t0 + N], r=[g_T], w=[gts[b][i]])
                    pss = []
                    for i, wgt in enumerate((wro, who, wlo)):
                        ps = nps()
                        for kc in range(4):
                            p.mm(ps[:, :N], wgt[:, kc, m * 128:(m + 1) * 128], bt3[i][:, kc, :N], kc == 0, kc == 3, r=[wgt, bt3[i]], w=[ps])
                        pss.append(ps)
                    p.tt("dve", t1[b][:, :N], pss[0][:, :N], gts[b][0][:, :N], ALU.mult, r=[pss[0], gts[b][0]], w=[t1[b]])
                    p.tt("dve", t2[b][:, :N], pss[1][:, :N], gts[b][1][:, :N], ALU.mult, r=[pss[1], gts[b][1]], w=[t2[b]])
                    p.tt("pool", t1[b][:, :N], t1[b][:, :N], t2[b][:, :N], ALU.add, r=[t1[b], t2[b]], w=[t1[b]])
                    p.tt("dve", t2[b][:, :N], pss[2][:, :N], gts[b][2][:, :N], ALU.mult, r=[pss[2], gts[b][2]], w=[t2[b]])
                    p.tt("pool", mg[:, m, :N], t1[b][:, :N], t2[b][:, :N], ALU.add, r=[t1[b], t2[b]], w=[mg])
                for m in range(KC):
                    ps = nps()
                    for kc in range(KC):
                        p.mm(ps[:, :N], wo[:, kc, m * 128:(m + 1) * 128], mg[:, kc, :N], kc == 0, kc == KC - 1, r=[wo, mg], w=[ps])
                    p.stt("dve", xn[:, m, :N], ps[:, :N], modv[:, 2, m, col:col + 1], xo[:, m, :N], ALU.mult, ALU.add, r=[ps, modv, xo], w=[xn])
                p.dma("sp", xT[:, :, t0:t0 + N].rearrange("k p n -> p k n"), xn[:, :, :N], r=[xn], w=[xT], merge=True)
                p.act(sq[:, :, :N], xn[:, :, :N], AF.Square, r=[xn], w=[sq])
                ps = nps()
                for kc in range(KC):
                    p.mm(ps[:, :N], ones, sq[:, kc, :N], kc == 0, kc == KC - 1, r=[cst, sq], w=[ps])
                p.act(rstd[:, :N], ps[:, :N], AF.Ln, r=[ps], w=[rstd], bias=EPS, scale=1.0 / D)
                p.act(rstd[:, :N], rstd[:, :N], AF.Exp, r=[rstd], w=[rstd], scale=-0.5)
                for kc in range(KC):
                    p.stt("dve", sq[:, kc, :N], xn[:, kc, :N], modv[:, 3, kc, col:col + 1], rstd[:, :N], ALU.mult, ALU.mult, r=[xn, modv, rstd], w=[sq])
                    p.act(sq[:, kc, :N], sq[:, kc, :N], AF.Identity, r=[sq, modv], w=[sq], bias=modv[:, 4, kc, col:col + 1])
                p.copy("pool", h2b[:, :, :N], sq[:, :, :N], r=[sq], w=[h2b])
                p.dma("sp", h2T_d[:, :, t0:t0 + N].rearrange("k p n -> p k n"), h2b[:, :, :N], r=[h2b], w=[h2T_d], merge=True)
                for s in range(N // 128):
                    ps = nps()
                    for kc in range(KC):
                        p.mm(ps[:, 0:NEXP], sq[:, kc, s * 128:(s + 1) * 128], wr[:, kc, :], kc == 0, kc == KC - 1, r=[sq, wr], w=[ps])
                    p.tt("dve", lgt[:], ps[:, 0:NEXP], rbrow[:], ALU.add, r=[ps, rbrow], w=[lgt])
                    p.op("dve", lambda e: e.max(out=top8[:], in_=lgt[:]), r=[lgt], w=[top8])
                    p.ts("dve", wrow[:], lgt[:], top8[:, 3:4], None, ALU.is_ge, None, r=[lgt, top8], w=[wrow])
                    p.ts("dve", ssum[:, 0:1], top8[:, 0:1], -1.0, None, ALU.mult, None, r=[top8], w=[ssum])
                    p.act(lgt[:], lgt[:], AF.Exp, r=[lgt, ssum], w=[lgt], bias=ssum[:, 0:1])
                    p.tt("dve", wrow[:], wrow[:], lgt[:], ALU.mult, r=[wrow, lgt], w=[wrow])
                    p.op("dve", lambda e: e.reduce_sum(out=ssum[:, 1:2], in_=wrow[:], axis=AX.X), r=[wrow], w=[ssum])
                    p.op("dve", lambda e: e.reciprocal(out=ssum[:, 1:2], in_=ssum[:, 1:2]), r=[ssum], w=[ssum])
                    p.ts("dve", wrow[:], wrow[:], ssum[:, 1:2], None, ALU.mult, None, r=[wrow, ssum], w=[wrow])
                    p.dma("sp", wmoe_d[t0 + s * 128:t0 + (s + 1) * 128, :], wrow[:], r=[wrow], w=[wmoe_d], merge=True)

        with p.scope("F%d" % l):
            mtiles = [tl for tl in tiles if not (last and tl[2])]
            groups = [mtiles[i:i + 2] for i in range(0, len(mtiles), 2)]
            wgu = [p.sb([128, KC, 2 * D], BF16, "wgu%d" % i) for i in range(2)]
            wdn = [p.sb([128, KC, D], BF16, "wdn%d" % i) for i in range(2)]
            h2 = p.sb([128, KC, 1024], BF16, "mh2")
            accs = [p.sb([128, D], F32, "acc%d" % i) for i in range(8)]
            wsb = [p.sb([128, NEXP], F32, "mw%d" % i) for i in range(8)]
            aT = [p.sb([128, KC, 512], BF16, "aT%d" % i) for i in range(2)]
            g1 = [p.sb([128, 512], F32, "mg%d" % i) for i in range(3)]
            s1 = [p.sb([128, 512], F32, "ms%d" % i) for i in range(3)]
            u1 = [p.sb([128, 512], F32, "mu%d" % i) for i in range(3)]
            bdn = p.sb([NEXP, D], F32, "bdn")
            bu1 = p.sb([128, NEXP, 8], F32, "bu1")
            p.ts("dve", bu1[:], fv[:, FV_BGU:FV_BGU + 512].rearrange("p (e c) -> p e c", e=NEXP)[:, :, 8:16], 1.0, None, ALU.add, None, r=[fv], w=[bu1])
            wT = p.sb([NEXP, 128], F32, "wT")
            xo = p.sb([128, 512], F32, "fxo")
            xn2 = p.sb([128, 512], F32, "fxn")
            p.dma("sp", bdn[:], b_dn[l], w=[bdn])
            ei = 0
            for grp in groups:
                gt0 = grp[0][0]
                gN = sum(tl[1] for tl in grp)
                nsub = gN // 128
                p.dma("sp", h2[:, :, :gN], h2T_d[:, :, gt0:gt0 + gN].rearrange("k p n -> p k n"), r=[h2T_d], w=[h2])
                for s in range(nsub):
                    p.dma("sp", wsb[s][:], wmoe_d[gt0 + s * 128:gt0 + (s + 1) * 128, :], r=[wmoe_d], w=[wsb[s]])
                for e in range(NEXP):
                    wb = ei % 2
                    ei += 1
                    p.dma("sp", wgu[wb][:], wgu_bf[l % 2][e].h.rearrange("(k p) n -> p k n", p=128), r=[wgu_bf[l % 2][e]], w=[wgu[wb]])
                    p.dma("sp", wdn[wb][:], wdn_bf[l % 2][e].h.rearrange("(k p) n -> p k n", p=128), r=[wdn_bf[l % 2][e]], w=[wdn[wb]])
                    if grp is groups[0] and not last:
                        convert_expert(l + 1, e)
                    off = 0
                    for ti, (t0, N, isctx) in enumerate(grp):
                        a = aT[ti % 2]
                        for m in range(KC):
                            b = m % 3
                            pg = nps()
                            for kc in range(KC):
                                p.mm(pg[:, :N], wgu[wb][:, kc, m * 128:(m + 1) * 128], h2[:, kc, off:off + N], kc == 0, kc == KC - 1, r=[wgu[wb], h2], w=[pg])
                            pu = nps()
                            for kc in range(KC):
                                p.mm(pu[:, :N], wgu[wb][:, kc, D + m * 128:D + (m + 1) * 128], h2[:, kc, off:off + N], kc == 0, kc == KC - 1, r=[wgu[wb], h2], w=[pu])
                            bg = fv[:, FV_BGU + e * 16 + m:FV_BGU + e * 16 + m + 1]
                            bu = fv[:, FV_BGU + e * 16 + 8 + m:FV_BGU + e * 16 + 8 + m + 1]
                            p.ts("dve", g1[b][:, :N], pg[:, :N], bg, 7.0, ALU.add, ALU.min, r=[pg, fv], w=[g1[b]])
                            p.act(s1[b][:, :N], g1[b][:, :N], AF.Gelu_apprx_sigmoid, r=[g1[b]], w=[s1[b]])
                            p.ts("dve", u1[b][:, :N], pu[:, :N], bu1[:, e, m:m + 1], 8.0, ALU.add, ALU.min, r=[pu, bu1], w=[u1[b]])
                            p.stt("dve", a[:, m, :N], u1[b][:, :N], -6.0, s1[b][:, :N], ALU.max, ALU.mult, r=[u1[b], s1[b]], w=[a])
                        for s in range(N // 128):
                            si = off // 128 + s
                            for hf_ in range(2):
                                pd = nps()
                                for kc in range(KC):
                                    p.mm(pd[:], a[:, kc, s * 128:(s + 1) * 128], wdn[wb][:, kc, hf_ * 512:(hf_ + 1) * 512], kc == 0, kc == KC - 1, r=[a, wdn[wb]], w=[pd])
                                cs = slice(hf_ * 512, (hf_ + 1) * 512)
                                if e == 0:
                                    p.ts("dve", accs[si][:, cs], pd[:], wsb[si][:, e:e + 1], None, ALU.mult, None, r=[pd, wsb[si]], w=[accs[si]])
                                else:
                                    p.stt("dve", accs[si][:, cs], pd[:], wsb[si][:, e:e + 1], accs[si][:, cs], ALU.mult, ALU.add, r=[pd, wsb[si], accs[si]], w=[accs[si]])
                        off += N
                off = 0
                for (t0, N, isctx) in grp:
                    col = 0 if isctx else 1
                    for s in range(N // 128):
                        si = off // 128 + s
                        pt = nps()
                        p.tr(pt[0:NEXP, 0:128], wsb[si][:], ident, r=[wsb[si], cst], w=[pt])
                        p.copy("dve", wT[:], pt[0:NEXP, 0:128], r=[pt], w=[wT])
                        for hf_ in range(2):
                            pb = nps()
                            p.mm(pb[:], wT[:], bdn[:, hf_ * 512:(hf_ + 1) * 512], True, True, r=[wT, bdn], w=[pb])
                            cs = slice(hf_ * 512, (hf_ + 1) * 512)
                            p.tt("dve", accs[si][:, cs], accs[si][:, cs], pb[:], ALU.add, r=[accs[si], pb], w=[accs[si]])
                    for m in range(KC):
                        p.dma("sp", xo[:, :N], xT[m, :, t0:t0 + N], r=[xT], w=[xo])
                        pt = nps()
                        for s in range(N // 128):
                            si = off // 128 + s
                            p.tr(pt[:, s * 128:(s + 1) * 128], accs[si][:, m * 128:(m + 1) * 128], ident, r=[accs[si], cst], w=[pt])
                        p.stt("dve", xn2[:, :N], pt[:, :N], modv[:, 5, m, col:col + 1], xo[:, :N], ALU.mult, ALU.add, r=[pt, modv, xo], w=[xn2])
                        p.dma("sp", xT[m, :, t0:t0 + N], xn2[:, :N], r=[xn2], w=[xT], merge=True)
                    off += N

    with p.scope():
        xt = p.sb([128, KC, 512], F32, "fx")
        sq = p.sb([128, KC, 512], F32, "fsq")
        rstd = p.sb([128, 512], F32, "frstd")
        ot = [p.sb([128, D], F32, "fot%d" % i) for i in range(2)]
        p.dma("sp", fv[:], fv_d[0], w=[fv])
        for (t0, N, isctx) in tiles:
            if isctx:
                continue
            p.dma("sp", xt[:, :, :N], xT[:, :, t0:t0 + N].rearrange("k p n -> p k n"), r=[xT], w=[xt])
            p.act(sq[:, :, :N], xt[:, :, :N], AF.Square, r=[xt], w=[sq])
            ps = nps()
            for kc in range(KC):
                p.mm(ps[:, :N], ones, sq[:, kc, :N], kc == 0, kc == KC - 1, r=[cst, sq], w=[ps])
            p.act(rstd[:, :N], ps[:, :N], AF.Ln, r=[ps], w=[rstd], bias=EPS, scale=1.0 / D)
            p.act(rstd[:, :N], rstd[:, :N], AF.Exp, r=[rstd], w=[rstd], scale=-0.5)
            for kc in range(KC):
                p.stt("dve", sq[:, kc, :N], xt[:, kc, :N], fv[:, FV_FG + kc:FV_FG + kc + 1], rstd[:, :N], ALU.mult, ALU.mult, r=[xt, fv, rstd], w=[sq])
            for s in range(N // 128):
                o = ot[s % 2]
                for half in range(2):
                    ps = nps()
                    for j in range(4):
                        kc = half * 4 + j
                        p.tr(ps[:, j * 128:(j + 1) * 128], sq[:, kc, s * 128:(s + 1) * 128], ident, r=[sq, cst], w=[ps])
                    p.copy("dve" if half == 0 else "act", o[:, half * 512:(half + 1) * 512], ps[:], r=[ps], w=[o])
                r0 = t0 - CTX + s * 128
                p.dma("sp", out_d[r0:r0 + 128, :], o[:], r=[o], w=[out_d], merge=True)
    p.finish()
    return nc, p


def prep_shared(inputs, depth, lat):
    f = lambda a: np.ascontiguousarray(np.asarray(a, dtype=np.float32))
    fm = lambda v, n: np.asarray(v, np.float32).reshape(n, 128).T
    fv = np.zeros((depth, 128, NFV), np.float32)
    for l in range(depth):
        fv[l, :, FV_N1:FV_N1 + 8] = fm(inputs["norm1_g"][l], 8)
        fv[l, :, FV_N2:FV_N2 + 8] = fm(inputs["norm2_g"][l], 8)
        fv[l, :, FV_MODB:FV_MODB + 48] = fm(inputs["mod_b"][l], 48)
        cw = np.asarray(inputs["lru_conv_w"][l], np.float32)
        for w in range(4):
            fv[l, :, FV_CW + w * 4:FV_CW + w * 4 + 4] = fm(cw[w], 4)
        fv[l, :, FV_CB:FV_CB + 4] = fm(inputs["lru_conv_b"][l], 4)
        for d in range(2):
            fv[l, :, FV_BA + d * 4:FV_BA + d * 4 + 4] = fm(inputs["lru_ba"][l][d], 4)
            fv[l, :, FV_BX + d * 4:FV_BX + d * 4 + 4] = fm(inputs["lru_bx"][l][d], 4)
            fv[l, :, FV_LAM + d * 4:FV_LAM + d * 4 + 4] = fm(inputs["lru_lambda"][l][d], 4)
        fv[l, :, FV_FG:FV_FG + 8] = fm(inputs["final_g"], 8)
        bgu = np.asarray(inputs["exp_b_gu"][l], np.float32)
        fv[l, :, FV_BGU:FV_BGU + 512] = bgu.reshape(NEXP, 16, 128).transpose(2, 0, 1).reshape(128, 512)
    lbl = np.asarray(inputs["hgrn_lb_logits"], np.float32)
    if lbl.shape[0] < 4:
        lbl = np.concatenate([lbl, np.full((4 - lbl.shape[0], 2, 512), -100.0, np.float32)], axis=0)
    lblT = lbl.reshape(lbl.shape[0], 2, 4, 128).transpose(3, 0, 1, 2).reshape(128, lbl.shape[0] * 8)
    lblT4 = np.zeros((128, 32), np.float32)
    lblT4[:, :lblT.shape[1]] = lblT
    bd = np.zeros((depth, 2, 2, 4, 128, 128), np.float32)
    for l in range(depth):
        for ai, nm in enumerate(("lru_wa", "lru_wx")):
            wsrc = np.asarray(inputs[nm][l], np.float32)
            for d in range(2):
                for c in range(4):
                    for j in range(2):
                        bd[l, ai, d, c, j * 64:(j + 1) * 64, j * 64:(j + 1) * 64] = wsrc[d, c * 2 + j]
    sh = {
        "consts": make_consts(), "rot": make_rot(lat), "fv": fv, "lblT": lblT4,
        "hgrn_lb_logits": f(lbl).reshape(1, -1),
        "mod_w": f(inputs["mod_w"][:depth]), "w_in": f(inputs["w_in"][:depth]),
        "ret_decay": f(inputs["ret_decay"][:depth]).reshape(depth, 16),
        "ret_gn_g": f(inputs["ret_gn_g"][:depth]), "hgrn_gn_g": f(inputs["hgrn_gn_g"][:depth]),
        "w_ret_o": f(inputs["w_ret_o"][:depth]), "w_hgrn_o": f(inputs["w_hgrn_o"][:depth]), "w_lru_o": f(inputs["w_lru_o"][:depth]),
        "w_out": f(inputs["w_out"][:depth]), "lru_bd": bd,
        "router_w": f(inputs["router_w"][:depth]), "router_b": f(inputs["router_b"][:depth]),
        "exp_w_gu": f(inputs["exp_w_gu"][:depth]), "exp_w_down": f(inputs["exp_w_down"][:depth]), "exp_b_down": f(inputs["exp_b_down"][:depth]),
    }
    return sh


def core_inputs(inputs, b, sh):
    x = np.asarray(inputs["x"][b], np.float32)
    ctx = np.asarray(inputs["ctx"][b], np.float32)
    xin = np.ascontiguousarray(np.concatenate([ctx, x], axis=0))
    sc = np.stack([np.asarray(inputs["c_ctx"], np.float32), np.asarray(inputs["c"][b], np.float32)], axis=-1)
    scin = np.ascontiguousarray(sc.reshape(KC, 128, 2).transpose(1, 0, 2).reshape(128, KC * 2))
    m = dict(sh)
    m["xin"] = xin
    m["scin"] = scin
    return m


_CACHE = {}


def kernel(**inputs):
    B, L, _ = inputs["x"].shape
    depth = inputs["w_in"].shape[0]
    key = (L, depth)
    if key not in _CACHE:
        _CACHE[key] = build(L, depth)[0]
    nc = _CACHE[key]
    sh = prep_shared(inputs, depth, L)
    n = 8
    in_maps = [core_inputs(inputs, c % B, sh) for c in range(n)]
    res = run_bass_kernel_spmd(nc, in_maps, core_ids=list(range(n)))
    out = np.stack([np.asarray(res.results[b]["out"], np.float32) for b in range(B)], axis=0)
    return out
```

```python
import contextlib
import numpy as np
import concourse.bass as bass
import concourse.mybir as mybir
from concourse.bass_utils import run_bass_kernel_spmd

F32 = mybir.dt.float32
BF16 = mybir.dt.bfloat16
ALU = mybir.AluOpType
AF = mybir.ActivationFunctionType
AX = mybir.AxisListType

D = 1024
KC = 8
CTX = 256
NEXP = 32
EPS = 1e-6
IN_COLS = 8704


class T:
    __slots__ = ("h", "w", "r", "name")

    def __init__(self, h, name):
        self.h = h
        self.name = name
        self.w = {}
        self.r = {}

    def __getitem__(self, k):
        return self.h[k]


class Prog:
    NDMA = 8

    def __init__(self, nc, same_engine_sync=True):
        self.nc = nc
        self.stack = [contextlib.ExitStack()]
        self.eng = {"pe": nc.tensor, "act": nc.scalar, "dve": nc.vector, "pool": nc.gpsimd, "sp": nc.sync}
        self.semh = {}
        self.cnt = {}
        self.known = {k: {} for k in self.eng}
        for k in self.eng:
            self.semh[k] = self.stack[0].enter_context(nc.semaphore("s_" + k))
            self.cnt[k] = 0
        self.dq = {}
        for q in ("sp", "pool", "act"):
            sems = []
            for i in range(self.NDMA):
                key = "d_%s%d" % (q, i)
                self.semh[key] = self.stack[0].enter_context(nc.semaphore(key))
                self.cnt[key] = 0
                sems.append(key)
            self.dq[q] = [sems, 0]
        self.same = same_engine_sync
        self.uid = 0
        self.ninst = 0

    @contextlib.contextmanager
    def scope(self, name=None):
        es = contextlib.ExitStack()
        self.stack.append(es)
        if name is not None:
            es.enter_context(self.nc.named_scope(name))
        try:
            yield
        finally:
            self.barrier()
            self.stack.pop()
            es.close()

    def sb(self, shape, dt=F32, name=None):
        self.uid += 1
        name = (name or "t") + "_%d" % self.uid
        h = self.stack[-1].enter_context(self.nc.sbuf_tensor(name, list(shape), dt))
        return T(h, name)

    def ps(self, shape, dt=F32, name=None):
        self.uid += 1
        name = (name or "p") + "_%d" % self.uid
        h = self.stack[-1].enter_context(self.nc.psum_tensor(name, list(shape), dt))
        return T(h, name)

    def dram(self, name, shape, dt, kind="Internal"):
        h = self.nc.dram_tensor(name, list(shape), dt, kind=kind)
        return T(h.ap(), name)

    def _wait(self, ek, need):
        e = self.eng[ek]
        kn = self.known[ek]
        for s, v in need.items():
            if s == ek and (not self.same or ek == "pe"):
                continue
            if kn.get(s, 0) < v:
                e.wait_ge(self.semh[s], v)
                kn[s] = v

    @staticmethod
    def _merge(d, s):
        for k, v in s.items():
            if d.get(k, 0) < v:
                d[k] = v

    def _need(self, r, w, skip_waw=False):
        need = {}
        for t in r:
            self._merge(need, t.w)
        for t in w:
            if not skip_waw:
                self._merge(need, t.w)
            self._merge(need, t.r)
        return need

    def _record(self, tok, r, w, merge=False):
        for t in w:
            if merge:
                self._merge(t.w, tok)
            else:
                t.w = dict(tok)
                t.r = {}
        for t in r:
            if t not in w:
                self._merge(t.r, tok)

    def op(self, ek, fn, r=(), w=()):
        self._wait(ek, self._need(r, w))
        inst = fn(self.eng[ek])
        self.cnt[ek] += 1
        inst.then_inc(self.semh[ek], 1)
        self.ninst += 1
        self._record({ek: self.cnt[ek]}, r, w)
        return inst

    def dma(self, q, out_ap, in_ap, r=(), w=(), merge=False):
        sems, i = self.dq[q]
        key = sems[i % len(sems)]
        self.dq[q][1] = i + 1
        need = self._need(r, w, skip_waw=merge)
        if self.cnt[key] > 0:
            need[key] = max(need.get(key, 0), self.cnt[key])
        self._wait(q, need)
        inst = self.eng[q].dma_start(out=out_ap, in_=in_ap)
        self.cnt[key] += 16
        inst.then_inc(self.semh[key], 16)
        self.ninst += 1
        self._record({key: self.cnt[key]}, r, w, merge=merge)
        return inst

    def barrier(self):
        allv = {k: v for k, v in self.cnt.items() if v > 0}
        for ek in self.eng:
            self._wait(ek, dict(allv))

    def finish(self):
        self.barrier()
        while self.stack:
            self.stack.pop().close()

    def tt(self, ek, out, in0, in1, op, r, w):
        return self.op(ek, lambda e: e.tensor_tensor(out=out, in0=in0, in1=in1, op=op), r=r, w=w)

    def ts(self, ek, out, in0, s1, s2, op0, op1, r, w):
        if s2 is None:
            return self.op(ek, lambda e: e.tensor_scalar(out=out, in0=in0, scalar1=s1, scalar2=None, op0=op0), r=r, w=w)
        return self.op(ek, lambda e: e.tensor_scalar(out=out, in0=in0, scalar1=s1, scalar2=s2, op0=op0, op1=op1), r=r, w=w)

    def stt(self, ek, out, in0, scalar, in1, op0, op1, r, w):
        return self.op(ek, lambda e: e.scalar_tensor_tensor(out=out, in0=in0, scalar=scalar, in1=in1, op0=op0, op1=op1), r=r, w=w)

    def act(self, out, in_, func, r, w, bias=None, scale=None):
        kw = {}
        if bias is not None:
            kw["bias"] = bias
        if scale is not None:
            kw["scale"] = scale
        return self.op("act", lambda e: e.activation(out=out, in_=in_, func=func, **kw), r=r, w=w)

    def copy(self, ek, out, in_, r, w):
        if ek == "act":
            return self.op("act", lambda e: e.copy(out=out, in_=in_), r=r, w=w)
        return self.op(ek, lambda e: e.tensor_copy(out=out, in_=in_), r=r, w=w)

    def mm(self, out, lhsT, rhs, start, stop, r, w):
        return self.op("pe", lambda e: e.matmul(out, lhsT=lhsT, rhs=rhs, start=start, stop=stop), r=r, w=w)

    def tr(self, out, in_, ident, r, w):
        return self.op("pe", lambda e: e.transpose(out=out, in_=in_, identity=ident), r=r, w=w)


C_ID = 0
C_MF = 128
C_MB = 256
C_TF = 384
C_TB = 448
C_RF = 512
C_RB = 576
C_IDX = 640
C_ONE = 646
NCONST = 774

FV_N1 = 0
FV_N2 = 8
FV_MODB = 16
FV_CW = 64
FV_CB = 80
FV_BA = 84
FV_BX = 92
FV_LAM = 100
FV_FG = 108
FV_BGU = 116
NFV = 628

G_RQ, G_RK, G_RV, G_RG, G_HQ, G_HFF, G_HFB, G_HI, G_HGT, G_LX, G_LGT = [512 * i for i in range(11)]
G_GR = 5632
G_GH = 6656
G_GL = 7680


def make_consts():
    c = np.zeros((128, NCONST), np.float32)
    p = np.arange(128)
    c[:, C_ID:C_ID + 128] = np.eye(128)
    c[:, C_MF:C_MF + 128] = (p[None, :] >= p[:, None])
    c[:, C_MB:C_MB + 128] = (p[:, None] > p[None, :])
    q = np.arange(64)
    c[:64, C_TF:C_TF + 64] = (q[:, None] <= q[None, :])
    c[:64, C_TB:C_TB + 64] = (q[:, None] >= q[None, :])
    c[:64, C_RF:C_RF + 64] = (q[:, None] > q[None, :])
    c[:64, C_RB:C_RB + 64] = (q[:, None] < q[None, :])
    c[:, C_IDX + 0] = p + 1
    c[:, C_IDX + 1] = 127 - p
    c[:, C_IDX + 2] = 128 - p
    c[:, C_IDX + 3] = p
    c[:, C_IDX + 4] = -(p + 1)
    c[:, C_IDX + 5] = p - 128
    c[:, C_ONE:C_ONE + 128] = 1.0
    return c


def make_rot(lat):
    n = np.arange(lat)
    row = (n // 64).astype(np.float32)
    col = (n % 64).astype(np.float32)
    inv = (10000.0 ** (-np.arange(16, dtype=np.float32) / 16)).astype(np.float32)
    ang = np.concatenate([row[:, None] * inv, col[:, None] * inv], axis=-1).astype(np.float32)
    cs, sn = np.cos(ang), np.sin(ang)
    return np.concatenate([cs, sn, 0.125 * cs, 0.125 * sn], axis=-1).astype(np.float32)


def build(lat, depth, dbg=()):
    S = CTX + lat
    nc = bass.Bass("TRN2", target_bir_lowering=False)
    p = Prog(nc)
    tiles = [(0, 256, True)] + [(CTX + 512 * i, 512, False) for i in range(lat // 512)]
    NT128 = S // 128

    def din(name, shape):
        return p.dram(name, shape, F32, kind="ExternalInput")

    xin = din("xin", [S, D])
    scin = din("scin", [128, KC * 2])
    consts_d = din("consts", [128, NCONST])
    rot_d = din("rot", [lat, 128])
    fv_d = din("fv", [depth, 128, NFV])
    lblT_d = din("lblT", [128, 4 * 8])
    lbl_d = din("hgrn_lb_logits", [1, 4 * 2 * 512])
    mod_w = din("mod_w", [depth, D, 6 * D])
    w_in = din("w_in", [depth, D, IN_COLS])
    ret_decay = din("ret_decay", [depth, 16])
    ret_gn = din("ret_gn_g", [depth, 512])
    hg_gn = din("hgrn_gn_g", [depth, 512])
    w_ret_o = din("w_ret_o", [depth, 512, D])
    w_hg_o = din("w_hgrn_o", [depth, 512, D])
    w_lru_o = din("w_lru_o", [depth, 512, D])
    w_out = din("w_out", [depth, D, D])
    lru_bd = din("lru_bd", [depth, 2, 2, 4, 128, 128])
    router_w = din("router_w", [depth, D, NEXP])
    router_b = din("router_b", [depth, NEXP])
    w_gu = din("exp_w_gu", [depth, NEXP, D, 2 * D])
    w_dn = din("exp_w_down", [depth, NEXP, D, D])
    b_dn = din("exp_b_down", [depth, NEXP, D])
    out_d = p.dram("out", [lat, D], F32, kind="ExternalOutput")

    def scr(name, shape, dt=F32):
        kind = "ExternalOutput" if name in dbg else "Internal"
        return p.dram(name, shape, dt, kind=kind)

    xT = scr("xT", [KC, 128, S])
    r_q = scr("r_q", [S, 512], BF16)
    r_k = scr("r_k", [S, 512], BF16)
    r_v = scr("r_v", [S, 512], BF16)
    r_g = scr("r_g", [S, 512])
    h_qT = scr("h_qT", [4, 128, S])
    h_kT = [scr("h_kfT", [4, 128, S]), scr("h_kbT", [4, 128, S])]
    h_k = [scr("h_kf", [S, 512]), scr("h_kb", [S, 512])]
    h_lf = [scr("h_lff", [S, 512]), scr("h_lfb", [S, 512])]
    h_v = scr("h_v", [S, 512], BF16)
    h_g = scr("h_g", [S, 512])
    l_xT = scr("l_xT", [4, 128, S])
    l_gT = scr("l_gT", [4, 128, S])
    g_T = scr("g_T", [3, KC, 128, S])
    o_ret = scr("o_ret", [S, 512])
    o_hg = scr("o_hg", [S, 512])
    retnT = scr("retnT", [4, 128, S], BF16)
    hgnT = scr("hgnT", [4, 128, S], BF16)
    lruT = scr("lruT", [4, 128, S], BF16)
    h2T_d = scr("h2T", [KC, 128, S], BF16)
    lbrow_d = scr("lbrow", [4, 128, 2048])
    wmoe_d = scr("wmoe", [S, NEXP])
    wgu_bf = [[p.dram("wgubf_%d_%d" % (i, e), [D, 2 * D], BF16) for e in range(NEXP)] for i in range(2)]
    wdn_bf = [[p.dram("wdnbf_%d_%d" % (i, e), [D, D], BF16) for e in range(NEXP)] for i in range(2)]

    def convert_expert(ll, e):
        par = ll % 2
        for q4 in range(4):
            rs = slice(q4 * 256, (q4 + 1) * 256)
            p.dma("pool", wgu_bf[par][e][rs, :], w_gu[ll, e, rs, :], w=[wgu_bf[par][e]], merge=True)
        for q2 in range(2):
            rs = slice(q2 * 512, (q2 + 1) * 512)
            p.dma("pool", wdn_bf[par][e][rs, :], w_dn[ll, e, rs, :], w=[wdn_bf[par][e]], merge=True)

    cst = p.sb([128, NCONST], F32, "cst")
    identb = p.sb([128, 128], BF16, "identb")
    sT = p.sb([128, KC * 2], F32, "sT")
    lbT = p.sb([128, 32], F32, "lbT")
    omlT = p.sb([128, 32], F32, "omlT")
    fv = p.sb([128, NFV], F32, "fv")
    modv = p.sb([128, 6, KC, 2], F32, "modv")
    psb = [p.ps([128, 512], F32, "psf%d" % i) for i in range(6)]
    psh = [p.ps([128, 1024], BF16, "psh%d" % i) for i in range(2)]
    rr = {"f": 0, "h": 0}

    def nps():
        rr["f"] += 1
        return psb[rr["f"] % 6]

    def npsh():
        rr["h"] += 1
        return psh[rr["h"] % 2]

    ident = cst[:, C_ID:C_ID + 128]
    ones = cst[:, C_ONE:C_ONE + 128]

    p.dma("sp", cst[:], consts_d[:], w=[cst])
    p.dma("sp", sT[:], scin[:], w=[sT])
    p.copy("dve", identb[:], ident, r=[cst], w=[identb])
    with p.scope():
        t0 = p.sb([128, KC * 2])
        p.act(t0[:], sT[:], AF.Sigmoid, r=[sT], w=[t0])
        p.tt("dve", sT[:], sT[:], t0[:], ALU.mult, r=[sT, t0], w=[sT])
        e0 = p.sb([128, 32])
        p.dma("sp", e0[:], lblT_d[:], w=[e0])
        p.act(e0[:], e0[:], AF.Exp, r=[e0], w=[e0])
        sm = p.sb([128, 8])
        p.tt("dve", sm[:], e0[:, 0:8], e0[:, 8:16], ALU.add, r=[e0], w=[sm])
        p.tt("dve", sm[:], sm[:], e0[:, 16:24], ALU.add, r=[e0, sm], w=[sm])
        p.tt("dve", sm[:], sm[:], e0[:, 24:32], ALU.add, r=[e0, sm], w=[sm])
        p.op("dve", lambda e: e.reciprocal(out=sm[:], in_=sm[:]), r=[sm], w=[sm])
        p.op("dve", lambda e: e.memset(lbT[:, 0:8], 0.0), w=[lbT])
        for l in range(1, 4):
            p.tt("dve", e0[:, 8 * l:8 * l + 8], e0[:, 8 * l:8 * l + 8], sm[:], ALU.mult, r=[e0, sm], w=[e0])
            p.tt("dve", lbT[:, 8 * l:8 * l + 8], lbT[:, 8 * l - 8:8 * l], e0[:, 8 * l:8 * l + 8], ALU.add, r=[e0, lbT], w=[lbT])
        p.ts("dve", omlT[:], lbT[:], -1.0, 1.0, ALU.mult, ALU.add, r=[lbT], w=[omlT])
        er = p.sb([128, 4, 1024])
        p.dma("sp", er[:].rearrange("p l n -> p (l n)"), lbl_d[:].partition_broadcast(128), w=[er])
        p.act(er[:], er[:], AF.Exp, r=[er], w=[er])
        sr = p.sb([128, 1024])
        p.tt("dve", sr[:], er[:, 0, :], er[:, 1, :], ALU.add, r=[er], w=[sr])
        p.tt("dve", sr[:], sr[:], er[:, 2, :], ALU.add, r=[er, sr], w=[sr])
        p.tt("dve", sr[:], sr[:], er[:, 3, :], ALU.add, r=[er, sr], w=[sr])
        p.op("dve", lambda e: e.reciprocal(out=sr[:], in_=sr[:]), r=[sr], w=[sr])
        lbr = p.sb([128, 2048])
        p.op("dve", lambda e: e.memset(lbr[:, 0:1024], 0.0), w=[lbr])
        for l in range(4):
            if l > 0:
                p.tt("dve", er[:, l, :], er[:, l, :], sr[:], ALU.mult, r=[er, sr], w=[er])
                p.tt("dve", lbr[:, 0:1024], lbr[:, 0:1024], er[:, l, :], ALU.add, r=[er, lbr], w=[lbr])
            p.ts("dve", lbr[:, 1024:2048], lbr[:, 0:1024], -1.0, 1.0, ALU.mult, ALU.add, r=[lbr], w=[lbr])
            p.dma("sp", lbrow_d[l], lbr[:], r=[lbr], w=[lbrow_d], merge=True)

    for e in range(NEXP):
        convert_expert(0, e)

    with p.scope():
        xts = [p.sb([128, D], F32, "xt") for _ in range(2)]
        xo = [p.sb([128, KC, 128], F32, "xo") for _ in range(2)]
        for t in range(NT128):
            xt = xts[t % 2]
            p.dma("sp", xt[:], xin[t * 128:(t + 1) * 128, :], w=[xt])
            o = xo[t % 2]
            for half in range(2):
                ps = nps()
                for j in range(4):
                    kc = half * 4 + j
                    p.tr(ps[:, j * 128:(j + 1) * 128], xt[:, kc * 128:(kc + 1) * 128], ident, r=[xt, cst], w=[ps])
                p.copy("dve" if half == 0 else "act", o[:, half * 4:half * 4 + 4, :].rearrange("p k n -> p (k n)"), ps[:], r=[ps], w=[o])
            p.dma("sp", xT[:, :, t * 128:(t + 1) * 128].rearrange("k p n -> p k n"), o[:], r=[o], w=[xT], merge=True)

    for l in range(depth):
        last = (l == depth - 1)
        p.dma("sp", fv[:], fv_d[l], w=[fv])
        with p.scope("mod%d" % l):
            mraw = p.sb([128, 48, 2], F32, "mraw")
            wts = [p.sb([128, KC, 512], F32, "modw") for _ in range(2)]
            for g in range(12):
                wt = wts[g % 2]
                p.dma("sp", wt[:], mod_w[l, :, g * 512:(g + 1) * 512].rearrange("(k p) n -> p k n", p=128), w=[wt])
                ps = nps()
                for m in range(4):
                    for kc in range(KC):
                        p.mm(ps[:, m * 2:m * 2 + 2], wt[:, kc, m * 128:(m + 1) * 128], sT[:, kc * 2:kc * 2 + 2],
                             kc == 0, kc == KC - 1, r=[wt, sT], w=[ps])
                p.copy("dve", mraw[:, g * 4:g * 4 + 4, :].rearrange("p a b -> p (a b)"), ps[:, 0:8], r=[ps], w=[mraw])
            p.tt("dve", mraw[:], mraw[:], fv[:, FV_MODB:FV_MODB + 48].unsqueeze(2).to_broadcast([128, 48, 2]), ALU.add, r=[mraw, fv], w=[mraw])
            for (dst, src, ng) in ((0, 1, FV_N1), (3, 4, FV_N2)):
                p.ts("dve", modv[:, dst], mraw[:, src * 8:src * 8 + 8, :], 1.0, None, ALU.add, None, r=[mraw], w=[modv])
                p.tt("dve", modv[:, dst], modv[:, dst], fv[:, ng:ng + 8].unsqueeze(2).to_broadcast([128, 8, 2]), ALU.mult, r=[modv, fv], w=[modv])
            for (dst, src) in ((1, 0), (2, 2), (4, 3), (5, 5)):
                p.copy("dve", modv[:, dst], mraw[:, src * 8:src * 8 + 8, :], r=[mraw], w=[modv])

        with p.scope("A%d" % l):
            hT = p.sb([128, KC, S], BF16, "hT")
            with p.scope():
                xt = p.sb([128, KC, 512], F32, "xa")
                sq = p.sb([128, KC, 512], F32, "sq")
                rstd = p.sb([128, 512], F32, "rstd")
                for (t0, N, isctx) in tiles:
                    col = 0 if isctx else 1
                    p.dma("sp", xt[:, :, :N], xT[:, :, t0:t0 + N].rearrange("k p n -> p k n"), r=[xT], w=[xt])
                    p.act(sq[:, :, :N], xt[:, :, :N], AF.Square, r=[xt], w=[sq])
                    ps = nps()
                    for kc in range(KC):
                        p.mm(ps[:, :N], ones, sq[:, kc, :N], kc == 0, kc == KC - 1, r=[cst, sq], w=[ps])
                    p.act(rstd[:, :N], ps[:, :N], AF.Ln, r=[ps], w=[rstd], bias=EPS, scale=1.0 / D)
                    p.act(rstd[:, :N], rstd[:, :N], AF.Exp, r=[rstd], w=[rstd], scale=-0.5)
                    for kc in range(KC):
                        p.stt("dve", sq[:, kc, :N], xt[:, kc, :N], modv[:, 0, kc, col:col + 1], rstd[:, :N], ALU.mult, ALU.mult,
                              r=[xt, modv, rstd], w=[sq])
                        p.act(hT[:, kc, t0:t0 + N], sq[:, kc, :N], AF.Identity, r=[sq, modv], w=[hT], bias=modv[:, 1, kc, col:col + 1])

            lbr = p.sb([128, 2048], F32, "lbr")
            p.dma("sp", lbr[:], lbrow_d[l], r=[lbrow_d], w=[lbr])
            wgs = [p.sb([128, KC, 512], BF16, "wg") for _ in range(2)]
            gi = [0]

            def load_wg(c0):
                wg = wgs[gi[0] % 2]
                gi[0] += 1
                p.dma("pool", wg[:], w_in[l, :, c0:c0 + 512].rearrange("(k p) n -> p k n", p=128), w=[wg])
                return wg

            rot = [p.sb([128, 128], F32, "rot") for _ in range(2)]
            tmpA = [p.sb([128, 512], F32, "tmpA") for _ in range(2)]
            tmpB = [p.sb([128, 512], F32, "tmpB") for _ in range(2)]
            obf = [p.sb([128, 512], BF16, "obf") for _ in range(2)]
            of32 = [p.sb([128, 512], F32, "of32") for _ in range(2)]
            of32b = [p.sb([128, 512], F32, "of32b") for _ in range(2)]
            cnt = [0]

            def tm_loop(wg, handler):
                for t in range(NT128):
                    ps = nps()
                    for kc in range(KC):
                        p.mm(ps[:], hT[:, kc, t * 128:(t + 1) * 128], wg[:, kc, :], kc == 0, kc == KC - 1, r=[hT, wg], w=[ps])
                    cnt[0] += 1
                    handler(t, ps, cnt[0] % 2)

            def fm_loop(wg, handler):
                for (t0, N, isctx) in tiles:
                    for m in range(4):
                        ps = nps()
                        for kc in range(KC):
                            p.mm(ps[:, :N], wg[:, kc, m * 128:(m + 1) * 128], hT[:, kc, t0:t0 + N], kc == 0, kc == KC - 1, r=[hT, wg], w=[ps])
                        cnt[0] += 1
                        handler(t0, N, m, ps, cnt[0] % 2)

            def store_tm(dst, t, src):
                p.dma("sp", dst[t * 128:(t + 1) * 128, :], src[:], r=[src], w=[dst], merge=True)

            def store_fm(dst3, m, t0, N, src):
                p.dma("sp", dst3[m, :, t0:t0 + N], src[:, :N], r=[src], w=[dst3], merge=True)

            def h_rot(dst, cofs):
                def h(t, ps, b):
                    o = obf[b]
                    if t < 2:
                        if cofs == 0:
                            p.copy("dve", o[:], ps[:], r=[ps], w=[o])
                        else:
                            p.ts("dve", o[:], ps[:], 0.125, None, ALU.mult, None, r=[ps], w=[o])
                    else:
                        rt = rot[b]
                        p.dma("sp", rt[:], rot_d[(t - 2) * 128:(t - 1) * 128, :], w=[rt])
                        pv = ps[:].rearrange("p (h two d) -> p h two d", h=8, two=2)
                        ov = o[:].rearrange("p (h two d) -> p h two d", h=8, two=2)
                        cs = rt[:, cofs:cofs + 32].unsqueeze(1).to_broadcast([128, 8, 32])
                        sn = rt[:, cofs + 32:cofs + 64].unsqueeze(1).to_broadcast([128, 8, 32])
                        a = tmpA[b][:, 0:256].rearrange("p (h d) -> p h d", h=8)
                        bb = tmpA[b][:, 256:512].rearrange("p (h d) -> p h d", h=8)
                        p.tt("dve", a, pv[:, :, 0, :], cs, ALU.mult, r=[ps, rt], w=[tmpA[b]])
                        p.tt("dve", bb, pv[:, :, 1, :], sn, ALU.mult, r=[ps, rt], w=[tmpA[b]])
                        p.tt("dve", ov[:, :, 0, :], a, bb, ALU.subtract, r=[tmpA[b]], w=[o])
                        p.tt("dve", a, pv[:, :, 0, :], sn, ALU.mult, r=[ps, rt], w=[tmpA[b]])
                        p.tt("dve", bb, pv[:, :, 1, :], cs, ALU.mult, r=[ps, rt], w=[tmpA[b]])
                        p.tt("dve", ov[:, :, 1, :], a, bb, ALU.add, r=[tmpA[b]], w=[o])
                    store_tm(dst, t, o)
                return h

            def h_copy_bf(dst):
                def h(t, ps, b):
                    p.copy("act", obf[b][:], ps[:], r=[ps], w=[obf[b]])
                    store_tm(dst, t, obf[b])
                return h

            def h_silu_tm(dst):
                def h(t, ps, b):
                    p.act(tmpA[b][:], ps[:], AF.Sigmoid, r=[ps], w=[tmpA[b]])
                    p.tt("dve", of32[b][:], tmpA[b][:], ps[:], ALU.mult, r=[ps, tmpA[b]], w=[of32[b]])
                    store_tm(dst, t, of32[b])
                return h

            def h_sig_tm(dst):
                def h(t, ps, b):
                    p.act(of32[b][:], ps[:], AF.Sigmoid, r=[ps], w=[of32[b]])
                    store_tm(dst, t, of32[b])
                return h

            def h_forget_tm(d):
                lb = lbr[:, d * 512:(d + 1) * 512]
                oml = lbr[:, 1024 + d * 512:1024 + (d + 1) * 512]

                def h(t, ps, b):
                    p.act(tmpA[b][:], ps[:], AF.Sigmoid, r=[ps], w=[tmpA[b]])
                    p.tt("dve", tmpA[b][:], tmpA[b][:], oml, ALU.mult, r=[tmpA[b], lbr], w=[tmpA[b]])
                    p.tt("dve", tmpB[b][:], tmpA[b][:], lb, ALU.add, r=[tmpA[b], lbr], w=[tmpB[b]])
                    p.tt("pool", of32[b][:], oml, tmpA[b][:], ALU.subtract, r=[tmpA[b], lbr], w=[of32[b]])
                    p.act(of32b[b][:], tmpB[b][:], AF.Ln, r=[tmpB[b]], w=[of32b[b]])
                    store_tm(h_k[d], t, of32[b])
                    store_tm(h_lf[d], t, of32b[b])
                return h

            def h_forget_fm(d):
                def h(t0, N, m, ps, b):
                    p.act(tmpA[b][:, :N], ps[:, :N], AF.Sigmoid, r=[ps], w=[tmpA[b]], scale=-1.0)
                    p.ts("dve", of32[b][:, :N], tmpA[b][:, :N], omlT[:, l * 8 + d * 4 + m:l * 8 + d * 4 + m + 1], None, ALU.mult, None,
                         r=[tmpA[b], omlT], w=[of32[b]])
                    store_fm(h_kT[d], m, t0, N, of32[b])
                return h

            def h_silu_fm(dst):
                def h(t0, N, m, ps, b):
                    p.act(tmpA[b][:, :N], ps[:, :N], AF.Sigmoid, r=[ps], w=[tmpA[b]])
                    p.tt("dve", of32[b][:, :N], tmpA[b][:, :N], ps[:, :N], ALU.mult, r=[ps, tmpA[b]], w=[of32[b]])
                    store_fm(dst, m, t0, N, of32[b])
                return h

            def h_copy_fm(dst):
                def h(t0, N, m, ps, b):
                    p.copy("act", of32[b][:, :N], ps[:, :N], r=[ps], w=[of32[b]])
                    store_fm(dst, m, t0, N, of32[b])
                return h

            def h_gelu_fm(dst):
                def h(t0, N, m, ps, b):
                    p.act(tmpA[b][:, :N], ps[:, :N], AF.Square, r=[ps], w=[tmpA[b]])
                    p.ts("dve", tmpA[b][:, :N], tmpA[b][:, :N], 0.044715, 1.0, ALU.mult, ALU.add, r=[tmpA[b]], w=[tmpA[b]])
                    p.tt("dve", tmpA[b][:, :N], tmpA[b][:, :N], ps[:, :N], ALU.mult, r=[tmpA[b], ps], w=[tmpA[b]])
                    p.act(tmpA[b][:, :N], tmpA[b][:, :N], AF.Sigmoid, r=[tmpA[b]], w=[tmpA[b]], scale=1.5957691216057308)
                    p.tt("dve", of32[b][:, :N], tmpA[b][:, :N], ps[:, :N], ALU.mult, r=[tmpA[b], ps], w=[of32[b]])
                    store_fm(dst, m, t0, N, of32[b])
                return h

            def h_sig_fm(dst, gidx, half):
                def h(t0, N, m, ps, b):
                    p.act(of32[b][:, :N], ps[:, :N], AF.Sigmoid, r=[ps], w=[of32[b]])
                    p.dma("sp", dst[gidx, half * 4 + m, :, t0:t0 + N], of32[b][:, :N], r=[of32[b]], w=[dst], merge=True)
                return h

            plan = [
                (G_RQ, "tm", h_rot(r_q, 0)), (G_RK, "tm", h_rot(r_k, 64)), (G_RV, "tm", h_copy_bf(r_v)), (G_RG, "tm", h_silu_tm(r_g)),
                (G_HQ, "fm", h_silu_fm(h_qT)),
                (G_HFF, "tm", h_forget_tm(0)), (G_HFF, "fm", h_forget_fm(0)),
                (G_HFB, "tm", h_forget_tm(1)), (G_HFB, "fm", h_forget_fm(1)),
                (G_HI, "tm", h_copy_bf(h_v)), (G_HGT, "tm", h_sig_tm(h_g)),
                (G_LX, "fm", h_copy_fm(l_xT)), (G_LGT, "fm", h_gelu_fm(l_gT)),
                (G_GR, "fm", h_sig_fm(g_T, 0, 0)), (G_GR + 512, "fm", h_sig_fm(g_T, 0, 1)),
                (G_GH, "fm", h_sig_fm(g_T, 1, 0)), (G_GH + 512, "fm", h_sig_fm(g_T, 1, 1)),
                (G_GL, "fm", h_sig_fm(g_T, 2, 0)), (G_GL + 512, "fm", h_sig_fm(g_T, 2, 1)),
            ]
            prev = (None, None)
            for (c0, kind, hd) in plan:
                if c0 != prev[0]:
                    wg = load_wg(c0)
                    prev = (c0, wg)
                (tm_loop if kind == "tm" else fm_loop)(prev[1], hd)

        def phase_B():
            lgb = p.sb([128, 16], F32, "lgb")
            p.dma("sp", lgb[:], ret_decay[l:l + 1, :].partition_broadcast(128), w=[lgb])
            p.act(lgb[:], lgb[:], AF.Exp, r=[lgb], w=[lgb], scale=-1.0)
            p.act(lgb[:], lgb[:], AF.Ln, r=[lgb], w=[lgb], bias=1.0)
            p.ts("dve", lgb[:], lgb[:], -1.0, None, ALU.mult, None, r=[lgb], w=[lgb])
            gnrow = p.sb([128, 512], F32, "gnrow")
            p.dma("sp", gnrow[:], ret_gn[l:l + 1, :].partition_broadcast(128), w=[gnrow])
            av = p.sb([128, 2, 8], F32, "av")
            bv = p.sb([128, 2, 8], F32, "bv")
            iv = p.sb([128, 2, 8], F32, "iv")
            Dt = p.sb([128, 2, 8, 128], F32, "Dt")
            Gt = p.sb([64, 2, 512], F32, "Gt")
            idx = lambda i: cst[:, C_IDX + i:C_IDX + i + 1]
            for d in range(2):
                lg = lgb[:, d * 8:d * 8 + 8]
                p.act(av[:, d, :], lg, AF.Exp, r=[lgb, cst], w=[av], scale=idx(0) if d == 0 else idx(2))
                p.act(bv[:, d, :], lg, AF.Exp, r=[lgb, cst], w=[bv], scale=idx(1) if d == 0 else idx(3))
                p.act(iv[:, d, :], lg, AF.Exp, r=[lgb, cst], w=[iv], scale=idx(4) if d == 0 else idx(5))
                msk = cst[:, C_MF:C_MF + 128] if d == 0 else cst[:, C_MB:C_MB + 128]
                for h in range(8):
                    p.ts("dve", Dt[:, d, h, :], msk, iv[:, d, h:h + 1], None, ALU.mult, None, r=[cst, iv], w=[Dt])
                p.act(Gt[:, d, :].rearrange("p (h v) -> p h v", h=8), lgb[0:64, d * 8:d * 8 + 8].unsqueeze(2).to_broadcast([64, 8, 64]),
                      AF.Exp, r=[lgb], w=[Gt], scale=128.0)

            Sst = p.sb([64, 512], F32, "Sst")
            Sbf = p.sb([64, 512], BF16, "Sbf")
            NB = 2
            qt = [p.sb([128, 512], BF16, "rq") for _ in range(NB)]
            kt = [p.sb([128, 512], BF16, "rk") for _ in range(NB)]
            vt = [p.sb([128, 512], BF16, "rv") for _ in range(NB)]
            qd = [p.sb([128, 512], BF16, "qd") for _ in range(NB)]
            kd = [p.sb([128, 512], BF16, "kd") for _ in range(NB)]
            qdT = [p.sb([64, 8, 128], BF16, "qdT") for _ in range(NB)]
            kTt = [p.sb([64, 8, 128], BF16, "kTt") for _ in range(NB)]
            Pm = [p.sb([128, 8, 128], BF16, "Pm") for _ in range(NB)]
            osb = [p.sb([128, 512], F32, "osb") for _ in range(NB)]
            oprev = [p.sb([128, 512], F32, "oprev") for _ in range(NB)]
            gate = [p.sb([128, 512], F32, "gate") for _ in range(NB)]
            ssq = [p.sb([128, 8], F32, "ssq") for _ in range(NB)]
            ybf = [p.sb([128, 512], BF16, "ybf") for _ in range(NB)]
            yT = [p.sb([128, 4, 128], BF16, "yT") for _ in range(NB)]
            for d in range(2):
                p.op("dve", lambda e: e.memset(Sst[:], 0.0), w=[Sst])
                p.op("dve", lambda e: e.memset(Sbf[:], 0.0), w=[Sbf])
                order = list(range(NT128)) if d == 0 else [1, 0] + list(range(NT128 - 1, 1, -1))
                for it, t in enumerate(order):
                    b = it % NB
                    yield
                    if last and d == 1 and t < 2:
                        pass
                    rows = slice(t * 128, (t + 1) * 128)
                    p.dma("sp", qt[b][:], r_q[rows, :], r=[r_q], w=[qt[b]])
                    p.dma("sp", kt[b][:], r_k[rows, :], r=[r_k], w=[kt[b]])
                    p.dma("sp", vt[b][:], r_v[rows, :], r=[r_v], w=[vt[b]])
                    v3 = lambda tl: tl[:].rearrange("p (h e) -> p h e", h=8)
                    p.tt("dve", v3(qd[b]), v3(qt[b]), av[:, d, :].unsqueeze(2).to_broadcast([128, 8, 64]), ALU.mult, r=[qt[b], av], w=[qd[b]])
                    p.tt("pool", v3(kd[b]), v3(kt[b]), bv[:, d, :].unsqueeze(2).to_broadcast([128, 8, 64]), ALU.mult, r=[kt[b], bv], w=[kd[b]])
                    ph = npsh()
                    for h in range(8):
                        p.tr(ph[0:64, h * 128:(h + 1) * 128], qd[b][:, h * 64:(h + 1) * 64], identb[:], r=[qd[b], identb], w=[ph])
                    p.copy("act", qdT[b][:].rearrange("p h n -> p (h n)"), ph[0:64, :], r=[ph], w=[qdT[b]])
                    ph = npsh()
                    for h in range(8):
                        p.tr(ph[0:64, h * 128:(h + 1) * 128], kt[b][:, h * 64:(h + 1) * 64], identb[:], r=[kt[b], identb], w=[ph])
                    p.copy("dve", kTt[b][:].rearrange("p h n -> p (h n)"), ph[0:64, :], r=[ph], w=[kTt[b]])
                    for hh in range(2):
                        ps = nps()
                        for j in range(4):
                            h = hh * 4 + j
                            p.mm(ps[:, j * 128:(j + 1) * 128], kTt[b][:, h, :], qdT[b][:, h, :], True, True, r=[kTt[b], qdT[b]], w=[ps])
                        p.tt("dve", Pm[b][:, hh * 4:hh * 4 + 4, :].rearrange("p h n -> p (h n)"), ps[:],
                             Dt[:, d, hh * 4:hh * 4 + 4, :].rearrange("p h n -> p (h n)"), ALU.mult, r=[ps, Dt], w=[Pm[b]])
                    po = nps()
                    for h in range(8):
                        cs = slice(h * 64, (h + 1) * 64)
                        p.mm(po[:, cs], Pm[b][:, h, :], vt[b][:, cs], True, False, r=[Pm[b], vt[b]], w=[po])
                        p.mm(po[:, cs], qdT[b][:, h, :], Sbf[:, cs], False, True, r=[qdT[b], Sbf], w=[po])
                    psS = nps()
                    for h in range(8):
                        cs = slice(h * 64, (h + 1) * 64)
                        p.mm(psS[0:64, cs], kd[b][:, cs], vt[b][:, cs], True, True, r=[kd[b], vt[b]], w=[psS])
                    p.tt("dve", Sst[:], Sst[:], Gt[:, d, :], ALU.mult, r=[Sst, Gt], w=[Sst])
                    p.tt("dve", Sst[:], Sst[:], psS[0:64, :], ALU.add, r=[Sst, psS], w=[Sst])
                    p.copy("act", Sbf[:], Sst[:], r=[Sst], w=[Sbf])
                    if d == 0:
                        p.copy("act", osb[b][:], po[:], r=[po], w=[osb[b]])
                        p.dma("sp", o_ret[rows, :], osb[b][:], r=[osb[b]], w=[o_ret], merge=True)
                    else:
                        if last and t < 2:
                            continue
                        p.dma("sp", oprev[b][:], o_ret[rows, :], r=[o_ret], w=[oprev[b]])
                        p.dma("sp", gate[b][:], r_g[rows, :], r=[r_g], w=[gate[b]])
                        p.tt("dve", osb[b][:], po[:], oprev[b][:], ALU.add, r=[po, oprev[b]], w=[osb[b]])
                        p.tt("pool", oprev[b][:], osb[b][:], osb[b][:], ALU.mult, r=[osb[b]], w=[oprev[b]])
                        p.op("dve", lambda e: e.reduce_sum(out=ssq[b][:], in_=oprev[b][:].rearrange("p (h e) -> p h e", h=8), axis=AX.X),
                             r=[oprev[b]], w=[ssq[b]])
                        p.act(ssq[b][:], ssq[b][:], AF.Ln, r=[ssq[b]], w=[ssq[b]], bias=EPS, scale=1.0 / 64)
                        p.act(ssq[b][:], ssq[b][:], AF.Exp, r=[ssq[b]], w=[ssq[b]], scale=-0.5)
                        p.tt("dve", v3(osb[b]), v3(osb[b]), ssq[b][:].unsqueeze(2).to_broadcast([128, 8, 64]), ALU.mult, r=[osb[b], ssq[b]], w=[osb[b]])
                        p.tt("pool", gate[b][:], gate[b][:], gnrow[:], ALU.mult, r=[gate[b], gnrow], w=[gate[b]])
                        p.tt("dve", ybf[b][:], osb[b][:], gate[b][:], ALU.mult, r=[osb[b], gate[b]], w=[ybf[b]])
                        ph = npsh()
                        for c in range(4):
                            p.tr(ph[:, c * 128:(c + 1) * 128], ybf[b][:, c * 128:(c + 1) * 128], identb[:], r=[ybf[b], identb], w=[ph])
                        p.copy("act", yT[b][:].rearrange("p c n -> p (c n)"), ph[:, 0:512], r=[ph], w=[yT[b]])
                        p.dma("sp", retnT[:, :, t * 128:(t + 1) * 128].rearrange("c p n -> p c n"), yT[b][:], r=[yT[b]], w=[retnT], merge=True)

        def phase_C():
            gnrow = p.sb([128, 512], F32, "hgnrow")
            p.dma("sp", gnrow[:], hg_gn[l:l + 1, :].partition_broadcast(128), w=[gnrow])
            Sst = p.sb([128, 4, 128], F32, "hS")
            Sbf = p.sb([128, 4, 128], BF16, "hSbf")
            NB = 2
            mk = lambda shape, dt, nm: [p.sb(shape, dt, nm) for _ in range(NB)]
            lft = mk([64, 512], F32, "lft")
            ktm = mk([64, 512], F32, "ktm")
            vt = mk([64, 512], BF16, "hv")
            qTt = mk([128, 4, 64], F32, "qTt")
            kTt = mk([128, 4, 64], F32, "hkT")
            gsb = mk([128, 4, 64], F32, "gsb")
            gref = mk([128, 8], F32, "gref")
            E1 = mk([128, 4, 64], F32, "E1")
            E3 = mk([128, 4, 64], F32, "E3")
            qtl = mk([128, 4, 64], BF16, "qtl")
            ktl = mk([128, 4, 64], BF16, "ktl")
            qg = mk([128, 4, 64], BF16, "qg")
            ER = mk([64, 512], F32, "ER")
            kdec = mk([64, 512], BF16, "kdec")
            Pm = mk([64, 4, 64], BF16, "hP")
            osb = mk([64, 512], F32, "hosb")
            oprev = mk([64, 512], F32, "hoprev")
            gate = mk([64, 512], F32, "hgate")
            ssq = mk([64, 4], F32, "hssq")
            ybf = mk([64, 512], BF16, "hybf")
            yT = mk([128, 4, 64], BF16, "hyT")
            NCH = S // 64
            M = 32
            mint = [p.sb([64, 4, 64], mybir.dt.int32, "mint%d" % i) for i in range(2)]
            zer = p.sb([64, 4, 64], F32, "zer")
            p.op("dve", lambda e: e.memset(zer[:], 0.0), w=[zer])
            for i, cc in enumerate((C_TF, C_TB)):
                p.copy("dve", mint[i][:], cst[0:64, cc:cc + 64].unsqueeze(1).to_broadcast([64, 4, 64]), r=[cst], w=[mint[i]])
            for d in range(2):
                p.op("dve", lambda e: e.memset(Sst[:], 0.0), w=[Sst])
                p.op("dve", lambda e: e.memset(Sbf[:], 0.0), w=[Sbf])
                tri = cst[0:64, C_TF:C_TF + 64] if d == 0 else cst[0:64, C_TB:C_TB + 64]
                rem = cst[0:64, C_RF:C_RF + 64] if d == 0 else cst[0:64, C_RB:C_RB + 64]
                lastcol = 63 if d == 0 else 0
                order = list(range(NCH)) if d == 0 else [3, 2, 1, 0] + list(range(NCH - 1, 3, -1))
                for it, c in enumerate(order):
                    b = it % NB
                    yield
                    rows = slice(c * 64, (c + 1) * 64)
                    p.dma("sp", lft[b][:], h_lf[d][rows, :], r=[h_lf[d]], w=[lft[b]])
                    p.dma("sp", ktm[b][:], h_k[d][rows, :], r=[h_k[d]], w=[ktm[b]])
                    p.dma("sp", vt[b][:], h_v[rows, :], r=[h_v], w=[vt[b]])
                    p.dma("sp", qTt[b][:], h_qT[:, :, c * 64:(c + 1) * 64].rearrange("h p n -> p h n"), r=[h_qT], w=[qTt[b]])
                    p.dma("sp", kTt[b][:], h_kT[d][:, :, c * 64:(c + 1) * 64].rearrange("h p n -> p h n"), r=[h_kT[d]], w=[kTt[b]])
                    pg = nps()
                    for h in range(4):
                        p.mm(pg[:, h * 64:(h + 1) * 64], lft[b][:, h * 128:(h + 1) * 128], tri, True, True, r=[lft[b], cst], w=[pg])
                    pr = nps()
                    for h in range(4):
                        p.mm(pr[0:64, h * 128:(h + 1) * 128], rem, lft[b][:, h * 128:(h + 1) * 128], True, True, r=[lft[b], cst], w=[pr])
                    p.copy("dve", gsb[b][:].rearrange("p h n -> p (h n)"), pg[:, 0:256], r=[pg], w=[gsb[b]])
                    p.copy("dve", gref[b][:, 0:4], gsb[b][:, :, M], r=[gsb[b]], w=[gref[b]])
                    p.ts("dve", gref[b][:, 4:8], gref[b][:, 0:4], -1.0, None, ALU.mult, None, r=[gref[b]], w=[gref[b]])
                    for h in range(4):
                        p.act(E1[b][:, h, :], gsb[b][:, h, :], AF.Exp, r=[gsb[b], gref[b]], w=[E1[b]], bias=gref[b][:, 4 + h:5 + h])
                    p.tt("dve", qtl[b][:], qTt[b][:], E1[b][:], ALU.mult, r=[qTt[b], E1[b]], w=[qtl[b]])
                    for h in range(4):
                        p.act(E1[b][:, h, :], gsb[b][:, h, :], AF.Exp, r=[gsb[b], gref[b], qtl[b]], w=[E1[b]], bias=gref[b][:, h:h + 1], scale=-1.0)
                    p.tt("dve", ktl[b][:], kTt[b][:], E1[b][:], ALU.mult, r=[kTt[b], E1[b]], w=[ktl[b]])
                    p.act(E3[b][:], gsb[b][:], AF.Exp, r=[gsb[b]], w=[E3[b]])
                    p.tt("pool", qg[b][:], qTt[b][:], E3[b][:], ALU.mult, r=[qTt[b], E3[b]], w=[qg[b]])
                    p.act(ER[b][:], pr[0:64, :], AF.Exp, r=[pr], w=[ER[b]])
                    p.tt("pool", kdec[b][:], ktm[b][:], ER[b][:], ALU.mult, r=[ktm[b], ER[b]], w=[kdec[b]])
                    psc = nps()
                    for h in range(4):
                        p.mm(psc[0:64, h * 64:(h + 1) * 64], ktl[b][:, h, :], qtl[b][:, h, :], True, True, r=[ktl[b], qtl[b]], w=[psc])
                    p.op("dve", lambda e: e.select(out=Pm[b][:], mask=mint[d][:], on_true=psc[0:64, 0:256].rearrange("p (h n) -> p h n", h=4),
                                                   on_false=zer[:]), r=[psc, mint[d], zer], w=[Pm[b]])
                    po = nps()
                    for h in range(4):
                        cs = slice(h * 128, (h + 1) * 128)
                        p.mm(po[0:64, cs], Pm[b][:, h, :], vt[b][:, cs], True, False, r=[Pm[b], vt[b]], w=[po])
                        p.mm(po[0:64, cs], qg[b][:, h, :], Sbf[:, h, :], False, True, r=[qg[b], Sbf], w=[po])
                    psS = nps()
                    for h in range(4):
                        cs = slice(h * 128, (h + 1) * 128)
                        p.mm(psS[:, cs], kdec[b][:, cs], vt[b][:, cs], True, True, r=[kdec[b], vt[b]], w=[psS])
                    for h in range(4):
                        p.stt("dve", Sst[:, h, :], Sst[:, h, :], E3[b][:, h, lastcol:lastcol + 1], psS[:, h * 128:(h + 1) * 128], ALU.mult, ALU.add,
                              r=[Sst, E3[b], psS], w=[Sst])
                    p.copy("act", Sbf[:], Sst[:], r=[Sst], w=[Sbf])
                    if d == 0:
                        p.copy("act", osb[b][:], po[0:64, :], r=[po], w=[osb[b]])
                        p.dma("sp", o_hg[rows, :], osb[b][:], r=[osb[b]], w=[o_hg], merge=True)
                    else:
                        if last and c < 4:
                            continue
                        p.dma("sp", oprev[b][:], o_hg[rows, :], r=[o_hg], w=[oprev[b]])
                        p.dma("sp", gate[b][:], h_g[rows, :], r=[h_g], w=[gate[b]])
                        p.tt("dve", osb[b][:], po[0:64, :], oprev[b][:], ALU.add, r=[po, oprev[b]], w=[osb[b]])
                        p.tt("pool", oprev[b][:], osb[b][:], osb[b][:], ALU.mult, r=[osb[b]], w=[oprev[b]])
                        p.op("dve", lambda e: e.reduce_sum(out=ssq[b][:], in_=oprev[b][:].rearrange("p (h e) -> p h e", h=4), axis=AX.X),
                             r=[oprev[b]], w=[ssq[b]])
                        p.act(ssq[b][:], ssq[b][:], AF.Ln, r=[ssq[b]], w=[ssq[b]], bias=EPS, scale=1.0 / 128)
                        p.act(ssq[b][:], ssq[b][:], AF.Exp, r=[ssq[b]], w=[ssq[b]], scale=-0.5)
                        o3 = osb[b][:].rearrange("p (h e) -> p h e", h=4)
                        p.tt("dve", o3, o3, ssq[b][:].unsqueeze(2).to_broadcast([64, 4, 128]), ALU.mult, r=[osb[b], ssq[b]], w=[osb[b]])
                        p.tt("pool", gate[b][:], gate[b][:], gnrow[0:64, :], ALU.mult, r=[gate[b], gnrow], w=[gate[b]])
                        p.tt("dve", ybf[b][:], osb[b][:], gate[b][:], ALU.mult, r=[osb[b], gate[b]], w=[ybf[b]])
                        ph = npsh()
                        for h in range(4):
                            p.tr(ph[:, h * 64:(h + 1) * 64], ybf[b][:, h * 128:(h + 1) * 128], identb[0:64, 0:64], r=[ybf[b], identb], w=[ph])
                        p.copy("act", yT[b][:].rearrange("p c n -> p (c n)"), ph[:, 0:256], r=[ph], w=[yT[b]])
                        p.dma("sp", hgnT[:, :, c * 64:(c + 1) * 64].rearrange("c p n -> p c n"), yT[b][:], r=[yT[b]], w=[hgnT], merge=True)

        with p.scope("BC%d" % l):
            gens = [phase_C(), phase_B()]
            quota = [2, 1]
            while gens:
                for gi in range(len(gens) - 1, -1, -1):
                    pass
                nxt = []
                for g_, q_ in zip(gens, quota):
                    ok = True
                    for _ in range(q_):
                        try:
                            next(g_)
                        except StopIteration:
                            ok = False
                            break
                    if ok:
                        nxt.append((g_, q_))
                gens = [x[0] for x in nxt]
                quota = [x[1] for x in nxt]

        with p.scope("D%d" % l):
            xs = p.sb([128, S], F32, "lx")
            xc = p.sb([128, S], F32, "lxc")
            at = p.sb([128, S], F32, "la")
            bt = p.sb([128, S], F32, "lb")
            hf = p.sb([128, S], F32, "lhf")
            hb = p.sb([128, S], F32, "lhb")
            gl = p.sb([128, S], F32, "lgl")
            ybf = p.sb([128, S], BF16, "lybf")
            wbd = p.sb([128, 128], F32, "wbd")
            wbx = p.sb([128, 128], F32, "wbx")
            cneg = p.sb([128, 8], F32, "cneg")
            p.act(cneg[:], fv[:, FV_LAM:FV_LAM + 8], AF.Exp, r=[fv], w=[cneg], scale=-1.0)
            p.act(cneg[:], cneg[:], AF.Ln, r=[cneg], w=[cneg], bias=1.0)
            p.ts("dve", cneg[:], cneg[:], -8.0, None, ALU.mult, None, r=[cneg], w=[cneg])
            tA = [p.sb([128, 512], F32, "ltA") for _ in range(2)]
            tB = [p.sb([128, 512], F32, "ltB") for _ in range(2)]
            segs = [(0, CTX), (CTX, S)]
            for c in range(4):
                p.dma("sp", xs[:], l_xT[c], r=[l_xT], w=[xs])
                p.dma("sp", gl[:], l_gT[c], r=[l_gT], w=[gl])
                cw = lambda w: fv[:, FV_CW + w * 4 + c:FV_CW + w * 4 + c + 1]
                p.ts("dve", xc[:], xs[:], cw(2), fv[:, FV_CB + c:FV_CB + c + 1], ALU.mult, ALU.add, r=[xs, fv], w=[xc])
                for w in (0, 1, 3):
                    o = w - 2
                    for (a, bnd) in segs:
                        lo, hi = max(a, a - o), min(bnd, bnd - o)
                        p.stt("dve", xc[:, lo:hi], xs[:, lo + o:hi + o], cw(w), xc[:, lo:hi], ALU.mult, ALU.add, r=[xs, fv, xc], w=[xc])
                for d in range(2):
                    p.dma("sp", wbd[:], lru_bd[l, 0, d, c], w=[wbd])
                    p.dma("sp", wbx[:], lru_bd[l, 1, d, c], w=[wbx])
                    for i, (t0, N, isctx) in enumerate(tiles):
                        b = i % 2
                        pa = nps()
                        p.mm(pa[:, :N], wbd[:], xc[:, t0:t0 + N], True, True, r=[wbd, xc], w=[pa])
                        px = nps()
                        p.mm(px[:, :N], wbx[:], xc[:, t0:t0 + N], True, True, r=[wbx, xc], w=[px])
                        p.act(tA[b][:, :N], pa[:, :N], AF.Sigmoid, r=[pa, fv], w=[tA[b]], bias=fv[:, FV_BA + d * 4 + c:FV_BA + d * 4 + c + 1])
                        p.act(tB[b][:, :N], px[:, :N], AF.Sigmoid, r=[px, fv], w=[tB[b]], bias=fv[:, FV_BX + d * 4 + c:FV_BX + d * 4 + c + 1])
                        p.act(at[:, t0:t0 + N], tA[b][:, :N], AF.Exp, r=[tA[b], cneg], w=[at], scale=cneg[:, d * 4 + c:d * 4 + c + 1])
                        p.tt("dve", tB[b][:, :N], tB[b][:, :N], xc[:, t0:t0 + N], ALU.mult, r=[tB[b], xc], w=[tB[b]])
                        p.tt("dve", tA[b][:, :N], at[:, t0:t0 + N], at[:, t0:t0 + N], ALU.mult, r=[at], w=[tA[b]])
                        p.ts("dve", tA[b][:, :N], tA[b][:, :N], -1.0, 1.0, ALU.mult, ALU.add, r=[tA[b]], w=[tA[b]])
                        p.act(tA[b][:, :N], tA[b][:, :N], AF.Ln, r=[tA[b]], w=[tA[b]])
                        p.act(tA[b][:, :N], tA[b][:, :N], AF.Exp, r=[tA[b]], w=[tA[b]], scale=0.5)
                        p.tt("dve", bt[:, t0:t0 + N], tA[b][:, :N], tB[b][:, :N], ALU.mult, r=[tA[b], tB[b]], w=[bt])
                    PC = 256
                    if d == 0:
                        for s0 in range(0, S, PC):
                            init = 0.0 if s0 == 0 else hf[:, s0 - 1:s0]
                            p.op("dve", lambda e: e.tensor_tensor_scan(out=hf[:, s0:s0 + PC], data0=at[:, s0:s0 + PC], data1=bt[:, s0:s0 + PC],
                                                                       initial=init, op0=ALU.mult, op1=ALU.add), r=[at, bt, hf], w=[hf])
                    else:
                        pieces = [(0, CTX, None)] + [(s0, s0 + PC, None) for s0 in range(S - PC, CTX - 1, -PC)]
                        prev_first = None
                        for (a0, a1, _) in pieces:
                            init = 0.0 if prev_first is None else hb[:, prev_first:prev_first + 1]
                            rv = lambda tl: tl[:, a0:a1][:, ::-1]
                            p.op("dve", lambda e: e.tensor_tensor_scan(out=rv(hb), data0=rv(at), data1=rv(bt),
                                                                       initial=init, op0=ALU.mult, op1=ALU.add), r=[at, bt, hb], w=[hb])
                            prev_first = a0
                p.tt("dve", hf[:], hf[:], hb[:], ALU.add, r=[hf, hb], w=[hf])
                p.tt("dve", ybf[:], hf[:], gl[:], ALU.mult, r=[hf, gl], w=[ybf])
                p.dma("sp", lruT[c], ybf[:], r=[ybf], w=[lruT], merge=True)

        with p.scope("E%d" % l):
            wro = p.sb([128, 4, D], BF16, "wro")
            who = p.sb([128, 4, D], BF16, "who")
            wlo = p.sb([128, 4, D], BF16, "wlo")
            wo = p.sb([128, KC, D], BF16, "wo")
            wr = p.sb([128, KC, NEXP], F32, "wr")
            rbrow = p.sb([128, NEXP], F32, "rbrow")
            p.dma("pool", wro[:], w_ret_o[l].rearrange("(k p) n -> p k n", p=128), w=[wro])
            p.dma("pool", who[:], w_hg_o[l].rearrange("(k p) n -> p k n", p=128), w=[who])
            p.dma("pool", wlo[:], w_lru_o[l].rearrange("(k p) n -> p k n", p=128), w=[wlo])
            p.dma("pool", wo[:], w_out[l].rearrange("(k p) n -> p k n", p=128), w=[wo])
            p.dma("sp", wr[:], router_w[l].rearrange("(k p) n -> p k n", p=128), w=[wr])
            p.dma("sp", rbrow[:], router_b[l:l + 1, :].partition_broadcast(128), w=[rbrow])
            bt3 = [p.sb([128, 4, 512], BF16, "br%d" % i) for i in range(3)]
            gts = [[p.sb([128, 512], F32, "gt%d" % i) for i in range(3)] for _ in range(2)]
            mg = p.sb([128, KC, 512], BF16, "mg")
            t1 = [p.sb([128, 512], F32, "et1") for _ in range(2)]
            t2 = [p.sb([128, 512], F32, "et2") for _ in range(2)]
            xo = p.sb([128, KC, 512], F32, "exo")
            xn = p.sb([128, KC, 512], F32, "exn")
            sq = p.sb([128, KC, 512], F32, "esq")
            rstd = p.sb([128, 512], F32, "erstd")
            h2b = p.sb([128, KC, 512], BF16, "h2b")
            lgt = p.sb([128, NEXP], F32, "lgt")
            top8 = p.sb([128, 8], F32, "top8")
            wrow = p.sb([128, NEXP], F32, "wrow")
            ssum = p.sb([128, 2], F32, "ssum")
            for (t0, N, isctx) in tiles:
                if last and isctx:
                    continue
                col = 0 if isctx else 1
                for i, src in enumerate((retnT, hgnT, lruT)):
                    p.dma("sp", bt3[i][:, :, :N], src[:, :, t0:t0 + N].rearrange("c p n -> p c n"), r=[src], w=[bt3[i]])
                p.dma("sp", xo[:, :, :N], xT[:, :, t0:t0 + N].rearrange("k p n -> p k n"), r=[xT], w=[xo])
                for m in range(KC):
                    b = m % 2
                    for i in range(3):
                        p.dma("sp", gts[b][i][:, :N], g_T[i, m, :, t0:t0 + N], r=[g_T], w=[gts[b][i]])
                    pss = []
                    for i, wgt in enumerate((wro, who, wlo)):
                        ps = nps()
                        for kc in range(4):
                            p.mm(ps[:, :N], wgt[:, kc, m * 128:(m + 1) * 128], bt3[i][:, kc, :N], kc == 0, kc == 3, r=[wgt, bt3[i]], w=[ps])
                        pss.append(ps)
                    p.tt("dve", t1[b][:, :N], pss[0][:, :N], gts[b][0][:, :N], ALU.mult, r=[pss[0], gts[b][0]], w=[t1[b]])
                    p.tt("dve", t2[b][:, :N], pss[1][:, :N], gts[b][1][:, :N], ALU.mult, r=[pss[1], gts[b][1]], w=[t2[b]])
                    p.tt("pool", t1[b][:, :N], t1[b][:, :N], t2[b][:, :N], ALU.add, r=[t1[b], t2[b]], w=[t1[b]])
                    p.tt("dve", t2[b][:, :N], pss[2][:, :N], gts[b][2][:, :N], ALU.mult, r=[pss[2], gts[b][2]], w=[t2[b]])
                    p.tt("pool", mg[:, m, :N], t1[b][:, :N], t2[b][:, :N], ALU.add, r=[t1[b], t2[b]], w=[mg])
                for m in range(KC):
                    ps = nps()
                    for kc in range(KC):
                        p.mm(ps[:, :N], wo[:, kc, m * 128:(m + 1) * 128], mg[:, kc, :N], kc == 0, kc == KC - 1, r=[wo, mg], w=[ps])
                    p.stt("dve", xn[:, m, :N], ps[:, :N], modv[:, 2, m, col:col + 1], xo[:, m, :N], ALU.mult, ALU.add, r=[ps, modv, xo], w=[xn])
                p.dma("sp", xT[:, :, t0:t0 + N].rearrange("k p n -> p k n"), xn[:, :, :N], r=[xn], w=[xT], merge=True)
                p.act(sq[:, :, :N], xn[:, :, :N], AF.Square, r=[xn], w=[sq])
                ps = nps()
                for kc in range(KC):
                    p.mm(ps[:, :N], ones, sq[:, kc, :N], kc == 0, kc == KC - 1, r=[cst, sq], w=[ps])
                p.act(rstd[:, :N], ps[:, :N], AF.Ln, r=[ps], w=[rstd], bias=EPS, scale=1.0 / D)
                p.act(rstd[:, :N], rstd[:, :N], AF.Exp, r=[rstd], w=[rstd], scale=-0.5)
                for kc in range(KC):
                    p.stt("dve", sq[:, kc, :N], xn[:, kc, :N], modv[:, 3, kc, col:col + 1], rstd[:, :N], ALU.mult, ALU.mult, r=[xn, modv, rstd], w=[sq])
                    p.act(sq[:, kc, :N], sq[:, kc, :N], AF.Identity, r=[sq, modv], w=[sq], bias=modv[:, 4, kc, col:col + 1])
                p.copy("pool", h2b[:, :, :N], sq[:, :, :N], r=[sq], w=[h2b])
                p.dma("sp", h2T_d[:, :, t0:t0 + N].rearrange("k p n -> p k n"), h2b[:, :, :N], r=[h2b], w=[h2T_d], merge=True)
                for s in range(N // 128):
                    ps = nps()
                    for kc in range(KC):
                        p.mm(ps[:, 0:NEXP], sq[:, kc, s * 128:(s + 1) * 128], wr[:, kc, :], kc == 0, kc == KC - 1, r=[sq, wr], w=[ps])
                    p.tt("dve", lgt[:], ps[:, 0:NEXP], rbrow[:], ALU.add, r=[ps, rbrow], w=[lgt])
                    p.op("dve", lambda e: e.max(out=top8[:], in_=lgt[:]), r=[lgt], w=[top8])
                    p.ts("dve", wrow[:], lgt[:], top8[:, 3:4], None, ALU.is_ge, None, r=[lgt, top8], w=[wrow])
                    p.ts("dve", ssum[:, 0:1], top8[:, 0:1], -1.0, None, ALU.mult, None, r=[top8], w=[ssum])
                    p.act(lgt[:], lgt[:], AF.Exp, r=[lgt, ssum], w=[lgt], bias=ssum[:, 0:1])
                    p.tt("dve", wrow[:], wrow[:], lgt[:], ALU.mult, r=[wrow, lgt], w=[wrow])
                    p.op("dve", lambda e: e.reduce_sum(out=ssum[:, 1:2], in_=wrow[:], axis=AX.X), r=[wrow], w=[ssum])
                    p.op("dve", lambda e: e.reciprocal(out=ssum[:, 1:2], in_=ssum[:, 1:2]), r=[ssum], w=[ssum])
                    p.ts("dve", wrow[:], wrow[:], ssum[:, 1:2], None, ALU.mult, None, r=[wrow, ssum], w=[wrow])
                    p.dma("sp", wmoe_d[t0 + s * 128:t0 + (s + 1) * 128, :], wrow[:], r=[wrow], w=[wmoe_d], merge=True)

        with p.scope("F%d" % l):
            mtiles = [tl for tl in tiles if not (last and tl[2])]
            groups = [mtiles[i:i + 2] for i in range(0, len(mtiles), 2)]
            wgu = [p.sb([128, KC, 2 * D], BF16, "wgu%d" % i) for i in range(2)]
            wdn = [p.sb([128, KC, D], BF16, "wdn%d" % i) for i in range(2)]
            h2 = p.sb([128, KC, 1024], BF16, "mh2")
            accs = [p.sb([128, D], F32, "acc%d" % i) for i in range(8)]
            wsb = [p.sb([128, NEXP], F32, "mw%d" % i) for i in range(8)]
            aT = [p.sb([128, KC, 512], BF16, "aT%d" % i) for i in range(2)]
            g1 = [p.sb([128, 512], F32, "mg%d" % i) for i in range(3)]
            s1 = [p.sb([128, 512], F32, "ms%d" % i) for i in range(3)]
            u1 = [p.sb([128, 512], F32, "mu%d" % i) for i in range(3)]
            bdn = p.sb([NEXP, D], F32, "bdn")
            bu1 = p.sb([128, NEXP, 8], F32, "bu1")
            p.ts("dve", bu1[:], fv[:, FV_BGU:FV_BGU + 512].rearrange("p (e c) -> p e c", e=NEXP)[:, :, 8:16], 1.0, None, ALU.add, None, r=[fv], w=[bu1])
            wT = p.sb([NEXP, 128], F32, "wT")
            xo = p.sb([128, 512], F32, "fxo")
            xn2 = p.sb([128, 512], F32, "fxn")
            p.dma("sp", bdn[:], b_dn[l], w=[bdn])
            ei = 0
            for grp in groups:
                gt0 = grp[0][0]
                gN = sum(tl[1] for tl in grp)
                nsub = gN // 128
                p.dma("sp", h2[:, :, :gN], h2T_d[:, :, gt0:gt0 + gN].rearrange("k p n -> p k n"), r=[h2T_d], w=[h2])
                for s in range(nsub):
                    p.dma("sp", wsb[s][:], wmoe_d[gt0 + s * 128:gt0 + (s + 1) * 128, :], r=[wmoe_d], w=[wsb[s]])
                for e in range(NEXP):
                    wb = ei % 2
                    ei += 1
                    p.dma("sp", wgu[wb][:], wgu_bf[l % 2][e].h.rearrange("(k p) n -> p k n", p=128), r=[wgu_bf[l % 2][e]], w=[wgu[wb]])
                    p.dma("sp", wdn[wb][:], wdn_bf[l % 2][e].h.rearrange("(k p) n -> p k n", p=128), r=[wdn_bf[l % 2][e]], w=[wdn[wb]])
                    if (not last) and (e % len(groups)) == groups.index(grp):
                        convert_expert(l + 1, e)
                    off = 0
                    for ti, (t0, N, isctx) in enumerate(grp):
                        a = aT[ti % 2]
                        for m in range(KC):
                            b = m % 3
                            pg = nps()
                            for kc in range(KC):
                                p.mm(pg[:, :N], wgu[wb][:, kc, m * 128:(m + 1) * 128], h2[:, kc, off:off + N], kc == 0, kc == KC - 1, r=[wgu[wb], h2], w=[pg])
                            pu = nps()
                            for kc in range(KC):
                                p.mm(pu[:, :N], wgu[wb][:, kc, D + m * 128:D + (m + 1) * 128], h2[:, kc, off:off + N], kc == 0, kc == KC - 1, r=[wgu[wb], h2], w=[pu])
                            bg = fv[:, FV_BGU + e * 16 + m:FV_BGU + e * 16 + m + 1]
                            bu = fv[:, FV_BGU + e * 16 + 8 + m:FV_BGU + e * 16 + 8 + m + 1]
                            p.ts("dve", g1[b][:, :N], pg[:, :N], bg, 7.0, ALU.add, ALU.min, r=[pg, fv], w=[g1[b]])
                            p.act(s1[b][:, :N], g1[b][:, :N], AF.Gelu_apprx_sigmoid, r=[g1[b]], w=[s1[b]])
                            p.ts("dve", u1[b][:, :N], pu[:, :N], bu1[:, e, m:m + 1], 8.0, ALU.add, ALU.min, r=[pu, bu1], w=[u1[b]])
                            p.stt("dve", a[:, m, :N], u1[b][:, :N], -6.0, s1[b][:, :N], ALU.max, ALU.mult, r=[u1[b], s1[b]], w=[a])
                        for s in range(N // 128):
                            si = off // 128 + s
                            for hf_ in range(2):
                                pd = nps()
                                for kc in range(KC):
                                    p.mm(pd[:], a[:, kc, s * 128:(s + 1) * 128], wdn[wb][:, kc, hf_ * 512:(hf_ + 1) * 512], kc == 0, kc == KC - 1, r=[a, wdn[wb]], w=[pd])
                                cs = slice(hf_ * 512, (hf_ + 1) * 512)
                                if e == 0:
                                    p.ts("dve", accs[si][:, cs], pd[:], wsb[si][:, e:e + 1], None, ALU.mult, None, r=[pd, wsb[si]], w=[accs[si]])
                                else:
                                    p.stt("dve", accs[si][:, cs], pd[:], wsb[si][:, e:e + 1], accs[si][:, cs], ALU.mult, ALU.add, r=[pd, wsb[si], accs[si]], w=[accs[si]])
                        off += N
                off = 0
                for (t0, N, isctx) in grp:
                    col = 0 if isctx else 1
                    for s in range(N // 128):
                        si = off // 128 + s
                        pt = nps()
                        p.tr(pt[0:NEXP, 0:128], wsb[si][:], ident, r=[wsb[si], cst], w=[pt])
                        p.copy("dve", wT[:], pt[0:NEXP, 0:128], r=[pt], w=[wT])
                        for hf_ in range(2):
                            pb = nps()
                            p.mm(pb[:], wT[:], bdn[:, hf_ * 512:(hf_ + 1) * 512], True, True, r=[wT, bdn], w=[pb])
                            cs = slice(hf_ * 512, (hf_ + 1) * 512)
                            p.tt("dve", accs[si][:, cs], accs[si][:, cs], pb[:], ALU.add, r=[accs[si], pb], w=[accs[si]])
                    for m in range(KC):
                        p.dma("sp", xo[:, :N], xT[m, :, t0:t0 + N], r=[xT], w=[xo])
                        pt = nps()
                        for s in range(N // 128):
                            si = off // 128 + s
                            p.tr(pt[:, s * 128:(s + 1) * 128], accs[si][:, m * 128:(m + 1) * 128], ident, r=[accs[si], cst], w=[pt])
                        p.stt("dve", xn2[:, :N], pt[:, :N], modv[:, 5, m, col:col + 1], xo[:, :N], ALU.mult, ALU.add, r=[pt, modv, xo], w=[xn2])
                        p.dma("sp", xT[m, :, t0:t0 + N], xn2[:, :N], r=[xn2], w=[xT], merge=True)
                    off += N

    with p.scope():
        xt = p.sb([128, KC, 512], F32, "fx")
        sq = p.sb([128, KC, 512], F32, "fsq")
        rstd = p.sb([128, 512], F32, "frstd")
        ot = [p.sb([128, D], F32, "fot%d" % i) for i in range(2)]
        p.dma("sp", fv[:], fv_d[0], w=[fv])
        for (t0, N, isctx) in tiles:
            if isctx:
                continue
            p.dma("sp", xt[:, :, :N], xT[:, :, t0:t0 + N].rearrange("k p n -> p k n"), r=[xT], w=[xt])
            p.act(sq[:, :, :N], xt[:, :, :N], AF.Square, r=[xt], w=[sq])
            ps = nps()
            for kc in range(KC):
                p.mm(ps[:, :N], ones, sq[:, kc, :N], kc == 0, kc == KC - 1, r=[cst, sq], w=[ps])
            p.act(rstd[:, :N], ps[:, :N], AF.Ln, r=[ps], w=[rstd], bias=EPS, scale=1.0 / D)
            p.act(rstd[:, :N], rstd[:, :N], AF.Exp, r=[rstd], w=[rstd], scale=-0.5)
            for kc in range(KC):
                p.stt("dve", sq[:, kc, :N], xt[:, kc, :N], fv[:, FV_FG + kc:FV_FG + kc + 1], rstd[:, :N], ALU.mult, ALU.mult, r=[xt, fv, rstd], w=[sq])
            for s in range(N // 128):
                o = ot[s % 2]
                for half in range(2):
                    ps = nps()
                    for j in range(4):
                        kc = half * 4 + j
                        p.tr(ps[:, j * 128:(j + 1) * 128], sq[:, kc, s * 128:(s + 1) * 128], ident, r=[sq, cst], w=[ps])
                    p.copy("dve" if half == 0 else "act", o[:, half * 512:(half + 1) * 512], ps[:], r=[ps], w=[o])
                r0 = t0 - CTX + s * 128
                p.dma("sp", out_d[r0:r0 + 128, :], o[:], r=[o], w=[out_d], merge=True)
    p.finish()
    return nc, p


def prep_shared(inputs, depth, lat):
    f = lambda a: np.ascontiguousarray(np.asarray(a, dtype=np.float32))
    fm = lambda v, n: np.asarray(v, np.float32).reshape(n, 128).T
    fv = np.zeros((depth, 128, NFV), np.float32)
    for l in range(depth):
        fv[l, :, FV_N1:FV_N1 + 8] = fm(inputs["norm1_g"][l], 8)
        fv[l, :, FV_N2:FV_N2 + 8] = fm(inputs["norm2_g"][l], 8)
        fv[l, :, FV_MODB:FV_MODB + 48] = fm(inputs["mod_b"][l], 48)
        cw = np.asarray(inputs["lru_conv_w"][l], np.float32)
        for w in range(4):
            fv[l, :, FV_CW + w * 4:FV_CW + w * 4 + 4] = fm(cw[w], 4)
        fv[l, :, FV_CB:FV_CB + 4] = fm(inputs["lru_conv_b"][l], 4)
        for d in range(2):
            fv[l, :, FV_BA + d * 4:FV_BA + d * 4 + 4] = fm(inputs["lru_ba"][l][d], 4)
            fv[l, :, FV_BX + d * 4:FV_BX + d * 4 + 4] = fm(inputs["lru_bx"][l][d], 4)
            fv[l, :, FV_LAM + d * 4:FV_LAM + d * 4 + 4] = fm(inputs["lru_lambda"][l][d], 4)
        fv[l, :, FV_FG:FV_FG + 8] = fm(inputs["final_g"], 8)
        bgu = np.asarray(inputs["exp_b_gu"][l], np.float32)
        fv[l, :, FV_BGU:FV_BGU + 512] = bgu.reshape(NEXP, 16, 128).transpose(2, 0, 1).reshape(128, 512)
    lbl = np.asarray(inputs["hgrn_lb_logits"], np.float32)
    if lbl.shape[0] < 4:
        lbl = np.concatenate([lbl, np.full((4 - lbl.shape[0], 2, 512), -100.0, np.float32)], axis=0)
    lblT = lbl.reshape(lbl.shape[0], 2, 4, 128).transpose(3, 0, 1, 2).reshape(128, lbl.shape[0] * 8)
    lblT4 = np.zeros((128, 32), np.float32)
    lblT4[:, :lblT.shape[1]] = lblT
    bd = np.zeros((depth, 2, 2, 4, 128, 128), np.float32)
    for l in range(depth):
        for ai, nm in enumerate(("lru_wa", "lru_wx")):
            wsrc = np.asarray(inputs[nm][l], np.float32)
            for d in range(2):
                for c in range(4):
                    for j in range(2):
                        bd[l, ai, d, c, j * 64:(j + 1) * 64, j * 64:(j + 1) * 64] = wsrc[d, c * 2 + j]
    sh = {
        "consts": make_consts(), "rot": make_rot(lat), "fv": fv, "lblT": lblT4,
        "hgrn_lb_logits": f(lbl).reshape(1, -1),
        "mod_w": f(inputs["mod_w"][:depth]), "w_in": f(inputs["w_in"][:depth]),
        "ret_decay": f(inputs["ret_decay"][:depth]).reshape(depth, 16),
        "ret_gn_g": f(inputs["ret_gn_g"][:depth]), "hgrn_gn_g": f(inputs["hgrn_gn_g"][:depth]),
        "w_ret_o": f(inputs["w_ret_o"][:depth]), "w_hgrn_o": f(inputs["w_hgrn_o"][:depth]), "w_lru_o": f(inputs["w_lru_o"][:depth]),
        "w_out": f(inputs["w_out"][:depth]), "lru_bd": bd,
        "router_w": f(inputs["router_w"][:depth]), "router_b": f(inputs["router_b"][:depth]),
        "exp_w_gu": f(inputs["exp_w_gu"][:depth]), "exp_w_down": f(inputs["exp_w_down"][:depth]), "exp_b_down": f(inputs["exp_b_down"][:depth]),
    }
    return sh


def core_inputs(inputs, b, sh):
    x = np.asarray(inputs["x"][b], np.float32)
    ctx = np.asarray(inputs["ctx"][b], np.float32)
    xin = np.ascontiguousarray(np.concatenate([ctx, x], axis=0))
    sc = np.stack([np.asarray(inputs["c_ctx"], np.float32), np.asarray(inputs["c"][b], np.float32)], axis=-1)
    scin = np.ascontiguousarray(sc.reshape(KC, 128, 2).transpose(1, 0, 2).reshape(128, KC * 2))
    m = dict(sh)
    m["xin"] = xin
    m["scin"] = scin
    return m


_CACHE = {}


def kernel(**inputs):
    B, L, _ = inputs["x"].shape
    depth = inputs["w_in"].shape[0]
    key = (L, depth)
    if key not in _CACHE:
        _CACHE[key] = build(L, depth)[0]
    nc = _CACHE[key]
    sh = prep_shared(inputs, depth, L)
    n = 8
    in_maps = [core_inputs(inputs, c % B, sh) for c in range(n)]
    res = run_bass_kernel_spmd(nc, in_maps, core_ids=list(range(n)))
    out = np.stack([np.asarray(res.results[b]["out"], np.float32) for b in range(B)], axis=0)
    return out
```

```python
import contextlib
import numpy as np
import concourse.bass as bass
import concourse.mybir as mybir
from concourse.bass_utils import run_bass_kernel_spmd

F32 = mybir.dt.float32
BF16 = mybir.dt.bfloat16
ALU = mybir.AluOpType
AF = mybir.ActivationFunctionType
AX = mybir.AxisListType

D = 1024
KC = 8
CTX = 256
NEXP = 32
EPS = 1e-6
IN_COLS = 8704


class T:
    __slots__ = ("h", "w", "r", "name")

    def __init__(self, h, name):
        self.h = h
        self.name = name
        self.w = {}
        self.r = {}

    def __getitem__(self, k):
        return self.h[k]


class Prog:
    NDMA = 8

    def __init__(self, nc, same_engine_sync=True):
        self.nc = nc
        self.stack = [contextlib.ExitStack()]
        self.eng = {"pe": nc.tensor, "act": nc.scalar, "dve": nc.vector, "pool": nc.gpsimd, "sp": nc.sync}
        self.semh = {}
        self.cnt = {}
        self.known = {k: {} for k in self.eng}
        for k in self.eng:
            self.semh[k] = self.stack[0].enter_context(nc.semaphore("s_" + k))
            self.cnt[k] = 0
        self.dq = {}
        for q in ("sp", "pool", "act"):
            sems = []
            for i in range(self.NDMA):
                key = "d_%s%d" % (q, i)
                self.semh[key] = self.stack[0].enter_context(nc.semaphore(key))
                self.cnt[key] = 0
                sems.append(key)
            self.dq[q] = [sems, 0]
        self.same = same_engine_sync
        self.uid = 0
        self.ninst = 0

    @contextlib.contextmanager
    def scope(self, name=None):
        es = contextlib.ExitStack()
        self.stack.append(es)
        if name is not None:
            es.enter_context(self.nc.named_scope(name))
        try:
            yield
        finally:
            self.barrier()
            self.stack.pop()
            es.close()

    def sb(self, shape, dt=F32, name=None):
        self.uid += 1
        name = (name or "t") + "_%d" % self.uid
        h = self.stack[-1].enter_context(self.nc.sbuf_tensor(name, list(shape), dt))
        return T(h, name)

    def ps(self, shape, dt=F32, name=None):
        self.uid += 1
        name = (name or "p") + "_%d" % self.uid
        h = self.stack[-1].enter_context(self.nc.psum_tensor(name, list(shape), dt))
        return T(h, name)

    def dram(self, name, shape, dt, kind="Internal"):
        h = self.nc.dram_tensor(name, list(shape), dt, kind=kind)
        return T(h.ap(), name)

    def _wait(self, ek, need):
        e = self.eng[ek]
        kn = self.known[ek]
        for s, v in need.items():
            if s == ek and (not self.same or ek == "pe"):
                continue
            if kn.get(s, 0) < v:
                e.wait_ge(self.semh[s], v)
                kn[s] = v

    @staticmethod
    def _merge(d, s):
        for k, v in s.items():
            if d.get(k, 0) < v:
                d[k] = v

    def _need(self, r, w, skip_waw=False):
        need = {}
        for t in r:
            self._merge(need, t.w)
        for t in w:
            if not skip_waw:
                self._merge(need, t.w)
            self._merge(need, t.r)
        return need

    def _record(self, tok, r, w, merge=False):
        for t in w:
            if merge:
                self._merge(t.w, tok)
            else:
                t.w = dict(tok)
                t.r = {}
        for t in r:
            if t not in w:
                self._merge(t.r, tok)

    def op(self, ek, fn, r=(), w=()):
        self._wait(ek, self._need(r, w))
        inst = fn(self.eng[ek])
        self.cnt[ek] += 1
        inst.then_inc(self.semh[ek], 1)
        self.ninst += 1
        self._record({ek: self.cnt[ek]}, r, w)
        return inst

    def dma(self, q, out_ap, in_ap, r=(), w=(), merge=False):
        sems, i = self.dq[q]
        key = sems[i % len(sems)]
        self.dq[q][1] = i + 1
        need = self._need(r, w, skip_waw=merge)
        if self.cnt[key] > 0:
            need[key] = max(need.get(key, 0), self.cnt[key])
        self._wait(q, need)
        inst = self.eng[q].dma_start(out=out_ap, in_=in_ap)
        self.cnt[key] += 16
        inst.then_inc(self.semh[key], 16)
        self.ninst += 1
        self._record({key: self.cnt[key]}, r, w, merge=merge)
        return inst

    def barrier(self):
        allv = {k: v for k, v in self.cnt.items() if v > 0}
        for ek in self.eng:
            self._wait(ek, dict(allv))

    def finish(self):
        self.barrier()
        while self.stack:
            self.stack.pop().close()

    def tt(self, ek, out, in0, in1, op, r, w):
        return self.op(ek, lambda e: e.tensor_tensor(out=out, in0=in0, in1=in1, op=op), r=r, w=w)

    def ts(self, ek, out, in0, s1, s2, op0, op1, r, w):
        if s2 is None:
            return self.op(ek, lambda e: e.tensor_scalar(out=out, in0=in0, scalar1=s1, scalar2=None, op0=op0), r=r, w=w)
        return self.op(ek, lambda e: e.tensor_scalar(out=out, in0=in0, scalar1=s1, scalar2=s2, op0=op0, op1=op1), r=r, w=w)

    def stt(self, ek, out, in0, scalar, in1, op0, op1, r, w):
        return self.op(ek, lambda e: e.scalar_tensor_tensor(out=out, in0=in0, scalar=scalar, in1=in1, op0=op0, op1=op1), r=r, w=w)

    def act(self, out, in_, func, r, w, bias=None, scale=None):
        kw = {}
        if bias is not None:
            kw["bias"] = bias
        if scale is not None:
            kw["scale"] = scale
        return self.op("act", lambda e: e.activation(out=out, in_=in_, func=func, **kw), r=r, w=w)

    def copy(self, ek, out, in_, r, w):
        if ek == "act":
            return self.op("act", lambda e: e.copy(out=out, in_=in_), r=r, w=w)
        return self.op(ek, lambda e: e.tensor_copy(out=out, in_=in_), r=r, w=w)

    def mm(self, out, lhsT, rhs, start, stop, r, w):
        return self.op("pe", lambda e: e.matmul(out, lhsT=lhsT, rhs=rhs, start=start, stop=stop), r=r, w=w)

    def tr(self, out, in_, ident, r, w):
        return self.op("pe", lambda e: e.transpose(out=out, in_=in_, identity=ident), r=r, w=w)


C_ID = 0
C_MF = 128
C_MB = 256
C_TF = 384
C_TB = 448
C_RF = 512
C_RB = 576
C_IDX = 640
C_ONE = 646
NCONST = 774

FV_N1 = 0
FV_N2 = 8
FV_MODB = 16
FV_CW = 64
FV_CB = 80
FV_BA = 84
FV_BX = 92
FV_LAM = 100
FV_FG = 108
FV_BGU = 116
NFV = 628

G_RQ, G_RK, G_RV, G_RG, G_HQ, G_HFF, G_HFB, G_HI, G_HGT, G_LX, G_LGT = [512 * i for i in range(11)]
G_GR = 5632
G_GH = 6656
G_GL = 7680


def make_consts():
    c = np.zeros((128, NCONST), np.float32)
    p = np.arange(128)
    c[:, C_ID:C_ID + 128] = np.eye(128)
    c[:, C_MF:C_MF + 128] = (p[None, :] >= p[:, None])
    c[:, C_MB:C_MB + 128] = (p[:, None] > p[None, :])
    q = np.arange(64)
    c[:64, C_TF:C_TF + 64] = (q[:, None] <= q[None, :])
    c[:64, C_TB:C_TB + 64] = (q[:, None] >= q[None, :])
    c[:64, C_RF:C_RF + 64] = (q[:, None] > q[None, :])
    c[:64, C_RB:C_RB + 64] = (q[:, None] < q[None, :])
    c[:, C_IDX + 0] = p + 1
    c[:, C_IDX + 1] = 127 - p
    c[:, C_IDX + 2] = 128 - p
    c[:, C_IDX + 3] = p
    c[:, C_IDX + 4] = -(p + 1)
    c[:, C_IDX + 5] = p - 128
    c[:, C_ONE:C_ONE + 128] = 1.0
    return c


def make_rot(lat):
    n = np.arange(lat)
    row = (n // 64).astype(np.float32)
    col = (n % 64).astype(np.float32)
    inv = (10000.0 ** (-np.arange(16, dtype=np.float32) / 16)).astype(np.float32)
    ang = np.concatenate([row[:, None] * inv, col[:, None] * inv], axis=-1).astype(np.float32)
    cs, sn = np.cos(ang), np.sin(ang)
    return np.concatenate([cs, sn, 0.125 * cs, 0.125 * sn], axis=-1).astype(np.float32)


def build(lat, depth, dbg=()):
    S = CTX + lat
    nc = bass.Bass("TRN2", target_bir_lowering=False)
    p = Prog(nc)
    tiles = [(0, 256, True)] + [(CTX + 512 * i, 512, False) for i in range(lat // 512)]
    NT128 = S // 128

    def din(name, shape):
        return p.dram(name, shape, F32, kind="ExternalInput")

    xin = din("xin", [S, D])
    scin = din("scin", [128, KC * 2])
    consts_d = din("consts", [128, NCONST])
    rot_d = din("rot", [lat, 128])
    fv_d = din("fv", [depth, 128, NFV])
    lblT_d = din("lblT", [128, 4 * 8])
    lbl_d = din("hgrn_lb_logits", [1, 4 * 2 * 512])
    mod_w = din("mod_w", [depth, D, 6 * D])
    w_in = din("w_in", [depth, D, IN_COLS])
    ret_decay = din("ret_decay", [depth, 16])
    ret_gn = din("ret_gn_g", [depth, 512])
    hg_gn = din("hgrn_gn_g", [depth, 512])
    w_ret_o = din("w_ret_o", [depth, 512, D])
    w_hg_o = din("w_hgrn_o", [depth, 512, D])
    w_lru_o = din("w_lru_o", [depth, 512, D])
    w_out = din("w_out", [depth, D, D])
    lru_bd = din("lru_bd", [depth, 2, 2, 4, 128, 128])
    router_w = din("router_w", [depth, D, NEXP])
    router_b = din("router_b", [depth, NEXP])
    w_gu = din("exp_w_gu", [depth, NEXP, D, 2 * D])
    w_dn = din("exp_w_down", [depth, NEXP, D, D])
    b_dn = din("exp_b_down", [depth, NEXP, D])
    out_d = p.dram("out", [lat, D], F32, kind="ExternalOutput")

    def scr(name, shape, dt=F32):
        kind = "ExternalOutput" if name in dbg else "Internal"
        return p.dram(name, shape, dt, kind=kind)

    xT = scr("xT", [KC, 128, S])
    r_q = scr("r_q", [S, 512], BF16)
    r_k = scr("r_k", [S, 512], BF16)
    r_v = scr("r_v", [S, 512], BF16)
    r_g = scr("r_g", [S, 512])
    h_qT = scr("h_qT", [4, 128, S])
    h_kT = [scr("h_kfT", [4, 128, S]), scr("h_kbT", [4, 128, S])]
    h_k = [scr("h_kf", [S, 512]), scr("h_kb", [S, 512])]
    h_lf = [scr("h_lff", [S, 512]), scr("h_lfb", [S, 512])]
    h_v = scr("h_v", [S, 512], BF16)
    h_g = scr("h_g", [S, 512])
    l_xT = scr("l_xT", [4, 128, S])
    l_gT = scr("l_gT", [4, 128, S])
    g_T = scr("g_T", [3, KC, 128, S])
    o_ret = scr("o_ret", [S, 512])
    o_hg = scr("o_hg", [S, 512])
    retnT = scr("retnT", [4, 128, S], BF16)
    hgnT = scr("hgnT", [4, 128, S], BF16)
    lruT = scr("lruT", [4, 128, S], BF16)
    h2T_d = scr("h2T", [KC, 128, S], BF16)
    lbrow_d = scr("lbrow", [4, 128, 2048])
    wmoe_d = scr("wmoe", [S, NEXP])
    wgu_bf = [[p.dram("wgubf_%d_%d" % (i, e), [D, 2 * D], BF16) for e in range(NEXP)] for i in range(2)]
    wdn_bf = [[p.dram("wdnbf_%d_%d" % (i, e), [D, D], BF16) for e in range(NEXP)] for i in range(2)]

    def convert_expert(ll, e):
        par = ll % 2
        for q4 in range(4):
            rs = slice(q4 * 256, (q4 + 1) * 256)
            p.dma("pool", wgu_bf[par][e][rs, :], w_gu[ll, e, rs, :], w=[wgu_bf[par][e]], merge=True)
        for q2 in range(2):
            rs = slice(q2 * 512, (q2 + 1) * 512)
            p.dma("pool", wdn_bf[par][e][rs, :], w_dn[ll, e, rs, :], w=[wdn_bf[par][e]], merge=True)

    cst = p.sb([128, NCONST], F32, "cst")
    identb = p.sb([128, 128], BF16, "identb")
    sT = p.sb([128, KC * 2], F32, "sT")
    lbT = p.sb([128, 32], F32, "lbT")
    omlT = p.sb([128, 32], F32, "omlT")
    fv = p.sb([128, NFV], F32, "fv")
    modv = p.sb([128, 6, KC, 2], F32, "modv")
    psb = [p.ps([128, 512], F32, "psf%d" % i) for i in range(6)]
    psh = [p.ps([128, 1024], BF16, "psh%d" % i) for i in range(2)]
    rr = {"f": 0, "h": 0}

    def nps():
        rr["f"] += 1
        return psb[rr["f"] % 6]

    def npsh():
        rr["h"] += 1
        return psh[rr["h"] % 2]

    ident = cst[:, C_ID:C_ID + 128]
    ones = cst[:, C_ONE:C_ONE + 128]

    p.dma("sp", cst[:], consts_d[:], w=[cst])
    p.dma("sp", sT[:], scin[:], w=[sT])
    p.copy("dve", identb[:], ident, r=[cst], w=[identb])
    with p.scope():
        t0 = p.sb([128, KC * 2])
        p.act(t0[:], sT[:], AF.Sigmoid, r=[sT], w=[t0])
        p.tt("dve", sT[:], sT[:], t0[:], ALU.mult, r=[sT, t0], w=[sT])
        e0 = p.sb([128, 32])
        p.dma("sp", e0[:], lblT_d[:], w=[e0])
        p.act(e0[:], e0[:], AF.Exp, r=[e0], w=[e0])
        sm = p.sb([128, 8])
        p.tt("dve", sm[:], e0[:, 0:8], e0[:, 8:16], ALU.add, r=[e0], w=[sm])
        p.tt("dve", sm[:], sm[:], e0[:, 16:24], ALU.add, r=[e0, sm], w=[sm])
        p.tt("dve", sm[:], sm[:], e0[:, 24:32], ALU.add, r=[e0, sm], w=[sm])
        p.op("dve", lambda e: e.reciprocal(out=sm[:], in_=sm[:]), r=[sm], w=[sm])
        p.op("dve", lambda e: e.memset(lbT[:, 0:8], 0.0), w=[lbT])
        for l in range(1, 4):
            p.tt("dve", e0[:, 8 * l:8 * l + 8], e0[:, 8 * l:8 * l + 8], sm[:], ALU.mult, r=[e0, sm], w=[e0])
            p.tt("dve", lbT[:, 8 * l:8 * l + 8], lbT[:, 8 * l - 8:8 * l], e0[:, 8 * l:8 * l + 8], ALU.add, r=[e0, lbT], w=[lbT])
        p.ts("dve", omlT[:], lbT[:], -1.0, 1.0, ALU.mult, ALU.add, r=[lbT], w=[omlT])
        er = p.sb([128, 4, 1024])
        p.dma("sp", er[:].rearrange("p l n -> p (l n)"), lbl_d[:].partition_broadcast(128), w=[er])
        p.act(er[:], er[:], AF.Exp, r=[er], w=[er])
        sr = p.sb([128, 1024])
        p.tt("dve", sr[:], er[:, 0, :], er[:, 1, :], ALU.add, r=[er], w=[sr])
        p.tt("dve", sr[:], sr[:], er[:, 2, :], ALU.add, r=[er, sr], w=[sr])
        p.tt("dve", sr[:], sr[:], er[:, 3, :], ALU.add, r=[er, sr], w=[sr])
        p.op("dve", lambda e: e.reciprocal(out=sr[:], in_=sr[:]), r=[sr], w=[sr])
        lbr = p.sb([128, 2048])
        p.op("dve", lambda e: e.memset(lbr[:, 0:1024], 0.0), w=[lbr])
        for l in range(4):
            if l > 0:
                p.tt("dve", er[:, l, :], er[:, l, :], sr[:], ALU.mult, r=[er, sr], w=[er])
                p.tt("dve", lbr[:, 0:1024], lbr[:, 0:1024], er[:, l, :], ALU.add, r=[er, lbr], w=[lbr])
            p.ts("dve", lbr[:, 1024:2048], lbr[:, 0:1024], -1.0, 1.0, ALU.mult, ALU.add, r=[lbr], w=[lbr])
            p.dma("sp", lbrow_d[l], lbr[:], r=[lbr], w=[lbrow_d], merge=True)

    for e in range(NEXP):
        convert_expert(0, e)

    with p.scope():
        xts = [p.sb([128, D], F32, "xt") for _ in range(2)]
        xo = [p.sb([128, KC, 128], F32, "xo") for _ in range(2)]
        for t in range(NT128):
            xt = xts[t % 2]
            p.dma("sp", xt[:], xin[t * 128:(t + 1) * 128, :], w=[xt])
            o = xo[t % 2]
            for half in range(2):
                ps = nps()
                for j in range(4):
                    kc = half * 4 + j
                    p.tr(ps[:, j * 128:(j + 1) * 128], xt[:, kc * 128:(kc + 1) * 128], ident, r=[xt, cst], w=[ps])
                p.copy("dve" if half == 0 else "act", o[:, half * 4:half * 4 + 4, :].rearrange("p k n -> p (k n)"), ps[:], r=[ps], w=[o])
            p.dma("sp", xT[:, :, t * 128:(t + 1) * 128].rearrange("k p n -> p k n"), o[:], r=[o], w=[xT], merge=True)

    for l in range(depth):
        last = (l == depth - 1)
        p.dma("sp", fv[:], fv_d[l], w=[fv])
        with p.scope("mod%d" % l):
            mraw = p.sb([128, 48, 2], F32, "mraw")
            wts = [p.sb([128, KC, 512], F32, "modw") for _ in range(2)]
            for g in range(12):
                wt = wts[g % 2]
                p.dma("sp", wt[:], mod_w[l, :, g * 512:(g + 1) * 512].rearrange("(k p) n -> p k n", p=128), w=[wt])
                ps = nps()
                for m in range(4):
                    for kc in range(KC):
                        p.mm(ps[:, m * 2:m * 2 + 2], wt[:, kc, m * 128:(m + 1) * 128], sT[:, kc * 2:kc * 2 + 2],
                             kc == 0, kc == KC - 1, r=[wt, sT], w=[ps])
                p.copy("dve", mraw[:, g * 4:g * 4 + 4, :].rearrange("p a b -> p (a b)"), ps[:, 0:8], r=[ps], w=[mraw])
            p.tt("dve", mraw[:], mraw[:], fv[:, FV_MODB:FV_MODB + 48].unsqueeze(2).to_broadcast([128, 48, 2]), ALU.add, r=[mraw, fv], w=[mraw])
            for (dst, src, ng) in ((0, 1, FV_N1), (3, 4, FV_N2)):
                p.ts("dve", modv[:, dst], mraw[:, src * 8:src * 8 + 8, :], 1.0, None, ALU.add, None, r=[mraw], w=[modv])
                p.tt("dve", modv[:, dst], modv[:, dst], fv[:, ng:ng + 8].unsqueeze(2).to_broadcast([128, 8, 2]), ALU.mult, r=[modv, fv], w=[modv])
            for (dst, src) in ((1, 0), (2, 2), (4, 3), (5, 5)):
                p.copy("dve", modv[:, dst], mraw[:, src * 8:src * 8 + 8, :], r=[mraw], w=[modv])

        with p.scope("A%d" % l):
            hT = p.sb([128, KC, S], BF16, "hT")
            with p.scope():
                xt = p.sb([128, KC, 512], F32, "xa")
                sq = p.sb([128, KC, 512], F32, "sq")
                rstd = p.sb([128, 512], F32, "rstd")
                for (t0, N, isctx) in tiles:
                    col = 0 if isctx else 1
                    p.dma("sp", xt[:, :, :N], xT[:, :, t0:t0 + N].rearrange("k p n -> p k n"), r=[xT], w=[xt])
                    p.act(sq[:, :, :N], xt[:, :, :N], AF.Square, r=[xt], w=[sq])
                    ps = nps()
                    for kc in range(KC):
                        p.mm(ps[:, :N], ones, sq[:, kc, :N], kc == 0, kc == KC - 1, r=[cst, sq], w=[ps])
                    p.act(rstd[:, :N], ps[:, :N], AF.Ln, r=[ps], w=[rstd], bias=EPS, scale=1.0 / D)
                    p.act(rstd[:, :N], rstd[:, :N], AF.Exp, r=[rstd], w=[rstd], scale=-0.5)
                    for kc in range(KC):
                        p.stt("dve", sq[:, kc, :N], xt[:, kc, :N], modv[:, 0, kc, col:col + 1], rstd[:, :N], ALU.mult, ALU.mult,
                              r=[xt, modv, rstd], w=[sq])
                        p.act(hT[:, kc, t0:t0 + N], sq[:, kc, :N], AF.Identity, r=[sq, modv], w=[hT], bias=modv[:, 1, kc, col:col + 1])

            lbr = p.sb([128, 2048], F32, "lbr")
            p.dma("sp", lbr[:], lbrow_d[l], r=[lbrow_d], w=[lbr])
            wgs = [p.sb([128, KC, 512], BF16, "wg") for _ in range(2)]
            gi = [0]

            def load_wg(c0):
                wg = wgs[gi[0] % 2]
                gi[0] += 1
                p.dma("pool", wg[:], w_in[l, :, c0:c0 + 512].rearrange("(k p) n -> p k n", p=128), w=[wg])
                return wg

            rot = [p.sb([128, 128], F32, "rot") for _ in range(2)]
            tmpA = [p.sb([128, 512], F32, "tmpA") for _ in range(2)]
            tmpB = [p.sb([128, 512], F32, "tmpB") for _ in range(2)]
            obf = [p.sb([128, 512], BF16, "obf") for _ in range(2)]
            of32 = [p.sb([128, 512], F32, "of32") for _ in range(2)]
            of32b = [p.sb([128, 512], F32, "of32b") for _ in range(2)]
            cnt = [0]

            def tm_loop(wg, handler):
                for t in range(NT128):
                    ps = nps()
                    for kc in range(KC):
                        p.mm(ps[:], hT[:, kc, t * 128:(t + 1) * 128], wg[:, kc, :], kc == 0, kc == KC - 1, r=[hT, wg], w=[ps])
                    cnt[0] += 1
                    handler(t, ps, cnt[0] % 2)

            def fm_loop(wg, handler):
                for (t0, N, isctx) in tiles:
                    for m in range(4):
                        ps = nps()
                        for kc in range(KC):
                            p.mm(ps[:, :N], wg[:, kc, m * 128:(m + 1) * 128], hT[:, kc, t0:t0 + N], kc == 0, kc == KC - 1, r=[hT, wg], w=[ps])
                        cnt[0] += 1
                        handler(t0, N, m, ps, cnt[0] % 2)

            def store_tm(dst, t, src):
                p.dma("sp", dst[t * 128:(t + 1) * 128, :], src[:], r=[src], w=[dst], merge=True)

            def store_fm(dst3, m, t0, N, src):
                p.dma("sp", dst3[m, :, t0:t0 + N], src[:, :N], r=[src], w=[dst3], merge=True)

            def h_rot(dst, cofs):
                def h(t, ps, b):
                    o = obf[b]
                    if t < 2:
                        if cofs == 0:
                            p.copy("dve", o[:], ps[:], r=[ps], w=[o])
                        else:
                            p.ts("dve", o[:], ps[:], 0.125, None, ALU.mult, None, r=[ps], w=[o])
                    else:
                        rt = rot[b]
                        p.dma("sp", rt[:], rot_d[(t - 2) * 128:(t - 1) * 128, :], w=[rt])
                        pv = ps[:].rearrange("p (h two d) -> p h two d", h=8, two=2)
                        ov = o[:].rearrange("p (h two d) -> p h two d", h=8, two=2)
                        cs = rt[:, cofs:cofs + 32].unsqueeze(1).to_broadcast([128, 8, 32])
                        sn = rt[:, cofs + 32:cofs + 64].unsqueeze(1).to_broadcast([128, 8, 32])
                        a = tmpA[b][:, 0:256].rearrange("p (h d) -> p h d", h=8)
                        bb = tmpA[b][:, 256:512].rearrange("p (h d) -> p h d", h=8)
                        p.tt("dve", a, pv[:, :, 0, :], cs, ALU.mult, r=[ps, rt], w=[tmpA[b]])
                        p.tt("dve", bb, pv[:, :, 1, :], sn, ALU.mult, r=[ps, rt], w=[tmpA[b]])
                        p.tt("dve", ov[:, :, 0, :], a, bb, ALU.subtract, r=[tmpA[b]], w=[o])
                        p.tt("dve", a, pv[:, :, 0, :], sn, ALU.mult, r=[ps, rt], w=[tmpA[b]])
                        p.tt("dve", bb, pv[:, :, 1, :], cs, ALU.mult, r=[ps, rt], w=[tmpA[b]])
                        p.tt("dve", ov[:, :, 1, :], a, bb, ALU.add, r=[tmpA[b]], w=[o])
                    store_tm(dst, t, o)
                return h

            def h_copy_bf(dst):
                def h(t, ps, b):
                    p.copy("act", obf[b][:], ps[:], r=[ps], w=[obf[b]])
                    store_tm(dst, t, obf[b])
                return h

            def h_silu_tm(dst):
                def h(t, ps, b):
                    p.act(tmpA[b][:], ps[:], AF.Sigmoid, r=[ps], w=[tmpA[b]])
                    p.tt("dve", of32[b][:], tmpA[b][:], ps[:], ALU.mult, r=[ps, tmpA[b]], w=[of32[b]])
                    store_tm(dst, t, of32[b])
                return h

            def h_sig_tm(dst):
                def h(t, ps, b):
                    p.act(of32[b][:], ps[:], AF.Sigmoid, r=[ps], w=[of32[b]])
                    store_tm(dst, t, of32[b])
                return h

            def h_forget_tm(d):
                lb = lbr[:, d * 512:(d + 1) * 512]
                oml = lbr[:, 1024 + d * 512:1024 + (d + 1) * 512]

                def h(t, ps, b):
                    p.act(tmpA[b][:], ps[:], AF.Sigmoid, r=[ps], w=[tmpA[b]])
                    p.tt("dve", tmpA[b][:], tmpA[b][:], oml, ALU.mult, r=[tmpA[b], lbr], w=[tmpA[b]])
                    p.tt("dve", tmpB[b][:], tmpA[b][:], lb, ALU.add, r=[tmpA[b], lbr], w=[tmpB[b]])
                    p.tt("pool", of32[b][:], oml, tmpA[b][:], ALU.subtract, r=[tmpA[b], lbr], w=[of32[b]])
                    p.act(of32b[b][:], tmpB[b][:], AF.Ln, r=[tmpB[b]], w=[of32b[b]])
                    store_tm(h_k[d], t, of32[b])
                    store_tm(h_lf[d], t, of32b[b])
                return h

            def h_forget_fm(d):
                def h(t0, N, m, ps, b):
                    p.act(tmpA[b][:, :N], ps[:, :N], AF.Sigmoid, r=[ps], w=[tmpA[b]], scale=-1.0)
                    p.ts("dve", of32[b][:, :N], tmpA[b][:, :N], omlT[:, l * 8 + d * 4 + m:l * 8 + d * 4 + m + 1], None, ALU.mult, None,
                         r=[tmpA[b], omlT], w=[of32[b]])
                    store_fm(h_kT[d], m, t0, N, of32[b])
                return h

            def h_silu_fm(dst):
                def h(t0, N, m, ps, b):
                    p.act(tmpA[b][:, :N], ps[:, :N], AF.Sigmoid, r=[ps], w=[tmpA[b]])
                    p.tt("dve", of32[b][:, :N], tmpA[b][:, :N], ps[:, :N], ALU.mult, r=[ps, tmpA[b]], w=[of32[b]])
                    store_fm(dst, m, t0, N, of32[b])
                return h

            def h_copy_fm(dst):
                def h(t0, N, m, ps, b):
                    p.copy("act", of32[b][:, :N], ps[:, :N], r=[ps], w=[of32[b]])
                    store_fm(dst, m, t0, N, of32[b])
                return h

            def h_gelu_fm(dst):
                def h(t0, N, m, ps, b):
                    p.act(tmpA[b][:, :N], ps[:, :N], AF.Square, r=[ps], w=[tmpA[b]])
                    p.ts("dve", tmpA[b][:, :N], tmpA[b][:, :N], 0.044715, 1.0, ALU.mult, ALU.add, r=[tmpA[b]], w=[tmpA[b]])
                    p.tt("dve", tmpA[b][:, :N], tmpA[b][:, :N], ps[:, :N], ALU.mult, r=[tmpA[b], ps], w=[tmpA[b]])
                    p.act(tmpA[b][:, :N], tmpA[b][:, :N], AF.Sigmoid, r=[tmpA[b]], w=[tmpA[b]], scale=1.5957691216057308)
                    p.tt("dve", of32[b][:, :N], tmpA[b][:, :N], ps[:, :N], ALU.mult, r=[tmpA[b], ps], w=[of32[b]])
                    store_fm(dst, m, t0, N, of32[b])
                return h

            def h_sig_fm(dst, gidx, half):
                def h(t0, N, m, ps, b):
                    p.act(of32[b][:, :N], ps[:, :N], AF.Sigmoid, r=[ps], w=[of32[b]])
                    p.dma("sp", dst[gidx, half * 4 + m, :, t0:t0 + N], of32[b][:, :N], r=[of32[b]], w=[dst], merge=True)
                return h

            plan = [
                (G_RQ, "tm", h_rot(r_q, 0)), (G_RK, "tm", h_rot(r_k, 64)), (G_RV, "tm", h_copy_bf(r_v)), (G_RG, "tm", h_silu_tm(r_g)),
                (G_HQ, "fm", h_silu_fm(h_qT)),
                (G_HFF, "tm", h_forget_tm(0)), (G_HFF, "fm", h_forget_fm(0)),
                (G_HFB, "tm", h_forget_tm(1)), (G_HFB, "fm", h_forget_fm(1)),
                (G_HI, "tm", h_copy_bf(h_v)), (G_HGT, "tm", h_sig_tm(h_g)),
                (G_LX, "fm", h_copy_fm(l_xT)), (G_LGT, "fm", h_gelu_fm(l_gT)),
                (G_GR, "fm", h_sig_fm(g_T, 0, 0)), (G_GR + 512, "fm", h_sig_fm(g_T, 0, 1)),
                (G_GH, "fm", h_sig_fm(g_T, 1, 0)), (G_GH + 512, "fm", h_sig_fm(g_T, 1, 1)),
                (G_GL, "fm", h_sig_fm(g_T, 2, 0)), (G_GL + 512, "fm", h_sig_fm(g_T, 2, 1)),
            ]
            prev = (None, None)
            for (c0, kind, hd) in plan:
                if c0 != prev[0]:
                    wg = load_wg(c0)
                    prev = (c0, wg)
                (tm_loop if kind == "tm" else fm_loop)(prev[1], hd)

        def phase_B():
            lgb = p.sb([128, 16], F32, "lgb")
            p.dma("sp", lgb[:], ret_decay[l:l + 1, :].partition_broadcast(128), w=[lgb])
            p.act(lgb[:], lgb[:], AF.Exp, r=[lgb], w=[lgb], scale=-1.0)
            p.act(lgb[:], lgb[:], AF.Ln, r=[lgb], w=[lgb], bias=1.0)
            p.ts("dve", lgb[:], lgb[:], -1.0, None, ALU.mult, None, r=[lgb], w=[lgb])
            gnrow = p.sb([128, 512], F32, "gnrow")
            p.dma("sp", gnrow[:], ret_gn[l:l + 1, :].partition_broadcast(128), w=[gnrow])
            av = p.sb([128, 2, 8], F32, "av")
            bv = p.sb([128, 2, 8], F32, "bv")
            iv = p.sb([128, 2, 8], F32, "iv")
            Dt = p.sb([128, 2, 8, 128], F32, "Dt")
            Gt = p.sb([64, 2, 512], F32, "Gt")
            idx = lambda i: cst[:, C_IDX + i:C_IDX + i + 1]
            for d in range(2):
                lg = lgb[:, d * 8:d * 8 + 8]
                p.act(av[:, d, :], lg, AF.Exp, r=[lgb, cst], w=[av], scale=idx(0) if d == 0 else idx(2))
                p.act(bv[:, d, :], lg, AF.Exp, r=[lgb, cst], w=[bv], scale=idx(1) if d == 0 else idx(3))
                p.act(iv[:, d, :], lg, AF.Exp, r=[lgb, cst], w=[iv], scale=idx(4) if d == 0 else idx(5))
                msk = cst[:, C_MF:C_MF + 128] if d == 0 else cst[:, C_MB:C_MB + 128]
                for h in range(8):
                    p.ts("dve", Dt[:, d, h, :], msk, iv[:, d, h:h + 1], None, ALU.mult, None, r=[cst, iv], w=[Dt])
                p.act(Gt[:, d, :].rearrange("p (h v) -> p h v", h=8), lgb[0:64, d * 8:d * 8 + 8].unsqueeze(2).to_broadcast([64, 8, 64]),
                      AF.Exp, r=[lgb], w=[Gt], scale=128.0)

            Sst = p.sb([64, 512], F32, "Sst")
            Sbf = p.sb([64, 512], BF16, "Sbf")
            NB = 2
            qt = [p.sb([128, 512], BF16, "rq") for _ in range(NB)]
            kt = [p.sb([128, 512], BF16, "rk") for _ in range(NB)]
            vt = [p.sb([128, 512], BF16, "rv") for _ in range(NB)]
            qd = [p.sb([128, 512], BF16, "qd") for _ in range(NB)]
            kd = [p.sb([128, 512], BF16, "kd") for _ in range(NB)]
            qdT = [p.sb([64, 8, 128], BF16, "qdT") for _ in range(NB)]
            kTt = [p.sb([64, 8, 128], BF16, "kTt") for _ in range(NB)]
            Pm = [p.sb([128, 8, 128], BF16, "Pm") for _ in range(NB)]
            osb = [p.sb([128, 512], F32, "osb") for _ in range(NB)]
            oprev = [p.sb([128, 512], F32, "oprev") for _ in range(NB)]
            gate = [p.sb([128, 512], F32, "gate") for _ in range(NB)]
            ssq = [p.sb([128, 8], F32, "ssq") for _ in range(NB)]
            ybf = [p.sb([128, 512], BF16, "ybf") for _ in range(NB)]
            yT = [p.sb([128, 4, 128], BF16, "yT") for _ in range(NB)]
            for d in range(2):
                p.op("dve", lambda e: e.memset(Sst[:], 0.0), w=[Sst])
                p.op("dve", lambda e: e.memset(Sbf[:], 0.0), w=[Sbf])
                order = list(range(NT128)) if d == 0 else [1, 0] + list(range(NT128 - 1, 1, -1))
                for it, t in enumerate(order):
                    b = it % NB
                    yield
                    if last and d == 1 and t < 2:
                        pass
                    rows = slice(t * 128, (t + 1) * 128)
                    p.dma("sp", qt[b][:], r_q[rows, :], r=[r_q], w=[qt[b]])
                    p.dma("sp", kt[b][:], r_k[rows, :], r=[r_k], w=[kt[b]])
                    p.dma("sp", vt[b][:], r_v[rows, :], r=[r_v], w=[vt[b]])
                    yield
                    v3 = lambda tl: tl[:].rearrange("p (h e) -> p h e", h=8)
                    p.tt("dve", v3(qd[b]), v3(qt[b]), av[:, d, :].unsqueeze(2).to_broadcast([128, 8, 64]), ALU.mult, r=[qt[b], av], w=[qd[b]])
                    p.tt("pool", v3(kd[b]), v3(kt[b]), bv[:, d, :].unsqueeze(2).to_broadcast([128, 8, 64]), ALU.mult, r=[kt[b], bv], w=[kd[b]])
                    yield
                    ph = npsh()
                    for h in range(8):
                        p.tr(ph[0:64, h * 128:(h + 1) * 128], qd[b][:, h * 64:(h + 1) * 64], identb[:], r=[qd[b], identb], w=[ph])
                    p.copy("act", qdT[b][:].rearrange("p h n -> p (h n)"), ph[0:64, :], r=[ph], w=[qdT[b]])
                    yield
                    ph = npsh()
                    for h in range(8):
                        p.tr(ph[0:64, h * 128:(h + 1) * 128], kt[b][:, h * 64:(h + 1) * 64], identb[:], r=[kt[b], identb], w=[ph])
                    p.copy("dve", kTt[b][:].rearrange("p h n -> p (h n)"), ph[0:64, :], r=[ph], w=[kTt[b]])
                    yield
                    for hh in range(2):
                        ps = nps()
                        for j in range(4):
                            h = hh * 4 + j
                            p.mm(ps[:, j * 128:(j + 1) * 128], kTt[b][:, h, :], qdT[b][:, h, :], True, True, r=[kTt[b], qdT[b]], w=[ps])
                        p.tt("dve", Pm[b][:, hh * 4:hh * 4 + 4, :].rearrange("p h n -> p (h n)"), ps[:],
                             Dt[:, d, hh * 4:hh * 4 + 4, :].rearrange("p h n -> p (h n)"), ALU.mult, r=[ps, Dt], w=[Pm[b]])
                    yield
                    po = nps()
                    for h in range(8):
                        cs = slice(h * 64, (h + 1) * 64)
                        p.mm(po[:, cs], Pm[b][:, h, :], vt[b][:, cs], True, False, r=[Pm[b], vt[b]], w=[po])
                        p.mm(po[:, cs], qdT[b][:, h, :], Sbf[:, cs], False, True, r=[qdT[b], Sbf], w=[po])
                    yield
                    psS = nps()
                    for h in range(8):
                        cs = slice(h * 64, (h + 1) * 64)
                        p.mm(psS[0:64, cs], kd[b][:, cs], vt[b][:, cs], True, True, r=[kd[b], vt[b]], w=[psS])
                    yield
                    p.tt("dve", Sst[:], Sst[:], Gt[:, d, :], ALU.mult, r=[Sst, Gt], w=[Sst])
                    p.tt("dve", Sst[:], Sst[:], psS[0:64, :], ALU.add, r=[Sst, psS], w=[Sst])
                    p.copy("act", Sbf[:], Sst[:], r=[Sst], w=[Sbf])
                    if d == 0:
                        p.copy("act", osb[b][:], po[:], r=[po], w=[osb[b]])
                        p.dma("sp", o_ret[rows, :], osb[b][:], r=[osb[b]], w=[o_ret], merge=True)
                    else:
                        if last and t < 2:
                            continue
                        p.dma("sp", oprev[b][:], o_ret[rows, :], r=[o_ret], w=[oprev[b]])
                        p.dma("sp", gate[b][:], r_g[rows, :], r=[r_g], w=[gate[b]])
                        p.tt("dve", osb[b][:], po[:], oprev[b][:], ALU.add, r=[po, oprev[b]], w=[osb[b]])
                        yield
                        p.tt("pool", oprev[b][:], osb[b][:], osb[b][:], ALU.mult, r=[osb[b]], w=[oprev[b]])
                        p.op("dve", lambda e: e.reduce_sum(out=ssq[b][:], in_=oprev[b][:].rearrange("p (h e) -> p h e", h=8), axis=AX.X),
                             r=[oprev[b]], w=[ssq[b]])
                        p.act(ssq[b][:], ssq[b][:], AF.Ln, r=[ssq[b]], w=[ssq[b]], bias=EPS, scale=1.0 / 64)
                        p.act(ssq[b][:], ssq[b][:], AF.Exp, r=[ssq[b]], w=[ssq[b]], scale=-0.5)
                        p.tt("dve", v3(osb[b]), v3(osb[b]), ssq[b][:].unsqueeze(2).to_broadcast([128, 8, 64]), ALU.mult, r=[osb[b], ssq[b]], w=[osb[b]])
                        yield
                        p.tt("pool", gate[b][:], gate[b][:], gnrow[:], ALU.mult, r=[gate[b], gnrow], w=[gate[b]])
                        p.tt("dve", ybf[b][:], osb[b][:], gate[b][:], ALU.mult, r=[osb[b], gate[b]], w=[ybf[b]])
                        ph = npsh()
                        for c in range(4):
                            p.tr(ph[:, c * 128:(c + 1) * 128], ybf[b][:, c * 128:(c + 1) * 128], identb[:], r=[ybf[b], identb], w=[ph])
                        p.copy("act", yT[b][:].rearrange("p c n -> p (c n)"), ph[:, 0:512], r=[ph], w=[yT[b]])
                        p.dma("sp", retnT[:, :, t * 128:(t + 1) * 128].rearrange("c p n -> p c n"), yT[b][:], r=[yT[b]], w=[retnT], merge=True)

        def phase_C():
            gnrow = p.sb([128, 512], F32, "hgnrow")
            p.dma("sp", gnrow[:], hg_gn[l:l + 1, :].partition_broadcast(128), w=[gnrow])
            Sst = p.sb([128, 4, 128], F32, "hS")
            Sbf = p.sb([128, 4, 128], BF16, "hSbf")
            NB = 2
            mk = lambda shape, dt, nm: [p.sb(shape, dt, nm) for _ in range(NB)]
            lft = mk([64, 512], F32, "lft")
            ktm = mk([64, 512], F32, "ktm")
            vt = mk([64, 512], BF16, "hv")
            qTt = mk([128, 4, 64], F32, "qTt")
            kTt = mk([128, 4, 64], F32, "hkT")
            gsb = mk([128, 4, 64], F32, "gsb")
            gref = mk([128, 8], F32, "gref")
            E1 = mk([128, 4, 64], F32, "E1")
            E3 = mk([128, 4, 64], F32, "E3")
            qtl = mk([128, 4, 64], BF16, "qtl")
            ktl = mk([128, 4, 64], BF16, "ktl")
            qg = mk([128, 4, 64], BF16, "qg")
            ER = mk([64, 512], F32, "ER")
            kdec = mk([64, 512], BF16, "kdec")
            Pm = mk([64, 4, 64], BF16, "hP")
            osb = mk([64, 512], F32, "hosb")
            oprev = mk([64, 512], F32, "hoprev")
            gate = mk([64, 512], F32, "hgate")
            ssq = mk([64, 4], F32, "hssq")
            ybf = mk([64, 512], BF16, "hybf")
            yT = mk([128, 4, 64], BF16, "hyT")
            NCH = S // 64
            M = 32
            mint = [p.sb([64, 4, 64], mybir.dt.int32, "mint%d" % i) for i in range(2)]
            zer = p.sb([64, 4, 64], F32, "zer")
            p.op("dve", lambda e: e.memset(zer[:], 0.0), w=[zer])
            for i, cc in enumerate((C_TF, C_TB)):
                p.copy("dve", mint[i][:], cst[0:64, cc:cc + 64].unsqueeze(1).to_broadcast([64, 4, 64]), r=[cst], w=[mint[i]])
            for d in range(2):
                p.op("dve", lambda e: e.memset(Sst[:], 0.0), w=[Sst])
                p.op("dve", lambda e: e.memset(Sbf[:], 0.0), w=[Sbf])
                tri = cst[0:64, C_TF:C_TF + 64] if d == 0 else cst[0:64, C_TB:C_TB + 64]
                rem = cst[0:64, C_RF:C_RF + 64] if d == 0 else cst[0:64, C_RB:C_RB + 64]
                lastcol = 63 if d == 0 else 0
                order = list(range(NCH)) if d == 0 else [3, 2, 1, 0] + list(range(NCH - 1, 3, -1))
                for it, c in enumerate(order):
                    b = it % NB
                    yield
                    rows = slice(c * 64, (c + 1) * 64)
                    p.dma("sp", lft[b][:], h_lf[d][rows, :], r=[h_lf[d]], w=[lft[b]])
                    p.dma("sp", ktm[b][:], h_k[d][rows, :], r=[h_k[d]], w=[ktm[b]])
                    p.dma("sp", vt[b][:], h_v[rows, :], r=[h_v], w=[vt[b]])
                    p.dma("sp", qTt[b][:], h_qT[:, :, c * 64:(c + 1) * 64].rearrange("h p n -> p h n"), r=[h_qT], w=[qTt[b]])
                    p.dma("sp", kTt[b][:], h_kT[d][:, :, c * 64:(c + 1) * 64].rearrange("h p n -> p h n"), r=[h_kT[d]], w=[kTt[b]])
                    yield
                    pg = nps()
                    for h in range(4):
                        p.mm(pg[:, h * 64:(h + 1) * 64], lft[b][:, h * 128:(h + 1) * 128], tri, True, True, r=[lft[b], cst], w=[pg])
                    pr = nps()
                    for h in range(4):
                        p.mm(pr[0:64, h * 128:(h + 1) * 128], rem, lft[b][:, h * 128:(h + 1) * 128], True, True, r=[lft[b], cst], w=[pr])
                    yield
                    p.copy("dve", gsb[b][:].rearrange("p h n -> p (h n)"), pg[:, 0:256], r=[pg], w=[gsb[b]])
                    p.copy("dve", gref[b][:, 0:4], gsb[b][:, :, M], r=[gsb[b]], w=[gref[b]])
                    p.ts("dve", gref[b][:, 4:8], gref[b][:, 0:4], -1.0, None, ALU.mult, None, r=[gref[b]], w=[gref[b]])
                    yield
                    for h in range(4):
                        p.act(E1[b][:, h, :], gsb[b][:, h, :], AF.Exp, r=[gsb[b], gref[b]], w=[E1[b]], bias=gref[b][:, 4 + h:5 + h])
                    yield
                    p.tt("dve", qtl[b][:], qTt[b][:], E1[b][:], ALU.mult, r=[qTt[b], E1[b]], w=[qtl[b]])
                    for h in range(4):
                        p.act(E1[b][:, h, :], gsb[b][:, h, :], AF.Exp, r=[gsb[b], gref[b], qtl[b]], w=[E1[b]], bias=gref[b][:, h:h + 1], scale=-1.0)
                    p.tt("dve", ktl[b][:], kTt[b][:], E1[b][:], ALU.mult, r=[kTt[b], E1[b]], w=[ktl[b]])
                    yield
                    p.act(E3[b][:], gsb[b][:], AF.Exp, r=[gsb[b]], w=[E3[b]])
                    p.tt("pool", qg[b][:], qTt[b][:], E3[b][:], ALU.mult, r=[qTt[b], E3[b]], w=[qg[b]])
                    yield
                    p.act(ER[b][:], pr[0:64, :], AF.Exp, r=[pr], w=[ER[b]])
                    p.tt("pool", kdec[b][:], ktm[b][:], ER[b][:], ALU.mult, r=[ktm[b], ER[b]], w=[kdec[b]])
                    yield
                    psc = nps()
                    for h in range(4):
                        p.mm(psc[0:64, h * 64:(h + 1) * 64], ktl[b][:, h, :], qtl[b][:, h, :], True, True, r=[ktl[b], qtl[b]], w=[psc])
                    yield
                    p.op("dve", lambda e: e.select(out=Pm[b][:], mask=mint[d][:], on_true=psc[0:64, 0:256].rearrange("p (h n) -> p h n", h=4),
                                                   on_false=zer[:]), r=[psc, mint[d], zer], w=[Pm[b]])
                    yield
                    po = nps()
                    for h in range(4):
                        cs = slice(h * 128, (h + 1) * 128)
                        p.mm(po[0:64, cs], Pm[b][:, h, :], vt[b][:, cs], True, False, r=[Pm[b], vt[b]], w=[po])
                        p.mm(po[0:64, cs], qg[b][:, h, :], Sbf[:, h, :], False, True, r=[qg[b], Sbf], w=[po])
                    yield
                    psS = nps()
                    for h in range(4):
                        cs = slice(h * 128, (h + 1) * 128)
                        p.mm(psS[:, cs], kdec[b][:, cs], vt[b][:, cs], True, True, r=[kdec[b], vt[b]], w=[psS])
                    yield
                    for h in range(4):
                        p.stt("dve", Sst[:, h, :], Sst[:, h, :], E3[b][:, h, lastcol:lastcol + 1], psS[:, h * 128:(h + 1) * 128], ALU.mult, ALU.add,
                              r=[Sst, E3[b], psS], w=[Sst])
                    p.copy("act", Sbf[:], Sst[:], r=[Sst], w=[Sbf])
                    if d == 0:
                        p.copy("act", osb[b][:], po[0:64, :], r=[po], w=[osb[b]])
                        p.dma("sp", o_hg[rows, :], osb[b][:], r=[osb[b]], w=[o_hg], merge=True)
                    else:
                        if last and c < 4:
                            continue
                        p.dma("sp", oprev[b][:], o_hg[rows, :], r=[o_hg], w=[oprev[b]])
                        p.dma("sp", gate[b][:], h_g[rows, :], r=[h_g], w=[gate[b]])
                        p.tt("dve", osb[b][:], po[0:64, :], oprev[b][:], ALU.add, r=[po, oprev[b]], w=[osb[b]])
                        yield
                        p.tt("pool", oprev[b][:], osb[b][:], osb[b][:], ALU.mult, r=[osb[b]], w=[oprev[b]])
                        p.op("dve", lambda e: e.reduce_sum(out=ssq[b][:], in_=oprev[b][:].rearrange("p (h e) -> p h e", h=4), axis=AX.X),
                             r=[oprev[b]], w=[ssq[b]])
                        p.act(ssq[b][:], ssq[b][:], AF.Ln, r=[ssq[b]], w=[ssq[b]], bias=EPS, scale=1.0 / 128)
                        p.act(ssq[b][:], ssq[b][:], AF.Exp, r=[ssq[b]], w=[ssq[b]], scale=-0.5)
                        o3 = osb[b][:].rearrange("p (h e) -> p h e", h=4)
                        p.tt("dve", o3, o3, ssq[b][:].unsqueeze(2).to_broadcast([64, 4, 128]), ALU.mult, r=[osb[b], ssq[b]], w=[osb[b]])
                        yield
                        p.tt("pool", gate[b][:], gate[b][:], gnrow[0:64, :], ALU.mult, r=[gate[b], gnrow], w=[gate[b]])
                        p.tt("dve", ybf[b][:], osb[b][:], gate[b][:], ALU.mult, r=[osb[b], gate[b]], w=[ybf[b]])
                        ph = npsh()
                        for h in range(4):
                            p.tr(ph[:, h * 64:(h + 1) * 64], ybf[b][:, h * 128:(h + 1) * 128], identb[0:64, 0:64], r=[ybf[b], identb], w=[ph])
                        p.copy("act", yT[b][:].rearrange("p c n -> p (c n)"), ph[:, 0:256], r=[ph], w=[yT[b]])
                        p.dma("sp", hgnT[:, :, c * 64:(c + 1) * 64].rearrange("c p n -> p c n"), yT[b][:], r=[yT[b]], w=[hgnT], merge=True)

        with p.scope("BC%d" % l):
            gens = [phase_C(), phase_B()]
            quota = [3, 1]
            while gens:
                for gi in range(len(gens) - 1, -1, -1):
                    pass
                nxt = []
                for g_, q_ in zip(gens, quota):
                    ok = True
                    for _ in range(q_):
                        try:
                            next(g_)
                        except StopIteration:
                            ok = False
                            break
                    if ok:
                        nxt.append((g_, q_))
                gens = [x[0] for x in nxt]
                quota = [x[1] for x in nxt]

        with p.scope("D%d" % l):
            xs = p.sb([128, S], F32, "lx")
            xc = p.sb([128, S], F32, "lxc")
            at = p.sb([128, S], F32, "la")
            bt = p.sb([128, S], F32, "lb")
            hf = p.sb([128, S], F32, "lhf")
            hb = p.sb([128, S], F32, "lhb")
            gl = p.sb([128, S], F32, "lgl")
            ybf = p.sb([128, S], BF16, "lybf")
            wbd = p.sb([128, 128], F32, "wbd")
            wbx = p.sb([128, 128], F32, "wbx")
            cneg = p.sb([128, 8], F32, "cneg")
            p.act(cneg[:], fv[:, FV_LAM:FV_LAM + 8], AF.Exp, r=[fv], w=[cneg], scale=-1.0)
            p.act(cneg[:], cneg[:], AF.Ln, r=[cneg], w=[cneg], bias=1.0)
            p.ts("dve", cneg[:], cneg[:], -8.0, None, ALU.mult, None, r=[cneg], w=[cneg])
            tA = [p.sb([128, 512], F32, "ltA") for _ in range(2)]
            tB = [p.sb([128, 512], F32, "ltB") for _ in range(2)]
            segs = [(0, CTX), (CTX, S)]
            for c in range(4):
                p.dma("sp", xs[:], l_xT[c], r=[l_xT], w=[xs])
                p.dma("sp", gl[:], l_gT[c], r=[l_gT], w=[gl])
                cw = lambda w: fv[:, FV_CW + w * 4 + c:FV_CW + w * 4 + c + 1]
                p.ts("dve", xc[:], xs[:], cw(2), fv[:, FV_CB + c:FV_CB + c + 1], ALU.mult, ALU.add, r=[xs, fv], w=[xc])
                for w in (0, 1, 3):
                    o = w - 2
                    for (a, bnd) in segs:
                        lo, hi = max(a, a - o), min(bnd, bnd - o)
                        p.stt("dve", xc[:, lo:hi], xs[:, lo + o:hi + o], cw(w), xc[:, lo:hi], ALU.mult, ALU.add, r=[xs, fv, xc], w=[xc])
                for d in range(2):
                    p.dma("sp", wbd[:], lru_bd[l, 0, d, c], w=[wbd])
                    p.dma("sp", wbx[:], lru_bd[l, 1, d, c], w=[wbx])
                    for i, (t0, N, isctx) in enumerate(tiles):
                        b = i % 2
                        pa = nps()
                        p.mm(pa[:, :N], wbd[:], xc[:, t0:t0 + N], True, True, r=[wbd, xc], w=[pa])
                        px = nps()
                        p.mm(px[:, :N], wbx[:], xc[:, t0:t0 + N], True, True, r=[wbx, xc], w=[px])
                        p.act(tA[b][:, :N], pa[:, :N], AF.Sigmoid, r=[pa, fv], w=[tA[b]], bias=fv[:, FV_BA + d * 4 + c:FV_BA + d * 4 + c + 1])
                        p.act(tB[b][:, :N], px[:, :N], AF.Sigmoid, r=[px, fv], w=[tB[b]], bias=fv[:, FV_BX + d * 4 + c:FV_BX + d * 4 + c + 1])
                        p.act(at[:, t0:t0 + N], tA[b][:, :N], AF.Exp, r=[tA[b], cneg], w=[at], scale=cneg[:, d * 4 + c:d * 4 + c + 1])
                        p.tt("dve", tB[b][:, :N], tB[b][:, :N], xc[:, t0:t0 + N], ALU.mult, r=[tB[b], xc], w=[tB[b]])
                        p.tt("dve", tA[b][:, :N], at[:, t0:t0 + N], at[:, t0:t0 + N], ALU.mult, r=[at], w=[tA[b]])
                        p.ts("dve", tA[b][:, :N], tA[b][:, :N], -1.0, 1.0, ALU.mult, ALU.add, r=[tA[b]], w=[tA[b]])
                        p.act(tA[b][:, :N], tA[b][:, :N], AF.Ln, r=[tA[b]], w=[tA[b]])
                        p.act(tA[b][:, :N], tA[b][:, :N], AF.Exp, r=[tA[b]], w=[tA[b]], scale=0.5)
                        p.tt("dve", bt[:, t0:t0 + N], tA[b][:, :N], tB[b][:, :N], ALU.mult, r=[tA[b], tB[b]], w=[bt])
                    PC = 256
                    if d == 0:
                        for s0 in range(0, S, PC):
                            init = 0.0 if s0 == 0 else hf[:, s0 - 1:s0]
                            p.op("dve", lambda e: e.tensor_tensor_scan(out=hf[:, s0:s0 + PC], data0=at[:, s0:s0 + PC], data1=bt[:, s0:s0 + PC],
                                                                       initial=init, op0=ALU.mult, op1=ALU.add), r=[at, bt, hf], w=[hf])
                    else:
                        pieces = [(0, CTX, None)] + [(s0, s0 + PC, None) for s0 in range(S - PC, CTX - 1, -PC)]
                        prev_first = None
                        for (a0, a1, _) in pieces:
                            init = 0.0 if prev_first is None else hb[:, prev_first:prev_first + 1]
                            rv = lambda tl: tl[:, a0:a1][:, ::-1]
                            p.op("dve", lambda e: e.tensor_tensor_scan(out=rv(hb), data0=rv(at), data1=rv(bt),
                                                                       initial=init, op0=ALU.mult, op1=ALU.add), r=[at, bt, hb], w=[hb])
                            prev_first = a0
                p.tt("dve", hf[:], hf[:], hb[:], ALU.add, r=[hf, hb], w=[hf])
                p.tt("dve", ybf[:], hf[:], gl[:], ALU.mult, r=[hf, gl], w=[ybf])
                p.dma("sp", lruT[c], ybf[:], r=[ybf], w=[lruT], merge=True)

        with p.scope("E%d" % l):
            wro = p.sb([128, 4, D], BF16, "wro")
            who = p.sb([128, 4, D], BF16, "who")
            wlo = p.sb([128, 4, D], BF16, "wlo")
            wo = p.sb([128, KC, D], BF16, "wo")
            wr = p.sb([128, KC, NEXP], F32, "wr")
            rbrow = p.sb([128, NEXP], F32, "rbrow")
            p.dma("pool", wro[:], w_ret_o[l].rearrange("(k p) n -> p k n", p=128), w=[wro])
            p.dma("pool", who[:], w_hg_o[l].rearrange("(k p) n -> p k n", p=128), w=[who])
            p.dma("pool", wlo[:], w_lru_o[l].rearrange("(k p) n -> p k n", p=128), w=[wlo])
            p.dma("pool", wo[:], w_out[l].rearrange("(k p) n -> p k n", p=128), w=[wo])
            p.dma("sp", wr[:], router_w[l].rearrange("(k p) n -> p k n", p=128), w=[wr])
            p.dma("sp", rbrow[:], router_b[l:l + 1, :].partition_broadcast(128), w=[rbrow])
            bt3 = [p.sb([128, 4, 512], BF16, "br%d" % i) for i in range(3)]
            gts = [[p.sb([128, 512], F32, "gt%d" % i) for i in range(3)] for _ in range(2)]
            mg = p.sb([128, KC, 512], BF16, "mg")
            t1 = [p.sb([128, 512], F32, "et1") for _ in range(2)]
            t2 = [p.sb([128, 512], F32, "et2") for _ in range(2)]
            xo = p.sb([128, KC, 512], F32, "exo")
            xn = p.sb([128, KC, 512], F32, "exn")
            sq = p.sb([128, KC, 512], F32, "esq")
            rstd = p.sb([128, 512], F32, "erstd")
            h2b = p.sb([128, KC, 512], BF16, "h2b")
            lgt = p.sb([128, NEXP], F32, "lgt")
            top8 = p.sb([128, 8], F32, "top8")
            wrow = p.sb([128, NEXP], F32, "wrow")
            ssum = p.sb([128, 2], F32, "ssum")
            for (t0, N, isctx) in tiles:
                if last and isctx:
                    continue
                col = 0 if isctx else 1
                for i, src in enumerate((retnT, hgnT, lruT)):
                    p.dma("sp", bt3[i][:, :, :N], src[:, :, t0:t0 + N].rearrange("c p n -> p c n"), r=[src], w=[bt3[i]])
                p.dma("sp", xo[:, :, :N], xT[:, :, t0:t0 + N].rearrange("k p n -> p k n"), r=[xT], w=[xo])
                for m in range(KC):
                    b = m % 2
                    for i in range(3):
                        p.dma("sp", gts[b][i][:, :N], g_T[i, m, :, t0:t0 + N], r=[g_T], w=[gts[b][i]])
                    pss = []
                    for i, wgt in enumerate((wro, who, wlo)):
                        ps = nps()
                        for kc in range(4):
                            p.mm(ps[:, :N], wgt[:, kc, m * 128:(m + 1) * 128], bt3[i][:, kc, :N], kc == 0, kc == 3, r=[wgt, bt3[i]], w=[ps])
                        pss.append(ps)
                    p.tt("dve", t1[b][:, :N], pss[0][:, :N], gts[b][0][:, :N], ALU.mult, r=[pss[0], gts[b][0]], w=[t1[b]])
                    p.tt("dve", t2[b][:, :N], pss[1][:, :N], gts[b][1][:, :N], ALU.mult, r=[pss[1], gts[b][1]], w=[t2[b]])
                    p.tt("pool", t1[b][:, :N], t1[b][:, :N], t2[b][:, :N], ALU.add, r=[t1[b], t2[b]], w=[t1[b]])
                    p.tt("dve", t2[b][:, :N], pss[2][:, :N], gts[b][2][:, :N], ALU.mult, r=[pss[2], gts[b][2]], w=[t2[b]])
                    p.tt("pool", mg[:, m, :N], t1[b][:, :N], t2[b][:, :N], ALU.add, r=[t1[b], t2[b]], w=[mg])
                for m in range(KC):
                    ps = nps()
                    for kc in range(KC):
                        p.mm(ps[:, :N], wo[:, kc, m * 128:(m + 1) * 128], mg[:, kc, :N], kc == 0, kc == KC - 1, r=[wo, mg], w=[ps])
                    p.stt("dve", xn[:, m, :N], ps[:, :N], modv[:, 2, m, col:col + 1], xo[:, m, :N], ALU.mult, ALU.add, r=[ps, modv, xo], w=[xn])
                p.dma("sp", xT[:, :, t0:t0 + N].rearrange("k p n -> p k n"), xn[:, :, :N], r=[xn], w=[xT], merge=True)
                p.act(sq[:, :, :N], xn[:, :, :N], AF.Square, r=[xn], w=[sq])
                ps = nps()
                for kc in range(KC):
                    p.mm(ps[:, :N], ones, sq[:, kc, :N], kc == 0, kc == KC - 1, r=[cst, sq], w=[ps])
                p.act(rstd[:, :N], ps[:, :N], AF.Ln, r=[ps], w=[rstd], bias=EPS, scale=1.0 / D)
                p.act(rstd[:, :N], rstd[:, :N], AF.Exp, r=[rstd], w=[rstd], scale=-0.5)
                for kc in range(KC):
                    p.stt("dve", sq[:, kc, :N], xn[:, kc, :N], modv[:, 3, kc, col:col + 1], rstd[:, :N], ALU.mult, ALU.mult, r=[xn, modv, rstd], w=[sq])
                    p.act(sq[:, kc, :N], sq[:, kc, :N], AF.Identity, r=[sq, modv], w=[sq], bias=modv[:, 4, kc, col:col + 1])
                p.copy("pool", h2b[:, :, :N], sq[:, :, :N], r=[sq], w=[h2b])
                p.dma("sp", h2T_d[:, :, t0:t0 + N].rearrange("k p n -> p k n"), h2b[:, :, :N], r=[h2b], w=[h2T_d], merge=True)
                for s in range(N // 128):
                    ps = nps()
                    for kc in range(KC):
                        p.mm(ps[:, 0:NEXP], sq[:, kc, s * 128:(s + 1) * 128], wr[:, kc, :], kc == 0, kc == KC - 1, r=[sq, wr], w=[ps])
                    p.tt("dve", lgt[:], ps[:, 0:NEXP], rbrow[:], ALU.add, r=[ps, rbrow], w=[lgt])
                    p.op("dve", lambda e: e.max(out=top8[:], in_=lgt[:]), r=[lgt], w=[top8])
                    p.ts("dve", wrow[:], lgt[:], top8[:, 3:4], None, ALU.is_ge, None, r=[lgt, top8], w=[wrow])
                    p.ts("dve", ssum[:, 0:1], top8[:, 0:1], -1.0, None, ALU.mult, None, r=[top8], w=[ssum])
                    p.act(lgt[:], lgt[:], AF.Exp, r=[lgt, ssum], w=[lgt], bias=ssum[:, 0:1])
                    p.tt("dve", wrow[:], wrow[:], lgt[:], ALU.mult, r=[wrow, lgt], w=[wrow])
                    p.op("dve", lambda e: e.reduce_sum(out=ssum[:, 1:2], in_=wrow[:], axis=AX.X), r=[wrow], w=[ssum])
                    p.op("dve", lambda e: e.reciprocal(out=ssum[:, 1:2], in_=ssum[:, 1:2]), r=[ssum], w=[ssum])
                    p.ts("dve", wrow[:], wrow[:], ssum[:, 1:2], None, ALU.mult, None, r=[wrow, ssum], w=[wrow])
                    p.dma("sp", wmoe_d[t0 + s * 128:t0 + (s + 1) * 128, :], wrow[:], r=[wrow], w=[wmoe_d], merge=True)

        with p.scope("F%d" % l):
            mtiles = [tl for tl in tiles if not (last and tl[2])]
            groups = [mtiles[i:i + 2] for i in range(0, len(mtiles), 2)]
            wgu = [p.sb([128, KC, 2 * D], BF16, "wgu%d" % i) for i in range(2)]
            wdn = [p.sb([128, KC, D], BF16, "wdn%d" % i) for i in range(2)]
            h2 = p.sb([128, KC, 1024], BF16, "mh2")
            accs = [p.sb([128, D], F32, "acc%d" % i) for i in range(8)]
            wsb = [p.sb([128, NEXP], F32, "mw%d" % i) for i in range(8)]
            aT = [p.sb([128, KC, 512], BF16, "aT%d" % i) for i in range(2)]
            g1 = [p.sb([128, 512], F32, "mg%d" % i) for i in range(3)]
            s1 = [p.sb([128, 512], F32, "ms%d" % i) for i in range(3)]
            u1 = [p.sb([128, 512], F32, "mu%d" % i) for i in range(3)]
            bdn = p.sb([NEXP, D], F32, "bdn")
            bu1 = p.sb([128, NEXP, 8], F32, "bu1")
            p.ts("dve", bu1[:], fv[:, FV_BGU:FV_BGU + 512].rearrange("p (e c) -> p e c", e=NEXP)[:, :, 8:16], 1.0, None, ALU.add, None, r=[fv], w=[bu1])
            wT = p.sb([NEXP, 128], F32, "wT")
            xo = p.sb([128, 512], F32, "fxo")
            xn2 = p.sb([128, 512], F32, "fxn")
            p.dma("sp", bdn[:], b_dn[l], w=[bdn])
            ei = 0
            for grp in groups:
                gt0 = grp[0][0]
                gN = sum(tl[1] for tl in grp)
                nsub = gN // 128
                p.dma("sp", h2[:, :, :gN], h2T_d[:, :, gt0:gt0 + gN].rearrange("k p n -> p k n"), r=[h2T_d], w=[h2])
                for s in range(nsub):
                    p.dma("sp", wsb[s][:], wmoe_d[gt0 + s * 128:gt0 + (s + 1) * 128, :], r=[wmoe_d], w=[wsb[s]])
                for e in range(NEXP):
                    wb = ei % 2
                    ei += 1
                    p.dma("sp", wgu[wb][:], wgu_bf[l % 2][e].h.rearrange("(k p) n -> p k n", p=128), r=[wgu_bf[l % 2][e]], w=[wgu[wb]])
                    p.dma("sp", wdn[wb][:], wdn_bf[l % 2][e].h.rearrange("(k p) n -> p k n", p=128), r=[wdn_bf[l % 2][e]], w=[wdn[wb]])
                    if (not last) and (e % len(groups)) == groups.index(grp):
                        convert_expert(l + 1, e)
                    off = 0
                    for ti, (t0, N, isctx) in enumerate(grp):
                        a = aT[ti % 2]
                        for m in range(KC):
                            b = m % 3
                            pg = nps()
                            for kc in range(KC):
                                p.mm(pg[:, :N], wgu[wb][:, kc, m * 128:(m + 1) * 128], h2[:, kc, off:off + N], kc == 0, kc == KC - 1, r=[wgu[wb], h2], w=[pg])
                            pu = nps()
                            for kc in range(KC):
                                p.mm(pu[:, :N], wgu[wb][:, kc, D + m * 128:D + (m + 1) * 128], h2[:, kc, off:off + N], kc == 0, kc == KC - 1, r=[wgu[wb], h2], w=[pu])
                            bg = fv[:, FV_BGU + e * 16 + m:FV_BGU + e * 16 + m + 1]
                            bu = fv[:, FV_BGU + e * 16 + 8 + m:FV_BGU + e * 16 + 8 + m + 1]
                            p.ts("dve", g1[b][:, :N], pg[:, :N], bg, 7.0, ALU.add, ALU.min, r=[pg, fv], w=[g1[b]])
                            p.act(s1[b][:, :N], g1[b][:, :N], AF.Gelu_apprx_sigmoid, r=[g1[b]], w=[s1[b]])
                            p.ts("dve", u1[b][:, :N], pu[:, :N], bu1[:, e, m:m + 1], 8.0, ALU.add, ALU.min, r=[pu, bu1], w=[u1[b]])
                            p.stt("dve", a[:, m, :N], u1[b][:, :N], -6.0, s1[b][:, :N], ALU.max, ALU.mult, r=[u1[b], s1[b]], w=[a])
                        for s in range(N // 128):
                            si = off // 128 + s
                            for hf_ in range(2):
                                pd = nps()
                                for kc in range(KC):
                                    p.mm(pd[:], a[:, kc, s * 128:(s + 1) * 128], wdn[wb][:, kc, hf_ * 512:(hf_ + 1) * 512], kc == 0, kc == KC - 1, r=[a, wdn[wb]], w=[pd])
                                cs = slice(hf_ * 512, (hf_ + 1) * 512)
                                if e == 0:
                                    p.ts("dve", accs[si][:, cs], pd[:], wsb[si][:, e:e + 1], None, ALU.mult, None, r=[pd, wsb[si]], w=[accs[si]])
                                else:
                                    p.stt("dve", accs[si][:, cs], pd[:], wsb[si][:, e:e + 1], accs[si][:, cs], ALU.mult, ALU.add, r=[pd, wsb[si], accs[si]], w=[accs[si]])
                        off += N
                off = 0
                for (t0, N, isctx) in grp:
                    col = 0 if isctx else 1
                    for s in range(N // 128):
                        si = off // 128 + s
                        pt = nps()
                        p.tr(pt[0:NEXP, 0:128], wsb[si][:], ident, r=[wsb[si], cst], w=[pt])
                        p.copy("dve", wT[:], pt[0:NEXP, 0:128], r=[pt], w=[wT])
                        for hf_ in range(2):
                            pb = nps()
                            p.mm(pb[:], wT[:], bdn[:, hf_ * 512:(hf_ + 1) * 512], True, True, r=[wT, bdn], w=[pb])
                            cs = slice(hf_ * 512, (hf_ + 1) * 512)
                            p.tt("dve", accs[si][:, cs], accs[si][:, cs], pb[:], ALU.add, r=[accs[si], pb], w=[accs[si]])
                    for m in range(KC):
                        p.dma("sp", xo[:, :N], xT[m, :, t0:t0 + N], r=[xT], w=[xo])
                        pt = nps()
                        for s in range(N // 128):
                            si = off // 128 + s
                            p.tr(pt[:, s * 128:(s + 1) * 128], accs[si][:, m * 128:(m + 1) * 128], ident, r=[accs[si], cst], w=[pt])
                        p.stt("dve", xn2[:, :N], pt[:, :N], modv[:, 5, m, col:col + 1], xo[:, :N], ALU.mult, ALU.add, r=[pt, modv, xo], w=[xn2])
                        p.dma("sp", xT[m, :, t0:t0 + N], xn2[:, :N], r=[xn2], w=[xT], merge=True)
                    off += N

    with p.scope():
        xt = p.sb([128, KC, 512], F32, "fx")
        sq = p.sb([128, KC, 512], F32, "fsq")
        rstd = p.sb([128, 512], F32, "frstd")
        ot = [p.sb([128, D], F32, "fot%d" % i) for i in range(2)]
        p.dma("sp", fv[:], fv_d[0], w=[fv])
        for (t0, N, isctx) in tiles:
            if isctx:
                continue
            p.dma("sp", xt[:, :, :N], xT[:, :, t0:t0 + N].rearrange("k p n -> p k n"), r=[xT], w=[xt])
            p.act(sq[:, :, :N], xt[:, :, :N], AF.Square, r=[xt], w=[sq])
            ps = nps()
            for kc in range(KC):
                p.mm(ps[:, :N], ones, sq[:, kc, :N], kc == 0, kc == KC - 1, r=[cst, sq], w=[ps])
            p.act(rstd[:, :N], ps[:, :N], AF.Ln, r=[ps], w=[rstd], bias=EPS, scale=1.0 / D)
            p.act(rstd[:, :N], rstd[:, :N], AF.Exp, r=[rstd], w=[rstd], scale=-0.5)
            for kc in range(KC):
                p.stt("dve", sq[:, kc, :N], xt[:, kc, :N], fv[:, FV_FG + kc:FV_FG + kc + 1], rstd[:, :N], ALU.mult, ALU.mult, r=[xt, fv, rstd], w=[sq])
            for s in range(N // 128):
                o = ot[s % 2]
                for half in range(2):
                    ps = nps()
                    for j in range(4):
                        kc = half * 4 + j
                        p.tr(ps[:, j * 128:(j + 1) * 128], sq[:, kc, s * 128:(s + 1) * 128], ident, r=[sq, cst], w=[ps])
                    p.copy("dve" if half == 0 else "act", o[:, half * 512:(half + 1) * 512], ps[:], r=[ps], w=[o])
                r0 = t0 - CTX + s * 128
                p.dma("sp", out_d[r0:r0 + 128, :], o[:], r=[o], w=[out_d], merge=True)
    p.finish()
    return nc, p


def prep_shared(inputs, depth, lat):
    f = lambda a: np.ascontiguousarray(np.asarray(a, dtype=np.float32))
    fm = lambda v, n: np.asarray(v, np.float32).reshape(n, 128).T
    fv = np.zeros((depth, 128, NFV), np.float32)
    for l in range(depth):
        fv[l, :, FV_N1:FV_N1 + 8] = fm(inputs["norm1_g"][l], 8)
        fv[l, :, FV_N2:FV_N2 + 8] = fm(inputs["norm2_g"][l], 8)
        fv[l, :, FV_MODB:FV_MODB + 48] = fm(inputs["mod_b"][l], 48)
        cw = np.asarray(inputs["lru_conv_w"][l], np.float32)
        for w in range(4):
            fv[l, :, FV_CW + w * 4:FV_CW + w * 4 + 4] = fm(cw[w], 4)
        fv[l, :, FV_CB:FV_CB + 4] = fm(inputs["lru_conv_b"][l], 4)
        for d in range(2):
            fv[l, :, FV_BA + d * 4:FV_BA + d * 4 + 4] = fm(inputs["lru_ba"][l][d], 4)
            fv[l, :, FV_BX + d * 4:FV_BX + d * 4 + 4] = fm(inputs["lru_bx"][l][d], 4)
            fv[l, :, FV_LAM + d * 4:FV_LAM + d * 4 + 4] = fm(inputs["lru_lambda"][l][d], 4)
        fv[l, :, FV_FG:FV_FG + 8] = fm(inputs["final_g"], 8)
        bgu = np.asarray(inputs["exp_b_gu"][l], np.float32)
        fv[l, :, FV_BGU:FV_BGU + 512] = bgu.reshape(NEXP, 16, 128).transpose(2, 0, 1).reshape(128, 512)
    lbl = np.asarray(inputs["hgrn_lb_logits"], np.float32)
    if lbl.shape[0] < 4:
        lbl = np.concatenate([lbl, np.full((4 - lbl.shape[0], 2, 512), -100.0, np.float32)], axis=0)
    lblT = lbl.reshape(lbl.shape[0], 2, 4, 128).transpose(3, 0, 1, 2).reshape(128, lbl.shape[0] * 8)
    lblT4 = np.zeros((128, 32), np.float32)
    lblT4[:, :lblT.shape[1]] = lblT
    bd = np.zeros((depth, 2, 2, 4, 128, 128), np.float32)
    for l in range(depth):
        for ai, nm in enumerate(("lru_wa", "lru_wx")):
            wsrc = np.asarray(inputs[nm][l], np.float32)
            for d in range(2):
                for c in range(4):
                    for j in range(2):
                        bd[l, ai, d, c, j * 64:(j + 1) * 64, j * 64:(j + 1) * 64] = wsrc[d, c * 2 + j]
    sh = {
        "consts": make_consts(), "rot": make_rot(lat), "fv": fv, "lblT": lblT4,
        "hgrn_lb_logits": f(lbl).reshape(1, -1),
        "mod_w": f(inputs["mod_w"][:depth]), "w_in": f(inputs["w_in"][:depth]),
        "ret_decay": f(inputs["ret_decay"][:depth]).reshape(depth, 16),
        "ret_gn_g": f(inputs["ret_gn_g"][:depth]), "hgrn_gn_g": f(inputs["hgrn_gn_g"][:depth]),
        "w_ret_o": f(inputs["w_ret_o"][:depth]), "w_hgrn_o": f(inputs["w_hgrn_o"][:depth]), "w_lru_o": f(inputs["w_lru_o"][:depth]),
        "w_out": f(inputs["w_out"][:depth]), "lru_bd": bd,
        "router_w": f(inputs["router_w"][:depth]), "router_b": f(inputs["router_b"][:depth]),
        "exp_w_gu": f(inputs["exp_w_gu"][:depth]), "exp_w_down": f(inputs["exp_w_down"][:depth]), "exp_b_down": f(inputs["exp_b_down"][:depth]),
    }
    return sh


def core_inputs(inputs, b, sh):
    x = np.asarray(inputs["x"][b], np.float32)
    ctx = np.asarray(inputs["ctx"][b], np.float32)
    xin = np.ascontiguousarray(np.concatenate([ctx, x], axis=0))
    sc = np.stack([np.asarray(inputs["c_ctx"], np.float32), np.asarray(inputs["c"][b], np.float32)], axis=-1)
    scin = np.ascontiguousarray(sc.reshape(KC, 128, 2).transpose(1, 0, 2).reshape(128, KC * 2))
    m = dict(sh)
    m["xin"] = xin
    m["scin"] = scin
    return m


_CACHE = {}


def kernel(**inputs):
    B, L, _ = inputs["x"].shape
    depth = inputs["w_in"].shape[0]
    key = (L, depth)
    if key not in _CACHE:
        _CACHE[key] = build(L, depth)[0]
    nc = _CACHE[key]
    sh = prep_shared(inputs, depth, L)
    n = 8
    in_maps = [core_inputs(inputs, c % B, sh) for c in range(n)]
    res = run_bass_kernel_spmd(nc, in_maps, core_ids=list(range(n)))
    out = np.stack([np.asarray(res.results[b]["out"], np.float32) for b in range(B)], axis=0)
    return out
```

```python
import contextlib
import numpy as np
import concourse.bass as bass
import concourse.mybir as mybir
from concourse.bass_utils import run_bass_kernel_spmd

F32 = mybir.dt.float32
BF16 = mybir.dt.bfloat16
ALU = mybir.AluOpType
AF = mybir.ActivationFunctionType
AX = mybir.AxisListType

D = 1024
KC = 8
CTX = 256
NEXP = 32
EPS = 1e-6
IN_COLS = 8704


class T:
    __slots__ = ("h", "w", "r", "name")

    def __init__(self, h, name):
        self.h = h
        self.name = name
        self.w = {}
        self.r = {}

    def __getitem__(self, k):
        return self.h[k]


class Prog:
    NDMA = 8

    def __init__(self, nc, same_engine_sync=True):
        self.nc = nc
        self.stack = [contextlib.ExitStack()]
        self.eng = {"pe": nc.tensor, "act": nc.scalar, "dve": nc.vector, "pool": nc.gpsimd, "sp": nc.sync}
        self.semh = {}
        self.cnt = {}
        self.known = {k: {} for k in self.eng}
        for k in self.eng:
            self.semh[k] = self.stack[0].enter_context(nc.semaphore("s_" + k))
            self.cnt[k] = 0
        self.dq = {}
        for q in ("sp", "pool", "act"):
            sems = []
            for i in range(self.NDMA):
                key = "d_%s%d" % (q, i)
                self.semh[key] = self.stack[0].enter_context(nc.semaphore(key))
                self.cnt[key] = 0
                sems.append(key)
            self.dq[q] = [sems, 0]
        self.same = same_engine_sync
        self.uid = 0
        self.ninst = 0

    @contextlib.contextmanager
    def scope(self, name=None):
        es = contextlib.ExitStack()
        self.stack.append(es)
        if name is not None:
            es.enter_context(self.nc.named_scope(name))
        try:
            yield
        finally:
            self.barrier()
            self.stack.pop()
            es.close()

    def sb(self, shape, dt=F32, name=None):
        self.uid += 1
        name = (name or "t") + "_%d" % self.uid
        h = self.stack[-1].enter_context(self.nc.sbuf_tensor(name, list(shape), dt))
        return T(h, name)

    def ps(self, shape, dt=F32, name=None):
        self.uid += 1
        name = (name or "p") + "_%d" % self.uid
        h = self.stack[-1].enter_context(self.nc.psum_tensor(name, list(shape), dt))
        return T(h, name)

    def dram(self, name, shape, dt, kind="Internal"):
        h = self.nc.dram_tensor(name, list(shape), dt, kind=kind)
        return T(h.ap(), name)

    def _wait(self, ek, need):
        e = self.eng[ek]
        kn = self.known[ek]
        for s, v in need.items():
            if s == ek and (not self.same or ek == "pe"):
                continue
            if kn.get(s, 0) < v:
                e.wait_ge(self.semh[s], v)
                kn[s] = v

    @staticmethod
    def _merge(d, s):
        for k, v in s.items():
            if d.get(k, 0) < v:
                d[k] = v

    def _need(self, r, w, skip_waw=False):
        need = {}
        for t in r:
            self._merge(need, t.w)
        for t in w:
            if not skip_waw:
                self._merge(need, t.w)
            self._merge(need, t.r)
        return need

    def _record(self, tok, r, w, merge=False):
        for t in w:
            if merge:
                self._merge(t.w, tok)
            else:
                t.w = dict(tok)
                t.r = {}
        for t in r:
            if t not in w:
                self._merge(t.r, tok)

    def op(self, ek, fn, r=(), w=()):
        self._wait(ek, self._need(r, w))
        inst = fn(self.eng[ek])
        self.cnt[ek] += 1
        inst.then_inc(self.semh[ek], 1)
        self.ninst += 1
        self._record({ek: self.cnt[ek]}, r, w)
        return inst

    def dma(self, q, out_ap, in_ap, r=(), w=(), merge=False):
        sems, i = self.dq[q]
        key = sems[i % len(sems)]
        self.dq[q][1] = i + 1
        need = self._need(r, w, skip_waw=merge)
        if self.cnt[key] > 0:
            need[key] = max(need.get(key, 0), self.cnt[key])
        self._wait(q, need)
        inst = self.eng[q].dma_start(out=out_ap, in_=in_ap)
        self.cnt[key] += 16
        inst.then_inc(self.semh[key], 16)
        self.ninst += 1
        self._record({key: self.cnt[key]}, r, w, merge=merge)
        return inst

    def barrier(self):
        allv = {k: v for k, v in self.cnt.items() if v > 0}
        for ek in self.eng:
            self._wait(ek, dict(allv))

    def finish(self):
        self.barrier()
        while self.stack:
            self.stack.pop().close()

    def tt(self, ek, out, in0, in1, op, r, w):
        return self.op(ek, lambda e: e.tensor_tensor(out=out, in0=in0, in1=in1, op=op), r=r, w=w)

    def ts(self, ek, out, in0, s1, s2, op0, op1, r, w):
        if s2 is None:
            return self.op(ek, lambda e: e.tensor_scalar(out=out, in0=in0, scalar1=s1, scalar2=None, op0=op0), r=r, w=w)
        return self.op(ek, lambda e: e.tensor_scalar(out=out, in0=in0, scalar1=s1, scalar2=s2, op0=op0, op1=op1), r=r, w=w)

    def stt(self, ek, out, in0, scalar, in1, op0, op1, r, w):
        return self.op(ek, lambda e: e.scalar_tensor_tensor(out=out, in0=in0, scalar=scalar, in1=in1, op0=op0, op1=op1), r=r, w=w)

    def act(self, out, in_, func, r, w, bias=None, scale=None):
        kw = {}
        if bias is not None:
            kw["bias"] = bias
        if scale is not None:
            kw["scale"] = scale
        return self.op("act", lambda e: e.activation(out=out, in_=in_, func=func, **kw), r=r, w=w)

    def copy(self, ek, out, in_, r, w):
        if ek == "act":
            return self.op("act", lambda e: e.copy(out=out, in_=in_), r=r, w=w)
        return self.op(ek, lambda e: e.tensor_copy(out=out, in_=in_), r=r, w=w)

    def mm(self, out, lhsT, rhs, start, stop, r, w):
        return self.op("pe", lambda e: e.matmul(out, lhsT=lhsT, rhs=rhs, start=start, stop=stop), r=r, w=w)

    def tr(self, out, in_, ident, r, w):
        return self.op("pe", lambda e: e.transpose(out=out, in_=in_, identity=ident), r=r, w=w)


C_ID = 0
C_MF = 128
C_MB = 256
C_TF = 384
C_TB = 448
C_RF = 512
C_RB = 576
C_IDX = 640
C_ONE = 646
NCONST = 774

FV_N1 = 0
FV_N2 = 8
FV_MODB = 16
FV_CW = 64
FV_CB = 80
FV_BA = 84
FV_BX = 92
FV_LAM = 100
FV_FG = 108
FV_BGU = 116
NFV = 628

G_RQ, G_RK, G_RV, G_RG, G_HQ, G_HFF, G_HFB, G_HI, G_HGT, G_LX, G_LGT = [512 * i for i in range(11)]
G_GR = 5632
G_GH = 6656
G_GL = 7680


def make_consts():
    c = np.zeros((128, NCONST), np.float32)
    p = np.arange(128)
    c[:, C_ID:C_ID + 128] = np.eye(128)
    c[:, C_MF:C_MF + 128] = (p[None, :] >= p[:, None])
    c[:, C_MB:C_MB + 128] = (p[:, None] > p[None, :])
    q = np.arange(64)
    c[:64, C_TF:C_TF + 64] = (q[:, None] <= q[None, :])
    c[:64, C_TB:C_TB + 64] = (q[:, None] >= q[None, :])
    c[:64, C_RF:C_RF + 64] = (q[:, None] > q[None, :])
    c[:64, C_RB:C_RB + 64] = (q[:, None] < q[None, :])
    c[:, C_IDX + 0] = p + 1
    c[:, C_IDX + 1] = 127 - p
    c[:, C_IDX + 2] = 128 - p
    c[:, C_IDX + 3] = p
    c[:, C_IDX + 4] = -(p + 1)
    c[:, C_IDX + 5] = p - 128
    c[:, C_ONE:C_ONE + 128] = 1.0
    return c


def make_rot(lat):
    n = np.arange(lat)
    row = (n // 64).astype(np.float32)
    col = (n % 64).astype(np.float32)
    inv = (10000.0 ** (-np.arange(16, dtype=np.float32) / 16)).astype(np.float32)
    ang = np.concatenate([row[:, None] * inv, col[:, None] * inv], axis=-1).astype(np.float32)
    cs, sn = np.cos(ang), np.sin(ang)
    return np.concatenate([cs, sn, 0.125 * cs, 0.125 * sn], axis=-1).astype(np.float32)


def build(lat, depth, dbg=()):
    S = CTX + lat
    nc = bass.Bass("TRN2", target_bir_lowering=False)
    p = Prog(nc)
    tiles = [(0, 256, True)] + [(CTX + 512 * i, 512, False) for i in range(lat // 512)]
    NT128 = S // 128

    def din(name, shape):
        return p.dram(name, shape, F32, kind="ExternalInput")

    xin = din("xin", [S, D])
    scin = din("scin", [128, KC * 2])
    consts_d = din("consts", [128, NCONST])
    rot_d = din("rot", [lat, 128])
    fv_d = din("fv", [depth, 128, NFV])
    lblT_d = din("lblT", [128, 4 * 8])
    lbl_d = din("hgrn_lb_logits", [1, 4 * 2 * 512])
    mod_w = din("mod_w", [depth, D, 6 * D])
    w_in = din("w_in", [depth, D, IN_COLS])
    ret_decay = din("ret_decay", [depth, 16])
    ret_gn = din("ret_gn_g", [depth, 512])
    hg_gn = din("hgrn_gn_g", [depth, 512])
    w_ret_o = din("w_ret_o", [depth, 512, D])
    w_hg_o = din("w_hgrn_o", [depth, 512, D])
    w_lru_o = din("w_lru_o", [depth, 512, D])
    w_out = din("w_out", [depth, D, D])
    lru_bd = din("lru_bd", [depth, 2, 2, 4, 128, 128])
    router_w = din("router_w", [depth, D, NEXP])
    router_b = din("router_b", [depth, NEXP])
    w_gu = din("exp_w_gu", [depth, NEXP, D, 2 * D])
    w_dn = din("exp_w_down", [depth, NEXP, D, D])
    b_dn = din("exp_b_down", [depth, NEXP, D])
    out_d = p.dram("out", [lat, D], F32, kind="ExternalOutput")

    def scr(name, shape, dt=F32):
        kind = "ExternalOutput" if name in dbg else "Internal"
        return p.dram(name, shape, dt, kind=kind)

    xT = scr("xT", [KC, 128, S])
    r_q = scr("r_q", [S, 512], BF16)
    r_k = scr("r_k", [S, 512], BF16)
    r_v = scr("r_v", [S, 512], BF16)
    r_g = scr("r_g", [S, 512])
    h_qT = scr("h_qT", [4, 128, S])
    h_kT = [scr("h_kfT", [4, 128, S]), scr("h_kbT", [4, 128, S])]
    h_k = [scr("h_kf", [S, 512]), scr("h_kb", [S, 512])]
    h_lf = [scr("h_lff", [S, 512]), scr("h_lfb", [S, 512])]
    h_v = scr("h_v", [S, 512], BF16)
    h_g = scr("h_g", [S, 512])
    l_xT = scr("l_xT", [4, 128, S])
    l_gT = scr("l_gT", [4, 128, S])
    g_T = scr("g_T", [3, KC, 128, S])
    o_ret = scr("o_ret", [S, 512])
    o_hg = scr("o_hg", [S, 512])
    retnT = scr("retnT", [4, 128, S], BF16)
    hgnT = scr("hgnT", [4, 128, S], BF16)
    lruT = scr("lruT", [4, 128, S], BF16)
    h2T_d = scr("h2T", [KC, 128, S], BF16)
    lbrow_d = scr("lbrow", [4, 128, 2048])
    wmoe_d = scr("wmoe", [S, NEXP])
    wgu_bf = [[p.dram("wgubf_%d_%d" % (i, e), [D, 2 * D], BF16) for e in range(NEXP)] for i in range(2)]
    wdn_bf = [[p.dram("wdnbf_%d_%d" % (i, e), [D, D], BF16) for e in range(NEXP)] for i in range(2)]

    def convert_expert(ll, e):
        par = ll % 2
        for q4 in range(4):
            rs = slice(q4 * 256, (q4 + 1) * 256)
            p.dma("pool", wgu_bf[par][e][rs, :], w_gu[ll, e, rs, :], w=[wgu_bf[par][e]], merge=True)
        for q2 in range(2):
            rs = slice(q2 * 512, (q2 + 1) * 512)
            p.dma("pool", wdn_bf[par][e][rs, :], w_dn[ll, e, rs, :], w=[wdn_bf[par][e]], merge=True)

    cst = p.sb([128, NCONST], F32, "cst")
    identb = p.sb([128, 128], BF16, "identb")
    sT = p.sb([128, KC * 2], F32, "sT")
    lbT = p.sb([128, 32], F32, "lbT")
    omlT = p.sb([128, 32], F32, "omlT")
    fv = p.sb([128, NFV], F32, "fv")
    modv = p.sb([128, 6, KC, 2], F32, "modv")
    psb = [p.ps([128, 512], F32, "psf%d" % i) for i in range(6)]
    psh = [p.ps([128, 1024], BF16, "psh%d" % i) for i in range(2)]
    rr = {"f": 0, "h": 0}

    def nps():
        rr["f"] += 1
        return psb[rr["f"] % 6]

    def npsh():
        rr["h"] += 1
        return psh[rr["h"] % 2]

    ident = cst[:, C_ID:C_ID + 128]
    ones = cst[:, C_ONE:C_ONE + 128]

    p.dma("sp", cst[:], consts_d[:], w=[cst])
    p.dma("sp", sT[:], scin[:], w=[sT])
    p.copy("dve", identb[:], ident, r=[cst], w=[identb])
    with p.scope():
        t0 = p.sb([128, KC * 2])
        p.act(t0[:], sT[:], AF.Sigmoid, r=[sT], w=[t0])
        p.tt("dve", sT[:], sT[:], t0[:], ALU.mult, r=[sT, t0], w=[sT])
        e0 = p.sb([128, 32])
        p.dma("sp", e0[:], lblT_d[:], w=[e0])
        p.act(e0[:], e0[:], AF.Exp, r=[e0], w=[e0])
        sm = p.sb([128, 8])
        p.tt("dve", sm[:], e0[:, 0:8], e0[:, 8:16], ALU.add, r=[e0], w=[sm])
        p.tt("dve", sm[:], sm[:], e0[:, 16:24], ALU.add, r=[e0, sm], w=[sm])
        p.tt("dve", sm[:], sm[:], e0[:, 24:32], ALU.add, r=[e0, sm], w=[sm])
        p.op("dve", lambda e: e.reciprocal(out=sm[:], in_=sm[:]), r=[sm], w=[sm])
        p.op("dve", lambda e: e.memset(lbT[:, 0:8], 0.0), w=[lbT])
        for l in range(1, 4):
            p.tt("dve", e0[:, 8 * l:8 * l + 8], e0[:, 8 * l:8 * l + 8], sm[:], ALU.mult, r=[e0, sm], w=[e0])
            p.tt("dve", lbT[:, 8 * l:8 * l + 8], lbT[:, 8 * l - 8:8 * l], e0[:, 8 * l:8 * l + 8], ALU.add, r=[e0, lbT], w=[lbT])
        p.ts("dve", omlT[:], lbT[:], -1.0, 1.0, ALU.mult, ALU.add, r=[lbT], w=[omlT])
        er = p.sb([128, 4, 1024])
        p.dma("sp", er[:].rearrange("p l n -> p (l n)"), lbl_d[:].partition_broadcast(128), w=[er])
        p.act(er[:], er[:], AF.Exp, r=[er], w=[er])
        sr = p.sb([128, 1024])
        p.tt("dve", sr[:], er[:, 0, :], er[:, 1, :], ALU.add, r=[er], w=[sr])
        p.tt("dve", sr[:], sr[:], er[:, 2, :], ALU.add, r=[er, sr], w=[sr])
        p.tt("dve", sr[:], sr[:], er[:, 3, :], ALU.add, r=[er, sr], w=[sr])
        p.op("dve", lambda e: e.reciprocal(out=sr[:], in_=sr[:]), r=[sr], w=[sr])
        lbr = p.sb([128, 2048])
        p.op("dve", lambda e: e.memset(lbr[:, 0:1024], 0.0), w=[lbr])
        for l in range(4):
            if l > 0:
                p.tt("dve", er[:, l, :], er[:, l, :], sr[:], ALU.mult, r=[er, sr], w=[er])
                p.tt("dve", lbr[:, 0:1024], lbr[:, 0:1024], er[:, l, :], ALU.add, r=[er, lbr], w=[lbr])
            p.ts("dve", lbr[:, 1024:2048], lbr[:, 0:1024], -1.0, 1.0, ALU.mult, ALU.add, r=[lbr], w=[lbr])
            p.dma("sp", lbrow_d[l], lbr[:], r=[lbr], w=[lbrow_d], merge=True)

    for e in range(NEXP):
        convert_expert(0, e)

    with p.scope():
        xts = [p.sb([128, D], F32, "xt") for _ in range(2)]
        xo = [p.sb([128, KC, 128], F32, "xo") for _ in range(2)]
        for t in range(NT128):
            xt = xts[t % 2]
            p.dma("sp", xt[:], xin[t * 128:(t + 1) * 128, :], w=[xt])
            o = xo[t % 2]
            for half in range(2):
                ps = nps()
                for j in range(4):
                    kc = half * 4 + j
                    p.tr(ps[:, j * 128:(j + 1) * 128], xt[:, kc * 128:(kc + 1) * 128], ident, r=[xt, cst], w=[ps])
                p.copy("dve" if half == 0 else "act", o[:, half * 4:half * 4 + 4, :].rearrange("p k n -> p (k n)"), ps[:], r=[ps], w=[o])
            p.dma("sp", xT[:, :, t * 128:(t + 1) * 128].rearrange("k p n -> p k n"), o[:], r=[o], w=[xT], merge=True)

    for l in range(depth):
        last = (l == depth - 1)
        p.dma("sp", fv[:], fv_d[l], w=[fv])
        with p.scope("mod%d" % l):
            mraw = p.sb([128, 48, 2], F32, "mraw")
            wts = [p.sb([128, KC, 512], F32, "modw") for _ in range(2)]
            for g in range(12):
                wt = wts[g % 2]
                p.dma("sp", wt[:], mod_w[l, :, g * 512:(g + 1) * 512].rearrange("(k p) n -> p k n", p=128), w=[wt])
                ps = nps()
                for m in range(4):
                    for kc in range(KC):
                        p.mm(ps[:, m * 2:m * 2 + 2], wt[:, kc, m * 128:(m + 1) * 128], sT[:, kc * 2:kc * 2 + 2],
                             kc == 0, kc == KC - 1, r=[wt, sT], w=[ps])
                p.copy("dve", mraw[:, g * 4:g * 4 + 4, :].rearrange("p a b -> p (a b)"), ps[:, 0:8], r=[ps], w=[mraw])
            p.tt("dve", mraw[:], mraw[:], fv[:, FV_MODB:FV_MODB + 48].unsqueeze(2).to_broadcast([128, 48, 2]), ALU.add, r=[mraw, fv], w=[mraw])
            for (dst, src, ng) in ((0, 1, FV_N1), (3, 4, FV_N2)):
                p.ts("dve", modv[:, dst], mraw[:, src * 8:src * 8 + 8, :], 1.0, None, ALU.add, None, r=[mraw], w=[modv])
                p.tt("dve", modv[:, dst], modv[:, dst], fv[:, ng:ng + 8].unsqueeze(2).to_broadcast([128, 8, 2]), ALU.mult, r=[modv, fv], w=[modv])
            for (dst, src) in ((1, 0), (2, 2), (4, 3), (5, 5)):
                p.copy("dve", modv[:, dst], mraw[:, src * 8:src * 8 + 8, :], r=[mraw], w=[modv])

        with p.scope("A%d" % l):
            hT = p.sb([128, KC, S], BF16, "hT")
            with p.scope():
                xt = p.sb([128, KC, 512], F32, "xa")
                sq = p.sb([128, KC, 512], F32, "sq")
                rstd = p.sb([128, 512], F32, "rstd")
                for (t0, N, isctx) in tiles:
                    col = 0 if isctx else 1
                    p.dma("sp", xt[:, :, :N], xT[:, :, t0:t0 + N].rearrange("k p n -> p k n"), r=[xT], w=[xt])
                    p.act(sq[:, :, :N], xt[:, :, :N], AF.Square, r=[xt], w=[sq])
                    ps = nps()
                    for kc in range(KC):
                        p.mm(ps[:, :N], ones, sq[:, kc, :N], kc == 0, kc == KC - 1, r=[cst, sq], w=[ps])
                    p.act(rstd[:, :N], ps[:, :N], AF.Ln, r=[ps], w=[rstd], bias=EPS, scale=1.0 / D)
                    p.act(rstd[:, :N], rstd[:, :N], AF.Exp, r=[rstd], w=[rstd], scale=-0.5)
                    for kc in range(KC):
                        p.stt("dve", sq[:, kc, :N], xt[:, kc, :N], modv[:, 0, kc, col:col + 1], rstd[:, :N], ALU.mult, ALU.mult,
                              r=[xt, modv, rstd], w=[sq])
                        p.act(hT[:, kc, t0:t0 + N], sq[:, kc, :N], AF.Identity, r=[sq, modv], w=[hT], bias=modv[:, 1, kc, col:col + 1])

            lbr = p.sb([128, 2048], F32, "lbr")
            p.dma("sp", lbr[:], lbrow_d[l], r=[lbrow_d], w=[lbr])
            wgs = [p.sb([128, KC, 512], BF16, "wg") for _ in range(2)]
            gi = [0]

            def load_wg(c0):
                wg = wgs[gi[0] % 2]
                gi[0] += 1
                p.dma("pool", wg[:], w_in[l, :, c0:c0 + 512].rearrange("(k p) n -> p k n", p=128), w=[wg])
                return wg

            rot = [p.sb([128, 128], F32, "rot") for _ in range(2)]
            tmpA = [p.sb([128, 512], F32, "tmpA") for _ in range(2)]
            tmpB = [p.sb([128, 512], F32, "tmpB") for _ in range(2)]
            obf = [p.sb([128, 512], BF16, "obf") for _ in range(2)]
            of32 = [p.sb([128, 512], F32, "of32") for _ in range(2)]
            of32b = [p.sb([128, 512], F32, "of32b") for _ in range(2)]
            cnt = [0]

            def tm_loop(wg, handler):
                for t in range(NT128):
                    ps = nps()
                    for kc in range(KC):
                        p.mm(ps[:], hT[:, kc, t * 128:(t + 1) * 128], wg[:, kc, :], kc == 0, kc == KC - 1, r=[hT, wg], w=[ps])
                    cnt[0] += 1
                    handler(t, ps, cnt[0] % 2)

            def fm_loop(wg, handler):
                for (t0, N, isctx) in tiles:
                    for m in range(4):
                        ps = nps()
                        for kc in range(KC):
                            p.mm(ps[:, :N], wg[:, kc, m * 128:(m + 1) * 128], hT[:, kc, t0:t0 + N], kc == 0, kc == KC - 1, r=[hT, wg], w=[ps])
                        cnt[0] += 1
                        handler(t0, N, m, ps, cnt[0] % 2)

            def store_tm(dst, t, src):
                p.dma("sp", dst[t * 128:(t + 1) * 128, :], src[:], r=[src], w=[dst], merge=True)

            def store_fm(dst3, m, t0, N, src):
                p.dma("sp", dst3[m, :, t0:t0 + N], src[:, :N], r=[src], w=[dst3], merge=True)

            def h_rot(dst, cofs):
                def h(t, ps, b):
                    o = obf[b]
                    if t < 2:
                        if cofs == 0:
                            p.copy("dve", o[:], ps[:], r=[ps], w=[o])
                        else:
                            p.ts("dve", o[:], ps[:], 0.125, None, ALU.mult, None, r=[ps], w=[o])
                    else:
                        rt = rot[b]
                        p.dma("sp", rt[:], rot_d[(t - 2) * 128:(t - 1) * 128, :], w=[rt])
                        pv = ps[:].rearrange("p (h two d) -> p h two d", h=8, two=2)
                        ov = o[:].rearrange("p (h two d) -> p h two d", h=8, two=2)
                        cs = rt[:, cofs:cofs + 32].unsqueeze(1).to_broadcast([128, 8, 32])
                        sn = rt[:, cofs + 32:cofs + 64].unsqueeze(1).to_broadcast([128, 8, 32])
                        a = tmpA[b][:, 0:256].rearrange("p (h d) -> p h d", h=8)
                        bb = tmpA[b][:, 256:512].rearrange("p (h d) -> p h d", h=8)
                        p.tt("dve", a, pv[:, :, 0, :], cs, ALU.mult, r=[ps, rt], w=[tmpA[b]])
                        p.tt("dve", bb, pv[:, :, 1, :], sn, ALU.mult, r=[ps, rt], w=[tmpA[b]])
                        p.tt("dve", ov[:, :, 0, :], a, bb, ALU.subtract, r=[tmpA[b]], w=[o])
                        p.tt("dve", a, pv[:, :, 0, :], sn, ALU.mult, r=[ps, rt], w=[tmpA[b]])
                        p.tt("dve", bb, pv[:, :, 1, :], cs, ALU.mult, r=[ps, rt], w=[tmpA[b]])
                        p.tt("dve", ov[:, :, 1, :], a, bb, ALU.add, r=[tmpA[b]], w=[o])
                    store_tm(dst, t, o)
                return h

            def h_copy_bf(dst):
                def h(t, ps, b):
                    p.copy("act", obf[b][:], ps[:], r=[ps], w=[obf[b]])
                    store_tm(dst, t, obf[b])
                return h

            def h_silu_tm(dst):
                def h(t, ps, b):
                    p.act(tmpA[b][:], ps[:], AF.Sigmoid, r=[ps], w=[tmpA[b]])
                    p.tt("dve", of32[b][:], tmpA[b][:], ps[:], ALU.mult, r=[ps, tmpA[b]], w=[of32[b]])
                    store_tm(dst, t, of32[b])
                return h

            def h_sig_tm(dst):
                def h(t, ps, b):
                    p.act(of32[b][:], ps[:], AF.Sigmoid, r=[ps], w=[of32[b]])
                    store_tm(dst, t, of32[b])
                return h

            def h_forget_tm(d):
                lb = lbr[:, d * 512:(d + 1) * 512]
                oml = lbr[:, 1024 + d * 512:1024 + (d + 1) * 512]

                def h(t, ps, b):
                    p.act(tmpA[b][:], ps[:], AF.Sigmoid, r=[ps], w=[tmpA[b]])
                    p.tt("dve", tmpA[b][:], tmpA[b][:], oml, ALU.mult, r=[tmpA[b], lbr], w=[tmpA[b]])
                    p.tt("dve", tmpB[b][:], tmpA[b][:], lb, ALU.add, r=[tmpA[b], lbr], w=[tmpB[b]])
                    p.tt("pool", of32[b][:], oml, tmpA[b][:], ALU.subtract, r=[tmpA[b], lbr], w=[of32[b]])
                    p.act(of32b[b][:], tmpB[b][:], AF.Ln, r=[tmpB[b]], w=[of32b[b]])
                    store_tm(h_k[d], t, of32[b])
                    store_tm(h_lf[d], t, of32b[b])
                return h

            def h_forget_fm(d):
                def h(t0, N, m, ps, b):
                    p.act(tmpA[b][:, :N], ps[:, :N], AF.Sigmoid, r=[ps], w=[tmpA[b]], scale=-1.0)
                    p.ts("dve", of32[b][:, :N], tmpA[b][:, :N], omlT[:, l * 8 + d * 4 + m:l * 8 + d * 4 + m + 1], None, ALU.mult, None,
                         r=[tmpA[b], omlT], w=[of32[b]])
                    store_fm(h_kT[d], m, t0, N, of32[b])
                return h

            def h_silu_fm(dst):
                def h(t0, N, m, ps, b):
                    p.act(tmpA[b][:, :N], ps[:, :N], AF.Sigmoid, r=[ps], w=[tmpA[b]])
                    p.tt("dve", of32[b][:, :N], tmpA[b][:, :N], ps[:, :N], ALU.mult, r=[ps, tmpA[b]], w=[of32[b]])
                    store_fm(dst, m, t0, N, of32[b])
                return h

            def h_copy_fm(dst):
                def h(t0, N, m, ps, b):
                    p.copy("act", of32[b][:, :N], ps[:, :N], r=[ps], w=[of32[b]])
                    store_fm(dst, m, t0, N, of32[b])
                return h

            def h_gelu_fm(dst):
                def h(t0, N, m, ps, b):
                    p.act(tmpA[b][:, :N], ps[:, :N], AF.Square, r=[ps], w=[tmpA[b]])
                    p.ts("dve", tmpA[b][:, :N], tmpA[b][:, :N], 0.044715, 1.0, ALU.mult, ALU.add, r=[tmpA[b]], w=[tmpA[b]])
                    p.tt("dve", tmpA[b][:, :N], tmpA[b][:, :N], ps[:, :N], ALU.mult, r=[tmpA[b], ps], w=[tmpA[b]])
                    p.act(tmpA[b][:, :N], tmpA[b][:, :N], AF.Sigmoid, r=[tmpA[b]], w=[tmpA[b]], scale=1.5957691216057308)
                    p.tt("dve", of32[b][:, :N], tmpA[b][:, :N], ps[:, :N], ALU.mult, r=[tmpA[b], ps], w=[of32[b]])
                    store_fm(dst, m, t0, N, of32[b])
                return h

            def h_sig_fm(dst, gidx, half):
                def h(t0, N, m, ps, b):
                    p.act(of32[b][:, :N], ps[:, :N], AF.Sigmoid, r=[ps], w=[of32[b]])
                    p.dma("sp", dst[gidx, half * 4 + m, :, t0:t0 + N], of32[b][:, :N], r=[of32[b]], w=[dst], merge=True)
                return h

            plan = [
                (G_RQ, "tm", h_rot(r_q, 0)), (G_RK, "tm", h_rot(r_k, 64)), (G_RV, "tm", h_copy_bf(r_v)), (G_RG, "tm", h_silu_tm(r_g)),
                (G_HQ, "fm", h_silu_fm(h_qT)),
                (G_HFF, "tm", h_forget_tm(0)), (G_HFF, "fm", h_forget_fm(0)),
                (G_HFB, "tm", h_forget_tm(1)), (G_HFB, "fm", h_forget_fm(1)),
                (G_HI, "tm", h_copy_bf(h_v)), (G_HGT, "tm", h_sig_tm(h_g)),
                (G_LX, "fm", h_copy_fm(l_xT)), (G_LGT, "fm", h_gelu_fm(l_gT)),
                (G_GR, "fm", h_sig_fm(g_T, 0, 0)), (G_GR + 512, "fm", h_sig_fm(g_T, 0, 1)),
                (G_GH, "fm", h_sig_fm(g_T, 1, 0)), (G_GH + 512, "fm", h_sig_fm(g_T, 1, 1)),
                (G_GL, "fm", h_sig_fm(g_T, 2, 0)), (G_GL + 512, "fm", h_sig_fm(g_T, 2, 1)),
            ]
            prev = (None, None)
            for (c0, kind, hd) in plan:
                if c0 != prev[0]:
                    wg = load_wg(c0)
                    prev = (c0, wg)
                (tm_loop if kind == "tm" else fm_loop)(prev[1], hd)

        def phase_B():
            lgb = p.sb([128, 16], F32, "lgb")
            p.dma("sp", lgb[:], ret_decay[l:l + 1, :].partition_broadcast(128), w=[lgb])
            p.act(lgb[:], lgb[:], AF.Exp, r=[lgb], w=[lgb], scale=-1.0)
            p.act(lgb[:], lgb[:], AF.Ln, r=[lgb], w=[lgb], bias=1.0)
            p.ts("dve", lgb[:], lgb[:], -1.0, None, ALU.mult, None, r=[lgb], w=[lgb])
            gnrow = p.sb([128, 512], F32, "gnrow")
            p.dma("sp", gnrow[:], ret_gn[l:l + 1, :].partition_broadcast(128), w=[gnrow])
            av = p.sb([128, 2, 8], F32, "av")
            bv = p.sb([128, 2, 8], F32, "bv")
            iv = p.sb([128, 2, 8], F32, "iv")
            Dt = p.sb([128, 2, 8, 128], F32, "Dt")
            Gt = p.sb([64, 2, 512], F32, "Gt")
            idx = lambda i: cst[:, C_IDX + i:C_IDX + i + 1]
            for d in range(2):
                lg = lgb[:, d * 8:d * 8 + 8]
                p.act(av[:, d, :], lg, AF.Exp, r=[lgb, cst], w=[av], scale=idx(0) if d == 0 else idx(2))
                p.act(bv[:, d, :], lg, AF.Exp, r=[lgb, cst], w=[bv], scale=idx(1) if d == 0 else idx(3))
                p.act(iv[:, d, :], lg, AF.Exp, r=[lgb, cst], w=[iv], scale=idx(4) if d == 0 else idx(5))
                msk = cst[:, C_MF:C_MF + 128] if d == 0 else cst[:, C_MB:C_MB + 128]
                for h in range(8):
                    p.ts("dve", Dt[:, d, h, :], msk, iv[:, d, h:h + 1], None, ALU.mult, None, r=[cst, iv], w=[Dt])
                p.act(Gt[:, d, :].rearrange("p (h v) -> p h v", h=8), lgb[0:64, d * 8:d * 8 + 8].unsqueeze(2).to_broadcast([64, 8, 64]),
                      AF.Exp, r=[lgb], w=[Gt], scale=128.0)

            Sst = p.sb([64, 512], F32, "Sst")
            Sbf = p.sb([64, 512], BF16, "Sbf")
            NB = 2
            qt = [p.sb([128, 512], BF16, "rq") for _ in range(NB)]
            kt = [p.sb([128, 512], BF16, "rk") for _ in range(NB)]
            vt = [p.sb([128, 512], BF16, "rv") for _ in range(NB)]
            qd = [p.sb([128, 512], BF16, "qd") for _ in range(NB)]
            kd = [p.sb([128, 512], BF16, "kd") for _ in range(NB)]
            qdT = [p.sb([64, 8, 128], BF16, "qdT") for _ in range(NB)]
            kTt = [p.sb([64, 8, 128], BF16, "kTt") for _ in range(NB)]
            Pm = [p.sb([128, 8, 128], BF16, "Pm") for _ in range(NB)]
            osb = [p.sb([128, 512], F32, "osb") for _ in range(NB)]
            oprev = [p.sb([128, 512], F32, "oprev") for _ in range(NB)]
            gate = [p.sb([128, 512], F32, "gate") for _ in range(NB)]
            ssq = [p.sb([128, 8], F32, "ssq") for _ in range(NB)]
            ybf = [p.sb([128, 512], BF16, "ybf") for _ in range(NB)]
            yT = [p.sb([128, 4, 128], BF16, "yT") for _ in range(NB)]
            for d in range(2):
                p.op("dve", lambda e: e.memset(Sst[:], 0.0), w=[Sst])
                p.op("dve", lambda e: e.memset(Sbf[:], 0.0), w=[Sbf])
                order = list(range(NT128)) if d == 0 else [1, 0] + list(range(NT128 - 1, 1, -1))
                for it, t in enumerate(order):
                    b = it % NB
                    yield
                    if last and d == 1 and t < 2:
                        pass
                    rows = slice(t * 128, (t + 1) * 128)
                    p.dma("sp", qt[b][:], r_q[rows, :], r=[r_q], w=[qt[b]])
                    p.dma("sp", kt[b][:], r_k[rows, :], r=[r_k], w=[kt[b]])
                    p.dma("sp", vt[b][:], r_v[rows, :], r=[r_v], w=[vt[b]])
                    yield
                    v3 = lambda tl: tl[:].rearrange("p (h e) -> p h e", h=8)
                    p.tt("dve", v3(qd[b]), v3(qt[b]), av[:, d, :].unsqueeze(2).to_broadcast([128, 8, 64]), ALU.mult, r=[qt[b], av], w=[qd[b]])
                    p.tt("pool", v3(kd[b]), v3(kt[b]), bv[:, d, :].unsqueeze(2).to_broadcast([128, 8, 64]), ALU.mult, r=[kt[b], bv], w=[kd[b]])
                    yield
                    ph = npsh()
                    for h in range(8):
                        p.tr(ph[0:64, h * 128:(h + 1) * 128], qd[b][:, h * 64:(h + 1) * 64], identb[:], r=[qd[b], identb], w=[ph])
                    p.copy("act", qdT[b][:].rearrange("p h n -> p (h n)"), ph[0:64, :], r=[ph], w=[qdT[b]])
                    yield
                    ph = npsh()
                    for h in range(8):
                        p.tr(ph[0:64, h * 128:(h + 1) * 128], kt[b][:, h * 64:(h + 1) * 64], identb[:], r=[kt[b], identb], w=[ph])
                    p.copy("dve", kTt[b][:].rearrange("p h n -> p (h n)"), ph[0:64, :], r=[ph], w=[kTt[b]])
                    yield
                    for hh in range(2):
                        ps = nps()
                        for j in range(4):
                            h = hh * 4 + j
                            p.mm(ps[:, j * 128:(j + 1) * 128], kTt[b][:, h, :], qdT[b][:, h, :], True, True, r=[kTt[b], qdT[b]], w=[ps])
                        p.tt("dve", Pm[b][:, hh * 4:hh * 4 + 4, :].rearrange("p h n -> p (h n)"), ps[:],
                             Dt[:, d, hh * 4:hh * 4 + 4, :].rearrange("p h n -> p (h n)"), ALU.mult, r=[ps, Dt], w=[Pm[b]])
                    yield
                    po = nps()
                    for h in range(8):
                        cs = slice(h * 64, (h + 1) * 64)
                        p.mm(po[:, cs], Pm[b][:, h, :], vt[b][:, cs], True, False, r=[Pm[b], vt[b]], w=[po])
                        p.mm(po[:, cs], qdT[b][:, h, :], Sbf[:, cs], False, True, r=[qdT[b], Sbf], w=[po])
                    yield
                    psS = nps()
                    for h in range(8):
                        cs = slice(h * 64, (h + 1) * 64)
                        p.mm(psS[0:64, cs], kd[b][:, cs], vt[b][:, cs], True, True, r=[kd[b], vt[b]], w=[psS])
                    yield
                    p.tt("dve", Sst[:], Sst[:], Gt[:, d, :], ALU.mult, r=[Sst, Gt], w=[Sst])
                    p.tt("dve", Sst[:], Sst[:], psS[0:64, :], ALU.add, r=[Sst, psS], w=[Sst])
                    p.copy("act", Sbf[:], Sst[:], r=[Sst], w=[Sbf])
                    if d == 0:
                        p.copy("act", osb[b][:], po[:], r=[po], w=[osb[b]])
                        p.dma("sp", o_ret[rows, :], osb[b][:], r=[osb[b]], w=[o_ret], merge=True)
                    else:
                        if last and t < 2:
                            continue
                        p.dma("sp", oprev[b][:], o_ret[rows, :], r=[o_ret], w=[oprev[b]])
                        p.dma("sp", gate[b][:], r_g[rows, :], r=[r_g], w=[gate[b]])
                        p.tt("dve", osb[b][:], po[:], oprev[b][:], ALU.add, r=[po, oprev[b]], w=[osb[b]])
                        yield
                        p.tt("pool", oprev[b][:], osb[b][:], osb[b][:], ALU.mult, r=[osb[b]], w=[oprev[b]])
                        p.op("dve", lambda e: e.reduce_sum(out=ssq[b][:], in_=oprev[b][:].rearrange("p (h e) -> p h e", h=8), axis=AX.X),
                             r=[oprev[b]], w=[ssq[b]])
                        p.act(ssq[b][:], ssq[b][:], AF.Ln, r=[ssq[b]], w=[ssq[b]], bias=EPS, scale=1.0 / 64)
                        p.act(ssq[b][:], ssq[b][:], AF.Exp, r=[ssq[b]], w=[ssq[b]], scale=-0.5)
                        p.tt("dve", v3(osb[b]), v3(osb[b]), ssq[b][:].unsqueeze(2).to_broadcast([128, 8, 64]), ALU.mult, r=[osb[b], ssq[b]], w=[osb[b]])
                        yield
                        p.tt("pool", gate[b][:], gate[b][:], gnrow[:], ALU.mult, r=[gate[b], gnrow], w=[gate[b]])
                        p.tt("dve", ybf[b][:], osb[b][:], gate[b][:], ALU.mult, r=[osb[b], gate[b]], w=[ybf[b]])
                        ph = npsh()
                        for c in range(4):
                            p.tr(ph[:, c * 128:(c + 1) * 128], ybf[b][:, c * 128:(c + 1) * 128], identb[:], r=[ybf[b], identb], w=[ph])
                        p.copy("act", yT[b][:].rearrange("p c n -> p (c n)"), ph[:, 0:512], r=[ph], w=[yT[b]])
                        p.dma("sp", retnT[:, :, t * 128:(t + 1) * 128].rearrange("c p n -> p c n"), yT[b][:], r=[yT[b]], w=[retnT], merge=True)

        def phase_C():
            gnrow = p.sb([128, 512], F32, "hgnrow")
            p.dma("sp", gnrow[:], hg_gn[l:l + 1, :].partition_broadcast(128), w=[gnrow])
            Sst = p.sb([128, 4, 128], F32, "hS")
            Sbf = p.sb([128, 4, 128], BF16, "hSbf")
            NB = 2
            mk = lambda shape, dt, nm: [p.sb(shape, dt, nm) for _ in range(NB)]
            lft = mk([64, 512], F32, "lft")
            ktm = mk([64, 512], F32, "ktm")
            vt = mk([64, 512], BF16, "hv")
            qTt = mk([128, 4, 64], F32, "qTt")
            kTt = mk([128, 4, 64], F32, "hkT")
            gsb = mk([128, 4, 64], F32, "gsb")
            gref = mk([128, 8], F32, "gref")
            E1 = mk([128, 4, 64], F32, "E1")
            E3 = mk([128, 4, 64], F32, "E3")
            E2 = mk([128, 4, 64], F32, "E2")
            qtl = mk([128, 4, 64], BF16, "qtl")
            ktl = mk([128, 4, 64], BF16, "ktl")
            qg = mk([128, 4, 64], BF16, "qg")
            ER = mk([64, 512], F32, "ER")
            kdec = mk([64, 512], BF16, "kdec")
            Pm = mk([64, 4, 64], BF16, "hP")
            osb = mk([64, 512], F32, "hosb")
            oprev = mk([64, 512], F32, "hoprev")
            gate = mk([64, 512], F32, "hgate")
            ssq = mk([64, 4], F32, "hssq")
            ybf = mk([64, 512], BF16, "hybf")
            yT = mk([128, 4, 64], BF16, "hyT")
            NCH = S // 64
            M = 32
            mint = [p.sb([64, 4, 64], mybir.dt.int32, "mint%d" % i) for i in range(2)]
            zer = p.sb([64, 4, 64], F32, "zer")
            p.op("dve", lambda e: e.memset(zer[:], 0.0), w=[zer])
            for i, cc in enumerate((C_TF, C_TB)):
                p.copy("dve", mint[i][:], cst[0:64, cc:cc + 64].unsqueeze(1).to_broadcast([64, 4, 64]), r=[cst], w=[mint[i]])
            for d in range(2):
                p.op("dve", lambda e: e.memset(Sst[:], 0.0), w=[Sst])
                p.op("dve", lambda e: e.memset(Sbf[:], 0.0), w=[Sbf])
                tri = cst[0:64, C_TF:C_TF + 64] if d == 0 else cst[0:64, C_TB:C_TB + 64]
                rem = cst[0:64, C_RF:C_RF + 64] if d == 0 else cst[0:64, C_RB:C_RB + 64]
                lastcol = 63 if d == 0 else 0
                order = list(range(NCH)) if d == 0 else [3, 2, 1, 0] + list(range(NCH - 1, 3, -1))
                for it, c in enumerate(order):
                    b = it % NB
                    yield
                    rows = slice(c * 64, (c + 1) * 64)
                    p.dma("sp", lft[b][:], h_lf[d][rows, :], r=[h_lf[d]], w=[lft[b]])
                    p.dma("sp", ktm[b][:], h_k[d][rows, :], r=[h_k[d]], w=[ktm[b]])
                    p.dma("sp", vt[b][:], h_v[rows, :], r=[h_v], w=[vt[b]])
                    p.dma("sp", qTt[b][:], h_qT[:, :, c * 64:(c + 1) * 64].rearrange("h p n -> p h n"), r=[h_qT], w=[qTt[b]])
                    p.dma("sp", kTt[b][:], h_kT[d][:, :, c * 64:(c + 1) * 64].rearrange("h p n -> p h n"), r=[h_kT[d]], w=[kTt[b]])
                    yield
                    pg = nps()
                    for h in range(4):
                        p.mm(pg[:, h * 64:(h + 1) * 64], lft[b][:, h * 128:(h + 1) * 128], tri, True, True, r=[lft[b], cst], w=[pg])
                    pr = nps()
                    for h in range(4):
                        p.mm(pr[0:64, h * 128:(h + 1) * 128], rem, lft[b][:, h * 128:(h + 1) * 128], True, True, r=[lft[b], cst], w=[pr])
                    yield
                    p.copy("dve", gsb[b][:].rearrange("p h n -> p (h n)"), pg[:, 0:256], r=[pg], w=[gsb[b]])
                    p.copy("dve", gref[b][:, 0:4], gsb[b][:, :, M], r=[gsb[b]], w=[gref[b]])
                    p.ts("dve", gref[b][:, 4:8], gref[b][:, 0:4], -1.0, None, ALU.mult, None, r=[gref[b]], w=[gref[b]])
                    yield
                    for h in range(4):
                        p.act(E1[b][:, h, :], gsb[b][:, h, :], AF.Exp, r=[gsb[b], gref[b]], w=[E1[b]], bias=gref[b][:, 4 + h:5 + h])
                    yield
                    p.tt("dve", qtl[b][:], qTt[b][:], E1[b][:], ALU.mult, r=[qTt[b], E1[b]], w=[qtl[b]])
                    for h in range(4):
                        p.act(E2[b][:, h, :], gsb[b][:, h, :], AF.Exp, r=[gsb[b], gref[b]], w=[E2[b]], bias=gref[b][:, h:h + 1], scale=-1.0)
                    p.tt("dve", ktl[b][:], kTt[b][:], E2[b][:], ALU.mult, r=[kTt[b], E2[b]], w=[ktl[b]])
                    yield
                    p.act(E3[b][:], gsb[b][:], AF.Exp, r=[gsb[b]], w=[E3[b]])
                    p.tt("pool", qg[b][:], qTt[b][:], E3[b][:], ALU.mult, r=[qTt[b], E3[b]], w=[qg[b]])
                    yield
                    p.act(ER[b][:], pr[0:64, :], AF.Exp, r=[pr], w=[ER[b]])
                    p.tt("pool", kdec[b][:], ktm[b][:], ER[b][:], ALU.mult, r=[ktm[b], ER[b]], w=[kdec[b]])
                    yield
                    psc = nps()
                    for h in range(4):
                        p.mm(psc[0:64, h * 64:(h + 1) * 64], ktl[b][:, h, :], qtl[b][:, h, :], True, True, r=[ktl[b], qtl[b]], w=[psc])
                    yield
                    p.op("dve", lambda e: e.select(out=Pm[b][:], mask=mint[d][:], on_true=psc[0:64, 0:256].rearrange("p (h n) -> p h n", h=4),
                                                   on_false=zer[:]), r=[psc, mint[d], zer], w=[Pm[b]])
                    yield
                    po = nps()
                    for h in range(4):
                        cs = slice(h * 128, (h + 1) * 128)
                        p.mm(po[0:64, cs], Pm[b][:, h, :], vt[b][:, cs], True, False, r=[Pm[b], vt[b]], w=[po])
                        p.mm(po[0:64, cs], qg[b][:, h, :], Sbf[:, h, :], False, True, r=[qg[b], Sbf], w=[po])
                    yield
                    psS = nps()
                    for h in range(4):
                        cs = slice(h * 128, (h + 1) * 128)
                        p.mm(psS[:, cs], kdec[b][:, cs], vt[b][:, cs], True, True, r=[kdec[b], vt[b]], w=[psS])
                    yield
                    for h in range(4):
                        p.stt("dve", Sst[:, h, :], Sst[:, h, :], E3[b][:, h, lastcol:lastcol + 1], psS[:, h * 128:(h + 1) * 128], ALU.mult, ALU.add,
                              r=[Sst, E3[b], psS], w=[Sst])
                    p.copy("act", Sbf[:], Sst[:], r=[Sst], w=[Sbf])
                    if d == 0:
                        p.copy("act", osb[b][:], po[0:64, :], r=[po], w=[osb[b]])
                        p.dma("sp", o_hg[rows, :], osb[b][:], r=[osb[b]], w=[o_hg], merge=True)
                    else:
                        if last and c < 4:
                            continue
                        p.dma("sp", oprev[b][:], o_hg[rows, :], r=[o_hg], w=[oprev[b]])
                        p.dma("sp", gate[b][:], h_g[rows, :], r=[h_g], w=[gate[b]])
                        p.tt("dve", osb[b][:], po[0:64, :], oprev[b][:], ALU.add, r=[po, oprev[b]], w=[osb[b]])
                        yield
                        p.tt("pool", oprev[b][:], osb[b][:], osb[b][:], ALU.mult, r=[osb[b]], w=[oprev[b]])
                        p.op("dve", lambda e: e.reduce_sum(out=ssq[b][:], in_=oprev[b][:].rearrange("p (h e) -> p h e", h=4), axis=AX.X),
                             r=[oprev[b]], w=[ssq[b]])
                        p.act(ssq[b][:], ssq[b][:], AF.Ln, r=[ssq[b]], w=[ssq[b]], bias=EPS, scale=1.0 / 128)
                        p.act(ssq[b][:], ssq[b][:], AF.Exp, r=[ssq[b]], w=[ssq[b]], scale=-0.5)
                        o3 = osb[b][:].rearrange("p (h e) -> p h e", h=4)
                        p.tt("dve", o3, o3, ssq[b][:].unsqueeze(2).to_broadcast([64, 4, 128]), ALU.mult, r=[osb[b], ssq[b]], w=[osb[b]])
                        yield
                        p.tt("pool", gate[b][:], gate[b][:], gnrow[0:64, :], ALU.mult, r=[gate[b], gnrow], w=[gate[b]])
                        p.tt("dve", ybf[b][:], osb[b][:], gate[b][:], ALU.mult, r=[osb[b], gate[b]], w=[ybf[b]])
                        ph = npsh()
                        for h in range(4):
                            p.tr(ph[:, h * 64:(h + 1) * 64], ybf[b][:, h * 128:(h + 1) * 128], identb[0:64, 0:64], r=[ybf[b], identb], w=[ph])
                        p.copy("act", yT[b][:].rearrange("p c n -> p (c n)"), ph[:, 0:256], r=[ph], w=[yT[b]])
                        p.dma("sp", hgnT[:, :, c * 64:(c + 1) * 64].rearrange("c p n -> p c n"), yT[b][:], r=[yT[b]], w=[hgnT], merge=True)

        with p.scope("BC%d" % l):
            gens = [phase_C(), phase_B()]
            quota = [3, 1]
            while gens:
                for gi in range(len(gens) - 1, -1, -1):
                    pass
                nxt = []
                for g_, q_ in zip(gens, quota):
                    ok = True
                    for _ in range(q_):
                        try:
                            next(g_)
                        except StopIteration:
                            ok = False
                            break
                    if ok:
                        nxt.append((g_, q_))
                gens = [x[0] for x in nxt]
                quota = [x[1] for x in nxt]

        with p.scope("D%d" % l):
            xs = p.sb([128, S], F32, "lx")
            xc = p.sb([128, S], F32, "lxc")
            at = p.sb([128, S], F32, "la")
            bt = p.sb([128, S], F32, "lb")
            hf = p.sb([128, S], F32, "lhf")
            hb = p.sb([128, S], F32, "lhb")
            gl = p.sb([128, S], F32, "lgl")
            ybf = p.sb([128, S], BF16, "lybf")
            wbd = p.sb([128, 128], F32, "wbd")
            wbx = p.sb([128, 128], F32, "wbx")
            cneg = p.sb([128, 8], F32, "cneg")
            p.act(cneg[:], fv[:, FV_LAM:FV_LAM + 8], AF.Exp, r=[fv], w=[cneg], scale=-1.0)
            p.act(cneg[:], cneg[:], AF.Ln, r=[cneg], w=[cneg], bias=1.0)
            p.ts("dve", cneg[:], cneg[:], -8.0, None, ALU.mult, None, r=[cneg], w=[cneg])
            tA = [p.sb([128, 512], F32, "ltA") for _ in range(2)]
            tB = [p.sb([128, 512], F32, "ltB") for _ in range(2)]
            segs = [(0, CTX), (CTX, S)]
            for c in range(4):
                p.dma("sp", xs[:], l_xT[c], r=[l_xT], w=[xs])
                p.dma("sp", gl[:], l_gT[c], r=[l_gT], w=[gl])
                cw = lambda w: fv[:, FV_CW + w * 4 + c:FV_CW + w * 4 + c + 1]
                p.ts("dve", xc[:], xs[:], cw(2), fv[:, FV_CB + c:FV_CB + c + 1], ALU.mult, ALU.add, r=[xs, fv], w=[xc])
                for w in (0, 1, 3):
                    o = w - 2
                    for (a, bnd) in segs:
                        lo, hi = max(a, a - o), min(bnd, bnd - o)
                        p.stt("dve", xc[:, lo:hi], xs[:, lo + o:hi + o], cw(w), xc[:, lo:hi], ALU.mult, ALU.add, r=[xs, fv, xc], w=[xc])
                for d in range(2):
                    p.dma("sp", wbd[:], lru_bd[l, 0, d, c], w=[wbd])
                    p.dma("sp", wbx[:], lru_bd[l, 1, d, c], w=[wbx])
                    for i, (t0, N, isctx) in enumerate(tiles):
                        b = i % 2
                        pa = nps()
                        p.mm(pa[:, :N], wbd[:], xc[:, t0:t0 + N], True, True, r=[wbd, xc], w=[pa])
                        px = nps()
                        p.mm(px[:, :N], wbx[:], xc[:, t0:t0 + N], True, True, r=[wbx, xc], w=[px])
                        p.act(tA[b][:, :N], pa[:, :N], AF.Sigmoid, r=[pa, fv], w=[tA[b]], bias=fv[:, FV_BA + d * 4 + c:FV_BA + d * 4 + c + 1])
                        p.act(tB[b][:, :N], px[:, :N], AF.Sigmoid, r=[px, fv], w=[tB[b]], bias=fv[:, FV_BX + d * 4 + c:FV_BX + d * 4 + c + 1])
                        p.act(at[:, t0:t0 + N], tA[b][:, :N], AF.Exp, r=[tA[b], cneg], w=[at], scale=cneg[:, d * 4 + c:d * 4 + c + 1])
                        p.tt("dve", tB[b][:, :N], tB[b][:, :N], xc[:, t0:t0 + N], ALU.mult, r=[tB[b], xc], w=[tB[b]])
                        p.tt("dve", tA[b][:, :N], at[:, t0:t0 + N], at[:, t0:t0 + N], ALU.mult, r=[at], w=[tA[b]])
                        p.ts("dve", tA[b][:, :N], tA[b][:, :N], -1.0, 1.0, ALU.mult, ALU.add, r=[tA[b]], w=[tA[b]])
                        p.act(tA[b][:, :N], tA[b][:, :N], AF.Ln, r=[tA[b]], w=[tA[b]])
                        p.act(tA[b][:, :N], tA[b][:, :N], AF.Exp, r=[tA[b]], w=[tA[b]], scale=0.5)
                        p.tt("dve", bt[:, t0:t0 + N], tA[b][:, :N], tB[b][:, :N], ALU.mult, r=[tA[b], tB[b]], w=[bt])
                    PC = 256
                    if d == 0:
                        for s0 in range(0, S, PC):
                            init = 0.0 if s0 == 0 else hf[:, s0 - 1:s0]
                            p.op("dve", lambda e: e.tensor_tensor_scan(out=hf[:, s0:s0 + PC], data0=at[:, s0:s0 + PC], data1=bt[:, s0:s0 + PC],
                                                                       initial=init, op0=ALU.mult, op1=ALU.add), r=[at, bt, hf], w=[hf])
                    else:
                        pieces = [(0, CTX, None)] + [(s0, s0 + PC, None) for s0 in range(S - PC, CTX - 1, -PC)]
                        prev_first = None
                        for (a0, a1, _) in pieces:
                            init = 0.0 if prev_first is None else hb[:, prev_first:prev_first + 1]
                            rv = lambda tl: tl[:, a0:a1][:, ::-1]
                            p.op("dve", lambda e: e.tensor_tensor_scan(out=rv(hb), data0=rv(at), data1=rv(bt),
                                                                       initial=init, op0=ALU.mult, op1=ALU.add), r=[at, bt, hb], w=[hb])
                            prev_first = a0
                p.tt("dve", hf[:], hf[:], hb[:], ALU.add, r=[hf, hb], w=[hf])
                p.tt("dve", ybf[:], hf[:], gl[:], ALU.mult, r=[hf, gl], w=[ybf])
                p.dma("sp", lruT[c], ybf[:], r=[ybf], w=[lruT], merge=True)

        with p.scope("E%d" % l):
            wro = p.sb([128, 4, D], BF16, "wro")
            who = p.sb([128, 4, D], BF16, "who")
            wlo = p.sb([128, 4, D], BF16, "wlo")
            wo = p.sb([128, KC, D], BF16, "wo")
            wr = p.sb([128, KC, NEXP], F32, "wr")
            rbrow = p.sb([128, NEXP], F32, "rbrow")
            p.dma("pool", wro[:], w_ret_o[l].rearrange("(k p) n -> p k n", p=128), w=[wro])
            p.dma("pool", who[:], w_hg_o[l].rearrange("(k p) n -> p k n", p=128), w=[who])
            p.dma("pool", wlo[:], w_lru_o[l].rearrange("(k p) n -> p k n", p=128), w=[wlo])
            p.dma("pool", wo[:], w_out[l].rearrange("(k p) n -> p k n", p=128), w=[wo])
            p.dma("sp", wr[:], router_w[l].rearrange("(k p) n -> p k n", p=128), w=[wr])
            p.dma("sp", rbrow[:], router_b[l:l + 1, :].partition_broadcast(128), w=[rbrow])
            bt3 = [p.sb([128, 4, 512], BF16, "br%d" % i) for i in range(3)]
            gts = [[p.sb([128, 512], F32, "gt%d" % i) for i in range(3)] for _ in range(2)]
            mg = p.sb([128, KC, 512], BF16, "mg")
            t1 = [p.sb([128, 512], F32, "et1") for _ in range(2)]
            t2 = [p.sb([128, 512], F32, "et2") for _ in range(2)]
            xo = p.sb([128, KC, 512], F32, "exo")
            xn = p.sb([128, KC, 512], F32, "exn")
            sq = p.sb([128, KC, 512], F32, "esq")
            rstd = p.sb([128, 512], F32, "erstd")
            h2b = p.sb([128, KC, 512], BF16, "h2b")
            lgt = p.sb([128, NEXP], F32, "lgt")
            top8 = p.sb([128, 8], F32, "top8")
            wrow = p.sb([128, NEXP], F32, "wrow")
            ssum = p.sb([128, 2], F32, "ssum")
            for (t0, N, isctx) in tiles:
                if last and isctx:
                    continue
                col = 0 if isctx else 1
                for i, src in enumerate((retnT, hgnT, lruT)):
                    p.dma("sp", bt3[i][:, :, :N], src[:, :, t0:t0 + N].rearrange("c p n -> p c n"), r=[src], w=[bt3[i]])
                p.dma("sp", xo[:, :, :N], xT[:, :, t0:t0 + N].rearrange("k p n -> p k n"), r=[xT], w=[xo])
                for m in range(KC):
                    b = m % 2
                    for i in range(3):
                        p.dma("sp", gts[b][i][:, :N], g_T[i, m, :, t0:t0 + N], r=[g_T], w=[gts[b][i]])
                    pss = []
                    for i, wgt in enumerate((wro, who, wlo)):
                        ps = nps()
                        for kc in range(4):
                            p.mm(ps[:, :N], wgt[:, kc, m * 128:(m + 1) * 128], bt3[i][:, kc, :N], kc == 0, kc == 3, r=[wgt, bt3[i]], w=[ps])
                        pss.append(ps)
                    p.tt("dve", t1[b][:, :N], pss[0][:, :N], gts[b][0][:, :N], ALU.mult, r=[pss[0], gts[b][0]], w=[t1[b]])
                    p.tt("dve", t2[b][:, :N], pss[1][:, :N], gts[b][1][:, :N], ALU.mult, r=[pss[1], gts[b][1]], w=[t2[b]])
                    p.tt("pool", t1[b][:, :N], t1[b][:, :N], t2[b][:, :N], ALU.add, r=[t1[b], t2[b]], w=[t1[b]])
                    p.tt("dve", t2[b][:, :N], pss[2][:, :N], gts[b][2][:, :N], ALU.mult, r=[pss[2], gts[b][2]], w=[t2[b]])
                    p.tt("pool", mg[:, m, :N], t1[b][:, :N], t2[b][:, :N], ALU.add, r=[t1[b], t2[b]], w=[mg])
                for m in range(KC):
                    ps = nps()
                    for kc in range(KC):
                        p.mm(ps[:, :N], wo[:, kc, m * 128:(m + 1) * 128], mg[:, kc, :N], kc == 0, kc == KC - 1, r=[wo, mg], w=[ps])
                    p.stt("dve", xn[:, m, :N], ps[:, :N], modv[:, 2, m, col:col + 1], xo[:, m, :N], ALU.mult, ALU.add, r=[ps, modv, xo], w=[xn])
                p.dma("sp", xT[:, :, t0:t0 + N].rearrange("k p n -> p k n"), xn[:, :, :N], r=[xn], w=[xT], merge=True)
                p.act(sq[:, :, :N], xn[:, :, :N], AF.Square, r=[xn], w=[sq])
                ps = nps()
                for kc in range(KC):
                    p.mm(ps[:, :N], ones, sq[:, kc, :N], kc == 0, kc == KC - 1, r=[cst, sq], w=[ps])
                p.act(rstd[:, :N], ps[:, :N], AF.Ln, r=[ps], w=[rstd], bias=EPS, scale=1.0 / D)
                p.act(rstd[:, :N], rstd[:, :N], AF.Exp, r=[rstd], w=[rstd], scale=-0.5)
                for kc in range(KC):
                    p.stt("dve", sq[:, kc, :N], xn[:, kc, :N], modv[:, 3, kc, col:col + 1], rstd[:, :N], ALU.mult, ALU.mult, r=[xn, modv, rstd], w=[sq])
                    p.act(sq[:, kc, :N], sq[:, kc, :N], AF.Identity, r=[sq, modv], w=[sq], bias=modv[:, 4, kc, col:col + 1])
                p.copy("pool", h2b[:, :, :N], sq[:, :, :N], r=[sq], w=[h2b])
                p.dma("sp", h2T_d[:, :, t0:t0 + N].rearrange("k p n -> p k n"), h2b[:, :, :N], r=[h2b], w=[h2T_d], merge=True)
                for s in range(N // 128):
                    ps = nps()
                    for kc in range(KC):
                        p.mm(ps[:, 0:NEXP], sq[:, kc, s * 128:(s + 1) * 128], wr[:, kc, :], kc == 0, kc == KC - 1, r=[sq, wr], w=[ps])
                    p.tt("dve", lgt[:], ps[:, 0:NEXP], rbrow[:], ALU.add, r=[ps, rbrow], w=[lgt])
                    p.op("dve", lambda e: e.max(out=top8[:], in_=lgt[:]), r=[lgt], w=[top8])
                    p.ts("dve", wrow[:], lgt[:], top8[:, 3:4], None, ALU.is_ge, None, r=[lgt, top8], w=[wrow])
                    p.ts("dve", ssum[:, 0:1], top8[:, 0:1], -1.0, None, ALU.mult, None, r=[top8], w=[ssum])
                    p.act(lgt[:], lgt[:], AF.Exp, r=[lgt, ssum], w=[lgt], bias=ssum[:, 0:1])
                    p.tt("dve", wrow[:], wrow[:], lgt[:], ALU.mult, r=[wrow, lgt], w=[wrow])
                    p.op("dve", lambda e: e.reduce_sum(out=ssum[:, 1:2], in_=wrow[:], axis=AX.X), r=[wrow], w=[ssum])
                    p.op("dve", lambda e: e.reciprocal(out=ssum[:, 1:2], in_=ssum[:, 1:2]), r=[ssum], w=[ssum])
                    p.ts("dve", wrow[:], wrow[:], ssum[:, 1:2], None, ALU.mult, None, r=[wrow, ssum], w=[wrow])
                    p.dma("sp", wmoe_d[t0 + s * 128:t0 + (s + 1) * 128, :], wrow[:], r=[wrow], w=[wmoe_d], merge=True)

        with p.scope("F%d" % l):
            mtiles = [tl for tl in tiles if not (last and tl[2])]
            groups = [mtiles[i:i + 2] for i in range(0, len(mtiles), 2)]
            wgu = [p.sb([128, KC, 2 * D], BF16, "wgu%d" % i) for i in range(2)]
            wdn = [p.sb([128, KC, D], BF16, "wdn%d" % i) for i in range(2)]
            h2 = p.sb([128, KC, 1024], BF16, "mh2")
            accs = [p.sb([128, D], F32, "acc%d" % i) for i in range(8)]
            wsb = [p.sb([128, NEXP], F32, "mw%d" % i) for i in range(8)]
            aT = [p.sb([128, KC, 512], BF16, "aT%d" % i) for i in range(2)]
            g1 = [p.sb([128, 512], F32, "mg%d" % i) for i in range(3)]
            s1 = [p.sb([128, 512], F32, "ms%d" % i) for i in range(3)]
            u1 = [p.sb([128, 512], F32, "mu%d" % i) for i in range(3)]
            bdn = p.sb([NEXP, D], F32, "bdn")
            bu1 = p.sb([128, NEXP, 8], F32, "bu1")
            p.ts("dve", bu1[:], fv[:, FV_BGU:FV_BGU + 512].rearrange("p (e c) -> p e c", e=NEXP)[:, :, 8:16], 1.0, None, ALU.add, None, r=[fv], w=[bu1])
            wT = p.sb([NEXP, 128], F32, "wT")
            xo = p.sb([128, 512], F32, "fxo")
            xn2 = p.sb([128, 512], F32, "fxn")
            p.dma("sp", bdn[:], b_dn[l], w=[bdn])
            ei = 0
            for grp in groups:
                gt0 = grp[0][0]
                gN = sum(tl[1] for tl in grp)
                nsub = gN // 128
                p.dma("sp", h2[:, :, :gN], h2T_d[:, :, gt0:gt0 + gN].rearrange("k p n -> p k n"), r=[h2T_d], w=[h2])
                for s in range(nsub):
                    p.dma("sp", wsb[s][:], wmoe_d[gt0 + s * 128:gt0 + (s + 1) * 128, :], r=[wmoe_d], w=[wsb[s]])
                for e in range(NEXP):
                    wb = ei % 2
                    ei += 1
                    p.dma("sp", wgu[wb][:], wgu_bf[l % 2][e].h.rearrange("(k p) n -> p k n", p=128), r=[wgu_bf[l % 2][e]], w=[wgu[wb]])
                    p.dma("sp", wdn[wb][:], wdn_bf[l % 2][e].h.rearrange("(k p) n -> p k n", p=128), r=[wdn_bf[l % 2][e]], w=[wdn[wb]])
                    if (not last) and (e % len(groups)) == groups.index(grp):
                        convert_expert(l + 1, e)
                    off = 0
                    for ti, (t0, N, isctx) in enumerate(grp):
                        a = aT[ti % 2]
                        for m in range(KC):
                            b = m % 3
                            pg = nps()
                            for kc in range(KC):
                                p.mm(pg[:, :N], wgu[wb][:, kc, m * 128:(m + 1) * 128], h2[:, kc, off:off + N], kc == 0, kc == KC - 1, r=[wgu[wb], h2], w=[pg])
                            pu = nps()
                            for kc in range(KC):
                                p.mm(pu[:, :N], wgu[wb][:, kc, D + m * 128:D + (m + 1) * 128], h2[:, kc, off:off + N], kc == 0, kc == KC - 1, r=[wgu[wb], h2], w=[pu])
                            bg = fv[:, FV_BGU + e * 16 + m:FV_BGU + e * 16 + m + 1]
                            bu = fv[:, FV_BGU + e * 16 + 8 + m:FV_BGU + e * 16 + 8 + m + 1]
                            p.ts("dve", g1[b][:, :N], pg[:, :N], bg, 7.0, ALU.add, ALU.min, r=[pg, fv], w=[g1[b]])
                            p.act(s1[b][:, :N], g1[b][:, :N], AF.Gelu_apprx_sigmoid, r=[g1[b]], w=[s1[b]])
                            p.ts("dve", u1[b][:, :N], pu[:, :N], bu1[:, e, m:m + 1], 8.0, ALU.add, ALU.min, r=[pu, bu1], w=[u1[b]])
                            p.stt("dve", a[:, m, :N], u1[b][:, :N], -6.0, s1[b][:, :N], ALU.max, ALU.mult, r=[u1[b], s1[b]], w=[a])
                        for s in range(N // 128):
                            si = off // 128 + s
                            for hf_ in range(2):
                                pd = nps()
                                for kc in range(KC):
                                    p.mm(pd[:], a[:, kc, s * 128:(s + 1) * 128], wdn[wb][:, kc, hf_ * 512:(hf_ + 1) * 512], kc == 0, kc == KC - 1, r=[a, wdn[wb]], w=[pd])
                                cs = slice(hf_ * 512, (hf_ + 1) * 512)
                                if e == 0:
                                    p.ts("dve", accs[si][:, cs], pd[:], wsb[si][:, e:e + 1], None, ALU.mult, None, r=[pd, wsb[si]], w=[accs[si]])
                                else:
                                    p.stt("dve", accs[si][:, cs], pd[:], wsb[si][:, e:e + 1], accs[si][:, cs], ALU.mult, ALU.add, r=[pd, wsb[si], accs[si]], w=[accs[si]])
                        off += N
                off = 0
                for (t0, N, isctx) in grp:
                    col = 0 if isctx else 1
                    for s in range(N // 128):
                        si = off // 128 + s
                        pt = nps()
                        p.tr(pt[0:NEXP, 0:128], wsb[si][:], ident, r=[wsb[si], cst], w=[pt])
                        p.copy("dve", wT[:], pt[0:NEXP, 0:128], r=[pt], w=[wT])
                        for hf_ in range(2):
                            pb = nps()
                            p.mm(pb[:], wT[:], bdn[:, hf_ * 512:(hf_ + 1) * 512], True, True, r=[wT, bdn], w=[pb])
                            cs = slice(hf_ * 512, (hf_ + 1) * 512)
                            p.tt("dve", accs[si][:, cs], accs[si][:, cs], pb[:], ALU.add, r=[accs[si], pb], w=[accs[si]])
                    for m in range(KC):
                        p.dma("sp", xo[:, :N], xT[m, :, t0:t0 + N], r=[xT], w=[xo])
                        pt = nps()
                        for s in range(N // 128):
                            si = off // 128 + s
                            p.tr(pt[:, s * 128:(s + 1) * 128], accs[si][:, m * 128:(m + 1) * 128], ident, r=[accs[si], cst], w=[pt])
                        p.stt("dve", xn2[:, :N], pt[:, :N], modv[:, 5, m, col:col + 1], xo[:, :N], ALU.mult, ALU.add, r=[pt, modv, xo], w=[xn2])
                        p.dma("sp", xT[m, :, t0:t0 + N], xn2[:, :N], r=[xn2], w=[xT], merge=True)
                    off += N

    with p.scope():
        xt = p.sb([128, KC, 512], F32, "fx")
        sq = p.sb([128, KC, 512], F32, "fsq")
        rstd = p.sb([128, 512], F32, "frstd")
        ot = [p.sb([128, D], F32, "fot%d" % i) for i in range(2)]
        p.dma("sp", fv[:], fv_d[0], w=[fv])
        for (t0, N, isctx) in tiles:
            if isctx:
                continue
            p.dma("sp", xt[:, :, :N], xT[:, :, t0:t0 + N].rearrange("k p n -> p k n"), r=[xT], w=[xt])
            p.act(sq[:, :, :N], xt[:, :, :N], AF.Square, r=[xt], w=[sq])
            ps = nps()
            for kc in range(KC):
                p.mm(ps[:, :N], ones, sq[:, kc, :N], kc == 0, kc == KC - 1, r=[cst, sq], w=[ps])
            p.act(rstd[:, :N], ps[:, :N], AF.Ln, r=[ps], w=[rstd], bias=EPS, scale=1.0 / D)
            p.act(rstd[:, :N], rstd[:, :N], AF.Exp, r=[rstd], w=[rstd], scale=-0.5)
            for kc in range(KC):
                p.stt("dve", sq[:, kc, :N], xt[:, kc, :N], fv[:, FV_FG + kc:FV_FG + kc + 1], rstd[:, :N], ALU.mult, ALU.mult, r=[xt, fv, rstd], w=[sq])
            for s in range(N // 128):
                o = ot[s % 2]
                for half in range(2):
                    ps = nps()
                    for j in range(4):
                        kc = half * 4 + j
                        p.tr(ps[:, j * 128:(j + 1) * 128], sq[:, kc, s * 128:(s + 1) * 128], ident, r=[sq, cst], w=[ps])
                    p.copy("dve" if half == 0 else "act", o[:, half * 512:(half + 1) * 512], ps[:], r=[ps], w=[o])
                r0 = t0 - CTX + s * 128
                p.dma("sp", out_d[r0:r0 + 128, :], o[:], r=[o], w=[out_d], merge=True)
    p.finish()
    return nc, p


def prep_shared(inputs, depth, lat):
    f = lambda a: np.ascontiguousarray(np.asarray(a, dtype=np.float32))
    fm = lambda v, n: np.asarray(v, np.float32).reshape(n, 128).T
    fv = np.zeros((depth, 128, NFV), np.float32)
    for l in range(depth):
        fv[l, :, FV_N1:FV_N1 + 8] = fm(inputs["norm1_g"][l], 8)
        fv[l, :, FV_N2:FV_N2 + 8] = fm(inputs["norm2_g"][l], 8)
        fv[l, :, FV_MODB:FV_MODB + 48] = fm(inputs["mod_b"][l], 48)
        cw = np.asarray(inputs["lru_conv_w"][l], np.float32)
        for w in range(4):
            fv[l, :, FV_CW + w * 4:FV_CW + w * 4 + 4] = fm(cw[w], 4)
        fv[l, :, FV_CB:FV_CB + 4] = fm(inputs["lru_conv_b"][l], 4)
        for d in range(2):
            fv[l, :, FV_BA + d * 4:FV_BA + d * 4 + 4] = fm(inputs["lru_ba"][l][d], 4)
            fv[l, :, FV_BX + d * 4:FV_BX + d * 4 + 4] = fm(inputs["lru_bx"][l][d], 4)
            fv[l, :, FV_LAM + d * 4:FV_LAM + d * 4 + 4] = fm(inputs["lru_lambda"][l][d], 4)
        fv[l, :, FV_FG:FV_FG + 8] = fm(inputs["final_g"], 8)
        bgu = np.asarray(inputs["exp_b_gu"][l], np.float32)
        fv[l, :, FV_BGU:FV_BGU + 512] = bgu.reshape(NEXP, 16, 128).transpose(2, 0, 1).reshape(128, 512)
    lbl = np.asarray(inputs["hgrn_lb_logits"], np.float32)
    if lbl.shape[0] < 4:
        lbl = np.concatenate([lbl, np.full((4 - lbl.shape[0], 2, 512), -100.0, np.float32)], axis=0)
    lblT = lbl.reshape(lbl.shape[0], 2, 4, 128).transpose(3, 0, 1, 2).reshape(128, lbl.shape[0] * 8)
    lblT4 = np.zeros((128, 32), np.float32)
    lblT4[:, :lblT.shape[1]] = lblT
    bd = np.zeros((depth, 2, 2, 4, 128, 128), np.float32)
    for l in range(depth):
        for ai, nm in enumerate(("lru_wa", "lru_wx")):
            wsrc = np.asarray(inputs[nm][l], np.float32)
            for d in range(2):
                for c in range(4):
                    for j in range(2):
                        bd[l, ai, d, c, j * 64:(j + 1) * 64, j * 64:(j + 1) * 64] = wsrc[d, c * 2 + j]
    sh = {
        "consts": make_consts(), "rot": make_rot(lat), "fv": fv, "lblT": lblT4,
        "hgrn_lb_logits": f(lbl).reshape(1, -1),
        "mod_w": f(inputs["mod_w"][:depth]), "w_in": f(inputs["w_in"][:depth]),
        "ret_decay": f(inputs["ret_decay"][:depth]).reshape(depth, 16),
        "ret_gn_g": f(inputs["ret_gn_g"][:depth]), "hgrn_gn_g": f(inputs["hgrn_gn_g"][:depth]),
        "w_ret_o": f(inputs["w_ret_o"][:depth]), "w_hgrn_o": f(inputs["w_hgrn_o"][:depth]), "w_lru_o": f(inputs["w_lru_o"][:depth]),
        "w_out": f(inputs["w_out"][:depth]), "lru_bd": bd,
        "router_w": f(inputs["router_w"][:depth]), "router_b": f(inputs["router_b"][:depth]),
        "exp_w_gu": f(inputs["exp_w_gu"][:depth]), "exp_w_down": f(inputs["exp_w_down"][:depth]), "exp_b_down": f(inputs["exp_b_down"][:depth]),
    }
    return sh


def core_inputs(inputs, b, sh):
    x = np.asarray(inputs["x"][b], np.float32)
    ctx = np.asarray(inputs["ctx"][b], np.float32)
    xin = np.ascontiguousarray(np.concatenate([ctx, x], axis=0))
    sc = np.stack([np.asarray(inputs["c_ctx"], np.float32), np.asarray(inputs["c"][b], np.float32)], axis=-1)
    scin = np.ascontiguousarray(sc.reshape(KC, 128, 2).transpose(1, 0, 2).reshape(128, KC * 2))
    m = dict(sh)
    m["xin"] = xin
    m["scin"] = scin
    return m


_CACHE = {}


def kernel(**inputs):
    B, L, _ = inputs["x"].shape
    depth = inputs["w_in"].shape[0]
    key = (L, depth)
    if key not in _CACHE:
        _CACHE[key] = build(L, depth)[0]
    nc = _CACHE[key]
    sh = prep_shared(inputs, depth, L)
    n = 8
    in_maps = [core_inputs(inputs, c % B, sh) for c in range(n)]
    res = run_bass_kernel_spmd(nc, in_maps, core_ids=list(range(n)))
    out = np.stack([np.asarray(res.results[b]["out"], np.float32) for b in range(B)], axis=0)
    return out
```
